# Optimizing a Trainium2 kernel written in Bass

```python
import jax, jax.numpy as jnp
from jax import lax
import numpy as np

D_MODEL = 1024
BATCH = 8
SEQ = 2048
DEPTH = 1
DEC_BATCH = 128
DEC_SEQ = 4
PAST_LEN = 8192
PAGE_SIZE = 128

D_MIX = D_MODEL
D_A = D_MIX // 2
CHUNK = 128
GM_GROUPS = 4
GM_GROUP_DIM = D_A // GM_GROUPS
D_B = D_MIX - D_A
HEAD_DIM = 64
N_HEADS = D_B // HEAD_DIM
N_KV_HEADS = 2
KV_REP = N_HEADS // N_KV_HEADS
KV_W = N_KV_HEADS * HEAD_DIM
IDX_HEADS = 8
IDX_DIM = 64
TOPK_MAX = 256
Q_BLOCK = 128
PROJ_SIZES = (D_A, D_A, N_HEADS * HEAD_DIM, KV_W, KV_W, IDX_HEADS * IDX_DIM, IDX_DIM, IDX_HEADS)
P_IN = sum(PROJ_SIZES)
N_MEM = 256
MEM_HEADS = 4
MEM_HEAD_DIM = 128
D_MEM = MEM_HEADS * MEM_HEAD_DIM
PK_HEADS = 8
N_KEYS = 128
N_EXPERTS = N_KEYS * N_KEYS
KEY_DIM = 128
KEY_HALF = KEY_DIM // 2
PK_TOPK = 16
PEER_BLOCK = 128
EPS = 1e-6

kernel_name = 'hybrid_gmlp_dsa_peer_decode_step'


def rmsnorm(x, g):
    xf = x.astype(jnp.float32)
    y = xf * lax.rsqrt(jnp.mean(xf * xf, axis=-1, keepdims=True) + EPS)
    return (y * g.astype(jnp.float32)).astype(x.dtype)


def layernorm(x, g, b):
    xf = x.astype(jnp.float32)
    mu = jnp.mean(xf, axis=-1, keepdims=True)
    xc = xf - mu
    var = jnp.mean(xc * xc, axis=-1, keepdims=True)
    return (xc * lax.rsqrt(var + EPS) * g.astype(jnp.float32) + b.astype(jnp.float32)).astype(x.dtype)


def split_proj(z):
    lead = z.shape[:-1]
    offs, acc = [], 0
    for s in PROJ_SIZES[:-1]:
        acc += s
        offs.append(acc)
    ua, va, q, k, v, qi, ki, wi = jnp.split(z, offs, axis=-1)
    return (ua, va,
            q.reshape(*lead, N_HEADS, HEAD_DIM),
            k.reshape(*lead, N_KV_HEADS, HEAD_DIM),
            v.reshape(*lead, N_KV_HEADS, HEAD_DIM),
            qi.reshape(*lead, IDX_HEADS, IDX_DIM),
            ki, wi)


def gmlp_mix(ua, va, ln_g, ln_b, ws, bs, n):
    Bt, T, _ = ua.shape
    u = jax.nn.gelu(ua)
    vn = layernorm(jax.nn.gelu(va), ln_g, ln_b)
    vc = vn.reshape(Bt, T // n, n, GM_GROUPS, GM_GROUP_DIM)
    w = jnp.where(jnp.tril(jnp.ones((n, n), dtype=bool)), ws[:, :n, :n], 0)
    mixed = jnp.einsum('gts,bcsgd->bctgd', w, vc) + bs[:, :n].T[None, None, :, :, None]
    return u * mixed.reshape(Bt, T, D_A), vn


def index_scores(qi, wi, ki):
    s = jnp.einsum('bthd,bld->bthl', qi, ki, preferred_element_type=jnp.float32)
    w = wi.astype(jnp.float32) * (IDX_HEADS ** -0.5)
    return jnp.einsum('bthl,bth->btl', jax.nn.relu(s), w)


def sparse_attend(q, k_sel, v_sel, valid):
    B, T = q.shape[:2]
    qg = q.reshape(B, T, N_KV_HEADS, KV_REP, HEAD_DIM)
    s = jnp.einsum('btgrd,btkgd->btgrk', qg, k_sel, preferred_element_type=jnp.float32) * (HEAD_DIM ** -0.5)
    s = jnp.where(valid[:, :, None, None, :], s, -jnp.inf)
    p = jax.nn.softmax(s, axis=-1)
    o = jnp.einsum('btgrk,btkgd->btgrd', p.astype(v_sel.dtype), v_sel)
    return o.reshape(B, T, N_HEADS * HEAD_DIM)


def dsa_prompt(q, k, v, qi, ki, wi):
    B, S = q.shape[:2]
    topk = min(TOPK_MAX, S // 4)
    spos = jnp.arange(S)
    bi = jnp.arange(B)[:, None, None]

    def blk(i):
        t0 = i * Q_BLOCK
        qb = lax.dynamic_slice_in_dim(q, t0, Q_BLOCK, axis=1)
        qib = lax.dynamic_slice_in_dim(qi, t0, Q_BLOCK, axis=1)
        wib = lax.dynamic_slice_in_dim(wi, t0, Q_BLOCK, axis=1)
        tpos = t0 + jnp.arange(Q_BLOCK)
        sc = index_scores(qib, wib, ki)
        sc = jnp.where((spos[None, :] <= tpos[:, None])[None], sc, -jnp.inf)
        _, idx = lax.top_k(sc, topk)
        valid = idx <= tpos[None, :, None]
        return sparse_attend(qb, k[bi, idx], v[bi, idx], valid)

    out = lax.map(blk, jnp.arange(S // Q_BLOCK))
    return out.transpose(1, 0, 2, 3).reshape(B, S, D_B)


def dsa_sample(q, k_new, v_new, qi, ki_new, wi, ck, cv, cki, page_table):
    DB, T = q.shape[:2]
    L = PAST_LEN + T
    topk = min(TOPK_MAX, L // 4)
    ki_past = cki[page_table].reshape(DB, PAST_LEN, IDX_DIM)
    ki_all = jnp.concatenate([ki_past, ki_new], axis=1)
    sc = index_scores(qi, wi, ki_all)
    tpos = PAST_LEN + jnp.arange(T)
    spos = jnp.arange(L)
    sc = jnp.where((spos[None, :] <= tpos[:, None])[None], sc, -jnp.inf)
    _, idx = lax.top_k(sc, topk)
    valid = idx <= tpos[None, :, None]
    is_new = (idx >= PAST_LEN)[..., None, None]
    bi = jnp.arange(DB)[:, None, None]
    past_idx = jnp.minimum(idx, PAST_LEN - 1)
    phys = page_table[bi, past_idx // PAGE_SIZE]
    off = past_idx % PAGE_SIZE
    new_idx = jnp.clip(idx - PAST_LEN, 0, T - 1)
    k_sel = jnp.where(is_new, k_new[bi, new_idx], ck[phys, off])
    v_sel = jnp.where(is_new, v_new[bi, new_idx], cv[phys, off])
    return sparse_attend(q, k_sel, v_sel, valid)


def mem_kv(mem, g, wkv):
    B = mem.shape[0]
    k, v = jnp.split(rmsnorm(mem, g) @ wkv, 2, axis=-1)
    return (k.reshape(B, N_MEM, MEM_HEADS, MEM_HEAD_DIM), v.reshape(B, N_MEM, MEM_HEADS, MEM_HEAD_DIM))


def mem_attend(h, mk, mv, wq, wo):
    B, T = h.shape[:2]
    q = (h @ wq).reshape(B, T, MEM_HEADS, MEM_HEAD_DIM)
    s = jnp.einsum('bthd,bmhd->bhtm', q, mk, preferred_element_type=jnp.float32) * (MEM_HEAD_DIM ** -0.5)
    p = jax.nn.softmax(s, axis=-1)
    o = jnp.einsum('bhtm,bmhd->bthd', p.astype(mv.dtype), mv).reshape(B, T, D_MEM)
    return o @ wo


def peer_block(xb, wq, keys, u_tab, v_tab):
    T = xb.shape[0]
    q = (xb @ wq).reshape(T, PK_HEADS, 2, KEY_HALF)
    s = jnp.einsum('thcd,hcnd->thcn', q, keys, preferred_element_type=jnp.float32)
    sv, si = lax.top_k(s, PK_TOPK)
    cand = (sv[:, :, 0, :, None] + sv[:, :, 1, None, :]).reshape(T, PK_HEADS, PK_TOPK * PK_TOPK)
    cid = (si[:, :, 0, :, None] * N_KEYS + si[:, :, 1, None, :]).reshape(T, PK_HEADS, PK_TOPK * PK_TOPK)
    fv, fi = lax.top_k(cand, PK_TOPK)
    eid = jnp.take_along_axis(cid, fi, axis=-1)
    g = jax.nn.softmax(fv, axis=-1)
    a = jax.nn.gelu(jnp.einsum('thkd,td->thk', u_tab[eid], xb))
    return jnp.einsum('thk,thkd->td', (g * a).astype(xb.dtype), v_tab[eid])


def peer_apply(h, blk, wq, keys, u_tab, v_tab):
    Bt, T, D = h.shape
    hb = h.reshape(Bt * T // blk, blk, D)
    out = lax.map(lambda xb: peer_block(xb, wq, keys, u_tab, v_tab), hb)
    return out.reshape(Bt, T, D)


def setup_inputs(seed: int = 0) -> dict:
    key = jax.random.key(seed)
    ks = jax.random.split(key, 32)
    n_pages = PAST_LEN // PAGE_SIZE
    n_pool = (DEC_BATCH * n_pages * 5) // 4
    nrm = jax.random.normal
    f32 = jnp.float32
    page_table = jax.random.permutation(ks[0], n_pool)[:DEC_BATCH * n_pages].reshape(DEC_BATCH, n_pages).astype(jnp.int32)
    return {
        'x_prompt': nrm(ks[1], (BATCH, SEQ, D_MODEL), f32),
        'x_sample': nrm(ks[2], (DEC_BATCH, DEC_SEQ, D_MODEL), f32),
        'cache_k': nrm(ks[3], (DEPTH, n_pool, PAGE_SIZE, N_KV_HEADS, HEAD_DIM), f32),
        'cache_v': nrm(ks[4], (DEPTH, n_pool, PAGE_SIZE, N_KV_HEADS, HEAD_DIM), f32),
        'cache_kidx': nrm(ks[5], (DEPTH, n_pool, PAGE_SIZE, IDX_DIM), f32),
        'cache_mem_k': nrm(ks[6], (DEPTH, DEC_BATCH, N_MEM, MEM_HEADS, MEM_HEAD_DIM), f32),
        'cache_mem_v': nrm(ks[7], (DEPTH, DEC_BATCH, N_MEM, MEM_HEADS, MEM_HEAD_DIM), f32),
        'page_table': page_table,
        'mem_prompt': nrm(ks[8], (BATCH, N_MEM, D_MODEL), f32),
        'norm_mix_g': 1.0 + 0.01 * nrm(ks[9], (DEPTH, D_MODEL), f32),
        'w_in': nrm(ks[10], (DEPTH, D_MODEL, P_IN), f32) * D_MODEL ** -0.5,
        'gm_ln_g': 1.0 + 0.01 * nrm(ks[11], (DEPTH, D_A), f32),
        'gm_ln_b': 0.01 * nrm(ks[12], (DEPTH, D_A), f32),
        'gm_ws': nrm(ks[13], (DEPTH, GM_GROUPS, CHUNK, CHUNK), f32) * CHUNK ** -0.5,
        'gm_bs': 1.0 + 0.01 * nrm(ks[14], (DEPTH, GM_GROUPS, CHUNK), f32),
        'w_out': nrm(ks[15], (DEPTH, D_MIX, D_MODEL), f32) * D_MIX ** -0.5,
        'norm_mem_g': 1.0 + 0.01 * nrm(ks[16], (DEPTH, D_MODEL), f32),
        'mem_norm_g': 1.0 + 0.01 * nrm(ks[17], (DEPTH, D_MODEL), f32),
        'mem_wq': nrm(ks[18], (DEPTH, D_MODEL, D_MEM), f32) * D_MODEL ** -0.5,
        'mem_wkv': nrm(ks[19], (DEPTH, D_MODEL, 2 * D_MEM), f32) * D_MODEL ** -0.5,
        'mem_wo': nrm(ks[20], (DEPTH, D_MEM, D_MODEL), f32) * D_MEM ** -0.5,
        'norm_ffn_g': 1.0 + 0.01 * nrm(ks[21], (DEPTH, D_MODEL), f32),
        'peer_wq': nrm(ks[22], (DEPTH, D_MODEL, PK_HEADS * KEY_DIM), f32) * D_MODEL ** -0.5,
        'peer_keys': nrm(ks[23], (DEPTH, PK_HEADS, 2, N_KEYS, KEY_HALF), f32) * KEY_HALF ** -0.5,
        'peer_u': nrm(ks[24], (DEPTH, N_EXPERTS, D_MODEL), f32) * D_MODEL ** -0.5,
        'peer_v': nrm(ks[25], (DEPTH, N_EXPERTS, D_MODEL), f32) * 0.3,
        'final_norm_g': 1.0 + 0.01 * nrm(ks[26], (D_MODEL,), f32),
    }


def reference(x_prompt, x_sample, cache_k, cache_v, cache_kidx, cache_mem_k, cache_mem_v, page_table, mem_prompt,
              norm_mix_g, w_in, gm_ln_g, gm_ln_b, gm_ws, gm_bs, w_out,
              norm_mem_g, mem_norm_g, mem_wq, mem_wkv, mem_wo,
              norm_ffn_g, peer_wq, peer_keys, peer_u, peer_v, final_norm_g):
    xp, xs = x_prompt, x_sample
    S = x_prompt.shape[1]
    T = x_sample.shape[1]
    kp_l, vp_l, kip_l, gvp_l, mkp_l, mvp_l = [], [], [], [], [], []
    ks_l, vs_l, kis_l, gvs_l = [], [], [], []
    for l in range(DEPTH):
        ua, va, q, k, v, qi, ki, wi = split_proj(rmsnorm(xp, norm_mix_g[l]) @ w_in[l])
        ya, vn_p = gmlp_mix(ua, va, gm_ln_g[l], gm_ln_b[l], gm_ws[l], gm_bs[l], CHUNK)
        yb = dsa_prompt(q, k, v, qi, ki, wi)
        xp = xp + jnp.concatenate([ya, yb], axis=-1) @ w_out[l]
        kp_l.append(k); vp_l.append(v); kip_l.append(ki); gvp_l.append(vn_p[:, S - CHUNK:])

        ua_s, va_s, q_s, k_s, v_s, qi_s, ki_s, wi_s = split_proj(rmsnorm(xs, norm_mix_g[l]) @ w_in[l])
        ya_s, vn_s = gmlp_mix(ua_s, va_s, gm_ln_g[l], gm_ln_b[l], gm_ws[l], gm_bs[l], T)
        yb_s = dsa_sample(q_s, k_s, v_s, qi_s, ki_s, wi_s, cache_k[l], cache_v[l], cache_kidx[l], page_table)
        xs = xs + jnp.concatenate([ya_s, yb_s], axis=-1) @ w_out[l]
        ks_l.append(k_s); vs_l.append(v_s); kis_l.append(ki_s); gvs_l.append(vn_s)

        mk, mv = mem_kv(mem_prompt, mem_norm_g[l], mem_wkv[l])
        xp = xp + mem_attend(rmsnorm(xp, norm_mem_g[l]), mk, mv, mem_wq[l], mem_wo[l])
        xs = xs + mem_attend(rmsnorm(xs, norm_mem_g[l]), cache_mem_k[l], cache_mem_v[l], mem_wq[l], mem_wo[l])
        mkp_l.append(mk); mvp_l.append(mv)

        xp = xp + peer_apply(rmsnorm(xp, norm_ffn_g[l]), PEER_BLOCK, peer_wq[l], peer_keys[l], peer_u[l], peer_v[l])
        xs = xs + peer_apply(rmsnorm(xs, norm_ffn_g[l]), T, peer_wq[l], peer_keys[l], peer_u[l], peer_v[l])

    y_prompt = rmsnorm(xp, final_norm_g)
    y_sample = rmsnorm(xs, final_norm_g)
    k_prompt = jnp.stack(kp_l); v_prompt = jnp.stack(vp_l); kidx_prompt = jnp.stack(kip_l)
    gmv_prompt = jnp.stack(gvp_l); memk_prompt = jnp.stack(mkp_l); memv_prompt = jnp.stack(mvp_l)
    k_sample = jnp.stack(ks_l); v_sample = jnp.stack(vs_l); kidx_sample = jnp.stack(kis_l)
    gmv_sample = jnp.stack(gvs_l)
    return (y_prompt, y_sample, k_prompt, v_prompt, kidx_prompt, gmv_prompt, memk_prompt, memv_prompt,
            k_sample, v_sample, kidx_sample, gmv_sample)
```

```python
import numpy as np
from contextlib import ExitStack
import concourse.bass as bass
import concourse.mybir as mybir
from concourse.bass_utils import run_bass_kernel_spmd

F32 = mybir.dt.float32
BF16 = mybir.dt.bfloat16
I32 = mybir.dt.int32
U32 = mybir.dt.uint32
AF = mybir.ActivationFunctionType
ALU = mybir.AluOpType
AX = mybir.AxisListType

NCORES = 8
D = 1024
SEQ = 2048
NT = SEQ // 128
P_IN = 2376
EPS = 1e-6
NEG = -1.0e30
DEC_B = 16
DEC_T = 4
NS = DEC_B * DEC_T
NPAGES = 64
NEXP = 16384


class Prog:
    ENG = ('sp', 'act', 'dve', 'pool', 'pe')

    def __init__(self, nc, stack, n_dma_sems=12):
        self.nc = nc
        self.stack = stack
        self.ops = {k: [] for k in self.ENG}
        self.cnt = {k: 0 for k in self.ENG}
        self.waited = {k: {} for k in self.ENG}
        self.res = {}
        self.sems = {}
        for k in ('act', 'dve', 'pool', 'pe'):
            self.sems[k] = stack.enter_context(nc.semaphore('prog_' + k))
        self.dma_sems = {}
        self.dma_rr = {}
        self.dma_uses = {}
        for q in ('sp', 'pool', 'act'):
            lst = []
            for i in range(n_dma_sems):
                key = 'dma_%s_%d' % (q, i)
                self.sems[key] = stack.enter_context(nc.semaphore(key))
                self.dma_uses[key] = 0
                lst.append(key)
            self.dma_sems[q] = lst
            self.dma_rr[q] = 0
        self.psum_names = set()

    def sb(self, name, shape, dtype, stack=None):
        return (stack or self.stack).enter_context(self.nc.sbuf_tensor(name, list(shape), dtype))

    def ps(self, name, shape, dtype):
        self.psum_names.add(name)
        return self.stack.enter_context(self.nc.psum_tensor(name, list(shape), dtype))

    @staticmethod
    def _key(x):
        if isinstance(x, (str, tuple)):
            return x
        t = getattr(x, 'tensor', x)
        n = getattr(t, 'name', None)
        if n is None:
            raise ValueError('cannot derive resource key from %r' % (x,))
        return n

    def _deps(self, reads, writes):
        deps = []
        for r in reads:
            st = self.res.get(self._key(r))
            if st and st['w']:
                deps.append(st['w'])
        for w in writes:
            st = self.res.get(self._key(w))
            if st:
                if st['w']:
                    deps.append(st['w'])
                deps.extend(st['r'])
        return deps

    def _commit(self, reads, writes, tok):
        for r in reads:
            st = self.res.setdefault(self._key(r), {'w': None, 'r': []})
            st['r'].append(tok)
        for w in writes:
            self.res[self._key(w)] = {'w': tok, 'r': []}

    def _filter_waits(self, eng, deps, skip_self=False):
        out = {}
        for (sk, val) in deps:
            if skip_self and sk == eng:
                continue
            if self.waited[eng].get(sk, 0) >= val:
                continue
            if out.get(sk, 0) < val:
                out[sk] = val
        for sk, val in out.items():
            self.waited[eng][sk] = val
        return list(out.items())

    def op(self, eng, fn, reads=(), writes=()):
        pr = [r for r in reads if self._key(r) in self.psum_names]
        if pr:
            reads = [r for r in reads if self._key(r) not in self.psum_names]
            writes = list(writes) + pr
        deps = self._deps(reads, writes)
        waits = self._filter_waits(eng, deps, skip_self=(eng == 'pe'))
        self.cnt[eng] += 1
        tok = (eng, self.cnt[eng])
        self._commit(reads, writes, tok)
        self.ops[eng].append((waits, fn, (eng, 1)))
        return tok

    def dma(self, q, fn, reads=(), writes=()):
        deps = self._deps(reads, writes)
        lst = self.dma_sems[q]
        sk = lst[self.dma_rr[q] % len(lst)]
        self.dma_rr[q] += 1
        if self.dma_uses[sk] > 0:
            deps.append((sk, 16 * self.dma_uses[sk]))
        waits = self._filter_waits(q, deps)
        self.dma_uses[sk] += 1
        tok = (sk, 16 * self.dma_uses[sk])
        self._commit(reads, writes, tok)
        self.ops[q].append((waits, fn, (sk, 16)))
        return tok

    def barrier(self):
        deps = [(k, self.cnt[k]) for k in ('act', 'dve', 'pool', 'pe') if self.cnt[k] > 0]
        deps += [(sk, 16 * n) for sk, n in self.dma_uses.items() if n > 0]
        for eng in self.ENG:
            waits = self._filter_waits(eng, [d for d in deps if d[0] != eng])
            if waits:
                self.ops[eng].append((waits, None, None))
        self.res = {}

    def finish(self):
        deps = [(sk, 16 * n) for sk, n in self.dma_uses.items() if n > 0]
        waits = self._filter_waits('sp', deps)
        self.ops['sp'].append((waits, None, None))

    def emit(self):
        nc = self.nc
        allsems = list(self.sems.values())
        with nc.Block() as b0:
            def clr(e):
                for s in allsems:
                    e.sem_clear(s)
            b0.sync(clr)
        with nc.Block() as block:
            for name, meth in (('sp', block.sync), ('act', block.scalar), ('dve', block.vector),
                               ('pool', block.gpsimd), ('pe', block.tensor)):
                ops = self.ops[name]
                if not ops:
                    continue

                def body(e, ops=ops):
                    for waits, fn, inc in ops:
                        for (sk, val) in waits:
                            e.wait_ge(self.sems[sk], val)
                        if fn is None:
                            continue
                        ins = fn(e)
                        if inc is not None:
                            ins.then_inc(self.sems[inc[0]], inc[1])
                meth(body)


def host_consts():
    c = {}
    c['ident'] = np.eye(128, dtype=np.float32)
    s = np.arange(128)
    c['trilT'] = (s[:, None] <= s[None, :]).astype(np.float32)
    c['negmask'] = np.where(s[None, :] <= s[:, None], 0.0, NEG).astype(np.float32)
    bt = np.arange(64)
    c['blk64'] = ((bt[:, None] // 4 == bt[None, :] // 4) & (bt[:, None] % 4 <= bt[None, :] % 4)).astype(np.float32)
    c['ones'] = np.ones((128, 128), dtype=np.float32)
    c['iota'] = np.tile(np.arange(128, dtype=np.float32)[None, :], (128, 1))
    c['pow2'] = np.tile((2.0 ** -(np.arange(48, dtype=np.float64) + 1)).astype(np.float32)[None, :], (128, 1))
    t4 = np.arange(4)
    c['negm4'] = np.where(t4[:, None] <= t4[None, :], 0.0, NEG).astype(np.float32)
    return c


def build(cfg):
    n_pool = cfg.get('n_pool', 10240)
    do = cfg.get('phases', ('P1', 'P2', 'P3', 'P4'))
    dbg = cfg.get('dbg', False)
    nc = bass.Bass("TRN2", target_bir_lowering=False)

    def din(name, shape, dt=F32):
        return nc.dram_tensor(name, list(shape), dt, kind="ExternalInput").ap()

    def dout(name, shape, dt=F32):
        return nc.dram_tensor(name, list(shape), dt, kind="ExternalOutput").ap()

    xp_d = din('xp', [SEQ, D])
    xs_d = din('xs', [NS, D])
    memp_d = din('memp', [256, D])
    w_in_d = din('w_in', [D, P_IN])
    w_out_d = din('w_out', [D, D])
    g_mix_d = din('g_mix', [D]); g_mem_d = din('g_mem', [D]); g_memn_d = din('g_memn', [D])
    g_ffn_d = din('g_ffn', [D]); g_fin_d = din('g_fin', [D])
    ln_g_d = din('ln_g', [512]); ln_b_d = din('ln_b', [512])
    gm_ws_d = din('gm_ws', [4, 128, 128]); gm_bs_d = din('gm_bs', [4, 128])
    wq_d = din('mem_wq', [D, 512]); wkv_d = din('mem_wkv', [D, D]); wo_d = din('mem_wo', [512, D])
    c_ident = din('c_ident', [128, 128]); c_trilT = din('c_trilT', [128, 128]); c_negmask = din('c_negmask', [128, 128])
    c_blk64 = din('c_blk64', [64, 64]); c_ones = din('c_ones', [128, 128]); c_iota = din('c_iota', [128, 128])
    pwq_d = din('peer_wq', [D, D]); pkeys_d = din('peer_keys', [16, 128, 64])
    pu_d = din('peer_u', [NEXP, D]); pv_d = din('peer_v', [NEXP, D])
    ckidx_d = din('cache_kidx', [n_pool, 8192]); ck_d = din('cache_k', [2 * n_pool, 8192]); cv_d = din('cache_v', [2 * n_pool, 8192])
    pt_d = din('page_table', [DEC_B, NPAGES], I32)
    cmk_d = din('cache_mem_k', [DEC_B, 256, 512]); cmv_d = din('cache_mem_v', [DEC_B, 256, 512])
    c_pow2 = din('c_pow2', [128, 48]); c_negm4 = din('c_negm4', [4, 4])

    y_p = dout('y_p', [SEQ, D]); y_s = dout('y_s', [NS, D])
    k_p = dout('k_p', [SEQ, 128]); v_p = dout('v_p', [SEQ, 128]); ki_p = dout('ki_p', [SEQ, 64])
    gmv_p = dout('gmv_p', [128, 512])
    memk_p = dout('memk_p', [256, 512]); memv_p = dout('memv_p', [256, 512])
    k_s = dout('k_s', [NS, 128]); v_s = dout('v_s', [NS, 128]); ki_s = dout('ki_s', [NS, 64]); gmv_s = dout('gmv_s', [NS, 512])
    if dbg:
        x2_dbg = dout('x2_dbg', [SEQ + NS, D])
    x2s = nc.dram_tensor('x2s', [SEQ + NS, D], F32, kind="Internal").ap()
    UTs = nc.dram_tensor('UTs', [128, 128, D], BF16, kind="Internal").ap()
    Vs = nc.dram_tensor('Vs', [128, 128, D], BF16, kind="Internal").ap()

    with ExitStack() as st:
        p = Prog(nc, st)
        pb = [p.ps('pb%d' % i, [128, 512], F32) for i in range(8)]
        rr = [0]
        nbmod = [6]

        def nb():
            b = pb[rr[0] % nbmod[0]]
            rr[0] += 1
            return b

        def mm(out, lhsT, rhs, start, stop, R, W):
            p.op('pe', lambda e: e.matmul(out, lhsT=lhsT, rhs=rhs, start=start, stop=stop), reads=R, writes=W)

        identf = p.sb('identf', [128, 128], F32)
        ident = p.sb('ident', [128, 128], BF16)
        trilT = p.sb('trilT', [128, 128], F32)
        negmask = p.sb('negmask', [128, 128], F32)
        ones_bf = p.sb('ones_bf', [128, 128], BF16)
        onesf = p.sb('onesf', [128, 128], F32)
        p.dma('sp', lambda e: e.dma_start(out=identf[:], in_=c_ident[:, :]), writes=[identf])
        p.dma('sp', lambda e: e.dma_start(out=trilT[:], in_=c_trilT[:, :]), writes=[trilT])
        p.dma('sp', lambda e: e.dma_start(out=negmask[:], in_=c_negmask[:, :]), writes=[negmask])
        p.dma('sp', lambda e: e.dma_start(out=onesf[:], in_=c_ones[:, :]), writes=[onesf])
        p.op('dve', lambda e: e.tensor_copy(out=ident[:], in_=identf[:]), reads=[identf], writes=[ident])
        p.op('dve', lambda e: e.tensor_copy(out=ones_bf[:], in_=onesf[:]), reads=[onesf], writes=[ones_bf])

        def tr(out, in_, K, R, W):
            p.op('pe', lambda e: e.transpose(out=out, in_=in_, identity=ident[0:K, 0:K]), reads=list(R) + [ident], writes=W)

        gcols = {}

        def load_gcol(name, g_d):
            t = p.sb('gc_' + name, [128, 8], F32)
            p.dma('sp', lambda e: e.dma_start(out=t[:], in_=g_d.rearrange("(c q) -> q c", q=128), allow_slow_non_contiguous=True), writes=[t])
            gcols[name] = t
            return t

        def load_weight(dst, w_d, nk, ncol, gcol, stage, eng_rot=[0]):
            for c in range(nk):
                stg = stage[c % len(stage)]
                p.dma('sp', lambda e, c=c, stg=stg: e.dma_start(out=stg[:, 0:ncol], in_=w_d[c * 128:(c + 1) * 128, :]), writes=[stg])
                eng = ('dve', 'pool')[eng_rot[0] % 2]
                eng_rot[0] += 1
                if gcol is not None:
                    p.op(eng, lambda e, c=c, stg=stg: e.tensor_scalar(out=dst[:, c, :], in0=stg[:, 0:ncol], scalar1=gcol[:, c:c + 1],
                                                                     scalar2=None, op0=ALU.mult), reads=[stg, gcol], writes=[dst])
                else:
                    p.op(eng, lambda e, c=c, stg=stg: e.tensor_copy(out=dst[:, c, :], in_=stg[:, 0:ncol]), reads=[stg], writes=[dst])

        def rmsnorm_rstd(x_t, T, rstd, scratch):
            ss = rstd['ss']
            p.op('act', lambda e: e.activation(out=scratch[0:T, :], in_=x_t[0:T, :], func=AF.Square, accum_out=ss[0:T, 0:1]),
                 reads=[x_t], writes=[scratch, ss])
            p.op('dve', lambda e: e.tensor_scalar(out=ss[0:T, 1:2], in0=ss[0:T, 0:1], scalar1=1.0 / D, scalar2=EPS, op0=ALU.mult, op1=ALU.add),
                 reads=[ss], writes=[ss])
            p.op('act', lambda e: e.activation(out=ss[0:T, 2:3], in_=ss[0:T, 1:2], func=AF.Sqrt), reads=[ss], writes=[ss])
            p.op('dve', lambda e: e.reciprocal(out=ss[0:T, 3:4], in_=ss[0:T, 2:3]), reads=[ss], writes=[ss])
            return ss[0:T, 3:4]

        def norm_T(x_t, T, tag, xn, xnT, ssd, scratch, gcol=None):
            rs = rmsnorm_rstd(x_t, T, ssd, scratch)
            p.op('act', lambda e: e.activation(out=xn[0:T, :], in_=x_t[0:T, :], func=AF.Copy, scale=rs), reads=[x_t, ssd['ss']], writes=[xn])
            bk = nb()
            bv = bk[:].bitcast(BF16)
            for c in range(8):
                tr(bv[:, c * T:(c + 1) * T], xn[0:T, c * 128:(c + 1) * 128], T, [xn], [bk])
            if gcol is None:
                p.op('dve', lambda e: e.tensor_copy(out=xnT[:, :, 0:T], in_=bv[:, 0:8 * T].rearrange("q (c t) -> q c t", c=8)),
                     reads=[bk], writes=[xnT])
            else:
                for c in range(8):
                    p.op('dve', lambda e, c=c: e.tensor_scalar(out=xnT[:, c, 0:T], in0=bv[:, c * T:(c + 1) * T], scalar1=gcol[:, c:c + 1], scalar2=None, op0=ALU.mult),
                         reads=[bk, gcol], writes=[xnT])

        ssd = {'ss': p.sb('ss', [128, 4], F32)}

        for nm, gd in (('mix', g_mix_d), ('mem', g_mem_d), ('memn', g_memn_d), ('ffn', g_ffn_d)):
            load_gcol(nm, gd)
        conv_done = [0]

        def convert_chunk(ec, u_, t_, v_):
            p.dma('pool', lambda e: e.dma_start(out=u_[:, :], in_=pu_d[ec * 128:(ec + 1) * 128, :]), writes=[u_])
            p.dma('pool', lambda e: e.dma_start(out=v_[:, :], in_=pv_d[ec * 128:(ec + 1) * 128, :]), writes=[v_])
            bk = nb()
            bv = bk[:].bitcast(BF16)
            for c in range(8):
                tr(bv[:, c * 128:(c + 1) * 128], u_[:, c * 128:(c + 1) * 128], 128, [u_], [bk])
            if ec % 2 == 0:
                p.op('act', lambda e: e.copy(out=t_[:, :], in_=bv[:, :]), reads=[bk], writes=[t_])
            else:
                p.op('pool', lambda e: e.tensor_copy(out=t_[:, :], in_=bv[:, :]), reads=[bk], writes=[t_]) if False else \
                    p.op('act', lambda e: e.copy(out=t_[:, :], in_=bv[:, :]), reads=[bk], writes=[t_])
            p.dma('sp', lambda e: e.dma_start(out=UTs[ec, :, :], in_=t_[:, :]), reads=[t_], writes=[('UTs', ec)])
            p.dma('sp', lambda e: e.dma_start(out=Vs[ec, :, :], in_=v_[:, :]), reads=[v_], writes=[('Vs', ec)])

        with ExitStack() as s2:
            w_out = p.sb('w_out_sb', [128, 8, D], BF16, s2)
            wq = p.sb('wq_sb', [128, 8, 512], BF16, s2)
            wo = p.sb('wo_sb', [128, 4, D], BF16, s2)
            x0 = p.sb('x0', [128, D], F32, s2)
            sq = p.sb('sq_scratch', [128, D], F32, s2)
            xn = p.sb('xn', [128, D], BF16, s2)
            xnT = p.sb('xnT', [128, 8, 128], BF16, s2)
            mkT = p.sb('mkT', [128, 4, 256], BF16, s2)
            mv_aug = p.sb('mv_aug', [128, 2, 4, 129], BF16, s2)
            WgT = p.sb('WgT', [128, 4, 128], BF16, s2)
            bsT = p.sb('bsT', [128, 4], F32, s2)
            W4T = p.sb('W4T', [64, 4, 64], BF16, s2)
            bsT4 = p.sb('bsT4', [64, 4], F32, s2)
            lng = p.sb('lng', [128, 512], F32, s2)
            lnb = p.sb('lnb', [128, 512], F32, s2)
            tau_c = p.sb('tau_c', [128, 1], F32, s2)
            p.op('pool', lambda e: e.memset(tau_c[:], -1.0e29), writes=[tau_c])
            p.dma('sp', lambda e: e.dma_start(out=lng[:], in_=ln_g_d.partition_broadcast(128)), writes=[lng])
            p.dma('sp', lambda e: e.dma_start(out=lnb[:], in_=ln_b_d.partition_broadcast(128)), writes=[lnb])

            kvf = p.sb('kvf', [128, 328], F32, s2)
            ub = p.sb('ub', [128, 512], F32, s2)
            gv = p.sb('gv', [128, 512], F32, s2)
            vn = p.sb('vn', [128, 512], F32, s2)
            vnb = p.sb('vnb', [128, 512], BF16, s2)
            bnst = p.sb('bnst', [128, 8], F32, s2)
            zq = p.sb('zq', [128, 1024], BF16, s2)
            kb = p.sb('kb', [128, 192], BF16, s2)
            qT = p.sb('qT', [64, 8, 128], BF16, s2)
            qiT = p.sb('qiT', [64, 8, 128], BF16, s2)
            wsc = p.sb('wsc', [128, 8], F32, s2)
            ycat = p.sb('ycat', [128, D], BF16, s2)
            yT = p.sb('yT', [128, 8, 128], BF16, s2)
            x1 = p.sb('x1', [128, D], F32, s2)
            rden = p.sb('rden', [128, 8], F32, s2)
            qmT = p.sb('qmT', [128, 4, 128], BF16, s2)
            PmT = p.sb('PmT', [128, 2, 4, 128], BF16, s2)
            om = p.sb('om', [128, 512], BF16, s2)
            omT = p.sb('omT', [128, 4, 128], BF16, s2)
            x0s = p.sb('x0s', [64, D], F32, s2)
            ycats = p.sb('ycats', [64, D], BF16, s2)
            kvf_s = p.sb('kvf_s', [64, 328], F32, s2)
            kb_s = p.sb('kb_s', [64, 192], BF16, s2)
            wsc_s = p.sb('wsc_s', [64, 8], F32, s2)
            qTs = p.sb('qTs', [64, 8, 64], BF16, s2)
            qiTs = p.sb('qiTs', [64, 8, 64], BF16, s2)
            sw = ExitStack()
            w_in = p.sb('w_in_sb', [128, 8, P_IN], BF16, sw)

            with ExitStack() as s1:
                stage = [p.sb('wstage%d' % i, [128, P_IN], F32, s1) for i in range(2)]
                load_weight(w_in, w_in_d, 8, P_IN, gcols['mix'], stage)
                load_weight(w_out, w_out_d, 8, D, None, stage)
                load_weight(wq, wq_d, 8, 512, gcols['mem'], stage)
                load_weight(wo, wo_d, 4, D, None, stage)
                wsb = p.sb('wsb', [128, 128], BF16, s1)
                for g in range(4):
                    stg = stage[g % 2]
                    p.dma('sp', lambda e, g=g, stg=stg: e.dma_start(out=stg[:, 0:128], in_=gm_ws_d[g, :, :]), writes=[stg])
                    p.op('dve', lambda e, stg=stg: e.tensor_copy(out=wsb[:], in_=stg[:, 0:128]), reads=[stg], writes=[wsb])
                    bk = nb()
                    bv = bk[:].bitcast(BF16)
                    tr(bv[:, 0:128], wsb[:, :], 128, [wsb], [bk])
                    p.op('dve', lambda e, g=g, bv=bv: e.tensor_tensor(out=WgT[:, g, :], in0=bv[:, 0:128], in1=trilT[:, :], op=ALU.mult),
                         reads=[bk, trilT], writes=[WgT])
                p.dma('sp', lambda e: e.dma_start(out=bsT[:], in_=gm_bs_d.rearrange("g t -> t g"), allow_slow_non_contiguous=True), writes=[bsT])
                w4f = p.sb('w4f', [64, 4, 64], F32, s1)
                blk64 = p.sb('blk64', [64, 64], F32, s1)
                p.dma('sp', lambda e: e.dma_start(out=blk64[:], in_=c_blk64[:, :]), writes=[blk64])
                p.op('pool', lambda e: e.memset(w4f[:], 0.0), writes=[w4f])
                for g in range(4):
                    for b in range(DEC_B):
                        p.dma('sp', lambda e, g=g, b=b: e.dma_start(
                            out=w4f[4 * b:4 * b + 4, g, 4 * b:4 * b + 4], in_=gm_ws_d[g, 0:4, 0:4].rearrange("t s -> s t"), allow_slow_non_contiguous=True),
                            writes=[w4f])
                    p.op('dve', lambda e, g=g: e.tensor_tensor(out=W4T[:, g, :], in0=w4f[:, g, :], in1=blk64[:, :], op=ALU.mult), reads=[w4f, blk64], writes=[W4T])
                for b in range(DEC_B):
                    p.dma('sp', lambda e, b=b: e.dma_start(out=bsT4[4 * b:4 * b + 4, :], in_=gm_bs_d[:, 0:4].rearrange("g t -> t g"), allow_slow_non_contiguous=True), writes=[bsT4])

                if 'P1' in do:
                    wkv = p.sb('wkv_sb', [128, 8, D], BF16, s1)
                    load_weight(wkv, wkv_d, 8, D, gcols['memn'], stage)
                    mkvf = p.sb('mkvf', [128, D], F32, s1)
                    mkb = p.sb('mkb', [128, 512], BF16, s1)
                    p.op('pool', lambda e: e.memset(mv_aug[:], 1.0), writes=[mv_aug])
                    for mt in range(2):
                        p.dma('sp', lambda e, mt=mt: e.dma_start(out=x0[:], in_=memp_d[mt * 128:(mt + 1) * 128, :]), writes=[x0])
                        norm_T(x0, 128, 'm', xn, xnT, ssd, sq)
                        b0, b1 = nb(), nb()
                        for half, bk in enumerate((b0, b1)):
                            for c in range(8):
                                mm(bk[:, :], xnT[:, c, :], wkv[:, c, half * 512:(half + 1) * 512], c == 0, c == 7, [xnT, wkv], [bk])
                        p.op('act', lambda e, b0=b0: e.copy(out=mkvf[:, 0:512], in_=b0[:, :]), reads=[b0], writes=[mkvf])
                        p.op('dve', lambda e, b1=b1: e.tensor_copy(out=mkvf[:, 512:1024], in_=b1[:, :]), reads=[b1], writes=[mkvf])
                        p.dma('sp', lambda e, mt=mt: e.dma_start(out=memk_p[mt * 128:(mt + 1) * 128, :], in_=mkvf[:, 0:512]), reads=[mkvf], writes=['memk_p'])
                        p.dma('sp', lambda e, mt=mt: e.dma_start(out=memv_p[mt * 128:(mt + 1) * 128, :], in_=mkvf[:, 512:1024]), reads=[mkvf], writes=['memv_p'])
                        p.op('dve', lambda e: e.tensor_copy(out=mkb[:], in_=mkvf[:, 0:512]), reads=[mkvf], writes=[mkb])
                        p.op('pool', lambda e, mt=mt: e.tensor_copy(out=mv_aug[:, mt, :, 0:128], in_=mkvf[:, 512:1024].rearrange("q (h d) -> q h d", h=4)),
                             reads=[mkvf], writes=[mv_aug])
                        bk = nb()
                        bv = bk[:].bitcast(BF16)
                        for h in range(4):
                            tr(bv[:, h * 128:(h + 1) * 128], mkb[:, h * 128:(h + 1) * 128], 128, [mkb], [bk])
                        p.op('act', lambda e, mt=mt, bv=bv: e.copy(out=mkT[:, :, mt * 128:(mt + 1) * 128], in_=bv[:, 0:512].rearrange("q (h m) -> q h m", h=4)),
                             reads=[bk], writes=[mkT])
            p.barrier()

            with ExitStack() as s3:
                kT = [p.sb('kT%d' % g, [64, SEQ], BF16, s3) for g in range(2)]
                kiT = p.sb('kiT', [64, SEQ], BF16, s3)
                v_aug = p.sb('v_aug', [128, NT, 2, 65], BF16, s3)
                sc = p.sb('sc', [128, SEQ], F32, s3)
                work = p.sb('work', [128, SEQ], F32, s3)
                rbuf = [p.sb('rbuf%d' % i, [128, 512], F32, s3) for i in range(2)]
                m8 = p.sb('m8', [128, 8], F32, s3)
                maskb = p.sb('maskb', [128, SEQ], BF16, s3)
                maskT = p.sb('maskT', [128, NT, 128], BF16, s3)
                Eb = [p.sb('Eb%d' % i, [128, 512], BF16, s3) for i in range(3)]
                PTb = [p.sb('PTb%d' % i, [128, 4, 128], BF16, s3) for i in range(3)]
                p.op('pool', lambda e: e.memset(v_aug[:], 1.0), writes=[v_aug])
                if 'P4' in do and cfg.get('interleave_conv', True):
                    cub = [p.sb('cub%d' % i, [128, D], BF16, s3) for i in range(2)]
                    cut = [p.sb('cut%d' % i, [128, D], BF16, s3) for i in range(2)]
                    cvb = [p.sb('cvb%d' % i, [128, D], BF16, s3) for i in range(2)]

                def proj_tile(src_ap, T, ti, is_sample, x0b, ycatb):
                    p.dma('sp', lambda e: e.dma_start(out=x0b[0:T, :], in_=src_ap), writes=[x0b])
                    norm_T(x0b, T, 'a', xn, xnT, ssd, sq)
                    banks = [nb() for _ in range(5)]
                    for n5, bk in enumerate(banks):
                        c0 = n5 * 512
                        w = min(512, P_IN - c0)
                        for c in range(8):
                            mm(bk[0:T, 0:w], xnT[:, c, 0:T], w_in[:, c, c0:c0 + w], c == 0, c == 7, [xnT, w_in], [bk])
                    p.op('act', lambda e: e.activation(out=ub[0:T, :], in_=banks[0][0:T, :], func=AF.Gelu_apprx_tanh), reads=[banks[0]], writes=[ub])
                    p.op('act', lambda e: e.activation(out=gv[0:T, :], in_=banks[1][0:T, :], func=AF.Gelu_apprx_tanh), reads=[banks[1]], writes=[gv])
                    p.op('dve', lambda e: e.tensor_copy(out=zq[0:T, 0:512], in_=banks[2][0:T, :]), reads=[banks[2]], writes=[zq])
                    p.op('dve', lambda e: e.tensor_copy(out=kvf[0:T, 0:256], in_=banks[3][0:T, 0:256]), reads=[banks[3]], writes=[kvf])
                    p.op('dve', lambda e: e.tensor_copy(out=zq[0:T, 512:768], in_=banks[3][0:T, 256:512]), reads=[banks[3]], writes=[zq])
                    p.op('act', lambda e: e.copy(out=zq[0:T, 768:1024], in_=banks[4][0:T, 0:256]), reads=[banks[4]], writes=[zq])
                    p.op('act', lambda e: e.copy(out=kvf[0:T, 256:328], in_=banks[4][0:T, 256:328]), reads=[banks[4]], writes=[kvf])
                    r0 = ti * 128
                    ko, vo, kio = (k_s, v_s, ki_s) if is_sample else (k_p, v_p, ki_p)
                    p.dma('sp', lambda e: e.dma_start(out=ko[r0:r0 + T, :], in_=kvf[0:T, 0:128]), reads=[kvf], writes=['ko'])
                    p.dma('sp', lambda e: e.dma_start(out=vo[r0:r0 + T, :], in_=kvf[0:T, 128:256]), reads=[kvf], writes=['vo'])
                    p.dma('sp', lambda e: e.dma_start(out=kio[r0:r0 + T, :], in_=kvf[0:T, 256:320]), reads=[kvf], writes=['kio'])
                    p.op('dve', lambda e: e.bn_stats(out=bnst[0:T, 0:6], in_=gv[0:T, :]), reads=[gv], writes=[bnst])
                    p.op('dve', lambda e: e.bn_aggr(out=bnst[0:T, 6:8], in_=bnst[0:T, 0:6]), reads=[bnst], writes=[bnst])
                    p.op('dve', lambda e: e.tensor_scalar(out=bnst[0:T, 0:1], in0=bnst[0:T, 7:8], scalar1=EPS, scalar2=None, op0=ALU.add), reads=[bnst], writes=[bnst])
                    p.op('act', lambda e: e.activation(out=bnst[0:T, 1:2], in_=bnst[0:T, 0:1], func=AF.Sqrt), reads=[bnst], writes=[bnst])
                    p.op('dve', lambda e: e.reciprocal(out=bnst[0:T, 2:3], in_=bnst[0:T, 1:2]), reads=[bnst], writes=[bnst])
                    p.op('dve', lambda e: e.tensor_scalar(out=vn[0:T, :], in0=gv[0:T, :], scalar1=bnst[0:T, 6:7], scalar2=bnst[0:T, 2:3],
                                                          op0=ALU.subtract, op1=ALU.mult), reads=[gv, bnst], writes=[vn])
                    p.op('dve', lambda e: e.tensor_tensor(out=vn[0:T, :], in0=vn[0:T, :], in1=lng[0:T, :], op=ALU.mult), reads=[vn, lng], writes=[vn])
                    p.op('dve', lambda e: e.tensor_tensor(out=vn[0:T, :], in0=vn[0:T, :], in1=lnb[0:T, :], op=ALU.add), reads=[vn, lnb], writes=[vn])
                    if is_sample:
                        p.dma('sp', lambda e: e.dma_start(out=gmv_s[0:T, :], in_=vn[0:T, :]), reads=[vn], writes=['gmv_s'])
                    elif ti == NT - 1:
                        p.dma('sp', lambda e: e.dma_start(out=gmv_p[:, :], in_=vn[0:T, :]), reads=[vn], writes=['gmv_p'])
                    p.op('pool', lambda e: e.tensor_copy(out=vnb[0:T, :], in_=vn[0:T, :]), reads=[vn], writes=[vnb])
                    bk = nb()
                    Wm, bsm = (W4T, bsT4) if is_sample else (WgT, bsT)
                    for g in range(4):
                        mm(bk[0:T, g * 128:(g + 1) * 128], Wm[0:T, g, 0:T], vnb[0:T, g * 128:(g + 1) * 128], True, True, [Wm, vnb], [bk])
                    for g in range(4):
                        p.op('dve', lambda e, g=g, bk=bk: e.scalar_tensor_tensor(out=ycatb[0:T, g * 128:(g + 1) * 128], in0=bk[0:T, g * 128:(g + 1) * 128],
                                                                                  scalar=bsm[0:T, g:g + 1], in1=ub[0:T, g * 128:(g + 1) * 128],
                                                                                  op0=ALU.add, op1=ALU.mult), reads=[bk, bsm, ub], writes=[ycatb])

                def feature_major(T, ti, qTb, qiTb):
                    p.op('pool', lambda e: e.tensor_copy(out=kb[0:T, 0:128], in_=kvf[0:T, 0:128]), reads=[kvf], writes=[kb])
                    p.op('pool', lambda e: e.tensor_copy(out=kb[0:T, 128:192], in_=kvf[0:T, 256:320]), reads=[kvf], writes=[kb])
                    p.op('pool', lambda e: e.tensor_scalar(out=wsc[0:T, :], in0=kvf[0:T, 320:328], scalar1=8.0 ** -0.5, scalar2=None, op0=ALU.mult),
                         reads=[kvf], writes=[wsc])
                    for (src, off, dst) in ((zq, 0, qTb), (zq, 512, qiTb)):
                        bk = nb()
                        bv = bk[:].bitcast(BF16)
                        for h in range(8):
                            tr(bv[0:64, h * T:(h + 1) * T], src[0:T, off + h * 64: off + (h + 1) * 64], T, [src], [bk])
                        p.op('act', lambda e, bv=bv, dst=dst: e.copy(out=dst[:, :, 0:T], in_=bv[0:64, 0:8 * T].rearrange("q (h t) -> q h t", h=8)),
                             reads=[bk], writes=[dst])

                def prompt_dsa(ti):
                    T = 128
                    L = 128 * (ti + 1)
                    c_lo = ti * 128
                    bk = nb()
                    bv = bk[:].bitcast(BF16)
                    for g in range(3):
                        tr(bv[0:64, g * 128:(g + 1) * 128], kb[:, g * 64:(g + 1) * 64], 128, [kb], [bk])
                    p.op('act', lambda e, bv=bv: e.copy(out=kT[0][:, c_lo:c_lo + 128], in_=bv[0:64, 0:128]), reads=[bk], writes=[kT[0]])
                    p.op('act', lambda e, bv=bv: e.copy(out=kT[1][:, c_lo:c_lo + 128], in_=bv[0:64, 128:256]), reads=[bk], writes=[kT[1]])
                    p.op('act', lambda e, bv=bv: e.copy(out=kiT[:, c_lo:c_lo + 128], in_=bv[0:64, 256:384]), reads=[bk], writes=[kiT])
                    p.op('pool', lambda e: e.tensor_copy(out=v_aug[:, ti, :, 0:64], in_=kvf[:, 128:256].rearrange("q (g d) -> q g d", g=2)),
                         reads=[kvf], writes=[v_aug])
                    ri = 0
                    for c0 in range(0, L, 512):
                        w = min(512, L - c0)
                        for h in range(8):
                            bk = nb()
                            mm(bk[:, 0:w], qiT[:, h, :], kiT[:, c0:c0 + w], True, True, [qiT, kiT], [bk])
                            rb = rbuf[ri % 2]
                            ri += 1
                            p.op('act', lambda e, bk=bk, rb=rb, w=w: e.activation(out=rb[:, 0:w], in_=bk[:, 0:w], func=AF.Relu), reads=[bk], writes=[rb])
                            if h == 0:
                                p.op('dve', lambda e, rb=rb, w=w, c0=c0: e.tensor_scalar(out=sc[:, c0:c0 + w], in0=rb[:, 0:w], scalar1=wsc[:, 0:1], scalar2=None, op0=ALU.mult),
                                     reads=[rb, wsc], writes=[sc])
                            else:
                                p.op('dve', lambda e, rb=rb, w=w, c0=c0, h=h: e.scalar_tensor_tensor(out=sc[:, c0:c0 + w], in0=rb[:, 0:w], scalar=wsc[:, h:h + 1],
                                                                                                     in1=sc[:, c0:c0 + w], op0=ALU.mult, op1=ALU.add),
                                     reads=[rb, wsc, sc], writes=[sc])
                    p.op('dve', lambda e: e.tensor_tensor(out=sc[:, c_lo:c_lo + 128], in0=sc[:, c_lo:c_lo + 128], in1=negmask[:, :], op=ALU.add),
                         reads=[sc, negmask], writes=[sc])
                    if ti >= 2:
                        for r in range(32):
                            src = sc if r == 0 else work
                            p.op('dve', lambda e, src=src: e.max(out=m8[:, :], in_=src[:, 0:L]), reads=[src], writes=[m8])
                            if r < 31:
                                p.op('dve', lambda e, src=src: e.match_replace(out=work[:, 0:L], in_to_replace=m8[:, :], in_values=src[:, 0:L], imm_value=NEG),
                                     reads=[src, m8], writes=[work])
                        tau = m8[:, 7:8]
                        tau_t = m8
                    else:
                        tau = tau_c[:, 0:1]
                        tau_t = tau_c
                    p.op('dve', lambda e: e.tensor_scalar(out=maskb[:, 0:L], in0=sc[:, 0:L], scalar1=tau, scalar2=None, op0=ALU.is_ge),
                         reads=[sc, tau_t], writes=[maskb])
                    for j0 in range(0, ti + 1, 8):
                        nj = min(8, ti + 1 - j0)
                        bk = nb()
                        bv = bk[:].bitcast(BF16)
                        for jj in range(nj):
                            tr(bv[:, jj * 128:(jj + 1) * 128], maskb[:, (j0 + jj) * 128:(j0 + jj + 1) * 128], 128, [maskb], [bk])
                        p.op('act', lambda e, bv=bv, j0=j0, nj=nj: e.copy(out=maskT[:, j0:j0 + nj, :], in_=bv[:, 0:nj * 128].rearrange("q (j t) -> q j t", j=nj)),
                             reads=[bk], writes=[maskT])
                    seq = [(g, j) for g in range(2) for j in range(ti + 1)]
                    PTl = {}

                    def att_scores(idx):
                        g, j = seq[idx]
                        bk = nb()
                        mm(bk[:, :], kT[g][:, j * 128:(j + 1) * 128], qT[:, 4 * g:4 * g + 4, :].rearrange("q h t -> q (h t)"), True, True, [kT[g], qT], [bk])
                        E = Eb[idx % 3]
                        PT = PTb[idx % 3]
                        p.op('act', lambda e: e.activation(out=E[:, :], in_=bk[:, :], func=AF.Exp, scale=0.125), reads=[bk], writes=[E])
                        p.op('pool', lambda e: e.tensor_tensor(out=PT[:, :, :], in0=E[:, :].rearrange("q (h t) -> q h t", h=4),
                                                               in1=maskT[:, j:j + 1, :].to_broadcast([128, 4, 128]), op=ALU.mult),
                             reads=[E, maskT], writes=[PT])
                        PTl[idx] = PT

                    def att_pv(idx):
                        g, j = seq[idx]
                        ob = pb[6 + g]
                        PT = PTl[idx]
                        for hh in range(4):
                            mm(ob[:, hh * 65:(hh + 1) * 65], PT[:, hh, :], v_aug[:, j, g, :], (j == 0 and hh == 0), (j == ti), [PT, v_aug], [ob])
                        if j == ti:
                            p.op('dve', lambda e: e.reciprocal(out=rden[:, 4 * g:4 * g + 4], in_=ob[:, 0:260].rearrange("q (h d) -> q h d", h=4)[:, :, 64]),
                                 reads=[ob], writes=[rden])
                            p.op('dve', lambda e: e.tensor_tensor(out=ycat[:, 512 + 256 * g:512 + 256 * (g + 1)].rearrange("q (h d) -> q h d", h=4),
                                                                  in0=ob[:, 0:260].rearrange("q (h d) -> q h d", h=4)[:, :, 0:64],
                                                                  in1=rden[:, 4 * g:4 * g + 4].unsqueeze(2).to_broadcast([128, 4, 64]), op=ALU.mult),
                                 reads=[ob, rden], writes=[ycat])
                    for idx in range(len(seq) + 1):
                        if idx < len(seq):
                            att_scores(idx)
                        if idx >= 1:
                            att_pv(idx - 1)

                def out_proj_and_mem(T, row0, is_sample, x0b, ycatb):
                    bk = nb()
                    bv = bk[:].bitcast(BF16)
                    for c in range(8):
                        tr(bv[:, c * T:(c + 1) * T], ycatb[0:T, c * 128:(c + 1) * 128], T, [ycatb], [bk])
                    p.op('act', lambda e, bv=bv: e.copy(out=yT[:, :, 0:T], in_=bv[:, 0:8 * T].rearrange("q (c t) -> q c t", c=8)), reads=[bk], writes=[yT])
                    for half in range(2):
                        bk = nb()
                        for c in range(8):
                            mm(bk[0:T, :], yT[:, c, 0:T], w_out[:, c, half * 512:(half + 1) * 512], c == 0, c == 7, [yT, w_out], [bk])
                        p.op('dve', lambda e, bk=bk, half=half: e.tensor_tensor(out=x1[0:T, half * 512:(half + 1) * 512], in0=bk[0:T, :],
                                                                                in1=x0b[0:T, half * 512:(half + 1) * 512], op=ALU.add),
                             reads=[bk, x0b], writes=[x1])
                    norm_T(x1, T, 'b', xn, xnT, ssd, sq)
                    bk = nb()
                    for h in range(4):
                        for c in range(8):
                            mm(bk[:, h * T:(h + 1) * T], wq[:, c, h * 128:(h + 1) * 128], xnT[:, c, 0:T], c == 0, c == 7, [wq, xnT], [bk])
                    p.op('act', lambda e, bk=bk: e.copy(out=qmT[:, :, 0:T], in_=bk[:, 0:4 * T].rearrange("q (h t) -> q h t", h=4)), reads=[bk], writes=[qmT])
                    if not is_sample:
                        for mt in range(2):
                            bk = nb()
                            for h in range(4):
                                mm(bk[:, h * 128:(h + 1) * 128], mkT[:, h, mt * 128:(mt + 1) * 128], qmT[:, h, :], True, True, [mkT, qmT], [bk])
                            p.op('act', lambda e, bk=bk, mt=mt: e.activation(out=PmT[:, mt, :, :], in_=bk[:, :].rearrange("q (h t) -> q h t", h=4),
                                                                             func=AF.Exp, scale=128.0 ** -0.5), reads=[bk], writes=[PmT])
                        for hp in range(2):
                            ob = pb[6 + hp]
                            for mt in range(2):
                                for hh in range(2):
                                    h = 2 * hp + hh
                                    mm(ob[:, hh * 129:(hh + 1) * 129], PmT[:, mt, h, :], mv_aug[:, mt, h, :], (mt == 0 and hh == 0), (mt == 1), [PmT, mv_aug], [ob])
                            p.op('dve', lambda e, ob=ob, hp=hp: e.reciprocal(out=rden[:, 2 * hp:2 * hp + 2], in_=ob[:, 0:258].rearrange("q (h d) -> q h d", h=2)[:, :, 128]),
                                 reads=[ob], writes=[rden])
                            p.op('dve', lambda e, ob=ob, hp=hp: e.tensor_tensor(out=om[:, 256 * hp:256 * (hp + 1)].rearrange("q (h d) -> q h d", h=2),
                                                                                in0=ob[:, 0:258].rearrange("q (h d) -> q h d", h=2)[:, :, 0:128],
                                                                                in1=rden[:, 2 * hp:2 * hp + 2].unsqueeze(2).to_broadcast([128, 2, 128]), op=ALU.mult),
                                 reads=[ob, rden], writes=[om])
                        bk = nb()
                        bv = bk[:].bitcast(BF16)
                        for h in range(4):
                            tr(bv[:, h * T:(h + 1) * T], om[0:T, h * 128:(h + 1) * 128], T, [om], [bk])
                        p.op('act', lambda e, bv=bv: e.copy(out=omT[:, :, 0:T], in_=bv[:, 0:4 * T].rearrange("q (h t) -> q h t", h=4)), reads=[bk], writes=[omT])
                    else:
                        sample_mem_attn()
                    for half in range(2):
                        bk = nb()
                        for h in range(4):
                            mm(bk[0:T, :], omT[:, h, 0:T], wo[:, h, half * 512:(half + 1) * 512], h == 0, h == 3, [omT, wo], [bk])
                        p.op('dve', lambda e, bk=bk, half=half: e.tensor_tensor(out=x0b[0:T, half * 512:(half + 1) * 512], in0=bk[0:T, :],
                                                                                in1=x1[0:T, half * 512:(half + 1) * 512], op=ALU.add),
                             reads=[bk, x1], writes=[x0b])
                    p.dma('sp', lambda e: e.dma_start(out=x2s[row0:row0 + T, :], in_=x0b[0:T, :]), reads=[x0b], writes=['x2s'])
                    if dbg:
                        p.dma('sp', lambda e: e.dma_start(out=x2_dbg[row0:row0 + T, :], in_=x0b[0:T, :]), reads=[x0b], writes=['x2_dbg'])

                def sample_mem_attn():
                    for b in range(DEC_B):
                        mf, mb, mT = sm['mf'], sm['mb'], sm['mT']
                        for which, src_d in ((0, cmk_d), (1, cmv_d)):
                            p.dma('sp', lambda e, b=b, src_d=src_d, which=which: e.dma_start(out=mf[which][:, :, :], in_=src_d[b, :, :].rearrange("(mt m) f -> m mt f", mt=2)),
                                  writes=[mf[which]])
                            p.op('pool' if which else 'dve', lambda e, which=which: e.tensor_copy(out=mb[which][:, :, :], in_=mf[which][:, :, :]), reads=[mf[which]], writes=[mb[which]])
                        bk = nb()
                        bv = bk[:].bitcast(BF16)
                        for mt in range(2):
                            for h in range(4):
                                tr(bv[:, (mt * 4 + h) * 128:(mt * 4 + h + 1) * 128], mb[0][:, mt, h * 128:(h + 1) * 128], 128, [mb[0]], [bk])
                        p.op('act', lambda e, bv=bv: e.copy(out=mT[:, :, :], in_=bv[:, :].rearrange("q (k m) -> q k m", k=8)), reads=[bk], writes=[mT])
                        bk = nb()
                        for mt in range(2):
                            for h in range(4):
                                mm(bk[:, (mt * 4 + h) * 4:(mt * 4 + h + 1) * 4], mT[:, mt * 4 + h, :], qmT[:, h, 4 * b:4 * b + 4], True, True, [mT, qmT], [bk])
                        Pm = sm['Pm']
                        p.op('act', lambda e, bk=bk: e.activation(out=Pm[:, :], in_=bk[:, 0:32], func=AF.Exp, scale=128.0 ** -0.5), reads=[bk], writes=[Pm])
                        o6, o7 = pb[6], pb[7]
                        for h in range(4):
                            for mt in range(2):
                                mm(o6[:, h * 4:(h + 1) * 4], mb[1][:, mt, h * 128:(h + 1) * 128], Pm[:, (mt * 4 + h) * 4:(mt * 4 + h + 1) * 4], mt == 0, mt == 1, [mb[1], Pm], [o6])
                        for h in range(4):
                            for mt in range(2):
                                mm(o7[:, h * 4:(h + 1) * 4], ones_bf[:, :], Pm[:, (mt * 4 + h) * 4:(mt * 4 + h + 1) * 4], mt == 0, mt == 1, [ones_bf, Pm], [o7])
                        rc = sm['rc']
                        p.op('dve', lambda e: e.reciprocal(out=rc[:, 0:16], in_=o7[:, 0:16]), reads=[o7], writes=[rc])
                        p.op('dve', lambda e, b=b: e.tensor_tensor(out=omT[:, :, 4 * b:4 * b + 4], in0=o6[:, 0:16].rearrange("q (h t) -> q h t", h=4),
                                                                 in1=rc[:, 0:16].rearrange("q (h t) -> q h t", h=4), op=ALU.mult), reads=[o6, rc], writes=[omT])

                sm = {}

                def sample_dsa(stk):
                    T = NS
                    NIT = 36
                    gbuf = p.sb('gbuf', [64, 8192], F32, stk)
                    cbuf = p.sb('cbuf', [64, 8192], BF16, stk)
                    KTb = p.sb('KTb', [128, 64, 64], BF16, stk)
                    kiTc = p.sb('kiTc', [64, 16, 64], BF16, stk)
                    pt_sb = p.sb('pt_sb', [64, 16], I32, stk)
                    idx2 = p.sb('idx2', [64, 16, 2], I32, stk)
                    rS = p.sb('rS', [64, 16, 32], F32, stk)
                    scTb = p.sb('scTb', [64, 128, 4], F32, stk)
                    scn = p.sb('scn', [4, 4], F32, stk)
                    rSn = p.sb('rSn', [4, 32], F32, stk)
                    Wbc = p.sb('Wbc', [64, 16, 32], F32, stk)
                    Dg = p.sb('Dg', [64, 16, 8, 4], F32, stk)
                    q2T = p.sb('q2T', [128, 4, 64], BF16, stk)
                    kT2n = p.sb('kT2n', [128, 64], BF16, stk)
                    kiTs = p.sb('kiTs', [64, 64], BF16, stk)
                    vnf = p.sb('vnf', [4, 16, 128], F32, stk)
                    vnew = p.sb('vnew', [4, 16, 128], BF16, stk)
                    negm4 = p.sb('negm4', [4, 4], F32, stk)
                    pow2 = p.sb('pow2', [64, 48], F32, stk)
                    tmpA = p.sb('tmpA', [64, 512], F32, stk)
                    hs = p.sb('hs', [64, 4], F32, stk)
                    hsb = p.sb('hsb', [64, 2], BF16, stk)
                    Wsc = p.sb('Wsc', [64, 2], F32, stk)
                    Wtab = p.sb('Wtab', [64, 48], F32, stk)
                    lo = p.sb('lo', [64, 4], F32, stk)
                    mid = p.sb('mid', [64, 4], F32, stk)
                    ge = p.sb('ge', [64, 4], F32, stk)
                    cmpb = p.sb('cmpb', [64, 128, 4], BF16, stk)
                    cntp = p.sb('cntp', [64, 4], F32, stk)
                    cmpn = p.sb('cmpn', [4, 4], F32, stk)
                    mask_s = p.sb('mask_s', [64, 128, 4], BF16, stk)
                    maskn = p.sb('maskn', [4, 4], BF16, stk)
                    Es = p.sb('Es', [64, 16, 32], BF16, stk)
                    PTs = p.sb('PTs', [64, 16, 32], BF16, stk)
                    En = p.sb('En', [4, 32], BF16, stk)
                    PTn = p.sb('PTn', [4, 32], BF16, stk)
                    rcs = p.sb('rcs', [128, 32], F32, stk)
                    ybTs = p.sb('ybTs', [128, 4, 64], BF16, stk)
                    sm['mf'] = [p.sb('mf%d' % i, [128, 2, 512], F32, stk) for i in range(2)]
                    sm['mb'] = [p.sb('mb%d' % i, [128, 2, 512], BF16, stk) for i in range(2)]
                    sm['mT'] = p.sb('mT', [128, 8, 128], BF16, stk)
                    sm['Pm'] = p.sb('Pm', [128, 32], BF16, stk)
                    sm['rc'] = p.sb('rc', [128, 16], F32, stk)

                    p.dma('sp', lambda e: e.dma_start(out=pt_sb[:, :], in_=pt_d.rearrange("b j -> j b"), allow_slow_non_contiguous=True), writes=[pt_sb])
                    p.dma('sp', lambda e: e.dma_start(out=negm4[:, :], in_=c_negm4[:, :]), writes=[negm4])
                    p.dma('sp', lambda e: e.dma_start(out=pow2[:, :], in_=c_pow2[0:64, :]), writes=[pow2])
                    for half in range(2):
                        p.op('dve', lambda e, half=half: e.tensor_scalar(out=idx2[:, :, half], in0=pt_sb[:, :], scalar1=2, scalar2=half, op0=ALU.mult, op1=ALU.add),
                             reads=[pt_sb], writes=[idx2])
                    p.op('dve', lambda e: e.tensor_tensor(out=Dg[:, :, :, :], in0=wsc_s[:, :].unsqueeze(1).unsqueeze(3).to_broadcast([64, 16, 8, 4]),
                                                          in1=identf[0:64, 0:64].rearrange("q (b t) -> q b t", b=16).unsqueeze(2).to_broadcast([64, 16, 8, 4]), op=ALU.mult),
                         reads=[wsc_s, identf], writes=[Dg])
                    bk = nb()
                    mm(bk[0:64, :], onesf[0:64, 0:64], Dg[:, :, :, :].rearrange("q b h t -> q (b h t)"), True, True, [onesf, Dg], [bk])
                    p.op('act', lambda e, bk=bk: e.copy(out=Wbc[:, :, :], in_=bk[0:64, :].rearrange("q (b x) -> q b x", b=16)), reads=[bk], writes=[Wbc])
                    p.op('pool', lambda e: e.tensor_copy(out=q2T[0:64, :, :], in_=qTs[:, 0:4, :]), reads=[qTs], writes=[q2T])
                    p.dma('sp', lambda e: e.dma_start(out=q2T[64:128, :, :], in_=qTs[:, 4:8, :]), reads=[qTs], writes=[q2T])
                    bk = nb()
                    bv = bk[:].bitcast(BF16)
                    tr(bv[:, 0:64], kb_s[:, 0:128], 64, [kb_s], [bk])
                    tr(bv[0:64, 64:128], kb_s[:, 128:192], 64, [kb_s], [bk])
                    p.op('act', lambda e, bv=bv: e.copy(out=kT2n[:, :], in_=bv[:, 0:64]), reads=[bk], writes=[kT2n])
                    p.op('act', lambda e, bv=bv: e.copy(out=kiTs[:, :], in_=bv[0:64, 64:128]), reads=[bk], writes=[kiTs])
                    p.dma('sp', lambda e: e.dma_start(out=vnf[:, :, :], in_=v_s.rearrange("(b t) d -> t b d", t=4)), reads=['vo'], writes=[vnf])
                    p.op('dve', lambda e: e.tensor_copy(out=vnew[:, :, :], in_=vnf[:, :, :]), reads=[vnf], writes=[vnew])

                    def gather(src_d, idx_ap, idx_t):
                        p.dma('pool', lambda e: e.indirect_dma_start(out=gbuf[:, :], out_offset=None, in_=src_d[:, :],
                                                                     in_offset=bass.IndirectOffsetOnAxis(ap=idx_ap, axis=0)), reads=[idx_t], writes=[gbuf])
                        p.op('act', lambda e: e.copy(out=cbuf[:, 0:4096], in_=gbuf[:, 0:4096]), reads=[gbuf], writes=[cbuf])
                        p.op('dve', lambda e: e.tensor_copy(out=cbuf[:, 4096:8192], in_=gbuf[:, 4096:8192]), reads=[gbuf], writes=[cbuf])

                    for b in range(cfg.get('n_sb', DEC_B)):
                        qi_b = qiTs[:, :, 4 * b:4 * b + 4]
                        gather(ckidx_d, pt_sb[:, b:b + 1], pt_sb)
                        for lc in range(8):
                            bk = nb()
                            bv = bk[:].bitcast(BF16)
                            for i in range(16):
                                l = lc * 16 + i
                                tr(bv[0:64, i * 64:(i + 1) * 64], cbuf[:, l * 64:(l + 1) * 64], 64, [cbuf], [bk])
                            p.op('act', lambda e, bv=bv: e.copy(out=kiTc[:, :, :], in_=bv[0:64, :].rearrange("q (i j) -> q i j", i=16)), reads=[bk], writes=[kiTc])
                            bk = nb()
                            for i in range(16):
                                mm(bk[0:64, i * 32:(i + 1) * 32], kiTc[:, i, :], qi_b, True, True, [kiTc, qiTs], [bk])
                            p.op('act', lambda e, bk=bk: e.activation(out=rS[:, :, :], in_=bk[0:64, :].rearrange("q (i x) -> q i x", i=16), func=AF.Relu), reads=[bk], writes=[rS])
                            p.op('dve', lambda e, b=b: e.tensor_tensor(out=rS[:, :, :], in0=rS[:, :, :], in1=Wbc[:, b:b + 1, :].to_broadcast([64, 16, 32]), op=ALU.mult),
                                 reads=[rS, Wbc], writes=[rS])
                            p.op('dve', lambda e, lc=lc: e.tensor_reduce(out=scTb[:, lc * 16:(lc + 1) * 16, :], in_=rS[:, :, :].rearrange("q i (h t) -> q i t h", h=8),
                                                                          op=ALU.add, axis=AX.X), reads=[rS], writes=[scTb])
                        if cfg.get('sb_stage', 9) < 2:
                            continue
                        bk = nb()
                        mm(bk[0:4, 0:32], kiTs[:, 4 * b:4 * b + 4], qi_b, True, True, [kiTs, qiTs], [bk])
                        p.op('act', lambda e, bk=bk: e.activation(out=rSn[:, :], in_=bk[0:4, 0:32], func=AF.Relu), reads=[bk], writes=[rSn])
                        p.op('dve', lambda e, b=b: e.tensor_tensor(out=rSn[:, :], in0=rSn[:, :], in1=Wbc[0:4, b, :], op=ALU.mult), reads=[rSn, Wbc], writes=[rSn])
                        p.op('dve', lambda e: e.tensor_reduce(out=scn[:, :], in_=rSn[:, :].rearrange("q (h t) -> q t h", h=8), op=ALU.add, axis=AX.X), reads=[rSn], writes=[scn])
                        p.op('dve', lambda e: e.tensor_reduce(out=hs[:, 0:1], in_=scTb[:, :, :].rearrange("q l t -> q (l t)"), op=ALU.add, axis=AX.X, apply_absolute_value=True),
                             reads=[scTb], writes=[hs])
                        p.op('dve', lambda e: e.tensor_reduce(out=hs[0:4, 1:2], in_=scn[:, :], op=ALU.add, axis=AX.X, apply_absolute_value=True), reads=[scn], writes=[hs])
                        p.op('dve', lambda e: e.tensor_tensor(out=scn[:, :], in0=scn[:, :], in1=negm4[:, :], op=ALU.add), reads=[scn, negm4], writes=[scn])
                        bk = nb()
                        mm(bk[0:64, 0:1], onesf[0:64, 0:64], hs[:, 0:1], True, False, [onesf, hs], [bk])
                        mm(bk[0:64, 0:1], onesf[0:4, 0:64], hs[0:4, 1:2], False, True, [onesf, hs], [bk])
                        p.op('dve', lambda e, bk=bk: e.tensor_scalar(out=Wsc[:, 0:1], in0=bk[0:64, 0:1], scalar1=2.2, scalar2=2.0, op0=ALU.mult, op1=ALU.add), reads=[bk], writes=[Wsc])
                        p.op('dve', lambda e: e.tensor_scalar(out=Wsc[:, 1:2], in0=Wsc[:, 0:1], scalar1=-0.5, scalar2=None, op0=ALU.mult), reads=[Wsc], writes=[Wsc])
                        p.op('dve', lambda e: e.tensor_scalar(out=Wtab[:, :], in0=pow2[:, :], scalar1=Wsc[:, 0:1], scalar2=None, op0=ALU.mult), reads=[pow2, Wsc], writes=[Wtab])
                        p.op('dve', lambda e: e.tensor_scalar(out=lo[:, :], in0=pow2[:, 0:4], scalar1=0.0, scalar2=Wsc[:, 1:2], op0=ALU.mult, op1=ALU.add), reads=[pow2, Wsc], writes=[lo])
                        for k in range(NIT):
                            p.op('dve', lambda e, k=k: e.tensor_scalar(out=mid[:, :], in0=lo[:, :], scalar1=Wtab[:, k:k + 1], scalar2=None, op0=ALU.add), reads=[lo, Wtab], writes=[mid])
                            p.op('dve', lambda e: e.tensor_tensor(out=cmpb[:, :, :], in0=scTb[:, :, :], in1=mid[:, :].unsqueeze(1).to_broadcast([64, 128, 4]), op=ALU.is_ge),
                                 reads=[scTb, mid], writes=[cmpb])
                            p.op('dve', lambda e: e.tensor_reduce(out=cntp[:, :], in_=cmpb[:, :, :].rearrange("q l t -> q t l"), op=ALU.add, axis=AX.X), reads=[cmpb], writes=[cntp])
                            p.op('dve', lambda e: e.tensor_tensor(out=cmpn[:, :], in0=scn[:, :], in1=mid[0:4, :], op=ALU.is_ge), reads=[scn, mid], writes=[cmpn])
                            bk = nb()
                            mm(bk[0:64, 0:4], onesf[0:64, 0:64], cntp[:, :], True, False, [onesf, cntp], [bk])
                            mm(bk[0:64, 0:4], onesf[0:4, 0:64], cmpn[:, :], False, True, [onesf, cmpn], [bk])
                            p.op('dve', lambda e, bk=bk: e.tensor_scalar(out=ge[:, :], in0=bk[0:64, 0:4], scalar1=255.5, scalar2=None, op0=ALU.is_ge), reads=[bk], writes=[ge])
                            p.op('dve', lambda e, k=k: e.scalar_tensor_tensor(out=lo[:, :], in0=ge[:, :], scalar=Wtab[:, k:k + 1], in1=lo[:, :], op0=ALU.mult, op1=ALU.add),
                                 reads=[ge, Wtab, lo], writes=[lo])
                        p.op('dve', lambda e: e.tensor_tensor(out=mask_s[:, :, :], in0=scTb[:, :, :], in1=lo[:, :].unsqueeze(1).to_broadcast([64, 128, 4]), op=ALU.is_ge),
                             reads=[scTb, lo], writes=[mask_s])
                        p.op('dve', lambda e: e.tensor_tensor(out=maskn[:, :], in0=scn[:, :], in1=lo[0:4, :], op=ALU.is_ge), reads=[scn, lo], writes=[maskn])
                        if cfg.get('sb_stage', 9) < 3:
                            continue
                        o6, o7 = pb[6], pb[7]
                        first = True
                        for half in range(2):
                            gather(ck_d, idx2[:, b, half:half + 1], idx2)
                            for lc in range(4):
                                bk = nb()
                                bv = bk[:].bitcast(BF16)
                                for i in range(16):
                                    l = lc * 16 + i
                                    tr(bv[:, i * 64:(i + 1) * 64], cbuf[:, l * 128:(l + 1) * 128], 64, [cbuf], [bk])
                                p.op('act', lambda e, bv=bv, lc=lc: e.copy(out=KTb[:, lc * 16:(lc + 1) * 16, :], in_=bv[:, :].rearrange("q (i j) -> q i j", i=16)), reads=[bk], writes=[KTb])
                            gather(cv_d, idx2[:, b, half:half + 1], idx2)
                            for lc in range(4):
                                bkg = [nb(), nb()]
                                for i in range(16):
                                    l = lc * 16 + i
                                    for g in range(2):
                                        mm(bkg[g][0:64, i * 16:(i + 1) * 16], KTb[g * 64:(g + 1) * 64, l, :], q2T[g * 64:(g + 1) * 64, :, 4 * b:4 * b + 4], True, True, [KTb, q2T], [bkg[g]])
                                for g in range(2):
                                    p.op('act', lambda e, g=g, bkg=bkg: e.activation(out=Es[:, :, g * 16:(g + 1) * 16], in_=bkg[g][0:64, 0:256].rearrange("q (i x) -> q i x", i=16),
                                                                                     func=AF.Exp, scale=0.125), reads=[bkg[g]], writes=[Es])
                                l0 = half * 64 + lc * 16
                                p.op('pool', lambda e, l0=l0: e.tensor_tensor(out=PTs[:, :, :].rearrange("q i (x t) -> q i x t", t=4), in0=Es[:, :, :].rearrange("q i (x t) -> q i x t", t=4),
                                                                             in1=mask_s[:, l0:l0 + 16, :].unsqueeze(2).to_broadcast([64, 16, 8, 4]), op=ALU.mult),
                                     reads=[Es, mask_s], writes=[PTs])
                                for i in range(16):
                                    l = lc * 16 + i
                                    mm(o6[:, 0:32], cbuf[:, l * 128:(l + 1) * 128], PTs[:, i, :], first, False, [cbuf, PTs], [o6])
                                    mm(o7[:, 0:32], ones_bf[0:64, :], PTs[:, i, :], first, False, [ones_bf, PTs], [o7])
                                    first = False
                        bkg = [nb(), nb()]
                        for g in range(2):
                            mm(bkg[g][0:4, 0:16], kT2n[g * 64:(g + 1) * 64, 4 * b:4 * b + 4], q2T[g * 64:(g + 1) * 64, :, 4 * b:4 * b + 4], True, True, [kT2n, q2T], [bkg[g]])
                        for g in range(2):
                            p.op('act', lambda e, g=g, bkg=bkg: e.activation(out=En[:, g * 16:(g + 1) * 16], in_=bkg[g][0:4, 0:16], func=AF.Exp, scale=0.125), reads=[bkg[g]], writes=[En])
                        p.op('pool', lambda e: e.tensor_tensor(out=PTn[:, :].rearrange("q (x t) -> q x t", t=4), in0=En[:, :].rearrange("q (x t) -> q x t", t=4),
                                                               in1=maskn[:, :].unsqueeze(1).to_broadcast([4, 8, 4]), op=ALU.mult), reads=[En, maskn], writes=[PTn])
                        mm(o6[:, 0:32], vnew[:, b, :], PTn[:, :], False, True, [vnew, PTn], [o6])
                        mm(o7[:, 0:32], ones_bf[0:4, :], PTn[:, :], False, True, [ones_bf, PTn], [o7])
                        p.op('dve', lambda e: e.reciprocal(out=rcs[:, :], in_=o7[:, 0:32]), reads=[o7], writes=[rcs])
                        for g in range(2):
                            p.op('dve', lambda e, g=g, b=b: e.tensor_tensor(out=ybTs[g * 64:(g + 1) * 64, :, 4 * b:4 * b + 4],
                                                                          in0=o6[g * 64:(g + 1) * 64, g * 16:(g + 1) * 16].rearrange("q (r t) -> q r t", r=4),
                                                                          in1=rcs[g * 64:(g + 1) * 64, g * 16:(g + 1) * 16].rearrange("q (r t) -> q r t", r=4), op=ALU.mult),
                                 reads=[o6, rcs], writes=[ybTs])
                    for r in range(4):
                        bk = nb()
                        bv = bk[:].bitcast(BF16)
                        tr(bv[0:64, 0:128], ybTs[:, r, :], 128, [ybTs], [bk])
                        p.op('act', lambda e, bv=bv, r=r: e.copy(out=ycats[:, 512:1024].rearrange("q (g r d) -> q g r d", g=2, r=4)[:, :, r, :],
                                                                 in_=bv[0:64, 0:128].rearrange("q (g d) -> q g d", g=2)), reads=[bk], writes=[ycats])


                if 'P3' in do:
                    proj_tile(xs_d[:, :], NS, 0, True, x0s, ycats)
                    feature_major(NS, 0, qTs, qiTs)
                    p.op('pool', lambda e: e.tensor_copy(out=kvf_s[:, :], in_=kvf[0:NS, :]), reads=[kvf], writes=[kvf_s])
                    p.op('pool', lambda e: e.tensor_copy(out=kb_s[:, :], in_=kb[0:NS, :]), reads=[kb], writes=[kb_s])
                    p.op('pool', lambda e: e.tensor_copy(out=wsc_s[:, :], in_=wsc[0:NS, :]), reads=[wsc], writes=[wsc_s])
                if 'P2' in do:
                    for ti in range(cfg.get('n_tiles', NT)):
                        proj_tile(xp_d[ti * 128:(ti + 1) * 128, :], 128, ti, False, x0, ycat)
                        feature_major(128, ti, qT, qiT)
                        if 'P4' in do and cfg.get('interleave_conv', True):
                            for ec in range(ti * 8, ti * 8 + 8):
                                convert_chunk(ec, cub[ec % 2], cut[ec % 2], cvb[ec % 2])
                            conv_done[0] = ti * 8 + 8
                        prompt_dsa(ti)
                        out_proj_and_mem(128, ti * 128, False, x0, ycat)
            sw.close()
            p.barrier()
            if 'P3' in do:
                with ExitStack() as s3s:
                    sample_dsa(s3s)
                    out_proj_and_mem(NS, SEQ, True, x0s, ycats)
            p.barrier()

        if 'P4' in do:
            nbmod[0] = 4
            with ExitStack() as s4:
                iota_f = p.sb('iota_f', [128, 128], F32, s4)
                gfin = p.sb('gfin', [128, D], F32, s4)
                p.dma('sp', lambda e: e.dma_start(out=iota_f[:], in_=c_iota[:, :]), writes=[iota_f])
                p.dma('sp', lambda e: e.dma_start(out=gfin[:], in_=g_fin_d.partition_broadcast(128)), writes=[gfin])
                pwq = p.sb('pwq_sb', [128, 8, D], BF16, s4)
                keysT = p.sb('keysT', [64, 16, 128], BF16, s4)
                with ExitStack() as s5:
                    stage = [p.sb('pstage%d' % i, [128, D], F32, s5) for i in range(2)]
                    load_weight(pwq, pwq_d, 8, D, None, stage)
                    kbf = p.sb('kbf', [128, 64], BF16, s5)
                    for hc in range(16):
                        stg = stage[hc % 2]
                        p.dma('sp', lambda e, hc=hc, stg=stg: e.dma_start(out=stg[:, 0:64], in_=pkeys_d[hc, :, :]), writes=[stg])
                        p.op('dve', lambda e, stg=stg: e.tensor_copy(out=kbf[:], in_=stg[:, 0:64]), reads=[stg], writes=[kbf])
                        bk = nb()
                        bv = bk[:].bitcast(BF16)
                        tr(bv[0:64, 0:128], kbf[:, :], 128, [kbf], [bk])
                        p.op('act', lambda e, hc=hc, bv=bv: e.copy(out=keysT[:, hc, :], in_=bv[0:64, 0:128]), reads=[bk], writes=[keysT])
                    ubf = [p.sb('ubf%d' % i, [128, D], BF16, s5) for i in range(2)]
                    utb = [p.sb('utb%d' % i, [128, D], BF16, s5) for i in range(2)]
                    vbf = [p.sb('vbf%d' % i, [128, D], BF16, s5) for i in range(2)]
                    for ec in range(conv_done[0], cfg.get('n_ec', 128)):
                        convert_chunk(ec, ubf[ec % 2], utb[ec % 2], vbf[ec % 2])

                p.barrier()
                xg = p.sb('xg', [128, 2, D], F32, s4)
                XT = p.sb('XT', [128, 8, 256], BF16, s4)
                hn = p.sb('hn', [128, D], BF16, s4)
                hnT = p.sb('hnT', [128, 8, 128], BF16, s4)
                sq4 = p.sb('sq4', [128, D], F32, s4)
                qpT = p.sb('qpT', [64, 16, 128], BF16, s4)
                ssb = p.sb('ssb', [128, 16, 128], F32, s4)
                wk = p.sb('wk', [128, 128], F32, s4)
                a16 = p.sb('a16', [128, 8, 16], F32, s4)
                b16 = p.sb('b16', [128, 8, 16], F32, s4)
                iau = p.sb('iau', [128, 8, 16], U32, s4)
                ibu = p.sb('ibu', [128, 8, 16], U32, s4)
                iaf = p.sb('iaf', [128, 8, 16], F32, s4)
                ibf = p.sb('ibf', [128, 8, 16], F32, s4)
                cand = p.sb('cand', [128, 8, 256], F32, s4)
                wkc = p.sb('wkc', [128, 256], F32, s4)
                c16 = p.sb('c16', [128, 8, 16], F32, s4)
                icu = p.sb('icu', [128, 8, 16], U32, s4)
                iju = p.sb('iju', [128, 2, 128], U32, s4)
                ijf = p.sb('ijf', [128, 2, 128], F32, s4)
                eq = p.sb('eq', [128, 128, 16], F32, s4)
                SEL = p.sb('SEL', [128, 3, 128], F32, s4)
                e16 = p.sb('e16', [128, 8, 16], F32, s4)
                z8 = p.sb('z8', [128, 16], F32, s4)
                selT = p.sb('selT', [128, 3, 256], F32, s4)
                Gt = p.sb('Gt', [128, 256, 128], BF16, s4)
                ohb1 = [p.sb('ohb1_%d' % i, [128, 8, 128], BF16, s4) for i in range(2)]
                ohb0 = [p.sb('ohb0_%d' % i, [128, 8, 128], BF16, s4) for i in range(2)]
                ohbg = [p.sb('ohbg_%d' % i, [128, 8, 128], BF16, s4) for i in range(2)]
                selTb = p.sb('selTb', [128, 3, 256], BF16, s4)
                iota_b = p.sb('iota_b', [128, 128], BF16, s4)
                p.op('pool', lambda e: e.tensor_copy(out=iota_b[:, :], in_=iota_f[:, :]), reads=[iota_f], writes=[iota_b])
                NBUF = 4
                UTc = [p.sb('UTc%d' % i, [128, 8, 128], BF16, s4) for i in range(NBUF)]
                Vc = [p.sb('Vc%d' % i, [128, D], BF16, s4) for i in range(NBUF)]
                gab = [p.sb('gab%d' % i, [128, 256], BF16, s4) for i in range(3)]
                GAb = [p.sb('GAb%d' % i, [128, 256], BF16, s4) for i in range(3)]
                pre = p.sb('pre', [128, D], F32, s4)
                yo = pre


                def peer_select(T, s):
                    norm_T(xg[:, s, :], T, 'f', hn, hnT, ssd, sq4, gcol=gcols['ffn'])
                    p.op('pool', lambda e: e.tensor_copy(out=XT[:, :, s * 128:s * 128 + T], in_=hnT[:, :, 0:T]), reads=[hnT], writes=[XT])
                    for q4 in range(4):
                        bk = nb()
                        for k4 in range(4):
                            hc = q4 * 4 + k4
                            for c in range(8):
                                mm(bk[0:64, k4 * T:(k4 + 1) * T], pwq[:, c, hc * 64:(hc + 1) * 64], hnT[:, c, 0:T], c == 0, c == 7, [pwq, hnT], [bk])
                        p.op('act', lambda e, bk=bk, q4=q4: e.copy(out=qpT[:, q4 * 4:(q4 + 1) * 4, 0:T], in_=bk[0:64, 0:4 * T].rearrange("q (k t) -> q k t", k=4)),
                             reads=[bk], writes=[qpT])
                    for q4 in range(4):
                        bk = nb()
                        for k4 in range(4):
                            hc = q4 * 4 + k4
                            mm(bk[0:T, k4 * 128:(k4 + 1) * 128], qpT[:, hc, 0:T], keysT[:, hc, :], True, True, [qpT, keysT], [bk])
                        p.op('act', lambda e, bk=bk, q4=q4: e.copy(out=ssb[0:T, q4 * 4:(q4 + 1) * 4, :], in_=bk[0:T, :].rearrange("q (k n) -> q k n", k=4)),
                             reads=[bk], writes=[ssb])
                    for h in range(8):
                        for cc, (vals, idxs) in enumerate(((a16, iau), (b16, ibu))):
                            src = ssb[0:T, 2 * h + cc, :]
                            p.op('dve', lambda e, src=src, vals=vals, h=h: e.max(out=vals[0:T, h, 0:8], in_=src), reads=[ssb], writes=[vals])
                            p.op('dve', lambda e, src=src, vals=vals, idxs=idxs, h=h: e.max_index(out=idxs[0:T, h, 0:8], in_max=vals[0:T, h, 0:8], in_values=src),
                                 reads=[ssb, vals], writes=[idxs])
                            p.op('dve', lambda e, src=src, vals=vals, h=h: e.match_replace(out=wk[0:T, :], in_to_replace=vals[0:T, h, 0:8], in_values=src, imm_value=NEG),
                                 reads=[ssb, vals], writes=[wk])
                            p.op('dve', lambda e, vals=vals, h=h: e.max(out=vals[0:T, h, 8:16], in_=wk[0:T, :]), reads=[wk], writes=[vals])
                            p.op('dve', lambda e, vals=vals, idxs=idxs, h=h: e.max_index(out=idxs[0:T, h, 8:16], in_max=vals[0:T, h, 8:16], in_values=wk[0:T, :]),
                                 reads=[wk, vals], writes=[idxs])
                    for h in range(8):
                        p.op('dve', lambda e, h=h: e.tensor_tensor(out=cand[0:T, h, :].rearrange("q (i j) -> q i j", i=16),
                                                                    in0=a16[0:T, h, :].unsqueeze(2).to_broadcast([T, 16, 16]),
                                                                    in1=b16[0:T, h, :].unsqueeze(1).to_broadcast([T, 16, 16]), op=ALU.add),
                             reads=[a16, b16], writes=[cand])
                    for h in range(8):
                        src = cand[0:T, h, :]
                        p.op('dve', lambda e, src=src, h=h: e.max(out=c16[0:T, h, 0:8], in_=src), reads=[cand], writes=[c16])
                        p.op('dve', lambda e, src=src, h=h: e.max_index(out=icu[0:T, h, 0:8], in_max=c16[0:T, h, 0:8], in_values=src), reads=[cand, c16], writes=[icu])
                        p.op('dve', lambda e, src=src, h=h: e.match_replace(out=wkc[0:T, :], in_to_replace=c16[0:T, h, 0:8], in_values=src, imm_value=NEG),
                             reads=[cand, c16], writes=[wkc])
                        p.op('dve', lambda e, h=h: e.max(out=c16[0:T, h, 8:16], in_=wkc[0:T, :]), reads=[wkc], writes=[c16])
                        p.op('dve', lambda e, h=h: e.max_index(out=icu[0:T, h, 8:16], in_max=c16[0:T, h, 8:16], in_values=wkc[0:T, :]), reads=[wkc, c16], writes=[icu])
                    icf = icu[0:T, :, :].rearrange("q h k -> q (h k)")
                    p.op('dve', lambda e: e.tensor_scalar(out=iju[0:T, 0, :], in0=icf, scalar1=4, scalar2=None, op0=ALU.logical_shift_right), reads=[icu], writes=[iju])
                    p.op('dve', lambda e: e.tensor_scalar(out=iju[0:T, 1, :], in0=icf, scalar1=15, scalar2=None, op0=ALU.bitwise_and), reads=[icu], writes=[iju])
                    p.op('dve', lambda e: e.tensor_copy(out=ijf[0:T, :, :], in_=iju[0:T, :, :]), reads=[iju], writes=[ijf])
                    p.op('dve', lambda e: e.tensor_copy(out=iaf[0:T, :, :], in_=iau[0:T, :, :]), reads=[iau], writes=[iaf])
                    p.op('dve', lambda e: e.tensor_copy(out=ibf[0:T, :, :], in_=ibu[0:T, :, :]), reads=[ibu], writes=[ibf])
                    for w_, srcf in ((0, iaf), (1, ibf)):
                        p.op('dve', lambda e, w_=w_: e.tensor_tensor(out=eq[0:T, :, :], in0=ijf[0:T, w_, :].unsqueeze(2).to_broadcast([T, 128, 16]),
                                                                     in1=iota_f[0:T, 0:16].unsqueeze(1).to_broadcast([T, 128, 16]), op=ALU.is_equal),
                             reads=[ijf, iota_f], writes=[eq])
                        p.op('dve', lambda e, srcf=srcf: e.tensor_tensor(out=eq[0:T, :, :].rearrange("q (h k) i -> q h k i", h=8),
                                                                         in0=eq[0:T, :, :].rearrange("q (h k) i -> q h k i", h=8),
                                                                         in1=srcf[0:T, :, :].unsqueeze(2).to_broadcast([T, 8, 16, 16]), op=ALU.mult),
                             reads=[eq, srcf], writes=[eq])
                        p.op('dve', lambda e, w_=w_: e.tensor_reduce(out=SEL[0:T, w_, :], in_=eq[0:T, :, :], op=ALU.add, axis=AX.X), reads=[eq], writes=[SEL])
                    p.op('dve', lambda e: e.tensor_tensor(out=e16[0:T, :, :], in0=c16[0:T, :, :], in1=c16[0:T, :, 0:1].to_broadcast([T, 8, 16]), op=ALU.subtract),
                         reads=[c16], writes=[e16])
                    p.op('act', lambda e: e.activation(out=e16[0:T, :, :], in_=e16[0:T, :, :], func=AF.Exp), reads=[e16], writes=[e16])
                    p.op('dve', lambda e: e.tensor_reduce(out=z8[0:T, 0:8], in_=e16[0:T, :, :], op=ALU.add, axis=AX.X), reads=[e16], writes=[z8])
                    p.op('dve', lambda e: e.reciprocal(out=z8[0:T, 8:16], in_=z8[0:T, 0:8]), reads=[z8], writes=[z8])
                    p.op('dve', lambda e: e.tensor_tensor(out=SEL[0:T, 2, :].rearrange("q (h k) -> q h k", h=8), in0=e16[0:T, :, :],
                                                          in1=z8[0:T, 8:16].unsqueeze(2).to_broadcast([T, 8, 16]), op=ALU.mult), reads=[e16, z8], writes=[SEL])
                    bk = nb()
                    for w_ in range(3):
                        p.op('pe', lambda e, w_=w_, bk=bk: e.transpose(out=bk[:, w_ * T:(w_ + 1) * T], in_=SEL[0:T, w_, :], identity=identf[0:T, 0:T]),
                             reads=[SEL, identf], writes=[bk])
                    p.op('act', lambda e, bk=bk: e.copy(out=selT[:, :, s * 128:s * 128 + T], in_=bk[:, 0:3 * T].rearrange("q (w t) -> q w t", w=3)),
                         reads=[bk], writes=[selT])
                    p.op('pool', lambda e: e.tensor_copy(out=selTb[:, :, s * 128:s * 128 + T], in_=selT[:, :, s * 128:s * 128 + T]), reads=[selT], writes=[selTb])

                def peer_group(row0, Tg, y_out, yrow0):
                    nsub = (Tg + 127) // 128
                    Ts = min(Tg, 128)
                    for s in range(nsub):
                        p.dma('sp', lambda e, s=s: e.dma_start(out=xg[0:Ts, s, :], in_=x2s[row0 + s * 128:row0 + s * 128 + Ts, :]), reads=['x2s'], writes=[xg])
                        peer_select(Ts, s)
                    for t0 in range(0, Tg, 8):
                        o1, o0, og = ohb1[(t0 // 8) % 2], ohb0[(t0 // 8) % 2], ohbg[(t0 // 8) % 2]
                        p.op('dve', lambda e, t0=t0, o1=o1: e.tensor_tensor(out=o1[:, :, :], in0=iota_b[:, :].unsqueeze(1).to_broadcast([128, 8, 128]),
                                                                          in1=selTb[:, 1, t0:t0 + 8].unsqueeze(2).to_broadcast([128, 8, 128]), op=ALU.is_equal),
                             reads=[iota_b, selTb], writes=[o1])
                        p.op('dve', lambda e, t0=t0, o0=o0: e.tensor_tensor(out=o0[:, :, :], in0=iota_b[:, :].unsqueeze(1).to_broadcast([128, 8, 128]),
                                                                          in1=selTb[:, 0, t0:t0 + 8].unsqueeze(2).to_broadcast([128, 8, 128]), op=ALU.is_equal),
                             reads=[iota_b, selTb], writes=[o0])
                        p.op('pool', lambda e, t0=t0, o0=o0, og=og: e.tensor_tensor(out=og[:, :, :], in0=o0[:, :, :],
                                                                                  in1=selTb[:, 2, t0:t0 + 8].unsqueeze(2).to_broadcast([128, 8, 128]), op=ALU.mult),
                             reads=[o0, selTb], writes=[og])
                        for q4 in range(2):
                            bk = nb()
                            for tt in range(4):
                                mm(bk[:, tt * 128:(tt + 1) * 128], o1[:, q4 * 4 + tt, :], og[:, q4 * 4 + tt, :], True, True, [o1, og], [bk])
                            p.op('act', lambda e, bk=bk, ta=t0 + q4 * 4: e.copy(out=Gt[:, ta:ta + 4, :], in_=bk[:, :].rearrange("q (t i) -> q t i", t=4)), reads=[bk], writes=[Gt])
                    acc = pb[4:8]
                    n_ec = cfg.get('n_ec', 128)

                    def fetch(ec):
                        p.dma('sp', lambda e, ec=ec: e.dma_start(out=UTc[ec % NBUF][:, :, :].rearrange("q c e -> q (c e)"), in_=UTs[ec, :, :]),
                              reads=[('UTs', ec)], writes=[UTc[ec % NBUF]])
                        p.dma('sp', lambda e, ec=ec: e.dma_start(out=Vc[ec % NBUF][:, :], in_=Vs[ec, :, :]), reads=[('Vs', ec)], writes=[Vc[ec % NBUF]])
                    for ec in range(min(NBUF, n_ec)):
                        fetch(ec)
                    for ec in range(n_ec + 1):
                        if ec < n_ec:
                            U_ = UTc[ec % NBUF]
                            bk = pb[ec % 2]
                            for c in range(8):
                                mm(bk[:, 0:Tg], U_[:, c, :], XT[:, c, 0:Tg], c == 0, c == 7, [U_, XT], [bk])
                            ga, GA = gab[ec % 3], GAb[ec % 3]
                            p.op('act', lambda e, bk=bk, ga=ga: e.activation(out=ga[:, 0:Tg], in_=bk[:, 0:Tg], func=AF.Gelu_apprx_tanh), reads=[bk], writes=[ga])
                            p.op('dve', lambda e, ga=ga, GA=GA, ec=ec: e.tensor_tensor(out=GA[:, 0:Tg], in0=ga[:, 0:Tg], in1=Gt[:, 0:Tg, ec], op=ALU.mult),
                                 reads=[ga, Gt], writes=[GA])
                        if ec >= 1:
                            pe_ = ec - 1
                            V_, GA = Vc[pe_ % NBUF], GAb[pe_ % 3]
                            for s in range(nsub):
                                for half in range(2):
                                    ab = acc[2 * s + half]
                                    mm(ab[0:Ts, :], GA[:, s * 128:s * 128 + Ts], V_[:, half * 512:(half + 1) * 512], pe_ == 0, pe_ == n_ec - 1, [GA, V_], [ab])
                            if pe_ + NBUF < n_ec:
                                fetch(pe_ + NBUF)
                    for s in range(nsub):
                        for half in range(2):
                            ab = acc[2 * s + half]
                            p.op('dve', lambda e, ab=ab, s=s, half=half: e.tensor_tensor(out=pre[0:Ts, half * 512:(half + 1) * 512], in0=ab[0:Ts, :],
                                                                                         in1=xg[0:Ts, s, half * 512:(half + 1) * 512], op=ALU.add),
                                 reads=[ab, xg], writes=[pre])
                        rs = rmsnorm_rstd(pre, Ts, ssd, sq4)
                        p.op('dve', lambda e, rs=rs: e.scalar_tensor_tensor(out=yo[0:Ts, :], in0=pre[0:Ts, :], scalar=rs, in1=gfin[0:Ts, :], op0=ALU.mult, op1=ALU.mult),
                             reads=[pre, ssd['ss'], gfin], writes=[yo])
                        p.dma('sp', lambda e, s=s: e.dma_start(out=y_out[yrow0 + s * 128:yrow0 + s * 128 + Ts, :], in_=yo[0:Ts, :]), reads=[yo], writes=['y_out'])

                for gi in range(cfg.get('n_groups', 8)):
                    peer_group(gi * 256, 256, y_p, gi * 256)
                if cfg.get('peer_sample', True):
                    peer_group(SEQ, NS, y_s, 0)

        p.finish()
        p.emit()
    return nc


_NC_CACHE = {}


def make_in_maps(inp, cfg, ncores=NCORES):
    c = host_consts()
    f = lambda a: np.ascontiguousarray(a, dtype=np.float32)
    maps = []
    for i in range(ncores):
        m = {
            'xp': f(inp['x_prompt'][i]),
            'xs': f(inp['x_sample'][DEC_B * i:DEC_B * (i + 1)].reshape(NS, D)),
            'memp': f(inp['mem_prompt'][i]),
            'w_in': f(inp['w_in'][0]), 'w_out': f(inp['w_out'][0]),
            'g_mix': f(inp['norm_mix_g'][0]), 'g_mem': f(inp['norm_mem_g'][0]), 'g_memn': f(inp['mem_norm_g'][0]),
            'g_ffn': f(inp['norm_ffn_g'][0]), 'g_fin': f(inp['final_norm_g']),
            'ln_g': f(inp['gm_ln_g'][0]), 'ln_b': f(inp['gm_ln_b'][0]),
            'gm_ws': f(inp['gm_ws'][0]), 'gm_bs': f(inp['gm_bs'][0]),
            'mem_wq': f(inp['mem_wq'][0]), 'mem_wkv': f(inp['mem_wkv'][0]), 'mem_wo': f(inp['mem_wo'][0]),
            'c_ident': c['ident'], 'c_trilT': c['trilT'], 'c_negmask': c['negmask'], 'c_blk64': c['blk64'], 'c_ones': c['ones'], 'c_iota': c['iota'],
            'peer_wq': f(inp['peer_wq'][0]), 'peer_keys': f(inp['peer_keys'][0].reshape(16, 128, 64)),
            'peer_u': f(inp['peer_u'][0]), 'peer_v': f(inp['peer_v'][0]),
            'cache_kidx': f(inp['cache_kidx'][0]).reshape(-1, 8192), 'cache_k': f(inp['cache_k'][0]).reshape(-1, 8192),
            'cache_v': f(inp['cache_v'][0]).reshape(-1, 8192),
            'page_table': np.ascontiguousarray(inp['page_table'][DEC_B * i:DEC_B * (i + 1)], dtype=np.int32),
            'cache_mem_k': f(inp['cache_mem_k'][0][DEC_B * i:DEC_B * (i + 1)]).reshape(DEC_B, 256, 512),
            'cache_mem_v': f(inp['cache_mem_v'][0][DEC_B * i:DEC_B * (i + 1)]).reshape(DEC_B, 256, 512),
            'c_pow2': c['pow2'], 'c_negm4': c['negm4'],
        }
        maps.append(m)
    return maps


def assemble(results, ncores=NCORES):
    cat = lambda k: np.stack([np.asarray(r[k], dtype=np.float32) for r in results])
    y_prompt = cat('y_p')
    y_sample = cat('y_s').reshape(ncores * DEC_B, DEC_T, D)
    k_prompt = cat('k_p').reshape(1, ncores, SEQ, 2, 64)
    v_prompt = cat('v_p').reshape(1, ncores, SEQ, 2, 64)
    kidx_prompt = cat('ki_p').reshape(1, ncores, SEQ, 64)
    gmv_prompt = cat('gmv_p').reshape(1, ncores, 128, 512)
    memk = cat('memk_p').reshape(1, ncores, 256, 4, 128)
    memv = cat('memv_p').reshape(1, ncores, 256, 4, 128)
    k_sample = cat('k_s').reshape(1, ncores * DEC_B, DEC_T, 2, 64)
    v_sample = cat('v_s').reshape(1, ncores * DEC_B, DEC_T, 2, 64)
    kidx_sample = cat('ki_s').reshape(1, ncores * DEC_B, DEC_T, 64)
    gmv_sample = cat('gmv_s').reshape(1, ncores * DEC_B, DEC_T, 512)
    return (y_prompt, y_sample, k_prompt, v_prompt, kidx_prompt, gmv_prompt, memk, memv,
            k_sample, v_sample, kidx_sample, gmv_sample)


def kernel(**inputs):
    cfg = {}
    nc = build(cfg)
    maps = make_in_maps(inputs, cfg)
    res = run_bass_kernel_spmd(nc, maps, core_ids=list(range(NCORES)))
    return assemble(res.results)
```

```python
import numpy as np
from contextlib import ExitStack
import concourse.bass as bass
import concourse.mybir as mybir
from concourse.bass_utils import run_bass_kernel_spmd

F32 = mybir.dt.float32
BF16 = mybir.dt.bfloat16
I32 = mybir.dt.int32
U32 = mybir.dt.uint32
AF = mybir.ActivationFunctionType
ALU = mybir.AluOpType
AX = mybir.AxisListType

NCORES = 8
D = 1024
SEQ = 2048
NT = SEQ // 128
P_IN = 2376
EPS = 1e-6
NEG = -1.0e30
DEC_B = 16
DEC_T = 4
NS = DEC_B * DEC_T
NPAGES = 64
NEXP = 16384


class Prog:
    ENG = ('sp', 'act', 'dve', 'pool', 'pe')

    def __init__(self, nc, stack, n_dma_sems=12):
        self.nc = nc
        self.stack = stack
        self.ops = {k: [] for k in self.ENG}
        self.cnt = {k: 0 for k in self.ENG}
        self.waited = {k: {} for k in self.ENG}
        self.res = {}
        self.sems = {}
        for k in ('act', 'dve', 'pool', 'pe'):
            self.sems[k] = stack.enter_context(nc.semaphore('prog_' + k))
        self.dma_sems = {}
        self.dma_rr = {}
        self.dma_uses = {}
        for q in ('sp', 'pool', 'act'):
            lst = []
            for i in range(n_dma_sems):
                key = 'dma_%s_%d' % (q, i)
                self.sems[key] = stack.enter_context(nc.semaphore(key))
                self.dma_uses[key] = 0
                lst.append(key)
            self.dma_sems[q] = lst
            self.dma_rr[q] = 0
        self.psum_names = set()

    def sb(self, name, shape, dtype, stack=None):
        return (stack or self.stack).enter_context(self.nc.sbuf_tensor(name, list(shape), dtype))

    def ps(self, name, shape, dtype):
        self.psum_names.add(name)
        return self.stack.enter_context(self.nc.psum_tensor(name, list(shape), dtype))

    @staticmethod
    def _key(x):
        if isinstance(x, (str, tuple)):
            return x
        t = getattr(x, 'tensor', x)
        n = getattr(t, 'name', None)
        if n is None:
            raise ValueError('cannot derive resource key from %r' % (x,))
        return n

    def _deps(self, reads, writes):
        deps = []
        for r in reads:
            st = self.res.get(self._key(r))
            if st and st['w']:
                deps.append(st['w'])
        for w in writes:
            st = self.res.get(self._key(w))
            if st:
                if st['w']:
                    deps.append(st['w'])
                deps.extend(st['r'])
        return deps

    def _commit(self, reads, writes, tok):
        for r in reads:
            st = self.res.setdefault(self._key(r), {'w': None, 'r': []})
            st['r'].append(tok)
        for w in writes:
            self.res[self._key(w)] = {'w': tok, 'r': []}

    def _filter_waits(self, eng, deps, skip_self=False):
        out = {}
        for (sk, val) in deps:
            if skip_self and sk == eng:
                continue
            if self.waited[eng].get(sk, 0) >= val:
                continue
            if out.get(sk, 0) < val:
                out[sk] = val
        for sk, val in out.items():
            self.waited[eng][sk] = val
        return list(out.items())

    def op(self, eng, fn, reads=(), writes=()):
        pr = [r for r in reads if self._key(r) in self.psum_names]
        if pr:
            reads = [r for r in reads if self._key(r) not in self.psum_names]
            writes = list(writes) + pr
        deps = self._deps(reads, writes)
        waits = self._filter_waits(eng, deps, skip_self=(eng == 'pe'))
        self.cnt[eng] += 1
        tok = (eng, self.cnt[eng])
        self._commit(reads, writes, tok)
        self.ops[eng].append((waits, fn, (eng, 1)))
        return tok

    def dma(self, q, fn, reads=(), writes=()):
        deps = self._deps(reads, writes)
        lst = self.dma_sems[q]
        sk = lst[self.dma_rr[q] % len(lst)]
        self.dma_rr[q] += 1
        if self.dma_uses[sk] > 0:
            deps.append((sk, 16 * self.dma_uses[sk]))
        waits = self._filter_waits(q, deps)
        self.dma_uses[sk] += 1
        tok = (sk, 16 * self.dma_uses[sk])
        self._commit(reads, writes, tok)
        self.ops[q].append((waits, fn, (sk, 16)))
        return tok

    def barrier(self):
        deps = [(k, self.cnt[k]) for k in ('act', 'dve', 'pool', 'pe') if self.cnt[k] > 0]
        deps += [(sk, 16 * n) for sk, n in self.dma_uses.items() if n > 0]
        for eng in self.ENG:
            waits = self._filter_waits(eng, [d for d in deps if d[0] != eng])
            if waits:
                self.ops[eng].append((waits, None, None))
        self.res = {}

    def finish(self):
        deps = [(sk, 16 * n) for sk, n in self.dma_uses.items() if n > 0]
        waits = self._filter_waits('sp', deps)
        self.ops['sp'].append((waits, None, None))

    def emit(self):
        nc = self.nc
        allsems = list(self.sems.values())
        with nc.Block() as b0:
            def clr(e):
                for s in allsems:
                    e.sem_clear(s)
            b0.sync(clr)
        with nc.Block() as block:
            for name, meth in (('sp', block.sync), ('act', block.scalar), ('dve', block.vector),
                               ('pool', block.gpsimd), ('pe', block.tensor)):
                ops = self.ops[name]
                if not ops:
                    continue

                def body(e, ops=ops):
                    for waits, fn, inc in ops:
                        for (sk, val) in waits:
                            e.wait_ge(self.sems[sk], val)
                        if fn is None:
                            continue
                        ins = fn(e)
                        if inc is not None:
                            ins.then_inc(self.sems[inc[0]], inc[1])
                meth(body)


def host_consts():
    c = {}
    c['ident'] = np.eye(128, dtype=np.float32)
    s = np.arange(128)
    c['trilT'] = (s[:, None] <= s[None, :]).astype(np.float32)
    c['negmask'] = np.where(s[None, :] <= s[:, None], 0.0, NEG).astype(np.float32)
    bt = np.arange(64)
    c['blk64'] = ((bt[:, None] // 4 == bt[None, :] // 4) & (bt[:, None] % 4 <= bt[None, :] % 4)).astype(np.float32)
    c['ones'] = np.ones((128, 128), dtype=np.float32)
    c['iota'] = np.tile(np.arange(128, dtype=np.float32)[None, :], (128, 1))
    c['pow2'] = np.tile((2.0 ** -(np.arange(48, dtype=np.float64) + 1)).astype(np.float32)[None, :], (128, 1))
    t4 = np.arange(4)
    c['negm4'] = np.where(t4[:, None] <= t4[None, :], 0.0, NEG).astype(np.float32)
    return c


def build(cfg):
    n_pool = cfg.get('n_pool', 10240)
    do = cfg.get('phases', ('P1', 'P2', 'P3', 'P4'))
    dbg = cfg.get('dbg', False)
    nc = bass.Bass("TRN2", target_bir_lowering=False)

    def din(name, shape, dt=F32):
        return nc.dram_tensor(name, list(shape), dt, kind="ExternalInput").ap()

    def dout(name, shape, dt=F32):
        return nc.dram_tensor(name, list(shape), dt, kind="ExternalOutput").ap()

    xp_d = din('xp', [SEQ, D])
    xs_d = din('xs', [NS, D])
    memp_d = din('memp', [256, D])
    w_in_d = din('w_in', [D, P_IN])
    w_out_d = din('w_out', [D, D])
    g_mix_d = din('g_mix', [D]); g_mem_d = din('g_mem', [D]); g_memn_d = din('g_memn', [D])
    g_ffn_d = din('g_ffn', [D]); g_fin_d = din('g_fin', [D])
    ln_g_d = din('ln_g', [512]); ln_b_d = din('ln_b', [512])
    gm_ws_d = din('gm_ws', [4, 128, 128]); gm_bs_d = din('gm_bs', [4, 128])
    wq_d = din('mem_wq', [D, 512]); wkv_d = din('mem_wkv', [D, D]); wo_d = din('mem_wo', [512, D])
    c_ident = din('c_ident', [128, 128]); c_trilT = din('c_trilT', [128, 128]); c_negmask = din('c_negmask', [128, 128])
    c_blk64 = din('c_blk64', [64, 64]); c_ones = din('c_ones', [128, 128]); c_iota = din('c_iota', [128, 128])
    pwq_d = din('peer_wq', [D, D]); pkeys_d = din('peer_keys', [16, 128, 64])
    pu_d = din('peer_u', [NEXP, D]); pv_d = din('peer_v', [NEXP, D])
    ckidx_d = din('cache_kidx', [n_pool, 8192]); ck_d = din('cache_k', [2 * n_pool, 8192]); cv_d = din('cache_v', [2 * n_pool, 8192])
    pt_d = din('page_table', [DEC_B, NPAGES], I32)
    cmk_d = din('cache_mem_k', [DEC_B, 256, 512]); cmv_d = din('cache_mem_v', [DEC_B, 256, 512])
    c_pow2 = din('c_pow2', [128, 48]); c_negm4 = din('c_negm4', [4, 4])

    y_p = dout('y_p', [SEQ, D]); y_s = dout('y_s', [NS, D])
    k_p = dout('k_p', [SEQ, 128]); v_p = dout('v_p', [SEQ, 128]); ki_p = dout('ki_p', [SEQ, 64])
    gmv_p = dout('gmv_p', [128, 512])
    memk_p = dout('memk_p', [256, 512]); memv_p = dout('memv_p', [256, 512])
    k_s = dout('k_s', [NS, 128]); v_s = dout('v_s', [NS, 128]); ki_s = dout('ki_s', [NS, 64]); gmv_s = dout('gmv_s', [NS, 512])
    if dbg:
        x2_dbg = dout('x2_dbg', [SEQ + NS, D])
    x2s = nc.dram_tensor('x2s', [SEQ + NS, D], F32, kind="Internal").ap()
    UTs = nc.dram_tensor('UTs', [128, 128, D], BF16, kind="Internal").ap()
    Vs = nc.dram_tensor('Vs', [128, 128, D], BF16, kind="Internal").ap()

    with ExitStack() as st:
        p = Prog(nc, st)
        pb = [p.ps('pb%d' % i, [128, 512], F32) for i in range(8)]
        rr = [0]
        nbmod = [6]

        def nb():
            b = pb[rr[0] % nbmod[0]]
            rr[0] += 1
            return b

        def mm(out, lhsT, rhs, start, stop, R, W):
            p.op('pe', lambda e: e.matmul(out, lhsT=lhsT, rhs=rhs, start=start, stop=stop), reads=R, writes=W)

        identf = p.sb('identf', [128, 128], F32)
        ident = p.sb('ident', [128, 128], BF16)
        trilT = p.sb('trilT', [128, 128], F32)
        negmask = p.sb('negmask', [128, 128], F32)
        ones_bf = p.sb('ones_bf', [128, 128], BF16)
        onesf = p.sb('onesf', [128, 128], F32)
        p.dma('sp', lambda e: e.dma_start(out=identf[:], in_=c_ident[:, :]), writes=[identf])
        p.dma('sp', lambda e: e.dma_start(out=trilT[:], in_=c_trilT[:, :]), writes=[trilT])
        p.dma('sp', lambda e: e.dma_start(out=negmask[:], in_=c_negmask[:, :]), writes=[negmask])
        p.dma('sp', lambda e: e.dma_start(out=onesf[:], in_=c_ones[:, :]), writes=[onesf])
        p.op('dve', lambda e: e.tensor_copy(out=ident[:], in_=identf[:]), reads=[identf], writes=[ident])
        p.op('dve', lambda e: e.tensor_copy(out=ones_bf[:], in_=onesf[:]), reads=[onesf], writes=[ones_bf])

        def tr(out, in_, K, R, W):
            p.op('pe', lambda e: e.transpose(out=out, in_=in_, identity=ident[0:K, 0:K]), reads=list(R) + [ident], writes=W)

        gcols = {}

        def load_gcol(name, g_d):
            t = p.sb('gc_' + name, [128, 8], F32)
            p.dma('sp', lambda e: e.dma_start(out=t[:], in_=g_d.rearrange("(c q) -> q c", q=128), allow_slow_non_contiguous=True), writes=[t])
            gcols[name] = t
            return t

        def load_weight(dst, w_d, nk, ncol, gcol, stage, eng_rot=[0]):
            for c in range(nk):
                stg = stage[c % len(stage)]
                p.dma('sp', lambda e, c=c, stg=stg: e.dma_start(out=stg[:, 0:ncol], in_=w_d[c * 128:(c + 1) * 128, :]), writes=[stg])
                eng = ('dve', 'pool')[eng_rot[0] % 2]
                eng_rot[0] += 1
                if gcol is not None:
                    p.op(eng, lambda e, c=c, stg=stg: e.tensor_scalar(out=dst[:, c, :], in0=stg[:, 0:ncol], scalar1=gcol[:, c:c + 1],
                                                                     scalar2=None, op0=ALU.mult), reads=[stg, gcol], writes=[dst])
                else:
                    p.op(eng, lambda e, c=c, stg=stg: e.tensor_copy(out=dst[:, c, :], in_=stg[:, 0:ncol]), reads=[stg], writes=[dst])

        def rmsnorm_rstd(x_t, T, rstd, scratch):
            ss = rstd['ss']
            p.op('act', lambda e: e.activation(out=scratch[0:T, :], in_=x_t[0:T, :], func=AF.Square, accum_out=ss[0:T, 0:1]),
                 reads=[x_t], writes=[scratch, ss])
            p.op('dve', lambda e: e.tensor_scalar(out=ss[0:T, 1:2], in0=ss[0:T, 0:1], scalar1=1.0 / D, scalar2=EPS, op0=ALU.mult, op1=ALU.add),
                 reads=[ss], writes=[ss])
            p.op('act', lambda e: e.activation(out=ss[0:T, 2:3], in_=ss[0:T, 1:2], func=AF.Sqrt), reads=[ss], writes=[ss])
            p.op('dve', lambda e: e.reciprocal(out=ss[0:T, 3:4], in_=ss[0:T, 2:3]), reads=[ss], writes=[ss])
            return ss[0:T, 3:4]

        def norm_T(x_t, T, tag, xn, xnT, ssd, scratch, gcol=None):
            rs = rmsnorm_rstd(x_t, T, ssd, scratch)
            p.op('act', lambda e: e.activation(out=xn[0:T, :], in_=x_t[0:T, :], func=AF.Copy, scale=rs), reads=[x_t, ssd['ss']], writes=[xn])
            bk = nb()
            bv = bk[:].bitcast(BF16)
            for c in range(8):
                tr(bv[:, c * T:(c + 1) * T], xn[0:T, c * 128:(c + 1) * 128], T, [xn], [bk])
            if gcol is None:
                p.op('dve', lambda e: e.tensor_copy(out=xnT[:, :, 0:T], in_=bv[:, 0:8 * T].rearrange("q (c t) -> q c t", c=8)),
                     reads=[bk], writes=[xnT])
            else:
                for c in range(8):
                    p.op('dve', lambda e, c=c: e.tensor_scalar(out=xnT[:, c, 0:T], in0=bv[:, c * T:(c + 1) * T], scalar1=gcol[:, c:c + 1], scalar2=None, op0=ALU.mult),
                         reads=[bk, gcol], writes=[xnT])

        ssd = {'ss': p.sb('ss', [128, 4], F32)}

        for nm, gd in (('mix', g_mix_d), ('mem', g_mem_d), ('memn', g_memn_d), ('ffn', g_ffn_d)):
            load_gcol(nm, gd)
        conv_done = [0]

        def convert_chunk(ec, u_, t_, v_):
            p.dma('pool', lambda e: e.dma_start(out=u_[:, :], in_=pu_d[ec * 128:(ec + 1) * 128, :]), writes=[u_])
            p.dma('pool', lambda e: e.dma_start(out=v_[:, :], in_=pv_d[ec * 128:(ec + 1) * 128, :]), writes=[v_])
            bk = nb()
            bv = bk[:].bitcast(BF16)
            for c in range(8):
                tr(bv[:, c * 128:(c + 1) * 128], u_[:, c * 128:(c + 1) * 128], 128, [u_], [bk])
            if ec % 2 == 0:
                p.op('act', lambda e: e.copy(out=t_[:, :], in_=bv[:, :]), reads=[bk], writes=[t_])
            else:
                p.op('pool', lambda e: e.tensor_copy(out=t_[:, :], in_=bv[:, :]), reads=[bk], writes=[t_]) if False else \
                    p.op('act', lambda e: e.copy(out=t_[:, :], in_=bv[:, :]), reads=[bk], writes=[t_])
            p.dma('sp', lambda e: e.dma_start(out=UTs[ec, :, :], in_=t_[:, :]), reads=[t_], writes=[('UTs', ec)])
            p.dma('sp', lambda e: e.dma_start(out=Vs[ec, :, :], in_=v_[:, :]), reads=[v_], writes=[('Vs', ec)])

        with ExitStack() as s2:
            w_out = p.sb('w_out_sb', [128, 8, D], BF16, s2)
            wq = p.sb('wq_sb', [128, 8, 512], BF16, s2)
            wo = p.sb('wo_sb', [128, 4, D], BF16, s2)
            x0 = p.sb('x0', [128, D], F32, s2)
            sq = p.sb('sq_scratch', [128, D], BF16, s2)
            xn = p.sb('xn', [128, D], BF16, s2)
            xnT = p.sb('xnT', [128, 8, 128], BF16, s2)
            mkT = p.sb('mkT', [128, 4, 256], BF16, s2)
            mv_aug = p.sb('mv_aug', [128, 2, 4, 129], BF16, s2)
            WgT = p.sb('WgT', [128, 4, 128], BF16, s2)
            bsT = p.sb('bsT', [128, 4], F32, s2)
            W4T = p.sb('W4T', [64, 4, 64], BF16, s2)
            bsT4 = p.sb('bsT4', [64, 4], F32, s2)
            lng = p.sb('lng', [128, 512], F32, s2)
            lnb = p.sb('lnb', [128, 512], F32, s2)
            tau_c = p.sb('tau_c', [128, 1], F32, s2)
            p.op('pool', lambda e: e.memset(tau_c[:], -1.0e29), writes=[tau_c])
            p.dma('sp', lambda e: e.dma_start(out=lng[:], in_=ln_g_d.partition_broadcast(128)), writes=[lng])
            p.dma('sp', lambda e: e.dma_start(out=lnb[:], in_=ln_b_d.partition_broadcast(128)), writes=[lnb])

            kvf = p.sb('kvf', [128, 328], F32, s2)
            bnst = p.sb('bnst', [128, 8], F32, s2)
            kb = p.sb('kb', [128, 192], BF16, s2)
            qT = p.sb('qT', [64, 8, 128], BF16, s2)
            qiT = p.sb('qiT', [64, 8, 128], BF16, s2)
            wsc = p.sb('wsc', [128, 8], F32, s2)
            ycat = p.sb('ycat', [128, D], BF16, s2)
            yT = p.sb('yT', [128, 8, 128], BF16, s2)
            x1 = p.sb('x1', [128, D], F32, s2)
            rden = p.sb('rden', [128, 8], F32, s2)
            qmT = p.sb('qmT', [128, 4, 128], BF16, s2)
            PmT = p.sb('PmT', [128, 2, 4, 128], BF16, s2)
            om = p.sb('om', [128, 512], BF16, s2)
            omT = p.sb('omT', [128, 4, 128], BF16, s2)
            x0s = p.sb('x0s', [64, D], F32, s2)
            ycats = p.sb('ycats', [64, D], BF16, s2)
            kvf_s = p.sb('kvf_s', [64, 328], F32, s2)
            kb_s = p.sb('kb_s', [64, 192], BF16, s2)
            wsc_s = p.sb('wsc_s', [64, 8], F32, s2)
            qTs = p.sb('qTs', [64, 8, 64], BF16, s2)
            qiTs = p.sb('qiTs', [64, 8, 64], BF16, s2)
            sw = ExitStack()
            ub = p.sb('ub', [128, 512], F32, sw)
            gv = p.sb('gv', [128, 512], F32, sw)
            vn = p.sb('vn', [128, 512], F32, sw)
            vnb = p.sb('vnb', [128, 512], BF16, sw)
            zq = p.sb('zq', [128, 1024], BF16, sw)
            w_in = p.sb('w_in_sb', [128, 8, P_IN], BF16, sw)

            with ExitStack() as s1:
                stage = [p.sb('wstage%d' % i, [128, P_IN], F32, s1) for i in range(2)]
                load_weight(w_in, w_in_d, 8, P_IN, gcols['mix'], stage)
                load_weight(w_out, w_out_d, 8, D, None, stage)
                load_weight(wq, wq_d, 8, 512, gcols['mem'], stage)
                load_weight(wo, wo_d, 4, D, None, stage)
                wsb = p.sb('wsb', [128, 128], BF16, s1)
                for g in range(4):
                    stg = stage[g % 2]
                    p.dma('sp', lambda e, g=g, stg=stg: e.dma_start(out=stg[:, 0:128], in_=gm_ws_d[g, :, :]), writes=[stg])
                    p.op('dve', lambda e, stg=stg: e.tensor_copy(out=wsb[:], in_=stg[:, 0:128]), reads=[stg], writes=[wsb])
                    bk = nb()
                    bv = bk[:].bitcast(BF16)
                    tr(bv[:, 0:128], wsb[:, :], 128, [wsb], [bk])
                    p.op('dve', lambda e, g=g, bv=bv: e.tensor_tensor(out=WgT[:, g, :], in0=bv[:, 0:128], in1=trilT[:, :], op=ALU.mult),
                         reads=[bk, trilT], writes=[WgT])
                p.dma('sp', lambda e: e.dma_start(out=bsT[:], in_=gm_bs_d.rearrange("g t -> t g"), allow_slow_non_contiguous=True), writes=[bsT])
                w4f = p.sb('w4f', [64, 4, 64], F32, s1)
                blk64 = p.sb('blk64', [64, 64], F32, s1)
                p.dma('sp', lambda e: e.dma_start(out=blk64[:], in_=c_blk64[:, :]), writes=[blk64])
                p.op('pool', lambda e: e.memset(w4f[:], 0.0), writes=[w4f])
                for g in range(4):
                    for b in range(DEC_B):
                        p.dma('sp', lambda e, g=g, b=b: e.dma_start(
                            out=w4f[4 * b:4 * b + 4, g, 4 * b:4 * b + 4], in_=gm_ws_d[g, 0:4, 0:4].rearrange("t s -> s t"), allow_slow_non_contiguous=True),
                            writes=[w4f])
                    p.op('dve', lambda e, g=g: e.tensor_tensor(out=W4T[:, g, :], in0=w4f[:, g, :], in1=blk64[:, :], op=ALU.mult), reads=[w4f, blk64], writes=[W4T])
                for b in range(DEC_B):
                    p.dma('sp', lambda e, b=b: e.dma_start(out=bsT4[4 * b:4 * b + 4, :], in_=gm_bs_d[:, 0:4].rearrange("g t -> t g"), allow_slow_non_contiguous=True), writes=[bsT4])

                if 'P1' in do:
                    wkv = p.sb('wkv_sb', [128, 8, D], BF16, s1)
                    load_weight(wkv, wkv_d, 8, D, gcols['memn'], stage)
                    mkvf = p.sb('mkvf', [128, D], F32, s1)
                    mkb = p.sb('mkb', [128, 512], BF16, s1)
                    p.op('pool', lambda e: e.memset(mv_aug[:], 1.0), writes=[mv_aug])
                    for mt in range(2):
                        p.dma('sp', lambda e, mt=mt: e.dma_start(out=x0[:], in_=memp_d[mt * 128:(mt + 1) * 128, :]), writes=[x0])
                        norm_T(x0, 128, 'm', xn, xnT, ssd, sq)
                        b0, b1 = nb(), nb()
                        for half, bk in enumerate((b0, b1)):
                            for c in range(8):
                                mm(bk[:, :], xnT[:, c, :], wkv[:, c, half * 512:(half + 1) * 512], c == 0, c == 7, [xnT, wkv], [bk])
                        p.op('act', lambda e, b0=b0: e.copy(out=mkvf[:, 0:512], in_=b0[:, :]), reads=[b0], writes=[mkvf])
                        p.op('dve', lambda e, b1=b1: e.tensor_copy(out=mkvf[:, 512:1024], in_=b1[:, :]), reads=[b1], writes=[mkvf])
                        p.dma('sp', lambda e, mt=mt: e.dma_start(out=memk_p[mt * 128:(mt + 1) * 128, :], in_=mkvf[:, 0:512]), reads=[mkvf], writes=['memk_p'])
                        p.dma('sp', lambda e, mt=mt: e.dma_start(out=memv_p[mt * 128:(mt + 1) * 128, :], in_=mkvf[:, 512:1024]), reads=[mkvf], writes=['memv_p'])
                        p.op('dve', lambda e: e.tensor_copy(out=mkb[:], in_=mkvf[:, 0:512]), reads=[mkvf], writes=[mkb])
                        p.op('pool', lambda e, mt=mt: e.tensor_copy(out=mv_aug[:, mt, :, 0:128], in_=mkvf[:, 512:1024].rearrange("q (h d) -> q h d", h=4)),
                             reads=[mkvf], writes=[mv_aug])
                        bk = nb()
                        bv = bk[:].bitcast(BF16)
                        for h in range(4):
                            tr(bv[:, h * 128:(h + 1) * 128], mkb[:, h * 128:(h + 1) * 128], 128, [mkb], [bk])
                        p.op('act', lambda e, mt=mt, bv=bv: e.copy(out=mkT[:, :, mt * 128:(mt + 1) * 128], in_=bv[:, 0:512].rearrange("q (h m) -> q h m", h=4)),
                             reads=[bk], writes=[mkT])
            p.barrier()

            with ExitStack() as s3:
                kT = [p.sb('kT%d' % g, [64, SEQ], BF16, s3) for g in range(2)]
                kiT = p.sb('kiT', [64, SEQ], BF16, s3)
                v_aug = p.sb('v_aug', [128, NT, 2, 65], BF16, s3)
                sc = p.sb('sc', [128, SEQ], F32, s3)
                work = p.sb('work', [128, SEQ], F32, s3)
                rbuf = [p.sb('rbuf%d' % i, [128, 512], F32, s3) for i in range(2)]
                m8 = p.sb('m8', [128, 8], F32, s3)
                maskb = p.sb('maskb', [128, SEQ], BF16, s3)
                maskT = p.sb('maskT', [128, NT, 128], BF16, s3)
                Eb = [p.sb('Eb%d' % i, [128, 512], BF16, s3) for i in range(3)]
                PTb = [p.sb('PTb%d' % i, [128, 4, 128], BF16, s3) for i in range(3)]
                p.op('pool', lambda e: e.memset(v_aug[:], 1.0), writes=[v_aug])
                if 'P4' in do and cfg.get('interleave_conv', True):
                    cub = [p.sb('cub%d' % i, [128, D], BF16, s3) for i in range(2)]
                    cut = [p.sb('cut%d' % i, [128, D], BF16, s3) for i in range(2)]
                    cvb = [p.sb('cvb%d' % i, [128, D], BF16, s3) for i in range(2)]

                def proj_tile(src_ap, T, ti, is_sample, x0b, ycatb):
                    p.dma('sp', lambda e: e.dma_start(out=x0b[0:T, :], in_=src_ap), writes=[x0b])
                    norm_T(x0b, T, 'a', xn, xnT, ssd, sq)
                    banks = [nb() for _ in range(5)]
                    for n5, bk in enumerate(banks):
                        c0 = n5 * 512
                        w = min(512, P_IN - c0)
                        for c in range(8):
                            mm(bk[0:T, 0:w], xnT[:, c, 0:T], w_in[:, c, c0:c0 + w], c == 0, c == 7, [xnT, w_in], [bk])
                    p.op('act', lambda e: e.activation(out=ub[0:T, :], in_=banks[0][0:T, :], func=AF.Gelu_apprx_tanh), reads=[banks[0]], writes=[ub])
                    p.op('act', lambda e: e.activation(out=gv[0:T, :], in_=banks[1][0:T, :], func=AF.Gelu_apprx_tanh), reads=[banks[1]], writes=[gv])
                    p.op('dve', lambda e: e.tensor_copy(out=zq[0:T, 0:512], in_=banks[2][0:T, :]), reads=[banks[2]], writes=[zq])
                    p.op('dve', lambda e: e.tensor_copy(out=kvf[0:T, 0:256], in_=banks[3][0:T, 0:256]), reads=[banks[3]], writes=[kvf])
                    p.op('dve', lambda e: e.tensor_copy(out=zq[0:T, 512:768], in_=banks[3][0:T, 256:512]), reads=[banks[3]], writes=[zq])
                    p.op('act', lambda e: e.copy(out=zq[0:T, 768:1024], in_=banks[4][0:T, 0:256]), reads=[banks[4]], writes=[zq])
                    p.op('act', lambda e: e.copy(out=kvf[0:T, 256:328], in_=banks[4][0:T, 256:328]), reads=[banks[4]], writes=[kvf])
                    r0 = ti * 128
                    ko, vo, kio = (k_s, v_s, ki_s) if is_sample else (k_p, v_p, ki_p)
                    p.dma('sp', lambda e: e.dma_start(out=ko[r0:r0 + T, :], in_=kvf[0:T, 0:128]), reads=[kvf], writes=['ko'])
                    p.dma('sp', lambda e: e.dma_start(out=vo[r0:r0 + T, :], in_=kvf[0:T, 128:256]), reads=[kvf], writes=['vo'])
                    p.dma('sp', lambda e: e.dma_start(out=kio[r0:r0 + T, :], in_=kvf[0:T, 256:320]), reads=[kvf], writes=['kio'])
                    p.op('dve', lambda e: e.bn_stats(out=bnst[0:T, 0:6], in_=gv[0:T, :]), reads=[gv], writes=[bnst])
                    p.op('dve', lambda e: e.bn_aggr(out=bnst[0:T, 6:8], in_=bnst[0:T, 0:6]), reads=[bnst], writes=[bnst])
                    p.op('dve', lambda e: e.tensor_scalar(out=bnst[0:T, 0:1], in0=bnst[0:T, 7:8], scalar1=EPS, scalar2=None, op0=ALU.add), reads=[bnst], writes=[bnst])
                    p.op('act', lambda e: e.activation(out=bnst[0:T, 1:2], in_=bnst[0:T, 0:1], func=AF.Sqrt), reads=[bnst], writes=[bnst])
                    p.op('dve', lambda e: e.reciprocal(out=bnst[0:T, 2:3], in_=bnst[0:T, 1:2]), reads=[bnst], writes=[bnst])
                    p.op('dve', lambda e: e.tensor_scalar(out=vn[0:T, :], in0=gv[0:T, :], scalar1=bnst[0:T, 6:7], scalar2=bnst[0:T, 2:3],
                                                          op0=ALU.subtract, op1=ALU.mult), reads=[gv, bnst], writes=[vn])
                    p.op('dve', lambda e: e.tensor_tensor(out=vn[0:T, :], in0=vn[0:T, :], in1=lng[0:T, :], op=ALU.mult), reads=[vn, lng], writes=[vn])
                    p.op('dve', lambda e: e.tensor_tensor(out=vn[0:T, :], in0=vn[0:T, :], in1=lnb[0:T, :], op=ALU.add), reads=[vn, lnb], writes=[vn])
                    if is_sample:
                        p.dma('sp', lambda e: e.dma_start(out=gmv_s[0:T, :], in_=vn[0:T, :]), reads=[vn], writes=['gmv_s'])
                    elif ti == NT - 1:
                        p.dma('sp', lambda e: e.dma_start(out=gmv_p[:, :], in_=vn[0:T, :]), reads=[vn], writes=['gmv_p'])
                    p.op('pool', lambda e: e.tensor_copy(out=vnb[0:T, :], in_=vn[0:T, :]), reads=[vn], writes=[vnb])
                    bk = nb()
                    Wm, bsm = (W4T, bsT4) if is_sample else (WgT, bsT)
                    for g in range(4):
                        mm(bk[0:T, g * 128:(g + 1) * 128], Wm[0:T, g, 0:T], vnb[0:T, g * 128:(g + 1) * 128], True, True, [Wm, vnb], [bk])
                    for g in range(4):
                        p.op('dve', lambda e, g=g, bk=bk: e.scalar_tensor_tensor(out=ycatb[0:T, g * 128:(g + 1) * 128], in0=bk[0:T, g * 128:(g + 1) * 128],
                                                                                  scalar=bsm[0:T, g:g + 1], in1=ub[0:T, g * 128:(g + 1) * 128],
                                                                                  op0=ALU.add, op1=ALU.mult), reads=[bk, bsm, ub], writes=[ycatb])

                def feature_major(T, ti, qTb, qiTb):
                    p.op('pool', lambda e: e.tensor_copy(out=kb[0:T, 0:128], in_=kvf[0:T, 0:128]), reads=[kvf], writes=[kb])
                    p.op('pool', lambda e: e.tensor_copy(out=kb[0:T, 128:192], in_=kvf[0:T, 256:320]), reads=[kvf], writes=[kb])
                    p.op('pool', lambda e: e.tensor_scalar(out=wsc[0:T, :], in0=kvf[0:T, 320:328], scalar1=8.0 ** -0.5, scalar2=None, op0=ALU.mult),
                         reads=[kvf], writes=[wsc])
                    for (src, off, dst) in ((zq, 0, qTb), (zq, 512, qiTb)):
                        bk = nb()
                        bv = bk[:].bitcast(BF16)
                        for h in range(8):
                            tr(bv[0:64, h * T:(h + 1) * T], src[0:T, off + h * 64: off + (h + 1) * 64], T, [src], [bk])
                        p.op('act', lambda e, bv=bv, dst=dst: e.copy(out=dst[:, :, 0:T], in_=bv[0:64, 0:8 * T].rearrange("q (h t) -> q h t", h=8)),
                             reads=[bk], writes=[dst])

                def prompt_dsa(ti):
                    T = 128
                    L = 128 * (ti + 1)
                    c_lo = ti * 128
                    bk = nb()
                    bv = bk[:].bitcast(BF16)
                    for g in range(3):
                        tr(bv[0:64, g * 128:(g + 1) * 128], kb[:, g * 64:(g + 1) * 64], 128, [kb], [bk])
                    p.op('act', lambda e, bv=bv: e.copy(out=kT[0][:, c_lo:c_lo + 128], in_=bv[0:64, 0:128]), reads=[bk], writes=[kT[0]])
                    p.op('act', lambda e, bv=bv: e.copy(out=kT[1][:, c_lo:c_lo + 128], in_=bv[0:64, 128:256]), reads=[bk], writes=[kT[1]])
                    p.op('act', lambda e, bv=bv: e.copy(out=kiT[:, c_lo:c_lo + 128], in_=bv[0:64, 256:384]), reads=[bk], writes=[kiT])
                    p.op('pool', lambda e: e.tensor_copy(out=v_aug[:, ti, :, 0:64], in_=kvf[:, 128:256].rearrange("q (g d) -> q g d", g=2)),
                         reads=[kvf], writes=[v_aug])
                    ri = 0
                    for c0 in range(0, L, 512):
                        w = min(512, L - c0)
                        for h in range(8):
                            bk = nb()
                            mm(bk[:, 0:w], qiT[:, h, :], kiT[:, c0:c0 + w], True, True, [qiT, kiT], [bk])
                            rb = rbuf[ri % 2]
                            ri += 1
                            p.op('act', lambda e, bk=bk, rb=rb, w=w: e.activation(out=rb[:, 0:w], in_=bk[:, 0:w], func=AF.Relu), reads=[bk], writes=[rb])
                            if h == 0:
                                p.op('dve', lambda e, rb=rb, w=w, c0=c0: e.tensor_scalar(out=sc[:, c0:c0 + w], in0=rb[:, 0:w], scalar1=wsc[:, 0:1], scalar2=None, op0=ALU.mult),
                                     reads=[rb, wsc], writes=[sc])
                            else:
                                p.op('dve', lambda e, rb=rb, w=w, c0=c0, h=h: e.scalar_tensor_tensor(out=sc[:, c0:c0 + w], in0=rb[:, 0:w], scalar=wsc[:, h:h + 1],
                                                                                                     in1=sc[:, c0:c0 + w], op0=ALU.mult, op1=ALU.add),
                                     reads=[rb, wsc, sc], writes=[sc])
                    p.op('dve', lambda e: e.tensor_tensor(out=sc[:, c_lo:c_lo + 128], in0=sc[:, c_lo:c_lo + 128], in1=negmask[:, :], op=ALU.add),
                         reads=[sc, negmask], writes=[sc])
                    if ti >= 2:
                        for r in range(32):
                            src = sc if r == 0 else work
                            p.op('dve', lambda e, src=src: e.max(out=m8[:, :], in_=src[:, 0:L]), reads=[src], writes=[m8])
                            if r < 31:
                                p.op('dve', lambda e, src=src: e.match_replace(out=work[:, 0:L], in_to_replace=m8[:, :], in_values=src[:, 0:L], imm_value=NEG),
                                     reads=[src, m8], writes=[work])
                        tau = m8[:, 7:8]
                        tau_t = m8
                    else:
                        tau = tau_c[:, 0:1]
                        tau_t = tau_c
                    p.op('dve', lambda e: e.tensor_scalar(out=maskb[:, 0:L], in0=sc[:, 0:L], scalar1=tau, scalar2=None, op0=ALU.is_ge),
                         reads=[sc, tau_t], writes=[maskb])
                    for j0 in range(0, ti + 1, 8):
                        nj = min(8, ti + 1 - j0)
                        bk = nb()
                        bv = bk[:].bitcast(BF16)
                        for jj in range(nj):
                            tr(bv[:, jj * 128:(jj + 1) * 128], maskb[:, (j0 + jj) * 128:(j0 + jj + 1) * 128], 128, [maskb], [bk])
                        p.op('act', lambda e, bv=bv, j0=j0, nj=nj: e.copy(out=maskT[:, j0:j0 + nj, :], in_=bv[:, 0:nj * 128].rearrange("q (j t) -> q j t", j=nj)),
                             reads=[bk], writes=[maskT])
                    seq = [(g, j) for g in range(2) for j in range(ti + 1)]
                    PTl = {}

                    def att_scores(idx):
                        g, j = seq[idx]
                        bk = nb()
                        mm(bk[:, :], kT[g][:, j * 128:(j + 1) * 128], qT[:, 4 * g:4 * g + 4, :].rearrange("q h t -> q (h t)"), True, True, [kT[g], qT], [bk])
                        E = Eb[idx % 3]
                        PT = PTb[idx % 3]
                        p.op('act', lambda e: e.activation(out=E[:, :], in_=bk[:, :], func=AF.Exp, scale=0.125), reads=[bk], writes=[E])
                        p.op('pool', lambda e: e.tensor_tensor(out=PT[:, :, :], in0=E[:, :].rearrange("q (h t) -> q h t", h=4),
                                                               in1=maskT[:, j:j + 1, :].to_broadcast([128, 4, 128]), op=ALU.mult),
                             reads=[E, maskT], writes=[PT])
                        PTl[idx] = PT

                    def att_pv(idx):
                        g, j = seq[idx]
                        ob = pb[6 + g]
                        PT = PTl[idx]
                        for hh in range(4):
                            mm(ob[:, hh * 65:(hh + 1) * 65], PT[:, hh, :], v_aug[:, j, g, :], (j == 0 and hh == 0), (j == ti), [PT, v_aug], [ob])
                        if j == ti:
                            p.op('dve', lambda e: e.reciprocal(out=rden[:, 4 * g:4 * g + 4], in_=ob[:, 0:260].rearrange("q (h d) -> q h d", h=4)[:, :, 64]),
                                 reads=[ob], writes=[rden])
                            p.op('dve', lambda e: e.tensor_tensor(out=ycat[:, 512 + 256 * g:512 + 256 * (g + 1)].rearrange("q (h d) -> q h d", h=4),
                                                                  in0=ob[:, 0:260].rearrange("q (h d) -> q h d", h=4)[:, :, 0:64],
                                                                  in1=rden[:, 4 * g:4 * g + 4].unsqueeze(2).to_broadcast([128, 4, 64]), op=ALU.mult),
                                 reads=[ob, rden], writes=[ycat])
                    for idx in range(len(seq) + 1):
                        if idx < len(seq):
                            att_scores(idx)
                        if idx >= 1:
                            att_pv(idx - 1)

                def out_proj_and_mem(T, row0, is_sample, x0b, ycatb):
                    bk = nb()
                    bv = bk[:].bitcast(BF16)
                    for c in range(8):
                        tr(bv[:, c * T:(c + 1) * T], ycatb[0:T, c * 128:(c + 1) * 128], T, [ycatb], [bk])
                    p.op('act', lambda e, bv=bv: e.copy(out=yT[:, :, 0:T], in_=bv[:, 0:8 * T].rearrange("q (c t) -> q c t", c=8)), reads=[bk], writes=[yT])
                    for half in range(2):
                        bk = nb()
                        for c in range(8):
                            mm(bk[0:T, :], yT[:, c, 0:T], w_out[:, c, half * 512:(half + 1) * 512], c == 0, c == 7, [yT, w_out], [bk])
                        p.op('dve', lambda e, bk=bk, half=half: e.tensor_tensor(out=x1[0:T, half * 512:(half + 1) * 512], in0=bk[0:T, :],
                                                                                in1=x0b[0:T, half * 512:(half + 1) * 512], op=ALU.add),
                             reads=[bk, x0b], writes=[x1])
                    norm_T(x1, T, 'b', xn, xnT, ssd, sq)
                    bk = nb()
                    for h in range(4):
                        for c in range(8):
                            mm(bk[:, h * T:(h + 1) * T], wq[:, c, h * 128:(h + 1) * 128], xnT[:, c, 0:T], c == 0, c == 7, [wq, xnT], [bk])
                    p.op('act', lambda e, bk=bk: e.copy(out=qmT[:, :, 0:T], in_=bk[:, 0:4 * T].rearrange("q (h t) -> q h t", h=4)), reads=[bk], writes=[qmT])
                    if not is_sample:
                        for mt in range(2):
                            bk = nb()
                            for h in range(4):
                                mm(bk[:, h * 128:(h + 1) * 128], mkT[:, h, mt * 128:(mt + 1) * 128], qmT[:, h, :], True, True, [mkT, qmT], [bk])
                            p.op('act', lambda e, bk=bk, mt=mt: e.activation(out=PmT[:, mt, :, :], in_=bk[:, :].rearrange("q (h t) -> q h t", h=4),
                                                                             func=AF.Exp, scale=128.0 ** -0.5), reads=[bk], writes=[PmT])
                        for hp in range(2):
                            ob = pb[6 + hp]
                            for mt in range(2):
                                for hh in range(2):
                                    h = 2 * hp + hh
                                    mm(ob[:, hh * 129:(hh + 1) * 129], PmT[:, mt, h, :], mv_aug[:, mt, h, :], (mt == 0 and hh == 0), (mt == 1), [PmT, mv_aug], [ob])
                            p.op('dve', lambda e, ob=ob, hp=hp: e.reciprocal(out=rden[:, 2 * hp:2 * hp + 2], in_=ob[:, 0:258].rearrange("q (h d) -> q h d", h=2)[:, :, 128]),
                                 reads=[ob], writes=[rden])
                            p.op('dve', lambda e, ob=ob, hp=hp: e.tensor_tensor(out=om[:, 256 * hp:256 * (hp + 1)].rearrange("q (h d) -> q h d", h=2),
                                                                                in0=ob[:, 0:258].rearrange("q (h d) -> q h d", h=2)[:, :, 0:128],
                                                                                in1=rden[:, 2 * hp:2 * hp + 2].unsqueeze(2).to_broadcast([128, 2, 128]), op=ALU.mult),
                                 reads=[ob, rden], writes=[om])
                        bk = nb()
                        bv = bk[:].bitcast(BF16)
                        for h in range(4):
                            tr(bv[:, h * T:(h + 1) * T], om[0:T, h * 128:(h + 1) * 128], T, [om], [bk])
                        p.op('act', lambda e, bv=bv: e.copy(out=omT[:, :, 0:T], in_=bv[:, 0:4 * T].rearrange("q (h t) -> q h t", h=4)), reads=[bk], writes=[omT])
                    else:
                        sample_mem_attn()
                    for half in range(2):
                        bk = nb()
                        for h in range(4):
                            mm(bk[0:T, :], omT[:, h, 0:T], wo[:, h, half * 512:(half + 1) * 512], h == 0, h == 3, [omT, wo], [bk])
                        p.op('dve', lambda e, bk=bk, half=half: e.tensor_tensor(out=x0b[0:T, half * 512:(half + 1) * 512], in0=bk[0:T, :],
                                                                                in1=x1[0:T, half * 512:(half + 1) * 512], op=ALU.add),
                             reads=[bk, x1], writes=[x0b])
                    p.dma('sp', lambda e: e.dma_start(out=x2s[row0:row0 + T, :], in_=x0b[0:T, :]), reads=[x0b], writes=['x2s'])
                    if dbg:
                        p.dma('sp', lambda e: e.dma_start(out=x2_dbg[row0:row0 + T, :], in_=x0b[0:T, :]), reads=[x0b], writes=['x2_dbg'])

                def sample_mem_attn():
                    for b in range(DEC_B):
                        mf, mb, mT = sm['mf'], sm['mb'], sm['mT']
                        for which, src_d in ((0, cmk_d), (1, cmv_d)):
                            p.dma('sp', lambda e, b=b, src_d=src_d, which=which: e.dma_start(out=mf[which][:, :, :], in_=src_d[b, :, :].rearrange("(mt m) f -> m mt f", mt=2)),
                                  writes=[mf[which]])
                            p.op('pool' if which else 'dve', lambda e, which=which: e.tensor_copy(out=mb[which][:, :, :], in_=mf[which][:, :, :]), reads=[mf[which]], writes=[mb[which]])
                        bk = nb()
                        bv = bk[:].bitcast(BF16)
                        for mt in range(2):
                            for h in range(4):
                                tr(bv[:, (mt * 4 + h) * 128:(mt * 4 + h + 1) * 128], mb[0][:, mt, h * 128:(h + 1) * 128], 128, [mb[0]], [bk])
                        p.op('act', lambda e, bv=bv: e.copy(out=mT[:, :, :], in_=bv[:, :].rearrange("q (k m) -> q k m", k=8)), reads=[bk], writes=[mT])
                        bk = nb()
                        for mt in range(2):
                            for h in range(4):
                                mm(bk[:, (mt * 4 + h) * 4:(mt * 4 + h + 1) * 4], mT[:, mt * 4 + h, :], qmT[:, h, 4 * b:4 * b + 4], True, True, [mT, qmT], [bk])
                        Pm = sm['Pm']
                        p.op('act', lambda e, bk=bk: e.activation(out=Pm[:, :], in_=bk[:, 0:32], func=AF.Exp, scale=128.0 ** -0.5), reads=[bk], writes=[Pm])
                        o6, o7 = pb[6], pb[7]
                        for h in range(4):
                            for mt in range(2):
                                mm(o6[:, h * 4:(h + 1) * 4], mb[1][:, mt, h * 128:(h + 1) * 128], Pm[:, (mt * 4 + h) * 4:(mt * 4 + h + 1) * 4], mt == 0, mt == 1, [mb[1], Pm], [o6])
                        for h in range(4):
                            for mt in range(2):
                                mm(o7[:, h * 4:(h + 1) * 4], ones_bf[:, :], Pm[:, (mt * 4 + h) * 4:(mt * 4 + h + 1) * 4], mt == 0, mt == 1, [ones_bf, Pm], [o7])
                        rc = sm['rc']
                        p.op('dve', lambda e: e.reciprocal(out=rc[:, 0:16], in_=o7[:, 0:16]), reads=[o7], writes=[rc])
                        p.op('dve', lambda e, b=b: e.tensor_tensor(out=omT[:, :, 4 * b:4 * b + 4], in0=o6[:, 0:16].rearrange("q (h t) -> q h t", h=4),
                                                                 in1=rc[:, 0:16].rearrange("q (h t) -> q h t", h=4), op=ALU.mult), reads=[o6, rc], writes=[omT])

                sm = {}

                def sample_dsa(stk):
                    T = NS
                    NIT = 36
                    gbuf = p.sb('gbuf', [64, 8192], F32, stk)
                    cbs = [p.sb('cbs%d' % i, [64, 8192], BF16, stk) for i in range(2)]
                    KTb = p.sb('KTb', [128, 64, 64], BF16, stk)
                    kiTc_l = [p.sb('kiTc%d' % i, [64, 16, 64], BF16, stk) for i in range(3)]
                    pt_sb = p.sb('pt_sb', [64, 16], I32, stk)
                    idx2 = p.sb('idx2', [64, 16, 2], I32, stk)
                    rS_l = [p.sb('rS%d' % i, [64, 16, 32], F32, stk) for i in range(3)]
                    NCH = cfg.get('nch', 4)
                    scTbs = [p.sb('scTb%d' % i, [64, 128, 4], F32, stk) for i in range(NCH)]
                    scns = [p.sb('scn%d' % i, [4, 4], F32, stk) for i in range(NCH)]
                    rSn_l = [p.sb('rSn%d' % i, [4, 32], F32, stk) for i in range(2)]
                    chunk_ctr = [0]
                    Wbc = p.sb('Wbc', [64, 16, 32], F32, stk)
                    Dg = p.sb('Dg', [64, 16, 8, 4], F32, stk)
                    q2T = p.sb('q2T', [128, 4, 64], BF16, stk)
                    kT2n = p.sb('kT2n', [128, 64], BF16, stk)
                    kiTs = p.sb('kiTs', [64, 64], BF16, stk)
                    vnf = p.sb('vnf', [4, 16, 128], F32, stk)
                    vnew = p.sb('vnew', [4, 16, 128], BF16, stk)
                    negm4 = p.sb('negm4', [4, 4], F32, stk)
                    pow2 = p.sb('pow2', [64, 48], F32, stk)
                    hs_l = [p.sb('hs%d' % i, [64, 4], F32, stk) for i in range(NCH)]
                    hsb = p.sb('hsb', [64, 2], BF16, stk)
                    Wsc_l = [p.sb('Wsc%d' % i, [64, 2], F32, stk) for i in range(NCH)]
                    Wtab_l = [p.sb('Wtab%d' % i, [64, 48], F32, stk) for i in range(NCH)]
                    lo_l = [p.sb('lo%d' % i, [64, 4], F32, stk) for i in range(NCH)]
                    mid_l = [p.sb('mid%d' % i, [64, 4], F32, stk) for i in range(NCH)]
                    ge_l = [p.sb('ge%d' % i, [64, 4], F32, stk) for i in range(NCH)]
                    cmpb_l = [p.sb('cmpb%d' % i, [64, 128, 4], BF16, stk) for i in range(NCH)]
                    cntp_l = [p.sb('cntp%d' % i, [64, 4], F32, stk) for i in range(NCH)]
                    cmpn_l = [p.sb('cmpn%d' % i, [4, 4], F32, stk) for i in range(NCH)]
                    mask_s_l = cmpb_l
                    maskn_l = [p.sb('maskn%d' % i, [4, 4], BF16, stk) for i in range(NCH)]
                    Ess = [p.sb('Es%d' % i, [64, 16, 32], BF16, stk) for i in range(2)]
                    PTss = [p.sb('PTs%d' % i, [64, 16, 32], BF16, stk) for i in range(2)]
                    En = p.sb('En', [4, 32], BF16, stk)
                    PTn = p.sb('PTn', [4, 32], BF16, stk)
                    rcs = p.sb('rcs', [128, 32], F32, stk)
                    ybTs = p.sb('ybTs', [128, 4, 64], BF16, stk)

                    p.dma('sp', lambda e: e.dma_start(out=pt_sb[:, :], in_=pt_d.rearrange("b j -> j b"), allow_slow_non_contiguous=True), writes=[pt_sb])
                    p.dma('sp', lambda e: e.dma_start(out=negm4[:, :], in_=c_negm4[:, :]), writes=[negm4])
                    p.dma('sp', lambda e: e.dma_start(out=pow2[:, :], in_=c_pow2[0:64, :]), writes=[pow2])
                    for half in range(2):
                        p.op('dve', lambda e, half=half: e.tensor_scalar(out=idx2[:, :, half], in0=pt_sb[:, :], scalar1=2, scalar2=half, op0=ALU.mult, op1=ALU.add),
                             reads=[pt_sb], writes=[idx2])
                    p.op('dve', lambda e: e.tensor_tensor(out=Dg[:, :, :, :], in0=wsc_s[:, :].unsqueeze(1).unsqueeze(3).to_broadcast([64, 16, 8, 4]),
                                                          in1=identf[0:64, 0:64].rearrange("q (b t) -> q b t", b=16).unsqueeze(2).to_broadcast([64, 16, 8, 4]), op=ALU.mult),
                         reads=[wsc_s, identf], writes=[Dg])
                    bk = nb()
                    mm(bk[0:64, :], onesf[0:64, 0:64], Dg[:, :, :, :].rearrange("q b h t -> q (b h t)"), True, True, [onesf, Dg], [bk])
                    p.op('act', lambda e, bk=bk: e.copy(out=Wbc[:, :, :], in_=bk[0:64, :].rearrange("q (b x) -> q b x", b=16)), reads=[bk], writes=[Wbc])
                    p.op('pool', lambda e: e.tensor_copy(out=q2T[0:64, :, :], in_=qTs[:, 0:4, :]), reads=[qTs], writes=[q2T])
                    p.dma('sp', lambda e: e.dma_start(out=q2T[64:128, :, :], in_=qTs[:, 4:8, :]), reads=[qTs], writes=[q2T])
                    bk = nb()
                    bv = bk[:].bitcast(BF16)
                    tr(bv[:, 0:64], kb_s[:, 0:128], 64, [kb_s], [bk])
                    tr(bv[0:64, 64:128], kb_s[:, 128:192], 64, [kb_s], [bk])
                    p.op('act', lambda e, bv=bv: e.copy(out=kT2n[:, :], in_=bv[:, 0:64]), reads=[bk], writes=[kT2n])
                    p.op('act', lambda e, bv=bv: e.copy(out=kiTs[:, :], in_=bv[0:64, 64:128]), reads=[bk], writes=[kiTs])
                    p.dma('sp', lambda e: e.dma_start(out=vnf[:, :, :], in_=v_s.rearrange("(b t) d -> t b d", t=4)), reads=['vo'], writes=[vnf])
                    p.op('dve', lambda e: e.tensor_copy(out=vnew[:, :, :], in_=vnf[:, :, :]), reads=[vnf], writes=[vnew])

                    nb_ = cfg.get('n_sb', DEC_B)
                    NITB = cfg.get('nit', 26)
                    gseq = []
                    for b0_ in range(0, nb_, NCH):
                        grp_ = list(range(b0_, min(b0_ + NCH, nb_)))
                        gseq += [('ki', b_, 0) for b_ in grp_]
                        for b_ in grp_:
                            gseq += [('K', b_, 0), ('V', b_, 0), ('K', b_, 1), ('V', b_, 1)]
                    gpos = {it: i for i, it in enumerate(gseq)}
                    g_next = [0]

                    def emit_gather(i):
                        kind, b_, half = gseq[i]
                        if kind == 'ki':
                            src, idx_ap, idx_t = ckidx_d, pt_sb[:, b_:b_ + 1], pt_sb
                        else:
                            src, idx_ap, idx_t = (ck_d if kind == 'K' else cv_d), idx2[:, b_, half:half + 1], idx2
                        p.dma('pool', lambda e: e.indirect_dma_start(out=gbuf[:, :], out_offset=None, in_=src[:, :],
                                                                     in_offset=bass.IndirectOffsetOnAxis(ap=idx_ap, axis=0)), reads=[idx_t], writes=[gbuf])

                    g_cast = [0]

                    def use(item):
                        i = gpos[item]
                        if g_next[0] == 0:
                            emit_gather(0)
                            g_next[0] = 1
                        while g_cast[0] <= i:
                            c = g_cast[0]
                            dst = cbs[c % 2]
                            p.op('act', lambda e, dst=dst: e.copy(out=dst[:, 0:4096], in_=gbuf[:, 0:4096]), reads=[gbuf], writes=[dst])
                            p.op('pool', lambda e, dst=dst: e.tensor_copy(out=dst[:, 4096:8192], in_=gbuf[:, 4096:8192]), reads=[gbuf], writes=[dst])
                            g_cast[0] += 1
                            if g_next[0] < len(gseq):
                                emit_gather(g_next[0])
                                g_next[0] += 1
                        return cbs[i % 2]

                    def ki_steps(b):
                        qi_b = qiTs[:, :, 4 * b:4 * b + 4]
                        scT = scTbs[b % NCH]
                        scn_ = scns[b % NCH]
                        steps = []

                        def chunk(lc):
                            kiTc = kiTc_l[chunk_ctr[0] % 3]
                            rS = rS_l[chunk_ctr[0] % 3]
                            chunk_ctr[0] += 1
                            cb = use(('ki', b, 0))
                            bk = nb()
                            bv = bk[:].bitcast(BF16)
                            for i in range(16):
                                l = lc * 16 + i
                                tr(bv[0:64, i * 64:(i + 1) * 64], cb[:, l * 64:(l + 1) * 64], 64, [cb], [bk])
                            p.op('act', lambda e: e.copy(out=kiTc[:, :, :], in_=bv[0:64, :].rearrange("q (i j) -> q i j", i=16)), reads=[bk], writes=[kiTc])
                            bk2 = nb()
                            for i in range(16):
                                mm(bk2[0:64, i * 32:(i + 1) * 32], kiTc[:, i, :], qi_b, True, True, [kiTc, qiTs], [bk2])
                            p.op('act', lambda e: e.activation(out=rS[:, :, :], in_=bk2[0:64, :].rearrange("q (i x) -> q i x", i=16), func=AF.Relu), reads=[bk2], writes=[rS])
                            p.op('pool', lambda e: e.tensor_tensor(out=rS[:, :, :], in0=rS[:, :, :], in1=Wbc[:, b:b + 1, :].to_broadcast([64, 16, 32]), op=ALU.mult),
                                 reads=[rS, Wbc], writes=[rS])
                            p.op('dve', lambda e: e.tensor_reduce(out=scT[:, lc * 16:(lc + 1) * 16, :], in_=rS[:, :, :].rearrange("q i (h t) -> q i t h", h=8),
                                                                  op=ALU.add, axis=AX.X), reads=[rS], writes=[scT])

                        def newkeys():
                            rSn = rSn_l[b % 2]
                            bk = nb()
                            mm(bk[0:4, 0:32], kiTs[:, 4 * b:4 * b + 4], qi_b, True, True, [kiTs, qiTs], [bk])
                            p.op('act', lambda e: e.activation(out=rSn[:, :], in_=bk[0:4, 0:32], func=AF.Relu), reads=[bk], writes=[rSn])
                            p.op('pool', lambda e: e.tensor_tensor(out=rSn[:, :], in0=rSn[:, :], in1=Wbc[0:4, b, :], op=ALU.mult), reads=[rSn, Wbc], writes=[rSn])
                            p.op('dve', lambda e: e.tensor_reduce(out=scn_[:, :], in_=rSn[:, :].rearrange("q (h t) -> q t h", h=8), op=ALU.add, axis=AX.X), reads=[rSn], writes=[scn_])
                        for lc in range(8):
                            steps.append(lambda lc=lc: chunk(lc))
                        steps.append(newkeys)
                        return steps

                    def bisect_setup(b):
                        c = b % NCH
                        scT, scn_, hs, Wsc, Wtab, lo, cmpb = scTbs[c], scns[c], hs_l[c], Wsc_l[c], Wtab_l[c], lo_l[c], cmpb_l[c]
                        p.op('act', lambda e: e.activation(out=cmpb[:, :, :].rearrange("q l t -> q (l t)"), in_=scT[:, :, :].rearrange("q l t -> q (l t)"), func=AF.Square, accum_out=hs[:, 0:1]),
                             reads=[scT], writes=[cmpb, hs])
                        p.op('act', lambda e: e.activation(out=cmpb[0:4, 0, :], in_=scn_[:, :], func=AF.Square, accum_out=hs[0:4, 1:2]), reads=[scn_], writes=[cmpb, hs])
                        p.op('dve', lambda e: e.tensor_tensor(out=scn_[:, :], in0=scn_[:, :], in1=negm4[:, :], op=ALU.add), reads=[scn_, negm4], writes=[scn_])
                        bk = nb()
                        mm(bk[0:64, 0:1], onesf[0:64, 0:64], hs[:, 0:1], True, False, [onesf, hs], [bk])
                        mm(bk[0:64, 0:1], onesf[0:4, 0:64], hs[0:4, 1:2], False, True, [onesf, hs], [bk])
                        p.op('act', lambda e, bk=bk: e.activation(out=Wsc[:, 0:1], in_=bk[0:64, 0:1], func=AF.Sqrt), reads=[bk], writes=[Wsc])
                        p.op('dve', lambda e: e.tensor_scalar(out=Wsc[:, 0:1], in0=Wsc[:, 0:1], scalar1=2.2, scalar2=2.0, op0=ALU.mult, op1=ALU.add), reads=[Wsc], writes=[Wsc])
                        p.op('dve', lambda e: e.tensor_scalar(out=Wsc[:, 1:2], in0=Wsc[:, 0:1], scalar1=-0.5, scalar2=None, op0=ALU.mult), reads=[Wsc], writes=[Wsc])
                        p.op('dve', lambda e: e.tensor_scalar(out=Wtab[:, :], in0=pow2[:, :], scalar1=Wsc[:, 0:1], scalar2=None, op0=ALU.mult), reads=[pow2, Wsc], writes=[Wtab])
                        p.op('dve', lambda e: e.tensor_scalar(out=lo[:, :], in0=pow2[:, 0:4], scalar1=0.0, scalar2=Wsc[:, 1:2], op0=ALU.mult, op1=ALU.add), reads=[pow2, Wsc], writes=[lo])

                    def bisect_iter(b, k):
                        c = b % NCH
                        scT, scn_, Wtab, lo, mid, ge, cmpb, cntp, cmpn = scTbs[c], scns[c], Wtab_l[c], lo_l[c], mid_l[c], ge_l[c], cmpb_l[c], cntp_l[c], cmpn_l[c]
                        p.op('dve', lambda e: e.tensor_scalar(out=mid[:, :], in0=lo[:, :], scalar1=Wtab[:, k:k + 1], scalar2=None, op0=ALU.add), reads=[lo, Wtab], writes=[mid])
                        p.op('dve', lambda e: e.tensor_tensor(out=cmpb[:, :, :], in0=scT[:, :, :], in1=mid[:, :].unsqueeze(1).to_broadcast([64, 128, 4]), op=ALU.is_ge),
                             reads=[scT, mid], writes=[cmpb])
                        p.op('dve', lambda e: e.tensor_reduce(out=cntp[:, :], in_=cmpb[:, :, :].rearrange("q l t -> q t l"), op=ALU.add, axis=AX.X), reads=[cmpb], writes=[cntp])
                        p.op('dve', lambda e: e.tensor_tensor(out=cmpn[:, :], in0=scn_[:, :], in1=mid[0:4, :], op=ALU.is_ge), reads=[scn_, mid], writes=[cmpn])
                        bk = nb()
                        mm(bk[0:64, 0:4], onesf[0:64, 0:64], cntp[:, :], True, False, [onesf, cntp], [bk])
                        mm(bk[0:64, 0:4], onesf[0:4, 0:64], cmpn[:, :], False, True, [onesf, cmpn], [bk])
                        p.op('dve', lambda e: e.tensor_scalar(out=ge[:, :], in0=bk[0:64, 0:4], scalar1=255.5, scalar2=None, op0=ALU.is_ge), reads=[bk], writes=[ge])
                        p.op('dve', lambda e: e.scalar_tensor_tensor(out=lo[:, :], in0=ge[:, :], scalar=Wtab[:, k:k + 1], in1=lo[:, :], op0=ALU.mult, op1=ALU.add),
                             reads=[ge, Wtab, lo], writes=[lo])

                    def bisect_finish(b):
                        c = b % NCH
                        scT, scn_, lo, mask_s, maskn = scTbs[c], scns[c], lo_l[c], mask_s_l[c], maskn_l[c]
                        p.op('dve', lambda e: e.tensor_tensor(out=mask_s[:, :, :], in0=scT[:, :, :], in1=lo[:, :].unsqueeze(1).to_broadcast([64, 128, 4]), op=ALU.is_ge),
                             reads=[scT, lo], writes=[mask_s])
                        p.op('dve', lambda e: e.tensor_tensor(out=maskn[:, :], in0=scn_[:, :], in1=lo[0:4, :], op=ALU.is_ge), reads=[scn_, lo], writes=[maskn])

                    def attend(b):
                        mask_s, maskn = mask_s_l[b % NCH], maskn_l[b % NCH]
                        o6, o7 = pb[6], pb[7]
                        first = True
                        for half in range(2):
                            cbK = use(('K', b, half))
                            for lc in range(4):
                                bk = nb()
                                bv = bk[:].bitcast(BF16)
                                for i in range(16):
                                    l = lc * 16 + i
                                    tr(bv[:, i * 64:(i + 1) * 64], cbK[:, l * 128:(l + 1) * 128], 64, [cbK], [bk])
                                p.op('act', lambda e, bv=bv, lc=lc: e.copy(out=KTb[:, lc * 16:(lc + 1) * 16, :], in_=bv[:, :].rearrange("q (i j) -> q i j", i=16)), reads=[bk], writes=[KTb])
                            cbV = use(('V', b, half))
                            prev = None
                            for lc in range(5):
                                if lc < 4:
                                    bkg = [nb(), nb()]
                                    for i in range(16):
                                        l = lc * 16 + i
                                        for g in range(2):
                                            mm(bkg[g][0:64, i * 16:(i + 1) * 16], KTb[g * 64:(g + 1) * 64, l, :], q2T[g * 64:(g + 1) * 64, :, 4 * b:4 * b + 4], True, True, [KTb, q2T], [bkg[g]])
                                    E_, P_ = Ess[lc % 2], PTss[lc % 2]
                                    for g in range(2):
                                        p.op('act', lambda e, g=g, bkg=bkg, E_=E_: e.activation(out=E_[:, :, g * 16:(g + 1) * 16], in_=bkg[g][0:64, 0:256].rearrange("q (i x) -> q i x", i=16),
                                                                                                  func=AF.Exp, scale=0.125), reads=[bkg[g]], writes=[E_])
                                    l0 = half * 64 + lc * 16
                                    p.op('pool', lambda e, l0=l0, E_=E_, P_=P_: e.tensor_tensor(out=P_[:, :, :].rearrange("q i (x t) -> q i x t", t=4), in0=E_[:, :, :].rearrange("q i (x t) -> q i x t", t=4),
                                                                                              in1=mask_s[:, l0:l0 + 16, :].unsqueeze(2).to_broadcast([64, 16, 8, 4]), op=ALU.mult),
                                         reads=[E_, mask_s], writes=[P_])
                                if prev is not None:
                                    plc, P_prev = prev
                                    for i in range(16):
                                        l = plc * 16 + i
                                        mm(o6[:, 0:32], cbV[:, l * 128:(l + 1) * 128], P_prev[:, i, :], first, False, [cbV, P_prev], [o6])
                                        mm(o7[:, 0:32], ones_bf[0:64, :], P_prev[:, i, :], first, False, [ones_bf, P_prev], [o7])
                                        first = False
                                prev = (lc, PTss[lc % 2]) if lc < 4 else None
                        bkg = [nb(), nb()]
                        for g in range(2):
                            mm(bkg[g][0:4, 0:16], kT2n[g * 64:(g + 1) * 64, 4 * b:4 * b + 4], q2T[g * 64:(g + 1) * 64, :, 4 * b:4 * b + 4], True, True, [kT2n, q2T], [bkg[g]])
                        for g in range(2):
                            p.op('act', lambda e, g=g, bkg=bkg: e.activation(out=En[:, g * 16:(g + 1) * 16], in_=bkg[g][0:4, 0:16], func=AF.Exp, scale=0.125), reads=[bkg[g]], writes=[En])
                        p.op('pool', lambda e: e.tensor_tensor(out=PTn[:, :].rearrange("q (x t) -> q x t", t=4), in0=En[:, :].rearrange("q (x t) -> q x t", t=4),
                                                               in1=maskn[:, :].unsqueeze(1).to_broadcast([4, 8, 4]), op=ALU.mult), reads=[En, maskn], writes=[PTn])
                        mm(o6[:, 0:32], vnew[:, b, :], PTn[:, :], False, True, [vnew, PTn], [o6])
                        mm(o7[:, 0:32], ones_bf[0:4, :], PTn[:, :], False, True, [ones_bf, PTn], [o7])
                        p.op('dve', lambda e: e.reciprocal(out=rcs[:, :], in_=o7[:, 0:32]), reads=[o7], writes=[rcs])
                        for g in range(2):
                            p.op('dve', lambda e, g=g: e.tensor_tensor(out=ybTs[g * 64:(g + 1) * 64, :, 4 * b:4 * b + 4],
                                                                     in0=o6[g * 64:(g + 1) * 64, g * 16:(g + 1) * 16].rearrange("q (r t) -> q r t", r=4),
                                                                     in1=rcs[g * 64:(g + 1) * 64, g * 16:(g + 1) * 16].rearrange("q (r t) -> q r t", r=4), op=ALU.mult),
                                 reads=[o6, rcs], writes=[ybTs])

                    for b0_ in range(0, nb_, NCH):
                        grp_ = list(range(b0_, min(b0_ + NCH, nb_)))
                        for b in grp_:
                            for st_ in ki_steps(b):
                                st_()
                        if cfg.get('sb_stage', 9) < 2:
                            continue
                        for b in grp_:
                            bisect_setup(b)
                        for k in range(NITB):
                            for b in grp_:
                                bisect_iter(b, k)
                        for b in grp_:
                            bisect_finish(b)
                        if cfg.get('sb_stage', 9) < 3:
                            continue
                        for b in grp_:
                            attend(b)
                    for r in range(4):
                        bk = nb()
                        bv = bk[:].bitcast(BF16)
                        tr(bv[0:64, 0:128], ybTs[:, r, :], 128, [ybTs], [bk])
                        p.op('act', lambda e, bv=bv, r=r: e.copy(out=ycats[:, 512:1024].rearrange("q (g r d) -> q g r d", g=2, r=4)[:, :, r, :],
                                                                 in_=bv[0:64, 0:128].rearrange("q (g d) -> q g d", g=2)), reads=[bk], writes=[ycats])


                if 'P3' in do:
                    proj_tile(xs_d[:, :], NS, 0, True, x0s, ycats)
                    feature_major(NS, 0, qTs, qiTs)
                    p.op('pool', lambda e: e.tensor_copy(out=kvf_s[:, :], in_=kvf[0:NS, :]), reads=[kvf], writes=[kvf_s])
                    p.op('pool', lambda e: e.tensor_copy(out=kb_s[:, :], in_=kb[0:NS, :]), reads=[kb], writes=[kb_s])
                    p.op('pool', lambda e: e.tensor_copy(out=wsc_s[:, :], in_=wsc[0:NS, :]), reads=[wsc], writes=[wsc_s])
                if 'P2' in do:
                    for ti in range(cfg.get('n_tiles', NT)):
                        proj_tile(xp_d[ti * 128:(ti + 1) * 128, :], 128, ti, False, x0, ycat)
                        feature_major(128, ti, qT, qiT)
                        if 'P4' in do and cfg.get('interleave_conv', True):
                            for ec in range(ti * 8, ti * 8 + 8):
                                convert_chunk(ec, cub[ec % 2], cut[ec % 2], cvb[ec % 2])
                            conv_done[0] = ti * 8 + 8
                        prompt_dsa(ti)
                        out_proj_and_mem(128, ti * 128, False, x0, ycat)
            sw.close()
            p.barrier()
            if 'P3' in do:
                with ExitStack() as s3s:
                    sample_dsa(s3s)
                p.barrier()
                with ExitStack() as s3m:
                    sm['mf'] = [p.sb('mf%d' % i, [128, 2, 512], F32, s3m) for i in range(2)]
                    sm['mb'] = [p.sb('mb%d' % i, [128, 2, 512], BF16, s3m) for i in range(2)]
                    sm['mT'] = p.sb('mT', [128, 8, 128], BF16, s3m)
                    sm['Pm'] = p.sb('Pm', [128, 32], BF16, s3m)
                    sm['rc'] = p.sb('rc', [128, 16], F32, s3m)
                    out_proj_and_mem(NS, SEQ, True, x0s, ycats)
            p.barrier()

        if 'P4' in do:
            nbmod[0] = 4
            with ExitStack() as s4:
                iota_f = p.sb('iota_f', [128, 128], F32, s4)
                gfin = p.sb('gfin', [128, D], F32, s4)
                p.dma('sp', lambda e: e.dma_start(out=iota_f[:], in_=c_iota[:, :]), writes=[iota_f])
                p.dma('sp', lambda e: e.dma_start(out=gfin[:], in_=g_fin_d.partition_broadcast(128)), writes=[gfin])
                pwq = p.sb('pwq_sb', [128, 8, D], BF16, s4)
                keysT = p.sb('keysT', [64, 16, 128], BF16, s4)
                with ExitStack() as s5:
                    stage = [p.sb('pstage%d' % i, [128, D], F32, s5) for i in range(2)]
                    load_weight(pwq, pwq_d, 8, D, None, stage)
                    kbf = p.sb('kbf', [128, 64], BF16, s5)
                    for hc in range(16):
                        stg = stage[hc % 2]
                        p.dma('sp', lambda e, hc=hc, stg=stg: e.dma_start(out=stg[:, 0:64], in_=pkeys_d[hc, :, :]), writes=[stg])
                        p.op('dve', lambda e, stg=stg: e.tensor_copy(out=kbf[:], in_=stg[:, 0:64]), reads=[stg], writes=[kbf])
                        bk = nb()
                        bv = bk[:].bitcast(BF16)
                        tr(bv[0:64, 0:128], kbf[:, :], 128, [kbf], [bk])
                        p.op('act', lambda e, hc=hc, bv=bv: e.copy(out=keysT[:, hc, :], in_=bv[0:64, 0:128]), reads=[bk], writes=[keysT])
                    ubf = [p.sb('ubf%d' % i, [128, D], BF16, s5) for i in range(2)]
                    utb = [p.sb('utb%d' % i, [128, D], BF16, s5) for i in range(2)]
                    vbf = [p.sb('vbf%d' % i, [128, D], BF16, s5) for i in range(2)]
                    for ec in range(conv_done[0], cfg.get('n_ec', 128)):
                        convert_chunk(ec, ubf[ec % 2], utb[ec % 2], vbf[ec % 2])

                p.barrier()
                xg = p.sb('xg', [128, 2, D], F32, s4)
                XT = p.sb('XT', [128, 8, 256], BF16, s4)
                hn = p.sb('hn', [128, D], BF16, s4)
                hnT = p.sb('hnT', [128, 8, 128], BF16, s4)
                sq4 = p.sb('sq4', [128, D], F32, s4)
                qpT = p.sb('qpT', [64, 16, 128], BF16, s4)
                ssb = p.sb('ssb', [128, 16, 128], F32, s4)
                wk = p.sb('wk', [128, 128], F32, s4)
                a16 = p.sb('a16', [128, 8, 16], F32, s4)
                b16 = p.sb('b16', [128, 8, 16], F32, s4)
                iau = p.sb('iau', [128, 8, 16], U32, s4)
                ibu = p.sb('ibu', [128, 8, 16], U32, s4)
                iaf = p.sb('iaf', [128, 8, 16], F32, s4)
                ibf = p.sb('ibf', [128, 8, 16], F32, s4)
                cand = p.sb('cand', [128, 8, 256], F32, s4)
                wkc = p.sb('wkc', [128, 256], F32, s4)
                c16 = p.sb('c16', [128, 8, 16], F32, s4)
                icu = p.sb('icu', [128, 8, 16], U32, s4)
                iju = p.sb('iju', [128, 2, 128], U32, s4)
                ijf = p.sb('ijf', [128, 2, 128], F32, s4)
                eq = p.sb('eq', [128, 128, 16], F32, s4)
                SEL = p.sb('SEL', [128, 3, 128], F32, s4)
                e16 = p.sb('e16', [128, 8, 16], F32, s4)
                z8 = p.sb('z8', [128, 16], F32, s4)
                selT = p.sb('selT', [128, 3, 256], F32, s4)
                Gt = p.sb('Gt', [128, 256, 128], BF16, s4)
                ohb1 = [p.sb('ohb1_%d' % i, [128, 8, 128], BF16, s4) for i in range(2)]
                ohb0 = [p.sb('ohb0_%d' % i, [128, 8, 128], BF16, s4) for i in range(2)]
                ohbg = [p.sb('ohbg_%d' % i, [128, 8, 128], BF16, s4) for i in range(2)]
                selTb = p.sb('selTb', [128, 3, 256], BF16, s4)
                iota_b = p.sb('iota_b', [128, 128], BF16, s4)
                p.op('pool', lambda e: e.tensor_copy(out=iota_b[:, :], in_=iota_f[:, :]), reads=[iota_f], writes=[iota_b])
                NBUF = 4
                UTc = [p.sb('UTc%d' % i, [128, 8, 128], BF16, s4) for i in range(NBUF)]
                Vc = [p.sb('Vc%d' % i, [128, D], BF16, s4) for i in range(NBUF)]
                gab = [p.sb('gab%d' % i, [128, 256], BF16, s4) for i in range(3)]
                GAb = [p.sb('GAb%d' % i, [128, 256], BF16, s4) for i in range(3)]
                pre = p.sb('pre', [128, D], F32, s4)
                yo = pre


                def peer_select(T, s):
                    norm_T(xg[:, s, :], T, 'f', hn, hnT, ssd, sq4, gcol=gcols['ffn'])
                    p.op('pool', lambda e: e.tensor_copy(out=XT[:, :, s * 128:s * 128 + T], in_=hnT[:, :, 0:T]), reads=[hnT], writes=[XT])
                    for q4 in range(4):
                        bk = nb()
                        for k4 in range(4):
                            hc = q4 * 4 + k4
                            for c in range(8):
                                mm(bk[0:64, k4 * T:(k4 + 1) * T], pwq[:, c, hc * 64:(hc + 1) * 64], hnT[:, c, 0:T], c == 0, c == 7, [pwq, hnT], [bk])
                        p.op('act', lambda e, bk=bk, q4=q4: e.copy(out=qpT[:, q4 * 4:(q4 + 1) * 4, 0:T], in_=bk[0:64, 0:4 * T].rearrange("q (k t) -> q k t", k=4)),
                             reads=[bk], writes=[qpT])
                    for q4 in range(4):
                        bk = nb()
                        for k4 in range(4):
                            hc = q4 * 4 + k4
                            mm(bk[0:T, k4 * 128:(k4 + 1) * 128], qpT[:, hc, 0:T], keysT[:, hc, :], True, True, [qpT, keysT], [bk])
                        p.op('act', lambda e, bk=bk, q4=q4: e.copy(out=ssb[0:T, q4 * 4:(q4 + 1) * 4, :], in_=bk[0:T, :].rearrange("q (k n) -> q k n", k=4)),
                             reads=[bk], writes=[ssb])
                    for h in range(8):
                        for cc, (vals, idxs) in enumerate(((a16, iau), (b16, ibu))):
                            src = ssb[0:T, 2 * h + cc, :]
                            p.op('dve', lambda e, src=src, vals=vals, h=h: e.max(out=vals[0:T, h, 0:8], in_=src), reads=[ssb], writes=[vals])
                            p.op('dve', lambda e, src=src, vals=vals, idxs=idxs, h=h: e.max_index(out=idxs[0:T, h, 0:8], in_max=vals[0:T, h, 0:8], in_values=src),
                                 reads=[ssb, vals], writes=[idxs])
                            p.op('dve', lambda e, src=src, vals=vals, h=h: e.match_replace(out=wk[0:T, :], in_to_replace=vals[0:T, h, 0:8], in_values=src, imm_value=NEG),
                                 reads=[ssb, vals], writes=[wk])
                            p.op('dve', lambda e, vals=vals, h=h: e.max(out=vals[0:T, h, 8:16], in_=wk[0:T, :]), reads=[wk], writes=[vals])
                            p.op('dve', lambda e, vals=vals, idxs=idxs, h=h: e.max_index(out=idxs[0:T, h, 8:16], in_max=vals[0:T, h, 8:16], in_values=wk[0:T, :]),
                                 reads=[wk, vals], writes=[idxs])
                    for h in range(8):
                        p.op('dve', lambda e, h=h: e.tensor_tensor(out=cand[0:T, h, :].rearrange("q (i j) -> q i j", i=16),
                                                                    in0=a16[0:T, h, :].unsqueeze(2).to_broadcast([T, 16, 16]),
                                                                    in1=b16[0:T, h, :].unsqueeze(1).to_broadcast([T, 16, 16]), op=ALU.add),
                             reads=[a16, b16], writes=[cand])
                    for h in range(8):
                        src = cand[0:T, h, :]
                        p.op('dve', lambda e, src=src, h=h: e.max(out=c16[0:T, h, 0:8], in_=src), reads=[cand], writes=[c16])
                        p.op('dve', lambda e, src=src, h=h: e.max_index(out=icu[0:T, h, 0:8], in_max=c16[0:T, h, 0:8], in_values=src), reads=[cand, c16], writes=[icu])
                        p.op('dve', lambda e, src=src, h=h: e.match_replace(out=wkc[0:T, :], in_to_replace=c16[0:T, h, 0:8], in_values=src, imm_value=NEG),
                             reads=[cand, c16], writes=[wkc])
                        p.op('dve', lambda e, h=h: e.max(out=c16[0:T, h, 8:16], in_=wkc[0:T, :]), reads=[wkc], writes=[c16])
                        p.op('dve', lambda e, h=h: e.max_index(out=icu[0:T, h, 8:16], in_max=c16[0:T, h, 8:16], in_values=wkc[0:T, :]), reads=[wkc, c16], writes=[icu])
                    icf = icu[0:T, :, :].rearrange("q h k -> q (h k)")
                    p.op('dve', lambda e: e.tensor_scalar(out=iju[0:T, 0, :], in0=icf, scalar1=4, scalar2=None, op0=ALU.logical_shift_right), reads=[icu], writes=[iju])
                    p.op('dve', lambda e: e.tensor_scalar(out=iju[0:T, 1, :], in0=icf, scalar1=15, scalar2=None, op0=ALU.bitwise_and), reads=[icu], writes=[iju])
                    p.op('dve', lambda e: e.tensor_copy(out=ijf[0:T, :, :], in_=iju[0:T, :, :]), reads=[iju], writes=[ijf])
                    p.op('dve', lambda e: e.tensor_copy(out=iaf[0:T, :, :], in_=iau[0:T, :, :]), reads=[iau], writes=[iaf])
                    p.op('dve', lambda e: e.tensor_copy(out=ibf[0:T, :, :], in_=ibu[0:T, :, :]), reads=[ibu], writes=[ibf])
                    for w_, srcf in ((0, iaf), (1, ibf)):
                        p.op('dve', lambda e, w_=w_: e.tensor_tensor(out=eq[0:T, :, :], in0=ijf[0:T, w_, :].unsqueeze(2).to_broadcast([T, 128, 16]),
                                                                     in1=iota_f[0:T, 0:16].unsqueeze(1).to_broadcast([T, 128, 16]), op=ALU.is_equal),
                             reads=[ijf, iota_f], writes=[eq])
                        p.op('dve', lambda e, srcf=srcf: e.tensor_tensor(out=eq[0:T, :, :].rearrange("q (h k) i -> q h k i", h=8),
                                                                         in0=eq[0:T, :, :].rearrange("q (h k) i -> q h k i", h=8),
                                                                         in1=srcf[0:T, :, :].unsqueeze(2).to_broadcast([T, 8, 16, 16]), op=ALU.mult),
                             reads=[eq, srcf], writes=[eq])
                        p.op('dve', lambda e, w_=w_: e.tensor_reduce(out=SEL[0:T, w_, :], in_=eq[0:T, :, :], op=ALU.add, axis=AX.X), reads=[eq], writes=[SEL])
                    p.op('dve', lambda e: e.tensor_tensor(out=e16[0:T, :, :], in0=c16[0:T, :, :], in1=c16[0:T, :, 0:1].to_broadcast([T, 8, 16]), op=ALU.subtract),
                         reads=[c16], writes=[e16])
                    p.op('act', lambda e: e.activation(out=e16[0:T, :, :], in_=e16[0:T, :, :], func=AF.Exp), reads=[e16], writes=[e16])
                    p.op('dve', lambda e: e.tensor_reduce(out=z8[0:T, 0:8], in_=e16[0:T, :, :], op=ALU.add, axis=AX.X), reads=[e16], writes=[z8])
                    p.op('dve', lambda e: e.reciprocal(out=z8[0:T, 8:16], in_=z8[0:T, 0:8]), reads=[z8], writes=[z8])
                    p.op('dve', lambda e: e.tensor_tensor(out=SEL[0:T, 2, :].rearrange("q (h k) -> q h k", h=8), in0=e16[0:T, :, :],
                                                          in1=z8[0:T, 8:16].unsqueeze(2).to_broadcast([T, 8, 16]), op=ALU.mult), reads=[e16, z8], writes=[SEL])
                    bk = nb()
                    for w_ in range(3):
                        p.op('pe', lambda e, w_=w_, bk=bk: e.transpose(out=bk[:, w_ * T:(w_ + 1) * T], in_=SEL[0:T, w_, :], identity=identf[0:T, 0:T]),
                             reads=[SEL, identf], writes=[bk])
                    p.op('act', lambda e, bk=bk: e.copy(out=selT[:, :, s * 128:s * 128 + T], in_=bk[:, 0:3 * T].rearrange("q (w t) -> q w t", w=3)),
                         reads=[bk], writes=[selT])
                    p.op('pool', lambda e: e.tensor_copy(out=selTb[:, :, s * 128:s * 128 + T], in_=selT[:, :, s * 128:s * 128 + T]), reads=[selT], writes=[selTb])

                def peer_group(row0, Tg, y_out, yrow0):
                    nsub = (Tg + 127) // 128
                    Ts = min(Tg, 128)
                    for s in range(nsub):
                        p.dma('sp', lambda e, s=s: e.dma_start(out=xg[0:Ts, s, :], in_=x2s[row0 + s * 128:row0 + s * 128 + Ts, :]), reads=['x2s'], writes=[xg])
                        peer_select(Ts, s)
                    for t0 in range(0, Tg, 8):
                        o1, o0, og = ohb1[(t0 // 8) % 2], ohb0[(t0 // 8) % 2], ohbg[(t0 // 8) % 2]
                        p.op('dve', lambda e, t0=t0, o1=o1: e.tensor_tensor(out=o1[:, :, :], in0=iota_b[:, :].unsqueeze(1).to_broadcast([128, 8, 128]),
                                                                          in1=selTb[:, 1, t0:t0 + 8].unsqueeze(2).to_broadcast([128, 8, 128]), op=ALU.is_equal),
                             reads=[iota_b, selTb], writes=[o1])
                        p.op('dve', lambda e, t0=t0, o0=o0: e.tensor_tensor(out=o0[:, :, :], in0=iota_b[:, :].unsqueeze(1).to_broadcast([128, 8, 128]),
                                                                          in1=selTb[:, 0, t0:t0 + 8].unsqueeze(2).to_broadcast([128, 8, 128]), op=ALU.is_equal),
                             reads=[iota_b, selTb], writes=[o0])
                        p.op('pool', lambda e, t0=t0, o0=o0, og=og: e.tensor_tensor(out=og[:, :, :], in0=o0[:, :, :],
                                                                                  in1=selTb[:, 2, t0:t0 + 8].unsqueeze(2).to_broadcast([128, 8, 128]), op=ALU.mult),
                             reads=[o0, selTb], writes=[og])
                        for q4 in range(2):
                            bk = nb()
                            for tt in range(4):
                                mm(bk[:, tt * 128:(tt + 1) * 128], o1[:, q4 * 4 + tt, :], og[:, q4 * 4 + tt, :], True, True, [o1, og], [bk])
                            p.op('act', lambda e, bk=bk, ta=t0 + q4 * 4: e.copy(out=Gt[:, ta:ta + 4, :], in_=bk[:, :].rearrange("q (t i) -> q t i", t=4)), reads=[bk], writes=[Gt])
                    acc = pb[4:8]
                    n_ec = cfg.get('n_ec', 128)

                    def fetch(ec):
                        p.dma('sp', lambda e, ec=ec: e.dma_start(out=UTc[ec % NBUF][:, :, :].rearrange("q c e -> q (c e)"), in_=UTs[ec, :, :]),
                              reads=[('UTs', ec)], writes=[UTc[ec % NBUF]])
                        p.dma('sp', lambda e, ec=ec: e.dma_start(out=Vc[ec % NBUF][:, :], in_=Vs[ec, :, :]), reads=[('Vs', ec)], writes=[Vc[ec % NBUF]])
                    for ec in range(min(NBUF, n_ec)):
                        fetch(ec)
                    for ec in range(n_ec + 1):
                        if ec < n_ec:
                            U_ = UTc[ec % NBUF]
                            bk = pb[ec % 2]
                            for c in range(8):
                                mm(bk[:, 0:Tg], U_[:, c, :], XT[:, c, 0:Tg], c == 0, c == 7, [U_, XT], [bk])
                            ga, GA = gab[ec % 3], GAb[ec % 3]
                            p.op('act', lambda e, bk=bk, ga=ga: e.activation(out=ga[:, 0:Tg], in_=bk[:, 0:Tg], func=AF.Gelu_apprx_tanh), reads=[bk], writes=[ga])
                            p.op('dve', lambda e, ga=ga, GA=GA, ec=ec: e.tensor_tensor(out=GA[:, 0:Tg], in0=ga[:, 0:Tg], in1=Gt[:, 0:Tg, ec], op=ALU.mult),
                                 reads=[ga, Gt], writes=[GA])
                        if ec >= 1:
                            pe_ = ec - 1
                            V_, GA = Vc[pe_ % NBUF], GAb[pe_ % 3]
                            for s in range(nsub):
                                for half in range(2):
                                    ab = acc[2 * s + half]
                                    mm(ab[0:Ts, :], GA[:, s * 128:s * 128 + Ts], V_[:, half * 512:(half + 1) * 512], pe_ == 0, pe_ == n_ec - 1, [GA, V_], [ab])
                            if pe_ + NBUF < n_ec:
                                fetch(pe_ + NBUF)
                    for s in range(nsub):
                        for half in range(2):
                            ab = acc[2 * s + half]
                            p.op('dve', lambda e, ab=ab, s=s, half=half: e.tensor_tensor(out=pre[0:Ts, half * 512:(half + 1) * 512], in0=ab[0:Ts, :],
                                                                                         in1=xg[0:Ts, s, half * 512:(half + 1) * 512], op=ALU.add),
                                 reads=[ab, xg], writes=[pre])
                        rs = rmsnorm_rstd(pre, Ts, ssd, sq4)
                        p.op('dve', lambda e, rs=rs: e.scalar_tensor_tensor(out=yo[0:Ts, :], in0=pre[0:Ts, :], scalar=rs, in1=gfin[0:Ts, :], op0=ALU.mult, op1=ALU.mult),
                             reads=[pre, ssd['ss'], gfin], writes=[yo])
                        p.dma('sp', lambda e, s=s: e.dma_start(out=y_out[yrow0 + s * 128:yrow0 + s * 128 + Ts, :], in_=yo[0:Ts, :]), reads=[yo], writes=['y_out'])

                for gi in range(cfg.get('n_groups', 8)):
                    peer_group(gi * 256, 256, y_p, gi * 256)
                if cfg.get('peer_sample', True):
                    peer_group(SEQ, NS, y_s, 0)

        p.finish()
        p.emit()
    return nc


_NC_CACHE = {}


def make_in_maps(inp, cfg, ncores=NCORES):
    c = host_consts()
    f = lambda a: np.ascontiguousarray(a, dtype=np.float32)
    maps = []
    for i in range(ncores):
        m = {
            'xp': f(inp['x_prompt'][i]),
            'xs': f(inp['x_sample'][DEC_B * i:DEC_B * (i + 1)].reshape(NS, D)),
            'memp': f(inp['mem_prompt'][i]),
            'w_in': f(inp['w_in'][0]), 'w_out': f(inp['w_out'][0]),
            'g_mix': f(inp['norm_mix_g'][0]), 'g_mem': f(inp['norm_mem_g'][0]), 'g_memn': f(inp['mem_norm_g'][0]),
            'g_ffn': f(inp['norm_ffn_g'][0]), 'g_fin': f(inp['final_norm_g']),
            'ln_g': f(inp['gm_ln_g'][0]), 'ln_b': f(inp['gm_ln_b'][0]),
            'gm_ws': f(inp['gm_ws'][0]), 'gm_bs': f(inp['gm_bs'][0]),
            'mem_wq': f(inp['mem_wq'][0]), 'mem_wkv': f(inp['mem_wkv'][0]), 'mem_wo': f(inp['mem_wo'][0]),
            'c_ident': c['ident'], 'c_trilT': c['trilT'], 'c_negmask': c['negmask'], 'c_blk64': c['blk64'], 'c_ones': c['ones'], 'c_iota': c['iota'],
            'peer_wq': f(inp['peer_wq'][0]), 'peer_keys': f(inp['peer_keys'][0].reshape(16, 128, 64)),
            'peer_u': f(inp['peer_u'][0]), 'peer_v': f(inp['peer_v'][0]),
            'cache_kidx': f(inp['cache_kidx'][0]).reshape(-1, 8192), 'cache_k': f(inp['cache_k'][0]).reshape(-1, 8192),
            'cache_v': f(inp['cache_v'][0]).reshape(-1, 8192),
            'page_table': np.ascontiguousarray(inp['page_table'][DEC_B * i:DEC_B * (i + 1)], dtype=np.int32),
            'cache_mem_k': f(inp['cache_mem_k'][0][DEC_B * i:DEC_B * (i + 1)]).reshape(DEC_B, 256, 512),
            'cache_mem_v': f(inp['cache_mem_v'][0][DEC_B * i:DEC_B * (i + 1)]).reshape(DEC_B, 256, 512),
            'c_pow2': c['pow2'], 'c_negm4': c['negm4'],
        }
        maps.append(m)
    return maps


def assemble(results, ncores=NCORES):
    cat = lambda k: np.stack([np.asarray(r[k], dtype=np.float32) for r in results])
    y_prompt = cat('y_p')
    y_sample = cat('y_s').reshape(ncores * DEC_B, DEC_T, D)
    k_prompt = cat('k_p').reshape(1, ncores, SEQ, 2, 64)
    v_prompt = cat('v_p').reshape(1, ncores, SEQ, 2, 64)
    kidx_prompt = cat('ki_p').reshape(1, ncores, SEQ, 64)
    gmv_prompt = cat('gmv_p').reshape(1, ncores, 128, 512)
    memk = cat('memk_p').reshape(1, ncores, 256, 4, 128)
    memv = cat('memv_p').reshape(1, ncores, 256, 4, 128)
    k_sample = cat('k_s').reshape(1, ncores * DEC_B, DEC_T, 2, 64)
    v_sample = cat('v_s').reshape(1, ncores * DEC_B, DEC_T, 2, 64)
    kidx_sample = cat('ki_s').reshape(1, ncores * DEC_B, DEC_T, 64)
    gmv_sample = cat('gmv_s').reshape(1, ncores * DEC_B, DEC_T, 512)
    return (y_prompt, y_sample, k_prompt, v_prompt, kidx_prompt, gmv_prompt, memk, memv,
            k_sample, v_sample, kidx_sample, gmv_sample)


def kernel(**inputs):
    cfg = {}
    nc = build(cfg)
    maps = make_in_maps(inputs, cfg)
    res = run_bass_kernel_spmd(nc, maps, core_ids=list(range(NCORES)))
    return assemble(res.results)
```

```python
import numpy as np
from contextlib import ExitStack
import concourse.bass as bass
import concourse.mybir as mybir
from concourse.bass_utils import run_bass_kernel_spmd

F32 = mybir.dt.float32
BF16 = mybir.dt.bfloat16
I32 = mybir.dt.int32
U32 = mybir.dt.uint32
AF = mybir.ActivationFunctionType
ALU = mybir.AluOpType
AX = mybir.AxisListType

NCORES = 8
D = 1024
SEQ = 2048
NT = SEQ // 128
P_IN = 2376
EPS = 1e-6
NEG = -1.0e30
DEC_B = 16
DEC_T = 4
NS = DEC_B * DEC_T
NPAGES = 64
NEXP = 16384


class Prog:
    ENG = ('sp', 'act', 'dve', 'pool', 'pe')

    def __init__(self, nc, stack, n_dma_sems=12):
        self.nc = nc
        self.stack = stack
        self.ops = {k: [] for k in self.ENG}
        self.cnt = {k: 0 for k in self.ENG}
        self.waited = {k: {} for k in self.ENG}
        self.res = {}
        self.sems = {}
        for k in ('act', 'dve', 'pool', 'pe'):
            self.sems[k] = stack.enter_context(nc.semaphore('prog_' + k))
        self.dma_sems = {}
        self.dma_rr = {}
        self.dma_uses = {}
        for q in ('sp', 'pool', 'act'):
            lst = []
            for i in range(n_dma_sems):
                key = 'dma_%s_%d' % (q, i)
                self.sems[key] = stack.enter_context(nc.semaphore(key))
                self.dma_uses[key] = 0
                lst.append(key)
            self.dma_sems[q] = lst
            self.dma_rr[q] = 0
        self.psum_names = set()

    def sb(self, name, shape, dtype, stack=None):
        return (stack or self.stack).enter_context(self.nc.sbuf_tensor(name, list(shape), dtype))

    def ps(self, name, shape, dtype):
        self.psum_names.add(name)
        return self.stack.enter_context(self.nc.psum_tensor(name, list(shape), dtype))

    @staticmethod
    def _key(x):
        if isinstance(x, (str, tuple)):
            return x
        t = getattr(x, 'tensor', x)
        n = getattr(t, 'name', None)
        if n is None:
            raise ValueError('cannot derive resource key from %r' % (x,))
        return n

    def _deps(self, reads, writes):
        deps = []
        for r in reads:
            st = self.res.get(self._key(r))
            if st and st['w']:
                deps.append(st['w'])
        for w in writes:
            st = self.res.get(self._key(w))
            if st:
                if st['w']:
                    deps.append(st['w'])
                deps.extend(st['r'])
        return deps

    def _commit(self, reads, writes, tok):
        for r in reads:
            st = self.res.setdefault(self._key(r), {'w': None, 'r': []})
            st['r'].append(tok)
        for w in writes:
            self.res[self._key(w)] = {'w': tok, 'r': []}

    def _filter_waits(self, eng, deps, skip_self=False):
        out = {}
        for (sk, val) in deps:
            if skip_self and sk == eng:
                continue
            if self.waited[eng].get(sk, 0) >= val:
                continue
            if out.get(sk, 0) < val:
                out[sk] = val
        for sk, val in out.items():
            self.waited[eng][sk] = val
        return list(out.items())

    def op(self, eng, fn, reads=(), writes=()):
        pr = [r for r in reads if self._key(r) in self.psum_names]
        if pr:
            reads = [r for r in reads if self._key(r) not in self.psum_names]
            writes = list(writes) + pr
        deps = self._deps(reads, writes)
        waits = self._filter_waits(eng, deps, skip_self=(eng == 'pe'))
        self.cnt[eng] += 1
        tok = (eng, self.cnt[eng])
        self._commit(reads, writes, tok)
        self.ops[eng].append((waits, fn, (eng, 1)))
        return tok

    def dma(self, q, fn, reads=(), writes=()):
        deps = self._deps(reads, writes)
        lst = self.dma_sems[q]
        sk = lst[self.dma_rr[q] % len(lst)]
        self.dma_rr[q] += 1
        if self.dma_uses[sk] > 0:
            deps.append((sk, 16 * self.dma_uses[sk]))
        waits = self._filter_waits(q, deps)
        self.dma_uses[sk] += 1
        tok = (sk, 16 * self.dma_uses[sk])
        self._commit(reads, writes, tok)
        self.ops[q].append((waits, fn, (sk, 16)))
        return tok

    def barrier(self):
        deps = [(k, self.cnt[k]) for k in ('act', 'dve', 'pool', 'pe') if self.cnt[k] > 0]
        deps += [(sk, 16 * n) for sk, n in self.dma_uses.items() if n > 0]
        for eng in self.ENG:
            waits = self._filter_waits(eng, [d for d in deps if d[0] != eng])
            if waits:
                self.ops[eng].append((waits, None, None))
        self.res = {}

    def finish(self):
        deps = [(sk, 16 * n) for sk, n in self.dma_uses.items() if n > 0]
        waits = self._filter_waits('sp', deps)
        self.ops['sp'].append((waits, None, None))

    def emit(self):
        nc = self.nc
        allsems = list(self.sems.values())
        with nc.Block() as b0:
            def clr(e):
                for s in allsems:
                    e.sem_clear(s)
            b0.sync(clr)
        with nc.Block() as block:
            for name, meth in (('sp', block.sync), ('act', block.scalar), ('dve', block.vector),
                               ('pool', block.gpsimd), ('pe', block.tensor)):
                ops = self.ops[name]
                if not ops:
                    continue

                def body(e, ops=ops):
                    for waits, fn, inc in ops:
                        for (sk, val) in waits:
                            e.wait_ge(self.sems[sk], val)
                        if fn is None:
                            continue
                        ins = fn(e)
                        if inc is not None:
                            ins.then_inc(self.sems[inc[0]], inc[1])
                meth(body)


def host_consts():
    c = {}
    c['ident'] = np.eye(128, dtype=np.float32)
    s = np.arange(128)
    c['trilT'] = (s[:, None] <= s[None, :]).astype(np.float32)
    c['negmask'] = np.where(s[None, :] <= s[:, None], 0.0, NEG).astype(np.float32)
    bt = np.arange(64)
    c['blk64'] = ((bt[:, None] // 4 == bt[None, :] // 4) & (bt[:, None] % 4 <= bt[None, :] % 4)).astype(np.float32)
    c['ones'] = np.ones((128, 128), dtype=np.float32)
    c['iota'] = np.tile(np.arange(128, dtype=np.float32)[None, :], (128, 1))
    c['pow2'] = np.tile((2.0 ** -(np.arange(48, dtype=np.float64) + 1)).astype(np.float32)[None, :], (128, 1))
    t4 = np.arange(4)
    c['negm4'] = np.where(t4[:, None] <= t4[None, :], 0.0, NEG).astype(np.float32)
    return c


def build(cfg):
    n_pool = cfg.get('n_pool', 10240)
    do = cfg.get('phases', ('P1', 'P2', 'P3', 'P4'))
    dbg = cfg.get('dbg', False)
    nc = bass.Bass("TRN2", target_bir_lowering=False)

    def din(name, shape, dt=F32):
        return nc.dram_tensor(name, list(shape), dt, kind="ExternalInput").ap()

    def dout(name, shape, dt=F32):
        return nc.dram_tensor(name, list(shape), dt, kind="ExternalOutput").ap()

    xp_d = din('xp', [SEQ, D])
    xs_d = din('xs', [NS, D])
    memp_d = din('memp', [256, D])
    w_in_d = din('w_in', [D, P_IN])
    w_out_d = din('w_out', [D, D])
    g_mix_d = din('g_mix', [D]); g_mem_d = din('g_mem', [D]); g_memn_d = din('g_memn', [D])
    g_ffn_d = din('g_ffn', [D]); g_fin_d = din('g_fin', [D])
    ln_g_d = din('ln_g', [512]); ln_b_d = din('ln_b', [512])
    gm_ws_d = din('gm_ws', [4, 128, 128]); gm_bs_d = din('gm_bs', [4, 128])
    wq_d = din('mem_wq', [D, 512]); wkv_d = din('mem_wkv', [D, D]); wo_d = din('mem_wo', [512, D])
    c_ident = din('c_ident', [128, 128]); c_trilT = din('c_trilT', [128, 128]); c_negmask = din('c_negmask', [128, 128])
    c_blk64 = din('c_blk64', [64, 64]); c_ones = din('c_ones', [128, 128]); c_iota = din('c_iota', [128, 128])
    pwq_d = din('peer_wq', [D, D]); pkeys_d = din('peer_keys', [16, 128, 64])
    pu_d = din('peer_u', [NEXP, D]); pv_d = din('peer_v', [NEXP, D])
    ckidx_d = din('cache_kidx', [n_pool, 8192]); ck_d = din('cache_k', [2 * n_pool, 8192]); cv_d = din('cache_v', [2 * n_pool, 8192])
    pt_d = din('page_table', [DEC_B, NPAGES], I32)
    cmk_d = din('cache_mem_k', [DEC_B, 256, 512]); cmv_d = din('cache_mem_v', [DEC_B, 256, 512])
    c_pow2 = din('c_pow2', [128, 48]); c_negm4 = din('c_negm4', [4, 4])

    y_p = dout('y_p', [SEQ, D]); y_s = dout('y_s', [NS, D])
    k_p = dout('k_p', [SEQ, 128]); v_p = dout('v_p', [SEQ, 128]); ki_p = dout('ki_p', [SEQ, 64])
    gmv_p = dout('gmv_p', [128, 512])
    memk_p = dout('memk_p', [256, 512]); memv_p = dout('memv_p', [256, 512])
    k_s = dout('k_s', [NS, 128]); v_s = dout('v_s', [NS, 128]); ki_s = dout('ki_s', [NS, 64]); gmv_s = dout('gmv_s', [NS, 512])
    if dbg:
        x2_dbg = dout('x2_dbg', [SEQ + NS, D])
    x2s = nc.dram_tensor('x2s', [SEQ + NS, D], F32, kind="Internal").ap()
    UTs = nc.dram_tensor('UTs', [128, 128, D], BF16, kind="Internal").ap()
    Vs = nc.dram_tensor('Vs', [128, 128, D], BF16, kind="Internal").ap()

    with ExitStack() as st:
        p = Prog(nc, st)
        pb = [p.ps('pb%d' % i, [128, 512], F32) for i in range(8)]
        rr = [0]
        nbmod = [6]

        def nb():
            b = pb[rr[0] % nbmod[0]]
            rr[0] += 1
            return b

        def mm(out, lhsT, rhs, start, stop, R, W):
            p.op('pe', lambda e: e.matmul(out, lhsT=lhsT, rhs=rhs, start=start, stop=stop), reads=R, writes=W)

        identf = p.sb('identf', [128, 128], F32)
        ident = p.sb('ident', [128, 128], BF16)
        trilT = p.sb('trilT', [128, 128], F32)
        negmask = p.sb('negmask', [128, 128], F32)
        ones_bf = p.sb('ones_bf', [128, 128], BF16)
        onesf = p.sb('onesf', [128, 128], F32)
        p.dma('sp', lambda e: e.dma_start(out=identf[:], in_=c_ident[:, :]), writes=[identf])
        p.dma('sp', lambda e: e.dma_start(out=trilT[:], in_=c_trilT[:, :]), writes=[trilT])
        p.dma('sp', lambda e: e.dma_start(out=negmask[:], in_=c_negmask[:, :]), writes=[negmask])
        p.dma('sp', lambda e: e.dma_start(out=onesf[:], in_=c_ones[:, :]), writes=[onesf])
        p.op('dve', lambda e: e.tensor_copy(out=ident[:], in_=identf[:]), reads=[identf], writes=[ident])
        p.op('dve', lambda e: e.tensor_copy(out=ones_bf[:], in_=onesf[:]), reads=[onesf], writes=[ones_bf])

        def tr(out, in_, K, R, W):
            p.op('pe', lambda e: e.transpose(out=out, in_=in_, identity=ident[0:K, 0:K]), reads=list(R) + [ident], writes=W)

        gcols = {}

        def load_gcol(name, g_d):
            t = p.sb('gc_' + name, [128, 8], F32)
            p.dma('sp', lambda e: e.dma_start(out=t[:], in_=g_d.rearrange("(c q) -> q c", q=128), allow_slow_non_contiguous=True), writes=[t])
            gcols[name] = t
            return t

        def load_weight(dst, w_d, nk, ncol, gcol, stage, eng_rot=[0]):
            for c in range(nk):
                stg = stage[c % len(stage)]
                p.dma('sp', lambda e, c=c, stg=stg: e.dma_start(out=stg[:, 0:ncol], in_=w_d[c * 128:(c + 1) * 128, :]), writes=[stg])
                eng = ('dve', 'pool')[eng_rot[0] % 2]
                eng_rot[0] += 1
                if gcol is not None:
                    p.op(eng, lambda e, c=c, stg=stg: e.tensor_scalar(out=dst[:, c, :], in0=stg[:, 0:ncol], scalar1=gcol[:, c:c + 1],
                                                                     scalar2=None, op0=ALU.mult), reads=[stg, gcol], writes=[dst])
                else:
                    p.op(eng, lambda e, c=c, stg=stg: e.tensor_copy(out=dst[:, c, :], in_=stg[:, 0:ncol]), reads=[stg], writes=[dst])

        def rmsnorm_rstd(x_t, T, rstd, scratch):
            ss = rstd['ss']
            p.op('act', lambda e: e.activation(out=scratch[0:T, :], in_=x_t[0:T, :], func=AF.Square, accum_out=ss[0:T, 0:1]),
                 reads=[x_t], writes=[scratch, ss])
            p.op('dve', lambda e: e.tensor_scalar(out=ss[0:T, 1:2], in0=ss[0:T, 0:1], scalar1=1.0 / D, scalar2=EPS, op0=ALU.mult, op1=ALU.add),
                 reads=[ss], writes=[ss])
            p.op('act', lambda e: e.activation(out=ss[0:T, 2:3], in_=ss[0:T, 1:2], func=AF.Sqrt), reads=[ss], writes=[ss])
            p.op('dve', lambda e: e.reciprocal(out=ss[0:T, 3:4], in_=ss[0:T, 2:3]), reads=[ss], writes=[ss])
            return ss[0:T, 3:4]

        def norm_T(x_t, T, tag, xn, xnT, ssd, scratch, gcol=None):
            rs = rmsnorm_rstd(x_t, T, ssd, scratch)
            p.op('act', lambda e: e.activation(out=xn[0:T, :], in_=x_t[0:T, :], func=AF.Copy, scale=rs), reads=[x_t, ssd['ss']], writes=[xn])
            bk = nb()
            bv = bk[:].bitcast(BF16)
            for c in range(8):
                tr(bv[:, c * T:(c + 1) * T], xn[0:T, c * 128:(c + 1) * 128], T, [xn], [bk])
            if gcol is None:
                p.op('dve', lambda e: e.tensor_copy(out=xnT[:, :, 0:T], in_=bv[:, 0:8 * T].rearrange("q (c t) -> q c t", c=8)),
                     reads=[bk], writes=[xnT])
            else:
                for c in range(8):
                    p.op('dve', lambda e, c=c: e.tensor_scalar(out=xnT[:, c, 0:T], in0=bv[:, c * T:(c + 1) * T], scalar1=gcol[:, c:c + 1], scalar2=None, op0=ALU.mult),
                         reads=[bk, gcol], writes=[xnT])

        ssd = {'ss': p.sb('ss', [128, 4], F32)}

        for nm, gd in (('mix', g_mix_d), ('mem', g_mem_d), ('memn', g_memn_d), ('ffn', g_ffn_d)):
            load_gcol(nm, gd)
        conv_done = [0]

        def convert_chunk(ec, u_, t_, v_):
            p.dma('pool', lambda e: e.dma_start(out=u_[:, :], in_=pu_d[ec * 128:(ec + 1) * 128, :]), writes=[u_])
            p.dma('pool', lambda e: e.dma_start(out=v_[:, :], in_=pv_d[ec * 128:(ec + 1) * 128, :]), writes=[v_])
            bk = nb()
            bv = bk[:].bitcast(BF16)
            for c in range(8):
                tr(bv[:, c * 128:(c + 1) * 128], u_[:, c * 128:(c + 1) * 128], 128, [u_], [bk])
            if ec % 2 == 0:
                p.op('act', lambda e: e.copy(out=t_[:, :], in_=bv[:, :]), reads=[bk], writes=[t_])
            else:
                p.op('pool', lambda e: e.tensor_copy(out=t_[:, :], in_=bv[:, :]), reads=[bk], writes=[t_]) if False else \
                    p.op('act', lambda e: e.copy(out=t_[:, :], in_=bv[:, :]), reads=[bk], writes=[t_])
            p.dma('sp', lambda e: e.dma_start(out=UTs[ec, :, :], in_=t_[:, :]), reads=[t_], writes=[('UTs', ec)])
            p.dma('sp', lambda e: e.dma_start(out=Vs[ec, :, :], in_=v_[:, :]), reads=[v_], writes=[('Vs', ec)])

        with ExitStack() as s2:
            w_out = p.sb('w_out_sb', [128, 8, D], BF16, s2)
            wq = p.sb('wq_sb', [128, 8, 512], BF16, s2)
            wo = p.sb('wo_sb', [128, 4, D], BF16, s2)
            sq = p.sb('sq_scratch', [128, D], BF16, s2)
            xn = p.sb('xn', [128, D], BF16, s2)
            xnT = p.sb('xnT', [128, 8, 128], BF16, s2)
            W4T = p.sb('W4T', [64, 4, 64], BF16, s2)
            bsT4 = p.sb('bsT4', [64, 4], F32, s2)
            tau_c = p.sb('tau_c', [128, 1], F32, s2)
            p.op('pool', lambda e: e.memset(tau_c[:], -1.0e29), writes=[tau_c])

            bnst = p.sb('bnst', [128, 8], F32, s2)
            wsc = p.sb('wsc', [128, 8], F32, s2)
            ycat = p.sb('ycat', [128, D], BF16, s2)
            yT = p.sb('yT', [128, 8, 128], BF16, s2)
            x1 = p.sb('x1', [128, D], F32, s2)
            rden = p.sb('rden', [128, 8], F32, s2)
            qmT = p.sb('qmT', [128, 4, 128], BF16, s2)
            PmT = p.sb('PmT', [128, 2, 4, 128], BF16, s2)
            om = p.sb('om', [128, 512], BF16, s2)
            omT = p.sb('omT', [128, 4, 128], BF16, s2)
            x0s = p.sb('x0s', [64, D], F32, s2)
            ycats = p.sb('ycats', [64, D], BF16, s2)
            kvf_s = p.sb('kvf_s', [64, 328], F32, s2)
            kb_s = p.sb('kb_s', [64, 192], BF16, s2)
            wsc_s = p.sb('wsc_s', [64, 8], F32, s2)
            qTs = p.sb('qTs', [64, 8, 64], BF16, s2)
            qiTs = p.sb('qiTs', [64, 8, 64], BF16, s2)
            sw = ExitStack()
            x0 = p.sb('x0', [128, D], F32, sw)
            mkT = p.sb('mkT', [128, 4, 256], BF16, sw)
            mv_aug = p.sb('mv_aug', [128, 2, 4, 129], BF16, sw)
            WgT = p.sb('WgT', [128, 4, 128], BF16, sw)
            bsT = p.sb('bsT', [128, 4], F32, sw)
            lng = p.sb('lng', [128, 512], F32, sw)
            lnb = p.sb('lnb', [128, 512], F32, sw)
            kvf = p.sb('kvf', [128, 328], F32, sw)
            kb = p.sb('kb', [128, 192], BF16, sw)
            qT = p.sb('qT', [64, 8, 128], BF16, sw)
            qiT = p.sb('qiT', [64, 8, 128], BF16, sw)
            p.dma('sp', lambda e: e.dma_start(out=lng[:], in_=ln_g_d.partition_broadcast(128)), writes=[lng])
            p.dma('sp', lambda e: e.dma_start(out=lnb[:], in_=ln_b_d.partition_broadcast(128)), writes=[lnb])
            ub = p.sb('ub', [128, 512], F32, sw)
            gv = p.sb('gv', [128, 512], F32, sw)
            vn = p.sb('vn', [128, 512], F32, sw)
            vnb = p.sb('vnb', [128, 512], BF16, sw)
            zq = p.sb('zq', [128, 1024], BF16, sw)
            w_in = p.sb('w_in_sb', [128, 8, P_IN], BF16, sw)

            with ExitStack() as s1:
                stage = [p.sb('wstage%d' % i, [128, P_IN], F32, s1) for i in range(2)]
                load_weight(w_in, w_in_d, 8, P_IN, gcols['mix'], stage)
                load_weight(w_out, w_out_d, 8, D, None, stage)
                load_weight(wq, wq_d, 8, 512, gcols['mem'], stage)
                load_weight(wo, wo_d, 4, D, None, stage)
                wsb = p.sb('wsb', [128, 128], BF16, s1)
                for g in range(4):
                    stg = stage[g % 2]
                    p.dma('sp', lambda e, g=g, stg=stg: e.dma_start(out=stg[:, 0:128], in_=gm_ws_d[g, :, :]), writes=[stg])
                    p.op('dve', lambda e, stg=stg: e.tensor_copy(out=wsb[:], in_=stg[:, 0:128]), reads=[stg], writes=[wsb])
                    bk = nb()
                    bv = bk[:].bitcast(BF16)
                    tr(bv[:, 0:128], wsb[:, :], 128, [wsb], [bk])
                    p.op('dve', lambda e, g=g, bv=bv: e.tensor_tensor(out=WgT[:, g, :], in0=bv[:, 0:128], in1=trilT[:, :], op=ALU.mult),
                         reads=[bk, trilT], writes=[WgT])
                p.dma('sp', lambda e: e.dma_start(out=bsT[:], in_=gm_bs_d.rearrange("g t -> t g"), allow_slow_non_contiguous=True), writes=[bsT])
                w4f = p.sb('w4f', [64, 4, 64], F32, s1)
                blk64 = p.sb('blk64', [64, 64], F32, s1)
                p.dma('sp', lambda e: e.dma_start(out=blk64[:], in_=c_blk64[:, :]), writes=[blk64])
                p.op('pool', lambda e: e.memset(w4f[:], 0.0), writes=[w4f])
                for g in range(4):
                    for b in range(DEC_B):
                        p.dma('sp', lambda e, g=g, b=b: e.dma_start(
                            out=w4f[4 * b:4 * b + 4, g, 4 * b:4 * b + 4], in_=gm_ws_d[g, 0:4, 0:4].rearrange("t s -> s t"), allow_slow_non_contiguous=True),
                            writes=[w4f])
                    p.op('dve', lambda e, g=g: e.tensor_tensor(out=W4T[:, g, :], in0=w4f[:, g, :], in1=blk64[:, :], op=ALU.mult), reads=[w4f, blk64], writes=[W4T])
                for b in range(DEC_B):
                    p.dma('sp', lambda e, b=b: e.dma_start(out=bsT4[4 * b:4 * b + 4, :], in_=gm_bs_d[:, 0:4].rearrange("g t -> t g"), allow_slow_non_contiguous=True), writes=[bsT4])

                if 'P1' in do:
                    wkv = p.sb('wkv_sb', [128, 8, D], BF16, s1)
                    load_weight(wkv, wkv_d, 8, D, gcols['memn'], stage)
                    mkvf = p.sb('mkvf', [128, D], F32, s1)
                    mkb = p.sb('mkb', [128, 512], BF16, s1)
                    p.op('pool', lambda e: e.memset(mv_aug[:], 1.0), writes=[mv_aug])
                    for mt in range(2):
                        p.dma('sp', lambda e, mt=mt: e.dma_start(out=x0[:], in_=memp_d[mt * 128:(mt + 1) * 128, :]), writes=[x0])
                        norm_T(x0, 128, 'm', xn, xnT, ssd, sq)
                        b0, b1 = nb(), nb()
                        for half, bk in enumerate((b0, b1)):
                            for c in range(8):
                                mm(bk[:, :], xnT[:, c, :], wkv[:, c, half * 512:(half + 1) * 512], c == 0, c == 7, [xnT, wkv], [bk])
                        p.op('act', lambda e, b0=b0: e.copy(out=mkvf[:, 0:512], in_=b0[:, :]), reads=[b0], writes=[mkvf])
                        p.op('dve', lambda e, b1=b1: e.tensor_copy(out=mkvf[:, 512:1024], in_=b1[:, :]), reads=[b1], writes=[mkvf])
                        p.dma('sp', lambda e, mt=mt: e.dma_start(out=memk_p[mt * 128:(mt + 1) * 128, :], in_=mkvf[:, 0:512]), reads=[mkvf], writes=['memk_p'])
                        p.dma('sp', lambda e, mt=mt: e.dma_start(out=memv_p[mt * 128:(mt + 1) * 128, :], in_=mkvf[:, 512:1024]), reads=[mkvf], writes=['memv_p'])
                        p.op('dve', lambda e: e.tensor_copy(out=mkb[:], in_=mkvf[:, 0:512]), reads=[mkvf], writes=[mkb])
                        p.op('pool', lambda e, mt=mt: e.tensor_copy(out=mv_aug[:, mt, :, 0:128], in_=mkvf[:, 512:1024].rearrange("q (h d) -> q h d", h=4)),
                             reads=[mkvf], writes=[mv_aug])
                        bk = nb()
                        bv = bk[:].bitcast(BF16)
                        for h in range(4):
                            tr(bv[:, h * 128:(h + 1) * 128], mkb[:, h * 128:(h + 1) * 128], 128, [mkb], [bk])
                        p.op('act', lambda e, mt=mt, bv=bv: e.copy(out=mkT[:, :, mt * 128:(mt + 1) * 128], in_=bv[:, 0:512].rearrange("q (h m) -> q h m", h=4)),
                             reads=[bk], writes=[mkT])
            p.barrier()

            with ExitStack() as s3:
                kT = [p.sb('kT%d' % g, [64, SEQ], BF16, s3) for g in range(2)]
                kiT = p.sb('kiT', [64, SEQ], BF16, s3)
                v_aug = p.sb('v_aug', [128, NT, 2, 65], BF16, s3)
                sc = p.sb('sc', [128, SEQ], F32, s3)
                bis = p.sb('bis', [128, 8], F32, s3)
                Wtp = p.sb('Wtp', [128, 48], F32, s3)
                midp = p.sb('midp', [128, 1], F32, s3)
                pow2p = p.sb('pow2p', [128, 48], F32, s3)
                p.dma('sp', lambda e: e.dma_start(out=pow2p[:, :], in_=c_pow2[:, :]), writes=[pow2p])
                rbuf = [p.sb('rbuf%d' % i, [128, 512], F32, s3) for i in range(2)]
                m8 = p.sb('m8', [128, 8], F32, s3)
                maskb = p.sb('maskb', [128, SEQ], BF16, s3)
                maskT = p.sb('maskT', [128, NT, 128], BF16, s3)
                Eb = [p.sb('Eb%d' % i, [128, 512], BF16, s3) for i in range(3)]
                PTb = [p.sb('PTb%d' % i, [128, 4, 128], BF16, s3) for i in range(3)]
                p.op('pool', lambda e: e.memset(v_aug[:], 1.0), writes=[v_aug])
                if 'P4' in do and cfg.get('interleave_conv', True):
                    cub = [p.sb('cub%d' % i, [128, D], BF16, s3) for i in range(2)]
                    cut = [p.sb('cut%d' % i, [128, D], BF16, s3) for i in range(2)]
                    cvb = [p.sb('cvb%d' % i, [128, D], BF16, s3) for i in range(2)]

                def proj_tile(src_ap, T, ti, is_sample, x0b, ycatb):
                    p.dma('sp', lambda e: e.dma_start(out=x0b[0:T, :], in_=src_ap), writes=[x0b])
                    norm_T(x0b, T, 'a', xn, xnT, ssd, sq)
                    banks = [nb() for _ in range(5)]
                    for n5, bk in enumerate(banks):
                        c0 = n5 * 512
                        w = min(512, P_IN - c0)
                        for c in range(8):
                            mm(bk[0:T, 0:w], xnT[:, c, 0:T], w_in[:, c, c0:c0 + w], c == 0, c == 7, [xnT, w_in], [bk])
                    p.op('act', lambda e: e.activation(out=ub[0:T, :], in_=banks[0][0:T, :], func=AF.Gelu_apprx_tanh), reads=[banks[0]], writes=[ub])
                    p.op('act', lambda e: e.activation(out=gv[0:T, :], in_=banks[1][0:T, :], func=AF.Gelu_apprx_tanh), reads=[banks[1]], writes=[gv])
                    p.op('dve', lambda e: e.tensor_copy(out=zq[0:T, 0:512], in_=banks[2][0:T, :]), reads=[banks[2]], writes=[zq])
                    p.op('dve', lambda e: e.tensor_copy(out=kvf[0:T, 0:256], in_=banks[3][0:T, 0:256]), reads=[banks[3]], writes=[kvf])
                    p.op('dve', lambda e: e.tensor_copy(out=zq[0:T, 512:768], in_=banks[3][0:T, 256:512]), reads=[banks[3]], writes=[zq])
                    p.op('act', lambda e: e.copy(out=zq[0:T, 768:1024], in_=banks[4][0:T, 0:256]), reads=[banks[4]], writes=[zq])
                    p.op('act', lambda e: e.copy(out=kvf[0:T, 256:328], in_=banks[4][0:T, 256:328]), reads=[banks[4]], writes=[kvf])
                    r0 = ti * 128
                    ko, vo, kio = (k_s, v_s, ki_s) if is_sample else (k_p, v_p, ki_p)
                    p.dma('sp', lambda e: e.dma_start(out=ko[r0:r0 + T, :], in_=kvf[0:T, 0:128]), reads=[kvf], writes=['ko'])
                    p.dma('sp', lambda e: e.dma_start(out=vo[r0:r0 + T, :], in_=kvf[0:T, 128:256]), reads=[kvf], writes=['vo'])
                    p.dma('sp', lambda e: e.dma_start(out=kio[r0:r0 + T, :], in_=kvf[0:T, 256:320]), reads=[kvf], writes=['kio'])
                    p.op('dve', lambda e: e.bn_stats(out=bnst[0:T, 0:6], in_=gv[0:T, :]), reads=[gv], writes=[bnst])
                    p.op('dve', lambda e: e.bn_aggr(out=bnst[0:T, 6:8], in_=bnst[0:T, 0:6]), reads=[bnst], writes=[bnst])
                    p.op('dve', lambda e: e.tensor_scalar(out=bnst[0:T, 0:1], in0=bnst[0:T, 7:8], scalar1=EPS, scalar2=None, op0=ALU.add), reads=[bnst], writes=[bnst])
                    p.op('act', lambda e: e.activation(out=bnst[0:T, 1:2], in_=bnst[0:T, 0:1], func=AF.Sqrt), reads=[bnst], writes=[bnst])
                    p.op('dve', lambda e: e.reciprocal(out=bnst[0:T, 2:3], in_=bnst[0:T, 1:2]), reads=[bnst], writes=[bnst])
                    p.op('dve', lambda e: e.tensor_scalar(out=vn[0:T, :], in0=gv[0:T, :], scalar1=bnst[0:T, 6:7], scalar2=bnst[0:T, 2:3],
                                                          op0=ALU.subtract, op1=ALU.mult), reads=[gv, bnst], writes=[vn])
                    p.op('dve', lambda e: e.tensor_tensor(out=vn[0:T, :], in0=vn[0:T, :], in1=lng[0:T, :], op=ALU.mult), reads=[vn, lng], writes=[vn])
                    p.op('dve', lambda e: e.tensor_tensor(out=vn[0:T, :], in0=vn[0:T, :], in1=lnb[0:T, :], op=ALU.add), reads=[vn, lnb], writes=[vn])
                    if is_sample:
                        p.dma('sp', lambda e: e.dma_start(out=gmv_s[0:T, :], in_=vn[0:T, :]), reads=[vn], writes=['gmv_s'])
                    elif ti == NT - 1:
                        p.dma('sp', lambda e: e.dma_start(out=gmv_p[:, :], in_=vn[0:T, :]), reads=[vn], writes=['gmv_p'])
                    p.op('pool', lambda e: e.tensor_copy(out=vnb[0:T, :], in_=vn[0:T, :]), reads=[vn], writes=[vnb])
                    bk = nb()
                    Wm, bsm = (W4T, bsT4) if is_sample else (WgT, bsT)
                    for g in range(4):
                        mm(bk[0:T, g * 128:(g + 1) * 128], Wm[0:T, g, 0:T], vnb[0:T, g * 128:(g + 1) * 128], True, True, [Wm, vnb], [bk])
                    for g in range(4):
                        p.op('dve', lambda e, g=g, bk=bk: e.scalar_tensor_tensor(out=ycatb[0:T, g * 128:(g + 1) * 128], in0=bk[0:T, g * 128:(g + 1) * 128],
                                                                                  scalar=bsm[0:T, g:g + 1], in1=ub[0:T, g * 128:(g + 1) * 128],
                                                                                  op0=ALU.add, op1=ALU.mult), reads=[bk, bsm, ub], writes=[ycatb])

                def feature_major(T, ti, qTb, qiTb):
                    p.op('pool', lambda e: e.tensor_copy(out=kb[0:T, 0:128], in_=kvf[0:T, 0:128]), reads=[kvf], writes=[kb])
                    p.op('pool', lambda e: e.tensor_copy(out=kb[0:T, 128:192], in_=kvf[0:T, 256:320]), reads=[kvf], writes=[kb])
                    p.op('pool', lambda e: e.tensor_scalar(out=wsc[0:T, :], in0=kvf[0:T, 320:328], scalar1=8.0 ** -0.5, scalar2=None, op0=ALU.mult),
                         reads=[kvf], writes=[wsc])
                    for (src, off, dst) in ((zq, 0, qTb), (zq, 512, qiTb)):
                        bk = nb()
                        bv = bk[:].bitcast(BF16)
                        for h in range(8):
                            tr(bv[0:64, h * T:(h + 1) * T], src[0:T, off + h * 64: off + (h + 1) * 64], T, [src], [bk])
                        p.op('act', lambda e, bv=bv, dst=dst: e.copy(out=dst[:, :, 0:T], in_=bv[0:64, 0:8 * T].rearrange("q (h t) -> q h t", h=8)),
                             reads=[bk], writes=[dst])

                def prompt_dsa(ti):
                    T = 128
                    L = 128 * (ti + 1)
                    c_lo = ti * 128
                    bk = nb()
                    bv = bk[:].bitcast(BF16)
                    for g in range(3):
                        tr(bv[0:64, g * 128:(g + 1) * 128], kb[:, g * 64:(g + 1) * 64], 128, [kb], [bk])
                    p.op('act', lambda e, bv=bv: e.copy(out=kT[0][:, c_lo:c_lo + 128], in_=bv[0:64, 0:128]), reads=[bk], writes=[kT[0]])
                    p.op('act', lambda e, bv=bv: e.copy(out=kT[1][:, c_lo:c_lo + 128], in_=bv[0:64, 128:256]), reads=[bk], writes=[kT[1]])
                    p.op('act', lambda e, bv=bv: e.copy(out=kiT[:, c_lo:c_lo + 128], in_=bv[0:64, 256:384]), reads=[bk], writes=[kiT])
                    p.op('pool', lambda e: e.tensor_copy(out=v_aug[:, ti, :, 0:64], in_=kvf[:, 128:256].rearrange("q (g d) -> q g d", g=2)),
                         reads=[kvf], writes=[v_aug])
                    ri = 0
                    for c0 in range(0, L, 512):
                        w = min(512, L - c0)
                        for h in range(8):
                            bk = nb()
                            mm(bk[:, 0:w], qiT[:, h, :], kiT[:, c0:c0 + w], True, True, [qiT, kiT], [bk])
                            rb = rbuf[ri % 2]
                            ri += 1
                            p.op('act', lambda e, bk=bk, rb=rb, w=w: e.activation(out=rb[:, 0:w], in_=bk[:, 0:w], func=AF.Relu), reads=[bk], writes=[rb])
                            if h == 0:
                                p.op('dve', lambda e, rb=rb, w=w, c0=c0: e.tensor_scalar(out=sc[:, c0:c0 + w], in0=rb[:, 0:w], scalar1=wsc[:, 0:1], scalar2=None, op0=ALU.mult),
                                     reads=[rb, wsc], writes=[sc])
                            else:
                                p.op('dve', lambda e, rb=rb, w=w, c0=c0, h=h: e.scalar_tensor_tensor(out=sc[:, c0:c0 + w], in0=rb[:, 0:w], scalar=wsc[:, h:h + 1],
                                                                                                     in1=sc[:, c0:c0 + w], op0=ALU.mult, op1=ALU.add),
                                     reads=[rb, wsc, sc], writes=[sc])
                    if ti >= 2:
                        p.op('act', lambda e: e.activation(out=maskb[:, 0:L], in_=sc[:, 0:L], func=AF.Square, accum_out=bis[:, 0:1]), reads=[sc], writes=[maskb, bis])
                    p.op('dve', lambda e: e.tensor_tensor(out=sc[:, c_lo:c_lo + 128], in0=sc[:, c_lo:c_lo + 128], in1=negmask[:, :], op=ALU.add),
                         reads=[sc, negmask], writes=[sc])
                    if ti >= 2:
                        NITP = cfg.get('nit_p', 24)
                        p.op('act', lambda e: e.activation(out=bis[:, 1:2], in_=bis[:, 0:1], func=AF.Sqrt), reads=[bis], writes=[bis])
                        p.op('dve', lambda e: e.tensor_scalar(out=bis[:, 2:3], in0=bis[:, 1:2], scalar1=2.2, scalar2=2.0, op0=ALU.mult, op1=ALU.add), reads=[bis], writes=[bis])
                        p.op('dve', lambda e: e.tensor_scalar(out=Wtp[:, :], in0=pow2p[:, :], scalar1=bis[:, 2:3], scalar2=None, op0=ALU.mult), reads=[pow2p, bis], writes=[Wtp])
                        p.op('dve', lambda e: e.memset(midp[:, :], 0.0), writes=[midp])
                        for k in range(NITP):
                            p.op('dve', lambda e: e.tensor_scalar(out=maskb[:, 0:L], in0=sc[:, 0:L], scalar1=midp[:, 0:1], scalar2=None, op0=ALU.is_ge, op1=ALU.add,
                                                                  accum_out=bis[:, 3:4]), reads=[sc, midp], writes=[maskb, bis])
                            p.op('dve', lambda e: e.tensor_scalar(out=bis[:, 4:5], in0=bis[:, 3:4], scalar1=255.5, scalar2=0.5, op0=ALU.is_ge, op1=ALU.subtract), reads=[bis], writes=[bis])
                            p.op('dve', lambda e, k=k: e.scalar_tensor_tensor(out=midp[:, :], in0=bis[:, 4:5], scalar=Wtp[:, k:k + 1], in1=midp[:, :], op0=ALU.mult, op1=ALU.add),
                                 reads=[bis, Wtp, midp], writes=[midp])
                        p.op('dve', lambda e: e.tensor_tensor(out=bis[:, 5:6], in0=midp[:, :], in1=Wtp[:, NITP:NITP + 1], op=ALU.subtract), reads=[midp, Wtp], writes=[bis])
                        tau = bis[:, 5:6]
                        tau_t = bis
                    else:
                        tau = tau_c[:, 0:1]
                        tau_t = tau_c
                    p.op('dve', lambda e: e.tensor_scalar(out=maskb[:, 0:L], in0=sc[:, 0:L], scalar1=tau, scalar2=None, op0=ALU.is_ge),
                         reads=[sc, tau_t], writes=[maskb])
                    for j0 in range(0, ti + 1, 8):
                        nj = min(8, ti + 1 - j0)
                        bk = nb()
                        bv = bk[:].bitcast(BF16)
                        for jj in range(nj):
                            tr(bv[:, jj * 128:(jj + 1) * 128], maskb[:, (j0 + jj) * 128:(j0 + jj + 1) * 128], 128, [maskb], [bk])
                        p.op('act', lambda e, bv=bv, j0=j0, nj=nj: e.copy(out=maskT[:, j0:j0 + nj, :], in_=bv[:, 0:nj * 128].rearrange("q (j t) -> q j t", j=nj)),
                             reads=[bk], writes=[maskT])
                    seq = [(g, j) for g in range(2) for j in range(ti + 1)]
                    PTl = {}

                    def att_scores(idx):
                        g, j = seq[idx]
                        bk = nb()
                        mm(bk[:, :], kT[g][:, j * 128:(j + 1) * 128], qT[:, 4 * g:4 * g + 4, :].rearrange("q h t -> q (h t)"), True, True, [kT[g], qT], [bk])
                        E = Eb[idx % 3]
                        PT = PTb[idx % 3]
                        p.op('act', lambda e: e.activation(out=E[:, :], in_=bk[:, :], func=AF.Exp, scale=0.125), reads=[bk], writes=[E])
                        p.op('pool', lambda e: e.tensor_tensor(out=PT[:, :, :], in0=E[:, :].rearrange("q (h t) -> q h t", h=4),
                                                               in1=maskT[:, j:j + 1, :].to_broadcast([128, 4, 128]), op=ALU.mult),
                             reads=[E, maskT], writes=[PT])
                        PTl[idx] = PT

                    def att_pv(idx):
                        g, j = seq[idx]
                        ob = pb[6 + g]
                        PT = PTl[idx]
                        for hh in range(4):
                            mm(ob[:, hh * 65:(hh + 1) * 65], PT[:, hh, :], v_aug[:, j, g, :], (j == 0 and hh == 0), (j == ti), [PT, v_aug], [ob])
                        if j == ti:
                            p.op('dve', lambda e: e.reciprocal(out=rden[:, 4 * g:4 * g + 4], in_=ob[:, 0:260].rearrange("q (h d) -> q h d", h=4)[:, :, 64]),
                                 reads=[ob], writes=[rden])
                            p.op('dve', lambda e: e.tensor_tensor(out=ycat[:, 512 + 256 * g:512 + 256 * (g + 1)].rearrange("q (h d) -> q h d", h=4),
                                                                  in0=ob[:, 0:260].rearrange("q (h d) -> q h d", h=4)[:, :, 0:64],
                                                                  in1=rden[:, 4 * g:4 * g + 4].unsqueeze(2).to_broadcast([128, 4, 64]), op=ALU.mult),
                                 reads=[ob, rden], writes=[ycat])
                    for idx in range(len(seq) + 1):
                        if idx < len(seq):
                            att_scores(idx)
                        if idx >= 1:
                            att_pv(idx - 1)

                def out_proj_and_mem(T, row0, is_sample, x0b, ycatb):
                    bk = nb()
                    bv = bk[:].bitcast(BF16)
                    for c in range(8):
                        tr(bv[:, c * T:(c + 1) * T], ycatb[0:T, c * 128:(c + 1) * 128], T, [ycatb], [bk])
                    p.op('act', lambda e, bv=bv: e.copy(out=yT[:, :, 0:T], in_=bv[:, 0:8 * T].rearrange("q (c t) -> q c t", c=8)), reads=[bk], writes=[yT])
                    for half in range(2):
                        bk = nb()
                        for c in range(8):
                            mm(bk[0:T, :], yT[:, c, 0:T], w_out[:, c, half * 512:(half + 1) * 512], c == 0, c == 7, [yT, w_out], [bk])
                        p.op('dve', lambda e, bk=bk, half=half: e.tensor_tensor(out=x1[0:T, half * 512:(half + 1) * 512], in0=bk[0:T, :],
                                                                                in1=x0b[0:T, half * 512:(half + 1) * 512], op=ALU.add),
                             reads=[bk, x0b], writes=[x1])
                    norm_T(x1, T, 'b', xn, xnT, ssd, sq)
                    bk = nb()
                    for h in range(4):
                        for c in range(8):
                            mm(bk[:, h * T:(h + 1) * T], wq[:, c, h * 128:(h + 1) * 128], xnT[:, c, 0:T], c == 0, c == 7, [wq, xnT], [bk])
                    p.op('act', lambda e, bk=bk: e.copy(out=qmT[:, :, 0:T], in_=bk[:, 0:4 * T].rearrange("q (h t) -> q h t", h=4)), reads=[bk], writes=[qmT])
                    if not is_sample:
                        for mt in range(2):
                            bk = nb()
                            for h in range(4):
                                mm(bk[:, h * 128:(h + 1) * 128], mkT[:, h, mt * 128:(mt + 1) * 128], qmT[:, h, :], True, True, [mkT, qmT], [bk])
                            p.op('act', lambda e, bk=bk, mt=mt: e.activation(out=PmT[:, mt, :, :], in_=bk[:, :].rearrange("q (h t) -> q h t", h=4),
                                                                             func=AF.Exp, scale=128.0 ** -0.5), reads=[bk], writes=[PmT])
                        for hp in range(2):
                            ob = pb[6 + hp]
                            for mt in range(2):
                                for hh in range(2):
                                    h = 2 * hp + hh
                                    mm(ob[:, hh * 129:(hh + 1) * 129], PmT[:, mt, h, :], mv_aug[:, mt, h, :], (mt == 0 and hh == 0), (mt == 1), [PmT, mv_aug], [ob])
                            p.op('dve', lambda e, ob=ob, hp=hp: e.reciprocal(out=rden[:, 2 * hp:2 * hp + 2], in_=ob[:, 0:258].rearrange("q (h d) -> q h d", h=2)[:, :, 128]),
                                 reads=[ob], writes=[rden])
                            p.op('dve', lambda e, ob=ob, hp=hp: e.tensor_tensor(out=om[:, 256 * hp:256 * (hp + 1)].rearrange("q (h d) -> q h d", h=2),
                                                                                in0=ob[:, 0:258].rearrange("q (h d) -> q h d", h=2)[:, :, 0:128],
                                                                                in1=rden[:, 2 * hp:2 * hp + 2].unsqueeze(2).to_broadcast([128, 2, 128]), op=ALU.mult),
                                 reads=[ob, rden], writes=[om])
                        bk = nb()
                        bv = bk[:].bitcast(BF16)
                        for h in range(4):
                            tr(bv[:, h * T:(h + 1) * T], om[0:T, h * 128:(h + 1) * 128], T, [om], [bk])
                        p.op('act', lambda e, bv=bv: e.copy(out=omT[:, :, 0:T], in_=bv[:, 0:4 * T].rearrange("q (h t) -> q h t", h=4)), reads=[bk], writes=[omT])
                    else:
                        sample_mem_attn()
                    for half in range(2):
                        bk = nb()
                        for h in range(4):
                            mm(bk[0:T, :], omT[:, h, 0:T], wo[:, h, half * 512:(half + 1) * 512], h == 0, h == 3, [omT, wo], [bk])
                        p.op('dve', lambda e, bk=bk, half=half: e.tensor_tensor(out=x0b[0:T, half * 512:(half + 1) * 512], in0=bk[0:T, :],
                                                                                in1=x1[0:T, half * 512:(half + 1) * 512], op=ALU.add),
                             reads=[bk, x1], writes=[x0b])
                    p.dma('sp', lambda e: e.dma_start(out=x2s[row0:row0 + T, :], in_=x0b[0:T, :]), reads=[x0b], writes=['x2s'])
                    if dbg:
                        p.dma('sp', lambda e: e.dma_start(out=x2_dbg[row0:row0 + T, :], in_=x0b[0:T, :]), reads=[x0b], writes=['x2_dbg'])

                def sample_mem_attn():
                    for b in range(DEC_B):
                        mf, mb, mT = sm['mf'], sm['mb'], sm['mT']
                        for which, src_d in ((0, cmk_d), (1, cmv_d)):
                            p.dma('sp', lambda e, b=b, src_d=src_d, which=which: e.dma_start(out=mf[which][:, :, :], in_=src_d[b, :, :].rearrange("(mt m) f -> m mt f", mt=2)),
                                  writes=[mf[which]])
                            p.op('pool' if which else 'dve', lambda e, which=which: e.tensor_copy(out=mb[which][:, :, :], in_=mf[which][:, :, :]), reads=[mf[which]], writes=[mb[which]])
                        bk = nb()
                        bv = bk[:].bitcast(BF16)
                        for mt in range(2):
                            for h in range(4):
                                tr(bv[:, (mt * 4 + h) * 128:(mt * 4 + h + 1) * 128], mb[0][:, mt, h * 128:(h + 1) * 128], 128, [mb[0]], [bk])
                        p.op('act', lambda e, bv=bv: e.copy(out=mT[:, :, :], in_=bv[:, :].rearrange("q (k m) -> q k m", k=8)), reads=[bk], writes=[mT])
                        bk = nb()
                        for mt in range(2):
                            for h in range(4):
                                mm(bk[:, (mt * 4 + h) * 4:(mt * 4 + h + 1) * 4], mT[:, mt * 4 + h, :], qmT[:, h, 4 * b:4 * b + 4], True, True, [mT, qmT], [bk])
                        Pm = sm['Pm']
                        p.op('act', lambda e, bk=bk: e.activation(out=Pm[:, :], in_=bk[:, 0:32], func=AF.Exp, scale=128.0 ** -0.5), reads=[bk], writes=[Pm])
                        o6, o7 = pb[6], pb[7]
                        for h in range(4):
                            for mt in range(2):
                                mm(o6[:, h * 4:(h + 1) * 4], mb[1][:, mt, h * 128:(h + 1) * 128], Pm[:, (mt * 4 + h) * 4:(mt * 4 + h + 1) * 4], mt == 0, mt == 1, [mb[1], Pm], [o6])
                        for h in range(4):
                            for mt in range(2):
                                mm(o7[:, h * 4:(h + 1) * 4], ones_bf[:, :], Pm[:, (mt * 4 + h) * 4:(mt * 4 + h + 1) * 4], mt == 0, mt == 1, [ones_bf, Pm], [o7])
                        rc = sm['rc']
                        p.op('dve', lambda e: e.reciprocal(out=rc[:, 0:16], in_=o7[:, 0:16]), reads=[o7], writes=[rc])
                        p.op('dve', lambda e, b=b: e.tensor_tensor(out=omT[:, :, 4 * b:4 * b + 4], in0=o6[:, 0:16].rearrange("q (h t) -> q h t", h=4),
                                                                 in1=rc[:, 0:16].rearrange("q (h t) -> q h t", h=4), op=ALU.mult), reads=[o6, rc], writes=[omT])

                sm = {}

                def sample_dsa(stk):
                    T = NS
                    NIT = 36
                    gbufs = [p.sb('gbuf%d' % i, [64, 8192], F32, stk) for i in range(cfg.get('n_gbuf', 2))]
                    cbs = [p.sb('cbs%d' % i, [64, 8192], BF16, stk) for i in range(cfg.get('n_cbuf', 1))]
                    KTb = p.sb('KTb', [128, 64, 64], BF16, stk)
                    kiTc_l = [p.sb('kiTc%d' % i, [64, 16, 64], BF16, stk) for i in range(3)]
                    pt_sb = p.sb('pt_sb', [64, 16], I32, stk)
                    idx2 = p.sb('idx2', [64, 16, 2], I32, stk)
                    rS_l = [p.sb('rS%d' % i, [64, 16, 32], F32, stk) for i in range(3)]
                    NCH = cfg.get('nch', 4)
                    scTbs = [p.sb('scTb%d' % i, [64, 128, 4], F32, stk) for i in range(NCH)]
                    scns = [p.sb('scn%d' % i, [4, 4], F32, stk) for i in range(NCH)]
                    rSn_l = [p.sb('rSn%d' % i, [4, 32], F32, stk) for i in range(2)]
                    chunk_ctr = [0]
                    Wbc = p.sb('Wbc', [64, 16, 32], F32, stk)
                    Dg = p.sb('Dg', [64, 16, 8, 4], F32, stk)
                    q2T = p.sb('q2T', [128, 4, 64], BF16, stk)
                    kT2n = p.sb('kT2n', [128, 64], BF16, stk)
                    kiTs = p.sb('kiTs', [64, 64], BF16, stk)
                    vnf = p.sb('vnf', [4, 16, 128], F32, stk)
                    vnew = p.sb('vnew', [4, 16, 128], BF16, stk)
                    negm4 = p.sb('negm4', [4, 4], F32, stk)
                    pow2 = p.sb('pow2', [64, 48], F32, stk)
                    hs_l = [p.sb('hs%d' % i, [64, 4], F32, stk) for i in range(NCH)]
                    hsb = p.sb('hsb', [64, 2], BF16, stk)
                    Wsc_l = [p.sb('Wsc%d' % i, [64, 2], F32, stk) for i in range(NCH)]
                    Wtab_l = [p.sb('Wtab%d' % i, [64, 48], F32, stk) for i in range(NCH)]
                    lo_l = [p.sb('lo%d' % i, [64, 4], F32, stk) for i in range(NCH)]
                    mid_l = [p.sb('mid%d' % i, [64, 4], F32, stk) for i in range(NCH)]
                    ge_l = [p.sb('ge%d' % i, [64, 4], F32, stk) for i in range(NCH)]
                    cmpb_l = [p.sb('cmpb%d' % i, [64, 128, 4], BF16, stk) for i in range(NCH)]
                    cntp_l = [p.sb('cntp%d' % i, [64, 4], F32, stk) for i in range(NCH)]
                    cmpn_l = [p.sb('cmpn%d' % i, [4, 4], F32, stk) for i in range(NCH)]
                    mask_s_l = cmpb_l
                    maskn_l = [p.sb('maskn%d' % i, [4, 4], BF16, stk) for i in range(NCH)]
                    Ess = [p.sb('Es%d' % i, [64, 16, 32], BF16, stk) for i in range(2)]
                    PTss = [p.sb('PTs%d' % i, [64, 16, 32], BF16, stk) for i in range(2)]
                    En = p.sb('En', [4, 32], BF16, stk)
                    PTn = p.sb('PTn', [4, 32], BF16, stk)
                    rcs = p.sb('rcs', [128, 32], F32, stk)
                    ybTs = p.sb('ybTs', [128, 4, 64], BF16, stk)

                    p.dma('sp', lambda e: e.dma_start(out=pt_sb[:, :], in_=pt_d.rearrange("b j -> j b"), allow_slow_non_contiguous=True), writes=[pt_sb])
                    p.dma('sp', lambda e: e.dma_start(out=negm4[:, :], in_=c_negm4[:, :]), writes=[negm4])
                    p.dma('sp', lambda e: e.dma_start(out=pow2[:, :], in_=c_pow2[0:64, :]), writes=[pow2])
                    for half in range(2):
                        p.op('dve', lambda e, half=half: e.tensor_scalar(out=idx2[:, :, half], in0=pt_sb[:, :], scalar1=2, scalar2=half, op0=ALU.mult, op1=ALU.add),
                             reads=[pt_sb], writes=[idx2])
                    p.op('dve', lambda e: e.tensor_tensor(out=Dg[:, :, :, :], in0=wsc_s[:, :].unsqueeze(1).unsqueeze(3).to_broadcast([64, 16, 8, 4]),
                                                          in1=identf[0:64, 0:64].rearrange("q (b t) -> q b t", b=16).unsqueeze(2).to_broadcast([64, 16, 8, 4]), op=ALU.mult),
                         reads=[wsc_s, identf], writes=[Dg])
                    bk = nb()
                    mm(bk[0:64, :], onesf[0:64, 0:64], Dg[:, :, :, :].rearrange("q b h t -> q (b h t)"), True, True, [onesf, Dg], [bk])
                    p.op('act', lambda e, bk=bk: e.copy(out=Wbc[:, :, :], in_=bk[0:64, :].rearrange("q (b x) -> q b x", b=16)), reads=[bk], writes=[Wbc])
                    p.op('pool', lambda e: e.tensor_copy(out=q2T[0:64, :, :], in_=qTs[:, 0:4, :]), reads=[qTs], writes=[q2T])
                    p.dma('sp', lambda e: e.dma_start(out=q2T[64:128, :, :], in_=qTs[:, 4:8, :]), reads=[qTs], writes=[q2T])
                    bk = nb()
                    bv = bk[:].bitcast(BF16)
                    tr(bv[:, 0:64], kb_s[:, 0:128], 64, [kb_s], [bk])
                    tr(bv[0:64, 64:128], kb_s[:, 128:192], 64, [kb_s], [bk])
                    p.op('act', lambda e, bv=bv: e.copy(out=kT2n[:, :], in_=bv[:, 0:64]), reads=[bk], writes=[kT2n])
                    p.op('act', lambda e, bv=bv: e.copy(out=kiTs[:, :], in_=bv[0:64, 64:128]), reads=[bk], writes=[kiTs])
                    p.dma('sp', lambda e: e.dma_start(out=vnf[:, :, :], in_=v_s.rearrange("(b t) d -> t b d", t=4)), reads=['vo'], writes=[vnf])
                    p.op('dve', lambda e: e.tensor_copy(out=vnew[:, :, :], in_=vnf[:, :, :]), reads=[vnf], writes=[vnew])

                    nb_ = cfg.get('n_sb', DEC_B)
                    NITB = cfg.get('nit', 26)
                    gseq = []
                    for b0_ in range(0, nb_, NCH):
                        grp_ = list(range(b0_, min(b0_ + NCH, nb_)))
                        gseq += [('ki', b_, 0) for b_ in grp_]
                        for b_ in grp_:
                            gseq += [('K', b_, 0), ('V', b_, 0), ('K', b_, 1), ('V', b_, 1)]
                    gpos = {it: i for i, it in enumerate(gseq)}
                    g_next = [0]

                    NGB = len(gbufs)

                    def emit_gather(i):
                        kind, b_, half = gseq[i]
                        gb = gbufs[i % NGB]
                        if kind == 'ki':
                            src, idx_ap, idx_t = ckidx_d, pt_sb[:, b_:b_ + 1], pt_sb
                        else:
                            src, idx_ap, idx_t = (ck_d if kind == 'K' else cv_d), idx2[:, b_, half:half + 1], idx2
                        p.dma('pool', lambda e: e.indirect_dma_start(out=gb[:, :], out_offset=None, in_=src[:, :],
                                                                     in_offset=bass.IndirectOffsetOnAxis(ap=idx_ap, axis=0)), reads=[idx_t], writes=[gb])

                    g_cast = [0]

                    def use(item):
                        i = gpos[item]
                        while g_next[0] < min(NGB, len(gseq)):
                            emit_gather(g_next[0])
                            g_next[0] += 1
                        while g_cast[0] <= i:
                            c = g_cast[0]
                            dst = cbs[c % len(cbs)]
                            gb = gbufs[c % NGB]
                            p.op('act', lambda e, dst=dst, gb=gb: e.copy(out=dst[:, 0:4096], in_=gb[:, 0:4096]), reads=[gb], writes=[dst])
                            p.op('dve', lambda e, dst=dst, gb=gb: e.tensor_copy(out=dst[:, 4096:8192], in_=gb[:, 4096:8192]), reads=[gb], writes=[dst])
                            g_cast[0] += 1
                            if g_next[0] < len(gseq):
                                emit_gather(g_next[0])
                                g_next[0] += 1
                        return cbs[i % len(cbs)]

                    def ki_steps(b):
                        qi_b = qiTs[:, :, 4 * b:4 * b + 4]
                        scT = scTbs[b % NCH]
                        scn_ = scns[b % NCH]
                        steps = []

                        def chunk(lc):
                            kiTc = kiTc_l[chunk_ctr[0] % 3]
                            rS = rS_l[chunk_ctr[0] % 3]
                            chunk_ctr[0] += 1
                            cb = use(('ki', b, 0))
                            bk = nb()
                            bv = bk[:].bitcast(BF16)
                            for i in range(16):
                                l = lc * 16 + i
                                tr(bv[0:64, i * 64:(i + 1) * 64], cb[:, l * 64:(l + 1) * 64], 64, [cb], [bk])
                            p.op('act', lambda e: e.copy(out=kiTc[:, :, :], in_=bv[0:64, :].rearrange("q (i j) -> q i j", i=16)), reads=[bk], writes=[kiTc])
                            bk2 = nb()
                            for i in range(16):
                                mm(bk2[0:64, i * 32:(i + 1) * 32], kiTc[:, i, :], qi_b, True, True, [kiTc, qiTs], [bk2])
                            p.op('act', lambda e: e.activation(out=rS[:, :, :], in_=bk2[0:64, :].rearrange("q (i x) -> q i x", i=16), func=AF.Relu), reads=[bk2], writes=[rS])
                            p.op('pool', lambda e: e.tensor_tensor(out=rS[:, :, :], in0=rS[:, :, :], in1=Wbc[:, b:b + 1, :].to_broadcast([64, 16, 32]), op=ALU.mult),
                                 reads=[rS, Wbc], writes=[rS])
                            p.op('dve', lambda e: e.tensor_reduce(out=scT[:, lc * 16:(lc + 1) * 16, :], in_=rS[:, :, :].rearrange("q i (h t) -> q i t h", h=8),
                                                                  op=ALU.add, axis=AX.X), reads=[rS], writes=[scT])

                        def newkeys():
                            rSn = rSn_l[b % 2]
                            bk = nb()
                            mm(bk[0:4, 0:32], kiTs[:, 4 * b:4 * b + 4], qi_b, True, True, [kiTs, qiTs], [bk])
                            p.op('act', lambda e: e.activation(out=rSn[:, :], in_=bk[0:4, 0:32], func=AF.Relu), reads=[bk], writes=[rSn])
                            p.op('pool', lambda e: e.tensor_tensor(out=rSn[:, :], in0=rSn[:, :], in1=Wbc[0:4, b, :], op=ALU.mult), reads=[rSn, Wbc], writes=[rSn])
                            p.op('dve', lambda e: e.tensor_reduce(out=scn_[:, :], in_=rSn[:, :].rearrange("q (h t) -> q t h", h=8), op=ALU.add, axis=AX.X), reads=[rSn], writes=[scn_])
                        for lc in range(8):
                            steps.append(lambda lc=lc: chunk(lc))
                        steps.append(newkeys)
                        return steps

                    def bisect_setup(b):
                        c = b % NCH
                        scT, scn_, hs, Wsc, Wtab, lo, cmpb = scTbs[c], scns[c], hs_l[c], Wsc_l[c], Wtab_l[c], lo_l[c], cmpb_l[c]
                        p.op('act', lambda e: e.activation(out=cmpb[:, :, :].rearrange("q l t -> q (l t)"), in_=scT[:, :, :].rearrange("q l t -> q (l t)"), func=AF.Square, accum_out=hs[:, 0:1]),
                             reads=[scT], writes=[cmpb, hs])
                        p.op('act', lambda e: e.activation(out=cmpb[0:4, 0, :], in_=scn_[:, :], func=AF.Square, accum_out=hs[0:4, 1:2]), reads=[scn_], writes=[cmpb, hs])
                        p.op('dve', lambda e: e.tensor_tensor(out=scn_[:, :], in0=scn_[:, :], in1=negm4[:, :], op=ALU.add), reads=[scn_, negm4], writes=[scn_])
                        bk = nb()
                        mm(bk[0:64, 0:1], onesf[0:64, 0:64], hs[:, 0:1], True, False, [onesf, hs], [bk])
                        mm(bk[0:64, 0:1], onesf[0:4, 0:64], hs[0:4, 1:2], False, True, [onesf, hs], [bk])
                        p.op('act', lambda e, bk=bk: e.activation(out=Wsc[:, 0:1], in_=bk[0:64, 0:1], func=AF.Sqrt), reads=[bk], writes=[Wsc])
                        p.op('dve', lambda e: e.tensor_scalar(out=Wsc[:, 0:1], in0=Wsc[:, 0:1], scalar1=2.2, scalar2=2.0, op0=ALU.mult, op1=ALU.add), reads=[Wsc], writes=[Wsc])
                        p.op('dve', lambda e: e.tensor_scalar(out=Wsc[:, 1:2], in0=Wsc[:, 0:1], scalar1=-0.5, scalar2=None, op0=ALU.mult), reads=[Wsc], writes=[Wsc])
                        p.op('dve', lambda e: e.tensor_scalar(out=Wtab[:, :], in0=pow2[:, :], scalar1=Wsc[:, 0:1], scalar2=None, op0=ALU.mult), reads=[pow2, Wsc], writes=[Wtab])
                        p.op('dve', lambda e: e.tensor_scalar(out=lo[:, :], in0=pow2[:, 0:4], scalar1=0.0, scalar2=Wsc[:, 1:2], op0=ALU.mult, op1=ALU.add), reads=[pow2, Wsc], writes=[lo])

                    def bisect_iter(b, k):
                        c = b % NCH
                        scT, scn_, Wtab, lo, mid, ge, cmpb, cntp, cmpn = scTbs[c], scns[c], Wtab_l[c], lo_l[c], mid_l[c], ge_l[c], cmpb_l[c], cntp_l[c], cmpn_l[c]
                        p.op('dve', lambda e: e.tensor_scalar(out=mid[:, :], in0=lo[:, :], scalar1=Wtab[:, k:k + 1], scalar2=None, op0=ALU.add), reads=[lo, Wtab], writes=[mid])
                        p.op('dve', lambda e: e.tensor_tensor(out=cmpb[:, :, :], in0=scT[:, :, :], in1=mid[:, :].unsqueeze(1).to_broadcast([64, 128, 4]), op=ALU.is_ge),
                             reads=[scT, mid], writes=[cmpb])
                        p.op('dve', lambda e: e.tensor_reduce(out=cntp[:, :], in_=cmpb[:, :, :].rearrange("q l t -> q t l"), op=ALU.add, axis=AX.X), reads=[cmpb], writes=[cntp])
                        p.op('dve', lambda e: e.tensor_tensor(out=cmpn[:, :], in0=scn_[:, :], in1=mid[0:4, :], op=ALU.is_ge), reads=[scn_, mid], writes=[cmpn])
                        bk = nb()
                        mm(bk[0:64, 0:4], onesf[0:64, 0:64], cntp[:, :], True, False, [onesf, cntp], [bk])
                        mm(bk[0:64, 0:4], onesf[0:4, 0:64], cmpn[:, :], False, True, [onesf, cmpn], [bk])
                        p.op('dve', lambda e: e.tensor_scalar(out=ge[:, :], in0=bk[0:64, 0:4], scalar1=255.5, scalar2=None, op0=ALU.is_ge), reads=[bk], writes=[ge])
                        p.op('dve', lambda e: e.scalar_tensor_tensor(out=lo[:, :], in0=ge[:, :], scalar=Wtab[:, k:k + 1], in1=lo[:, :], op0=ALU.mult, op1=ALU.add),
                             reads=[ge, Wtab, lo], writes=[lo])

                    def bisect_finish(b):
                        c = b % NCH
                        scT, scn_, lo, mask_s, maskn = scTbs[c], scns[c], lo_l[c], mask_s_l[c], maskn_l[c]
                        p.op('dve', lambda e: e.tensor_tensor(out=mask_s[:, :, :], in0=scT[:, :, :], in1=lo[:, :].unsqueeze(1).to_broadcast([64, 128, 4]), op=ALU.is_ge),
                             reads=[scT, lo], writes=[mask_s])
                        p.op('dve', lambda e: e.tensor_tensor(out=maskn[:, :], in0=scn_[:, :], in1=lo[0:4, :], op=ALU.is_ge), reads=[scn_, lo], writes=[maskn])

                    def attend(b):
                        mask_s, maskn = mask_s_l[b % NCH], maskn_l[b % NCH]
                        o6, o7 = pb[6], pb[7]
                        first = True
                        for half in range(2):
                            cbK = use(('K', b, half))
                            for lc in range(4):
                                bk = nb()
                                bv = bk[:].bitcast(BF16)
                                for i in range(16):
                                    l = lc * 16 + i
                                    tr(bv[:, i * 64:(i + 1) * 64], cbK[:, l * 128:(l + 1) * 128], 64, [cbK], [bk])
                                p.op('act', lambda e, bv=bv, lc=lc: e.copy(out=KTb[:, lc * 16:(lc + 1) * 16, :], in_=bv[:, :].rearrange("q (i j) -> q i j", i=16)), reads=[bk], writes=[KTb])
                            cbV = use(('V', b, half))
                            prev = None
                            for lc in range(5):
                                if lc < 4:
                                    bkg = [nb(), nb()]
                                    for i in range(16):
                                        l = lc * 16 + i
                                        for g in range(2):
                                            mm(bkg[g][0:64, i * 16:(i + 1) * 16], KTb[g * 64:(g + 1) * 64, l, :], q2T[g * 64:(g + 1) * 64, :, 4 * b:4 * b + 4], True, True, [KTb, q2T], [bkg[g]])
                                    E_, P_ = Ess[lc % 2], PTss[lc % 2]
                                    for g in range(2):
                                        p.op('act', lambda e, g=g, bkg=bkg, E_=E_: e.activation(out=E_[:, :, g * 16:(g + 1) * 16], in_=bkg[g][0:64, 0:256].rearrange("q (i x) -> q i x", i=16),
                                                                                                  func=AF.Exp, scale=0.125), reads=[bkg[g]], writes=[E_])
                                    l0 = half * 64 + lc * 16
                                    p.op('pool', lambda e, l0=l0, E_=E_, P_=P_: e.tensor_tensor(out=P_[:, :, :].rearrange("q i (x t) -> q i x t", t=4), in0=E_[:, :, :].rearrange("q i (x t) -> q i x t", t=4),
                                                                                              in1=mask_s[:, l0:l0 + 16, :].unsqueeze(2).to_broadcast([64, 16, 8, 4]), op=ALU.mult),
                                         reads=[E_, mask_s], writes=[P_])
                                if prev is not None:
                                    plc, P_prev = prev
                                    for i in range(16):
                                        l = plc * 16 + i
                                        mm(o6[:, 0:32], cbV[:, l * 128:(l + 1) * 128], P_prev[:, i, :], first, False, [cbV, P_prev], [o6])
                                        mm(o7[:, 0:32], ones_bf[0:64, :], P_prev[:, i, :], first, False, [ones_bf, P_prev], [o7])
                                        first = False
                                prev = (lc, PTss[lc % 2]) if lc < 4 else None
                        bkg = [nb(), nb()]
                        for g in range(2):
                            mm(bkg[g][0:4, 0:16], kT2n[g * 64:(g + 1) * 64, 4 * b:4 * b + 4], q2T[g * 64:(g + 1) * 64, :, 4 * b:4 * b + 4], True, True, [kT2n, q2T], [bkg[g]])
                        for g in range(2):
                            p.op('act', lambda e, g=g, bkg=bkg: e.activation(out=En[:, g * 16:(g + 1) * 16], in_=bkg[g][0:4, 0:16], func=AF.Exp, scale=0.125), reads=[bkg[g]], writes=[En])
                        p.op('pool', lambda e: e.tensor_tensor(out=PTn[:, :].rearrange("q (x t) -> q x t", t=4), in0=En[:, :].rearrange("q (x t) -> q x t", t=4),
                                                               in1=maskn[:, :].unsqueeze(1).to_broadcast([4, 8, 4]), op=ALU.mult), reads=[En, maskn], writes=[PTn])
                        mm(o6[:, 0:32], vnew[:, b, :], PTn[:, :], False, True, [vnew, PTn], [o6])
                        mm(o7[:, 0:32], ones_bf[0:4, :], PTn[:, :], False, True, [ones_bf, PTn], [o7])
                        p.op('dve', lambda e: e.reciprocal(out=rcs[:, :], in_=o7[:, 0:32]), reads=[o7], writes=[rcs])
                        for g in range(2):
                            p.op('dve', lambda e, g=g: e.tensor_tensor(out=ybTs[g * 64:(g + 1) * 64, :, 4 * b:4 * b + 4],
                                                                     in0=o6[g * 64:(g + 1) * 64, g * 16:(g + 1) * 16].rearrange("q (r t) -> q r t", r=4),
                                                                     in1=rcs[g * 64:(g + 1) * 64, g * 16:(g + 1) * 16].rearrange("q (r t) -> q r t", r=4), op=ALU.mult),
                                 reads=[o6, rcs], writes=[ybTs])

                    for b0_ in range(0, nb_, NCH):
                        grp_ = list(range(b0_, min(b0_ + NCH, nb_)))
                        for b in grp_:
                            for st_ in ki_steps(b):
                                st_()
                        if cfg.get('sb_stage', 9) < 2:
                            continue
                        for b in grp_:
                            bisect_setup(b)
                        for k in range(NITB):
                            for b in grp_:
                                bisect_iter(b, k)
                        for b in grp_:
                            bisect_finish(b)
                        if cfg.get('sb_stage', 9) < 3:
                            continue
                        for b in grp_:
                            attend(b)
                    for r in range(4):
                        bk = nb()
                        bv = bk[:].bitcast(BF16)
                        tr(bv[0:64, 0:128], ybTs[:, r, :], 128, [ybTs], [bk])
                        p.op('act', lambda e, bv=bv, r=r: e.copy(out=ycats[:, 512:1024].rearrange("q (g r d) -> q g r d", g=2, r=4)[:, :, r, :],
                                                                 in_=bv[0:64, 0:128].rearrange("q (g d) -> q g d", g=2)), reads=[bk], writes=[ycats])


                if 'P3' in do:
                    proj_tile(xs_d[:, :], NS, 0, True, x0s, ycats)
                    feature_major(NS, 0, qTs, qiTs)
                    p.op('pool', lambda e: e.tensor_copy(out=kvf_s[:, :], in_=kvf[0:NS, :]), reads=[kvf], writes=[kvf_s])
                    p.op('pool', lambda e: e.tensor_copy(out=kb_s[:, :], in_=kb[0:NS, :]), reads=[kb], writes=[kb_s])
                    p.op('pool', lambda e: e.tensor_copy(out=wsc_s[:, :], in_=wsc[0:NS, :]), reads=[wsc], writes=[wsc_s])
                if 'P2' in do:
                    for ti in range(cfg.get('n_tiles', NT)):
                        proj_tile(xp_d[ti * 128:(ti + 1) * 128, :], 128, ti, False, x0, ycat)
                        feature_major(128, ti, qT, qiT)
                        if 'P4' in do and cfg.get('interleave_conv', True):
                            for ec in range(ti * 8, ti * 8 + 8):
                                convert_chunk(ec, cub[ec % 2], cut[ec % 2], cvb[ec % 2])
                            conv_done[0] = ti * 8 + 8
                        prompt_dsa(ti)
                        out_proj_and_mem(128, ti * 128, False, x0, ycat)
            sw.close()
            p.barrier()
            if 'P3' in do:
                with ExitStack() as s3s:
                    sample_dsa(s3s)
                p.barrier()
                with ExitStack() as s3m:
                    sm['mf'] = [p.sb('mf%d' % i, [128, 2, 512], F32, s3m) for i in range(2)]
                    sm['mb'] = [p.sb('mb%d' % i, [128, 2, 512], BF16, s3m) for i in range(2)]
                    sm['mT'] = p.sb('mT', [128, 8, 128], BF16, s3m)
                    sm['Pm'] = p.sb('Pm', [128, 32], BF16, s3m)
                    sm['rc'] = p.sb('rc', [128, 16], F32, s3m)
                    out_proj_and_mem(NS, SEQ, True, x0s, ycats)
            p.barrier()

        if 'P4' in do:
            nbmod[0] = 4
            with ExitStack() as s4:
                iota_f = p.sb('iota_f', [128, 128], F32, s4)
                gfin = p.sb('gfin', [128, D], F32, s4)
                p.dma('sp', lambda e: e.dma_start(out=iota_f[:], in_=c_iota[:, :]), writes=[iota_f])
                p.dma('sp', lambda e: e.dma_start(out=gfin[:], in_=g_fin_d.partition_broadcast(128)), writes=[gfin])
                pwq = p.sb('pwq_sb', [128, 8, D], BF16, s4)
                keysT = p.sb('keysT', [64, 16, 128], BF16, s4)
                with ExitStack() as s5:
                    stage = [p.sb('pstage%d' % i, [128, D], F32, s5) for i in range(2)]
                    load_weight(pwq, pwq_d, 8, D, None, stage)
                    kbf = p.sb('kbf', [128, 64], BF16, s5)
                    for hc in range(16):
                        stg = stage[hc % 2]
                        p.dma('sp', lambda e, hc=hc, stg=stg: e.dma_start(out=stg[:, 0:64], in_=pkeys_d[hc, :, :]), writes=[stg])
                        p.op('dve', lambda e, stg=stg: e.tensor_copy(out=kbf[:], in_=stg[:, 0:64]), reads=[stg], writes=[kbf])
                        bk = nb()
                        bv = bk[:].bitcast(BF16)
                        tr(bv[0:64, 0:128], kbf[:, :], 128, [kbf], [bk])
                        p.op('act', lambda e, hc=hc, bv=bv: e.copy(out=keysT[:, hc, :], in_=bv[0:64, 0:128]), reads=[bk], writes=[keysT])
                    ubf = [p.sb('ubf%d' % i, [128, D], BF16, s5) for i in range(2)]
                    utb = [p.sb('utb%d' % i, [128, D], BF16, s5) for i in range(2)]
                    vbf = [p.sb('vbf%d' % i, [128, D], BF16, s5) for i in range(2)]
                    for ec in range(conv_done[0], cfg.get('n_ec', 128)):
                        convert_chunk(ec, ubf[ec % 2], utb[ec % 2], vbf[ec % 2])

                p.barrier()
                xg = p.sb('xg', [128, 2, D], F32, s4)
                XT = p.sb('XT', [128, 8, 256], BF16, s4)
                hn = p.sb('hn', [128, D], BF16, s4)
                hnT = p.sb('hnT', [128, 8, 128], BF16, s4)
                sq4 = p.sb('sq4', [128, D], F32, s4)
                qpT = p.sb('qpT', [64, 16, 128], BF16, s4)
                ssb = p.sb('ssb', [128, 16, 128], F32, s4)
                wk = p.sb('wk', [128, 128], F32, s4)
                a16 = p.sb('a16', [128, 8, 16], F32, s4)
                b16 = p.sb('b16', [128, 8, 16], F32, s4)
                iau = p.sb('iau', [128, 8, 16], U32, s4)
                ibu = p.sb('ibu', [128, 8, 16], U32, s4)
                iaf = p.sb('iaf', [128, 8, 16], F32, s4)
                ibf = p.sb('ibf', [128, 8, 16], F32, s4)
                cand = p.sb('cand', [128, 8, 256], F32, s4)
                wkc = p.sb('wkc', [128, 256], F32, s4)
                c16 = p.sb('c16', [128, 8, 16], F32, s4)
                icu = p.sb('icu', [128, 8, 16], U32, s4)
                iju = p.sb('iju', [128, 2, 128], U32, s4)
                ijf = p.sb('ijf', [128, 2, 128], F32, s4)
                eq = p.sb('eq', [128, 128, 16], F32, s4)
                SEL = p.sb('SEL', [128, 3, 128], F32, s4)
                e16 = p.sb('e16', [128, 8, 16], F32, s4)
                z8 = p.sb('z8', [128, 16], F32, s4)
                selT = p.sb('selT', [128, 3, 256], F32, s4)
                Gt = p.sb('Gt', [128, 256, 128], BF16, s4)
                ohb1 = [p.sb('ohb1_%d' % i, [128, 8, 128], BF16, s4) for i in range(2)]
                ohb0 = [p.sb('ohb0_%d' % i, [128, 8, 128], BF16, s4) for i in range(2)]
                ohbg = [p.sb('ohbg_%d' % i, [128, 8, 128], BF16, s4) for i in range(2)]
                selTb = p.sb('selTb', [128, 3, 256], BF16, s4)
                iota_b = p.sb('iota_b', [128, 128], BF16, s4)
                p.op('pool', lambda e: e.tensor_copy(out=iota_b[:, :], in_=iota_f[:, :]), reads=[iota_f], writes=[iota_b])
                NBUF = 4
                UTc = [p.sb('UTc%d' % i, [128, 8, 128], BF16, s4) for i in range(NBUF)]
                Vc = [p.sb('Vc%d' % i, [128, D], BF16, s4) for i in range(NBUF)]
                gab = [p.sb('gab%d' % i, [128, 256], BF16, s4) for i in range(3)]
                GAb = [p.sb('GAb%d' % i, [128, 256], BF16, s4) for i in range(3)]
                pre = p.sb('pre', [128, D], F32, s4)
                yo = pre


                def peer_select(T, s):
                    norm_T(xg[:, s, :], T, 'f', hn, hnT, ssd, sq4, gcol=gcols['ffn'])
                    p.op('pool', lambda e: e.tensor_copy(out=XT[:, :, s * 128:s * 128 + T], in_=hnT[:, :, 0:T]), reads=[hnT], writes=[XT])
                    for q4 in range(4):
                        bk = nb()
                        for k4 in range(4):
                            hc = q4 * 4 + k4
                            for c in range(8):
                                mm(bk[0:64, k4 * T:(k4 + 1) * T], pwq[:, c, hc * 64:(hc + 1) * 64], hnT[:, c, 0:T], c == 0, c == 7, [pwq, hnT], [bk])
                        p.op('act', lambda e, bk=bk, q4=q4: e.copy(out=qpT[:, q4 * 4:(q4 + 1) * 4, 0:T], in_=bk[0:64, 0:4 * T].rearrange("q (k t) -> q k t", k=4)),
                             reads=[bk], writes=[qpT])
                    for q4 in range(4):
                        bk = nb()
                        for k4 in range(4):
                            hc = q4 * 4 + k4
                            mm(bk[0:T, k4 * 128:(k4 + 1) * 128], qpT[:, hc, 0:T], keysT[:, hc, :], True, True, [qpT, keysT], [bk])
                        p.op('act', lambda e, bk=bk, q4=q4: e.copy(out=ssb[0:T, q4 * 4:(q4 + 1) * 4, :], in_=bk[0:T, :].rearrange("q (k n) -> q k n", k=4)),
                             reads=[bk], writes=[ssb])
                    for h in range(8):
                        for cc, (vals, idxs) in enumerate(((a16, iau), (b16, ibu))):
                            src = ssb[0:T, 2 * h + cc, :]
                            p.op('dve', lambda e, src=src, vals=vals, h=h: e.max(out=vals[0:T, h, 0:8], in_=src), reads=[ssb], writes=[vals])
                            p.op('dve', lambda e, src=src, vals=vals, idxs=idxs, h=h: e.max_index(out=idxs[0:T, h, 0:8], in_max=vals[0:T, h, 0:8], in_values=src),
                                 reads=[ssb, vals], writes=[idxs])
                            p.op('dve', lambda e, src=src, vals=vals, h=h: e.match_replace(out=wk[0:T, :], in_to_replace=vals[0:T, h, 0:8], in_values=src, imm_value=NEG),
                                 reads=[ssb, vals], writes=[wk])
                            p.op('dve', lambda e, vals=vals, h=h: e.max(out=vals[0:T, h, 8:16], in_=wk[0:T, :]), reads=[wk], writes=[vals])
                            p.op('dve', lambda e, vals=vals, idxs=idxs, h=h: e.max_index(out=idxs[0:T, h, 8:16], in_max=vals[0:T, h, 8:16], in_values=wk[0:T, :]),
                                 reads=[wk, vals], writes=[idxs])
                    for h in range(8):
                        p.op('dve', lambda e, h=h: e.tensor_tensor(out=cand[0:T, h, :].rearrange("q (i j) -> q i j", i=16),
                                                                    in0=a16[0:T, h, :].unsqueeze(2).to_broadcast([T, 16, 16]),
                                                                    in1=b16[0:T, h, :].unsqueeze(1).to_broadcast([T, 16, 16]), op=ALU.add),
                             reads=[a16, b16], writes=[cand])
                    for h in range(8):
                        src = cand[0:T, h, :]
                        p.op('dve', lambda e, src=src, h=h: e.max(out=c16[0:T, h, 0:8], in_=src), reads=[cand], writes=[c16])
                        p.op('dve', lambda e, src=src, h=h: e.max_index(out=icu[0:T, h, 0:8], in_max=c16[0:T, h, 0:8], in_values=src), reads=[cand, c16], writes=[icu])
                        p.op('dve', lambda e, src=src, h=h: e.match_replace(out=wkc[0:T, :], in_to_replace=c16[0:T, h, 0:8], in_values=src, imm_value=NEG),
                             reads=[cand, c16], writes=[wkc])
                        p.op('dve', lambda e, h=h: e.max(out=c16[0:T, h, 8:16], in_=wkc[0:T, :]), reads=[wkc], writes=[c16])
                        p.op('dve', lambda e, h=h: e.max_index(out=icu[0:T, h, 8:16], in_max=c16[0:T, h, 8:16], in_values=wkc[0:T, :]), reads=[wkc, c16], writes=[icu])
                    icf = icu[0:T, :, :].rearrange("q h k -> q (h k)")
                    p.op('dve', lambda e: e.tensor_scalar(out=iju[0:T, 0, :], in0=icf, scalar1=4, scalar2=None, op0=ALU.logical_shift_right), reads=[icu], writes=[iju])
                    p.op('dve', lambda e: e.tensor_scalar(out=iju[0:T, 1, :], in0=icf, scalar1=15, scalar2=None, op0=ALU.bitwise_and), reads=[icu], writes=[iju])
                    p.op('dve', lambda e: e.tensor_copy(out=ijf[0:T, :, :], in_=iju[0:T, :, :]), reads=[iju], writes=[ijf])
                    p.op('dve', lambda e: e.tensor_copy(out=iaf[0:T, :, :], in_=iau[0:T, :, :]), reads=[iau], writes=[iaf])
                    p.op('dve', lambda e: e.tensor_copy(out=ibf[0:T, :, :], in_=ibu[0:T, :, :]), reads=[ibu], writes=[ibf])
                    for w_, srcf in ((0, iaf), (1, ibf)):
                        p.op('dve', lambda e, w_=w_: e.tensor_tensor(out=eq[0:T, :, :], in0=ijf[0:T, w_, :].unsqueeze(2).to_broadcast([T, 128, 16]),
                                                                     in1=iota_f[0:T, 0:16].unsqueeze(1).to_broadcast([T, 128, 16]), op=ALU.is_equal),
                             reads=[ijf, iota_f], writes=[eq])
                        p.op('dve', lambda e, srcf=srcf: e.tensor_tensor(out=eq[0:T, :, :].rearrange("q (h k) i -> q h k i", h=8),
                                                                         in0=eq[0:T, :, :].rearrange("q (h k) i -> q h k i", h=8),
                                                                         in1=srcf[0:T, :, :].unsqueeze(2).to_broadcast([T, 8, 16, 16]), op=ALU.mult),
                             reads=[eq, srcf], writes=[eq])
                        p.op('dve', lambda e, w_=w_: e.tensor_reduce(out=SEL[0:T, w_, :], in_=eq[0:T, :, :], op=ALU.add, axis=AX.X), reads=[eq], writes=[SEL])
                    p.op('dve', lambda e: e.tensor_tensor(out=e16[0:T, :, :], in0=c16[0:T, :, :], in1=c16[0:T, :, 0:1].to_broadcast([T, 8, 16]), op=ALU.subtract),
                         reads=[c16], writes=[e16])
                    p.op('act', lambda e: e.activation(out=e16[0:T, :, :], in_=e16[0:T, :, :], func=AF.Exp), reads=[e16], writes=[e16])
                    p.op('dve', lambda e: e.tensor_reduce(out=z8[0:T, 0:8], in_=e16[0:T, :, :], op=ALU.add, axis=AX.X), reads=[e16], writes=[z8])
                    p.op('dve', lambda e: e.reciprocal(out=z8[0:T, 8:16], in_=z8[0:T, 0:8]), reads=[z8], writes=[z8])
                    p.op('dve', lambda e: e.tensor_tensor(out=SEL[0:T, 2, :].rearrange("q (h k) -> q h k", h=8), in0=e16[0:T, :, :],
                                                          in1=z8[0:T, 8:16].unsqueeze(2).to_broadcast([T, 8, 16]), op=ALU.mult), reads=[e16, z8], writes=[SEL])
                    bk = nb()
                    for w_ in range(3):
                        p.op('pe', lambda e, w_=w_, bk=bk: e.transpose(out=bk[:, w_ * T:(w_ + 1) * T], in_=SEL[0:T, w_, :], identity=identf[0:T, 0:T]),
                             reads=[SEL, identf], writes=[bk])
                    p.op('act', lambda e, bk=bk: e.copy(out=selT[:, :, s * 128:s * 128 + T], in_=bk[:, 0:3 * T].rearrange("q (w t) -> q w t", w=3)),
                         reads=[bk], writes=[selT])
                    p.op('pool', lambda e: e.tensor_copy(out=selTb[:, :, s * 128:s * 128 + T], in_=selT[:, :, s * 128:s * 128 + T]), reads=[selT], writes=[selTb])

                def peer_group(row0, Tg, y_out, yrow0):
                    nsub = (Tg + 127) // 128
                    Ts = min(Tg, 128)
                    for s in range(nsub):
                        p.dma('sp', lambda e, s=s: e.dma_start(out=xg[0:Ts, s, :], in_=x2s[row0 + s * 128:row0 + s * 128 + Ts, :]), reads=['x2s'], writes=[xg])
                        peer_select(Ts, s)
                    for t0 in range(0, Tg, 8):
                        o1, o0, og = ohb1[(t0 // 8) % 2], ohb0[(t0 // 8) % 2], ohbg[(t0 // 8) % 2]
                        p.op('dve', lambda e, t0=t0, o1=o1: e.tensor_tensor(out=o1[:, :, :], in0=iota_b[:, :].unsqueeze(1).to_broadcast([128, 8, 128]),
                                                                          in1=selTb[:, 1, t0:t0 + 8].unsqueeze(2).to_broadcast([128, 8, 128]), op=ALU.is_equal),
                             reads=[iota_b, selTb], writes=[o1])
                        p.op('dve', lambda e, t0=t0, o0=o0: e.tensor_tensor(out=o0[:, :, :], in0=iota_b[:, :].unsqueeze(1).to_broadcast([128, 8, 128]),
                                                                          in1=selTb[:, 0, t0:t0 + 8].unsqueeze(2).to_broadcast([128, 8, 128]), op=ALU.is_equal),
                             reads=[iota_b, selTb], writes=[o0])
                        p.op('pool', lambda e, t0=t0, o0=o0, og=og: e.tensor_tensor(out=og[:, :, :], in0=o0[:, :, :],
                                                                                  in1=selTb[:, 2, t0:t0 + 8].unsqueeze(2).to_broadcast([128, 8, 128]), op=ALU.mult),
                             reads=[o0, selTb], writes=[og])
                        for q4 in range(2):
                            bk = nb()
                            for tt in range(4):
                                mm(bk[:, tt * 128:(tt + 1) * 128], o1[:, q4 * 4 + tt, :], og[:, q4 * 4 + tt, :], True, True, [o1, og], [bk])
                            p.op('act', lambda e, bk=bk, ta=t0 + q4 * 4: e.copy(out=Gt[:, ta:ta + 4, :], in_=bk[:, :].rearrange("q (t i) -> q t i", t=4)), reads=[bk], writes=[Gt])
                    acc = pb[4:8]
                    n_ec = cfg.get('n_ec', 128)

                    def fetch(ec):
                        p.dma('sp', lambda e, ec=ec: e.dma_start(out=UTc[ec % NBUF][:, :, :].rearrange("q c e -> q (c e)"), in_=UTs[ec, :, :]),
                              reads=[('UTs', ec)], writes=[UTc[ec % NBUF]])
                        p.dma('sp', lambda e, ec=ec: e.dma_start(out=Vc[ec % NBUF][:, :], in_=Vs[ec, :, :]), reads=[('Vs', ec)], writes=[Vc[ec % NBUF]])
                    for ec in range(min(NBUF, n_ec)):
                        fetch(ec)
                    for ec in range(n_ec + 1):
                        if ec < n_ec:
                            U_ = UTc[ec % NBUF]
                            bk = pb[ec % 2]
                            for c in range(8):
                                mm(bk[:, 0:Tg], U_[:, c, :], XT[:, c, 0:Tg], c == 0, c == 7, [U_, XT], [bk])
                            ga, GA = gab[ec % 3], GAb[ec % 3]
                            p.op('act', lambda e, bk=bk, ga=ga: e.activation(out=ga[:, 0:Tg], in_=bk[:, 0:Tg], func=AF.Gelu_apprx_tanh), reads=[bk], writes=[ga])
                            p.op('dve', lambda e, ga=ga, GA=GA, ec=ec: e.tensor_tensor(out=GA[:, 0:Tg], in0=ga[:, 0:Tg], in1=Gt[:, 0:Tg, ec], op=ALU.mult),
                                 reads=[ga, Gt], writes=[GA])
                        if ec >= 1:
                            pe_ = ec - 1
                            V_, GA = Vc[pe_ % NBUF], GAb[pe_ % 3]
                            for s in range(nsub):
                                for half in range(2):
                                    ab = acc[2 * s + half]
                                    mm(ab[0:Ts, :], GA[:, s * 128:s * 128 + Ts], V_[:, half * 512:(half + 1) * 512], pe_ == 0, pe_ == n_ec - 1, [GA, V_], [ab])
                            if pe_ + NBUF < n_ec:
                                fetch(pe_ + NBUF)
                    for s in range(nsub):
                        for half in range(2):
                            ab = acc[2 * s + half]
                            p.op('dve', lambda e, ab=ab, s=s, half=half: e.tensor_tensor(out=pre[0:Ts, half * 512:(half + 1) * 512], in0=ab[0:Ts, :],
                                                                                         in1=xg[0:Ts, s, half * 512:(half + 1) * 512], op=ALU.add),
                                 reads=[ab, xg], writes=[pre])
                        rs = rmsnorm_rstd(pre, Ts, ssd, sq4)
                        p.op('dve', lambda e, rs=rs: e.scalar_tensor_tensor(out=yo[0:Ts, :], in0=pre[0:Ts, :], scalar=rs, in1=gfin[0:Ts, :], op0=ALU.mult, op1=ALU.mult),
                             reads=[pre, ssd['ss'], gfin], writes=[yo])
                        p.dma('sp', lambda e, s=s: e.dma_start(out=y_out[yrow0 + s * 128:yrow0 + s * 128 + Ts, :], in_=yo[0:Ts, :]), reads=[yo], writes=['y_out'])

                for gi in range(cfg.get('n_groups', 8)):
                    peer_group(gi * 256, 256, y_p, gi * 256)
                if cfg.get('peer_sample', True):
                    peer_group(SEQ, NS, y_s, 0)

        p.finish()
        p.emit()
    return nc


_NC_CACHE = {}


def make_in_maps(inp, cfg, ncores=NCORES):
    c = host_consts()
    f = lambda a: np.ascontiguousarray(a, dtype=np.float32)
    maps = []
    for i in range(ncores):
        m = {
            'xp': f(inp['x_prompt'][i]),
            'xs': f(inp['x_sample'][DEC_B * i:DEC_B * (i + 1)].reshape(NS, D)),
            'memp': f(inp['mem_prompt'][i]),
            'w_in': f(inp['w_in'][0]), 'w_out': f(inp['w_out'][0]),
            'g_mix': f(inp['norm_mix_g'][0]), 'g_mem': f(inp['norm_mem_g'][0]), 'g_memn': f(inp['mem_norm_g'][0]),
            'g_ffn': f(inp['norm_ffn_g'][0]), 'g_fin': f(inp['final_norm_g']),
            'ln_g': f(inp['gm_ln_g'][0]), 'ln_b': f(inp['gm_ln_b'][0]),
            'gm_ws': f(inp['gm_ws'][0]), 'gm_bs': f(inp['gm_bs'][0]),
            'mem_wq': f(inp['mem_wq'][0]), 'mem_wkv': f(inp['mem_wkv'][0]), 'mem_wo': f(inp['mem_wo'][0]),
            'c_ident': c['ident'], 'c_trilT': c['trilT'], 'c_negmask': c['negmask'], 'c_blk64': c['blk64'], 'c_ones': c['ones'], 'c_iota': c['iota'],
            'peer_wq': f(inp['peer_wq'][0]), 'peer_keys': f(inp['peer_keys'][0].reshape(16, 128, 64)),
            'peer_u': f(inp['peer_u'][0]), 'peer_v': f(inp['peer_v'][0]),
            'cache_kidx': f(inp['cache_kidx'][0]).reshape(-1, 8192), 'cache_k': f(inp['cache_k'][0]).reshape(-1, 8192),
            'cache_v': f(inp['cache_v'][0]).reshape(-1, 8192),
            'page_table': np.ascontiguousarray(inp['page_table'][DEC_B * i:DEC_B * (i + 1)], dtype=np.int32),
            'cache_mem_k': f(inp['cache_mem_k'][0][DEC_B * i:DEC_B * (i + 1)]).reshape(DEC_B, 256, 512),
            'cache_mem_v': f(inp['cache_mem_v'][0][DEC_B * i:DEC_B * (i + 1)]).reshape(DEC_B, 256, 512),
            'c_pow2': c['pow2'], 'c_negm4': c['negm4'],
        }
        maps.append(m)
    return maps


def assemble(results, ncores=NCORES):
    cat = lambda k: np.stack([np.asarray(r[k], dtype=np.float32) for r in results])
    y_prompt = cat('y_p')
    y_sample = cat('y_s').reshape(ncores * DEC_B, DEC_T, D)
    k_prompt = cat('k_p').reshape(1, ncores, SEQ, 2, 64)
    v_prompt = cat('v_p').reshape(1, ncores, SEQ, 2, 64)
    kidx_prompt = cat('ki_p').reshape(1, ncores, SEQ, 64)
    gmv_prompt = cat('gmv_p').reshape(1, ncores, 128, 512)
    memk = cat('memk_p').reshape(1, ncores, 256, 4, 128)
    memv = cat('memv_p').reshape(1, ncores, 256, 4, 128)
    k_sample = cat('k_s').reshape(1, ncores * DEC_B, DEC_T, 2, 64)
    v_sample = cat('v_s').reshape(1, ncores * DEC_B, DEC_T, 2, 64)
    kidx_sample = cat('ki_s').reshape(1, ncores * DEC_B, DEC_T, 64)
    gmv_sample = cat('gmv_s').reshape(1, ncores * DEC_B, DEC_T, 512)
    return (y_prompt, y_sample, k_prompt, v_prompt, kidx_prompt, gmv_prompt, memk, memv,
            k_sample, v_sample, kidx_sample, gmv_sample)


def kernel(**inputs):
    cfg = {}
    nc = build(cfg)
    maps = make_in_maps(inputs, cfg)
    res = run_bass_kernel_spmd(nc, maps, core_ids=list(range(NCORES)))
    return assemble(res.results)
```

```python
import numpy as np
from contextlib import ExitStack
import concourse.bass as bass
import concourse.mybir as mybir
from concourse.bass_utils import run_bass_kernel_spmd

F32 = mybir.dt.float32
BF16 = mybir.dt.bfloat16
I32 = mybir.dt.int32
U32 = mybir.dt.uint32
AF = mybir.ActivationFunctionType
ALU = mybir.AluOpType
AX = mybir.AxisListType

NCORES = 8
D = 1024
SEQ = 2048
NT = SEQ // 128
P_IN = 2376
EPS = 1e-6
NEG = -1.0e30
DEC_B = 16
DEC_T = 4
NS = DEC_B * DEC_T
NPAGES = 64
NEXP = 16384


class Prog:
    ENG = ('sp', 'act', 'dve', 'pool', 'pe')

    def __init__(self, nc, stack, n_dma_sems=12):
        self.nc = nc
        self.stack = stack
        self.ops = {k: [] for k in self.ENG}
        self.cnt = {k: 0 for k in self.ENG}
        self.waited = {k: {} for k in self.ENG}
        self.res = {}
        self.sems = {}
        for k in ('act', 'dve', 'pool', 'pe'):
            self.sems[k] = stack.enter_context(nc.semaphore('prog_' + k))
        self.dma_sems = {}
        self.dma_rr = {}
        self.dma_uses = {}
        for q in ('sp', 'pool', 'act'):
            lst = []
            for i in range(n_dma_sems):
                key = 'dma_%s_%d' % (q, i)
                self.sems[key] = stack.enter_context(nc.semaphore(key))
                self.dma_uses[key] = 0
                lst.append(key)
            self.dma_sems[q] = lst
            self.dma_rr[q] = 0
        self.psum_names = set()

    def sb(self, name, shape, dtype, stack=None):
        return (stack or self.stack).enter_context(self.nc.sbuf_tensor(name, list(shape), dtype))

    def ps(self, name, shape, dtype):
        self.psum_names.add(name)
        return self.stack.enter_context(self.nc.psum_tensor(name, list(shape), dtype))

    @staticmethod
    def _key(x):
        if isinstance(x, (str, tuple)):
            return x
        t = getattr(x, 'tensor', x)
        n = getattr(t, 'name', None)
        if n is None:
            raise ValueError('cannot derive resource key from %r' % (x,))
        return n

    def _deps(self, reads, writes):
        deps = []
        for r in reads:
            st = self.res.get(self._key(r))
            if st and st['w']:
                deps.append(st['w'])
        for w in writes:
            st = self.res.get(self._key(w))
            if st:
                if st['w']:
                    deps.append(st['w'])
                deps.extend(st['r'])
        return deps

    def _commit(self, reads, writes, tok):
        for r in reads:
            st = self.res.setdefault(self._key(r), {'w': None, 'r': []})
            st['r'].append(tok)
        for w in writes:
            self.res[self._key(w)] = {'w': tok, 'r': []}

    def _filter_waits(self, eng, deps, skip_self=False):
        out = {}
        for (sk, val) in deps:
            if skip_self and sk == eng:
                continue
            if self.waited[eng].get(sk, 0) >= val:
                continue
            if out.get(sk, 0) < val:
                out[sk] = val
        for sk, val in out.items():
            self.waited[eng][sk] = val
        return list(out.items())

    def op(self, eng, fn, reads=(), writes=()):
        pr = [r for r in reads if self._key(r) in self.psum_names]
        if pr:
            reads = [r for r in reads if self._key(r) not in self.psum_names]
            writes = list(writes) + pr
        deps = self._deps(reads, writes)
        waits = self._filter_waits(eng, deps, skip_self=(eng == 'pe'))
        self.cnt[eng] += 1
        tok = (eng, self.cnt[eng])
        self._commit(reads, writes, tok)
        self.ops[eng].append((waits, fn, (eng, 1)))
        return tok

    def dma(self, q, fn, reads=(), writes=()):
        deps = self._deps(reads, writes)
        lst = self.dma_sems[q]
        sk = lst[self.dma_rr[q] % len(lst)]
        self.dma_rr[q] += 1
        if self.dma_uses[sk] > 0:
            deps.append((sk, 16 * self.dma_uses[sk]))
        waits = self._filter_waits(q, deps)
        self.dma_uses[sk] += 1
        tok = (sk, 16 * self.dma_uses[sk])
        self._commit(reads, writes, tok)
        self.ops[q].append((waits, fn, (sk, 16)))
        return tok

    def barrier(self):
        deps = [(k, self.cnt[k]) for k in ('act', 'dve', 'pool', 'pe') if self.cnt[k] > 0]
        deps += [(sk, 16 * n) for sk, n in self.dma_uses.items() if n > 0]
        for eng in self.ENG:
            waits = self._filter_waits(eng, [d for d in deps if d[0] != eng])
            if waits:
                self.ops[eng].append((waits, None, None))
        self.res = {}

    def finish(self):
        deps = [(sk, 16 * n) for sk, n in self.dma_uses.items() if n > 0]
        waits = self._filter_waits('sp', deps)
        self.ops['sp'].append((waits, None, None))

    def emit(self):
        nc = self.nc
        allsems = list(self.sems.values())
        with nc.Block() as b0:
            def clr(e):
                for s in allsems:
                    e.sem_clear(s)
            b0.sync(clr)
        with nc.Block() as block:
            for name, meth in (('sp', block.sync), ('act', block.scalar), ('dve', block.vector),
                               ('pool', block.gpsimd), ('pe', block.tensor)):
                ops = self.ops[name]
                if not ops:
                    continue

                def body(e, ops=ops):
                    for waits, fn, inc in ops:
                        for (sk, val) in waits:
                            e.wait_ge(self.sems[sk], val)
                        if fn is None:
                            continue
                        ins = fn(e)
                        if inc is not None:
                            ins.then_inc(self.sems[inc[0]], inc[1])
                meth(body)


def host_consts():
    c = {}
    c['ident'] = np.eye(128, dtype=np.float32)
    s = np.arange(128)
    c['trilT'] = (s[:, None] <= s[None, :]).astype(np.float32)
    c['negmask'] = np.where(s[None, :] <= s[:, None], 0.0, NEG).astype(np.float32)
    bt = np.arange(64)
    c['blk64'] = ((bt[:, None] // 4 == bt[None, :] // 4) & (bt[:, None] % 4 <= bt[None, :] % 4)).astype(np.float32)
    c['ones'] = np.ones((128, 128), dtype=np.float32)
    c['iota'] = np.tile(np.arange(128, dtype=np.float32)[None, :], (128, 1))
    c['pow2'] = np.tile((2.0 ** -(np.arange(48, dtype=np.float64) + 1)).astype(np.float32)[None, :], (128, 1))
    t4 = np.arange(4)
    c['negm4'] = np.where(t4[:, None] <= t4[None, :], 0.0, NEG).astype(np.float32)
    return c


def build(cfg):
    n_pool = cfg.get('n_pool', 10240)
    do = cfg.get('phases', ('P1', 'P2', 'P3', 'P4'))
    dbg = cfg.get('dbg', False)
    nc = bass.Bass("TRN2", target_bir_lowering=False)

    def din(name, shape, dt=F32):
        return nc.dram_tensor(name, list(shape), dt, kind="ExternalInput").ap()

    def dout(name, shape, dt=F32):
        return nc.dram_tensor(name, list(shape), dt, kind="ExternalOutput").ap()

    xp_d = din('xp', [SEQ, D])
    xs_d = din('xs', [NS, D])
    memp_d = din('memp', [256, D])
    w_in_d = din('w_in', [D, P_IN])
    w_out_d = din('w_out', [D, D])
    g_mix_d = din('g_mix', [D]); g_mem_d = din('g_mem', [D]); g_memn_d = din('g_memn', [D])
    g_ffn_d = din('g_ffn', [D]); g_fin_d = din('g_fin', [D])
    ln_g_d = din('ln_g', [512]); ln_b_d = din('ln_b', [512])
    gm_ws_d = din('gm_ws', [4, 128, 128]); gm_bs_d = din('gm_bs', [4, 128])
    wq_d = din('mem_wq', [D, 512]); wkv_d = din('mem_wkv', [D, D]); wo_d = din('mem_wo', [512, D])
    c_ident = din('c_ident', [128, 128]); c_trilT = din('c_trilT', [128, 128]); c_negmask = din('c_negmask', [128, 128])
    c_blk64 = din('c_blk64', [64, 64]); c_ones = din('c_ones', [128, 128]); c_iota = din('c_iota', [128, 128])
    pwq_d = din('peer_wq', [D, D]); pkeys_d = din('peer_keys', [16, 128, 64])
    pu_d = din('peer_u', [NEXP, D]); pv_d = din('peer_v', [NEXP, D])
    ckidx_d = din('cache_kidx', [n_pool, 8192]); ck_d = din('cache_k', [2 * n_pool, 8192]); cv_d = din('cache_v', [2 * n_pool, 8192])
    pt_d = din('page_table', [DEC_B, NPAGES], I32)
    cmk_d = din('cache_mem_k', [DEC_B, 256, 512]); cmv_d = din('cache_mem_v', [DEC_B, 256, 512])
    c_pow2 = din('c_pow2', [128, 48]); c_negm4 = din('c_negm4', [4, 4])

    y_p = dout('y_p', [SEQ, D]); y_s = dout('y_s', [NS, D])
    k_p = dout('k_p', [SEQ, 128]); v_p = dout('v_p', [SEQ, 128]); ki_p = dout('ki_p', [SEQ, 64])
    gmv_p = dout('gmv_p', [128, 512])
    memk_p = dout('memk_p', [256, 512]); memv_p = dout('memv_p', [256, 512])
    k_s = dout('k_s', [NS, 128]); v_s = dout('v_s', [NS, 128]); ki_s = dout('ki_s', [NS, 64]); gmv_s = dout('gmv_s', [NS, 512])
    if dbg:
        x2_dbg = dout('x2_dbg', [SEQ + NS, D])
    x2s = nc.dram_tensor('x2s', [SEQ + NS, D], F32, kind="Internal").ap()
    UTs = nc.dram_tensor('UTs', [128, 128, D], BF16, kind="Internal").ap()
    Vs = nc.dram_tensor('Vs', [128, 128, D], BF16, kind="Internal").ap()

    with ExitStack() as st:
        p = Prog(nc, st)
        pb = [p.ps('pb%d' % i, [128, 512], F32) for i in range(8)]
        rr = [0]
        nbmod = [6]
        nbbase = [0]

        def nb():
            b = pb[nbbase[0] + rr[0] % nbmod[0]]
            rr[0] += 1
            return b

        def mm(out, lhsT, rhs, start, stop, R, W):
            p.op('pe', lambda e: e.matmul(out, lhsT=lhsT, rhs=rhs, start=start, stop=stop), reads=R, writes=W)

        identf = p.sb('identf', [128, 128], F32)
        ident = p.sb('ident', [128, 128], BF16)
        trilT = p.sb('trilT', [128, 128], F32)
        negmask = p.sb('negmask', [128, 128], F32)
        ones_bf = p.sb('ones_bf', [128, 128], BF16)
        onesf = p.sb('onesf', [128, 128], F32)
        p.dma('sp', lambda e: e.dma_start(out=identf[:], in_=c_ident[:, :]), writes=[identf])
        p.dma('sp', lambda e: e.dma_start(out=trilT[:], in_=c_trilT[:, :]), writes=[trilT])
        p.dma('sp', lambda e: e.dma_start(out=negmask[:], in_=c_negmask[:, :]), writes=[negmask])
        p.dma('sp', lambda e: e.dma_start(out=onesf[:], in_=c_ones[:, :]), writes=[onesf])
        p.op('dve', lambda e: e.tensor_copy(out=ident[:], in_=identf[:]), reads=[identf], writes=[ident])
        p.op('dve', lambda e: e.tensor_copy(out=ones_bf[:], in_=onesf[:]), reads=[onesf], writes=[ones_bf])

        def tr(out, in_, K, R, W):
            p.op('pe', lambda e: e.transpose(out=out, in_=in_, identity=ident[0:K, 0:K]), reads=list(R) + [ident], writes=W)

        gcols = {}

        def load_gcol(name, g_d):
            t = p.sb('gc_' + name, [128, 8], F32)
            p.dma('sp', lambda e: e.dma_start(out=t[:], in_=g_d.rearrange("(c q) -> q c", q=128), allow_slow_non_contiguous=True), writes=[t])
            gcols[name] = t
            return t

        def load_weight(dst, w_d, nk, ncol, gcol, stage, eng_rot=[0]):
            for c in range(nk):
                stg = stage[c % len(stage)]
                p.dma('sp', lambda e, c=c, stg=stg: e.dma_start(out=stg[:, 0:ncol], in_=w_d[c * 128:(c + 1) * 128, :]), writes=[stg])
                eng = ('dve', 'pool')[eng_rot[0] % 2]
                eng_rot[0] += 1
                if gcol is not None:
                    p.op(eng, lambda e, c=c, stg=stg: e.tensor_scalar(out=dst[:, c, :], in0=stg[:, 0:ncol], scalar1=gcol[:, c:c + 1],
                                                                     scalar2=None, op0=ALU.mult), reads=[stg, gcol], writes=[dst])
                else:
                    p.op(eng, lambda e, c=c, stg=stg: e.tensor_copy(out=dst[:, c, :], in_=stg[:, 0:ncol]), reads=[stg], writes=[dst])

        def rmsnorm_rstd(x_t, T, rstd, scratch):
            ss = rstd['ss']
            p.op('act', lambda e: e.activation(out=scratch[0:T, :], in_=x_t[0:T, :], func=AF.Square, accum_out=ss[0:T, 0:1]),
                 reads=[x_t], writes=[scratch, ss])
            p.op('dve', lambda e: e.tensor_scalar(out=ss[0:T, 1:2], in0=ss[0:T, 0:1], scalar1=1.0 / D, scalar2=EPS, op0=ALU.mult, op1=ALU.add),
                 reads=[ss], writes=[ss])
            p.op('act', lambda e: e.activation(out=ss[0:T, 2:3], in_=ss[0:T, 1:2], func=AF.Sqrt), reads=[ss], writes=[ss])
            p.op('dve', lambda e: e.reciprocal(out=ss[0:T, 3:4], in_=ss[0:T, 2:3]), reads=[ss], writes=[ss])
            return ss[0:T, 3:4]

        def norm_T(x_t, T, tag, xn, xnT, ssd, scratch, gcol=None):
            rs = rmsnorm_rstd(x_t, T, ssd, scratch)
            p.op('act', lambda e: e.activation(out=xn[0:T, :], in_=x_t[0:T, :], func=AF.Copy, scale=rs), reads=[x_t, ssd['ss']], writes=[xn])
            bk = nb()
            bv = bk[:].bitcast(BF16)
            for c in range(8):
                tr(bv[:, c * T:(c + 1) * T], xn[0:T, c * 128:(c + 1) * 128], T, [xn], [bk])
            if gcol is None:
                p.op('dve', lambda e: e.tensor_copy(out=xnT[:, :, 0:T], in_=bv[:, 0:8 * T].rearrange("q (c t) -> q c t", c=8)),
                     reads=[bk], writes=[xnT])
            else:
                for c in range(8):
                    p.op('dve', lambda e, c=c: e.tensor_scalar(out=xnT[:, c, 0:T], in0=bv[:, c * T:(c + 1) * T], scalar1=gcol[:, c:c + 1], scalar2=None, op0=ALU.mult),
                         reads=[bk, gcol], writes=[xnT])

        ssd = {'ss': p.sb('ss', [128, 4], F32)}

        for nm, gd in (('mix', g_mix_d), ('mem', g_mem_d), ('memn', g_memn_d), ('ffn', g_ffn_d)):
            load_gcol(nm, gd)
        conv_done = [0]

        def convert_chunk(ec, u_, t_, v_):
            p.dma('pool', lambda e: e.dma_start(out=u_[:, :], in_=pu_d[ec * 128:(ec + 1) * 128, :]), writes=[u_])
            p.dma('pool', lambda e: e.dma_start(out=v_[:, :], in_=pv_d[ec * 128:(ec + 1) * 128, :]), writes=[v_])
            bk = nb()
            bv = bk[:].bitcast(BF16)
            for c in range(8):
                tr(bv[:, c * 128:(c + 1) * 128], u_[:, c * 128:(c + 1) * 128], 128, [u_], [bk])
            if ec % 2 == 0:
                p.op('act', lambda e: e.copy(out=t_[:, :], in_=bv[:, :]), reads=[bk], writes=[t_])
            else:
                p.op('pool', lambda e: e.tensor_copy(out=t_[:, :], in_=bv[:, :]), reads=[bk], writes=[t_]) if False else \
                    p.op('act', lambda e: e.copy(out=t_[:, :], in_=bv[:, :]), reads=[bk], writes=[t_])
            p.dma('sp', lambda e: e.dma_start(out=UTs[ec, :, :], in_=t_[:, :]), reads=[t_], writes=[('UTs', ec)])
            p.dma('sp', lambda e: e.dma_start(out=Vs[ec, :, :], in_=v_[:, :]), reads=[v_], writes=[('Vs', ec)])

        with ExitStack() as s2:
            w_out = p.sb('w_out_sb', [128, 8, D], BF16, s2)
            wq = p.sb('wq_sb', [128, 8, 512], BF16, s2)
            wo = p.sb('wo_sb', [128, 4, D], BF16, s2)
            sq = p.sb('sq_scratch', [128, D], BF16, s2)
            xn = p.sb('xn', [128, D], BF16, s2)
            xnT = p.sb('xnT', [128, 8, 128], BF16, s2)
            W4T = p.sb('W4T', [64, 4, 64], BF16, s2)
            bsT4 = p.sb('bsT4', [64, 4], F32, s2)
            tau_c = p.sb('tau_c', [128, 1], F32, s2)
            p.op('pool', lambda e: e.memset(tau_c[:], -1.0e29), writes=[tau_c])

            bnst = p.sb('bnst', [128, 8], F32, s2)
            wsc = p.sb('wsc', [128, 8], F32, s2)
            ycat = p.sb('ycat', [128, D], BF16, s2)
            yT = p.sb('yT', [128, 8, 128], BF16, s2)
            x1 = p.sb('x1', [128, D], F32, s2)
            rden = p.sb('rden', [128, 8], F32, s2)
            qmT = p.sb('qmT', [128, 4, 128], BF16, s2)
            PmT = p.sb('PmT', [128, 2, 4, 128], BF16, s2)
            om = p.sb('om', [128, 512], BF16, s2)
            omT = p.sb('omT', [128, 4, 128], BF16, s2)
            x0s = p.sb('x0s', [64, D], F32, s2)
            ycats = p.sb('ycats', [64, D], BF16, s2)
            kvf_s = p.sb('kvf_s', [64, 328], F32, s2)
            kb_s = p.sb('kb_s', [64, 192], BF16, s2)
            wsc_s = p.sb('wsc_s', [64, 8], F32, s2)
            qTs = p.sb('qTs', [64, 8, 64], BF16, s2)
            qiTs = p.sb('qiTs', [64, 8, 64], BF16, s2)
            sw = ExitStack()
            x0 = p.sb('x0', [128, D], F32, sw)
            mkT = p.sb('mkT', [128, 4, 256], BF16, sw)
            mv_aug = p.sb('mv_aug', [128, 2, 4, 129], BF16, sw)
            WgT = p.sb('WgT', [128, 4, 128], BF16, sw)
            bsT = p.sb('bsT', [128, 4], F32, sw)
            lng = p.sb('lng', [128, 512], F32, sw)
            lnb = p.sb('lnb', [128, 512], F32, sw)
            kvf = p.sb('kvf', [128, 328], F32, sw)
            kb = p.sb('kb', [128, 192], BF16, sw)
            qT = p.sb('qT', [64, 8, 128], BF16, sw)
            qiT = p.sb('qiT', [64, 8, 128], BF16, sw)
            p.dma('sp', lambda e: e.dma_start(out=lng[:], in_=ln_g_d.partition_broadcast(128)), writes=[lng])
            p.dma('sp', lambda e: e.dma_start(out=lnb[:], in_=ln_b_d.partition_broadcast(128)), writes=[lnb])
            ub = p.sb('ub', [128, 512], F32, sw)
            gv = p.sb('gv', [128, 512], F32, sw)
            vn = p.sb('vn', [128, 512], F32, sw)
            vnb = p.sb('vnb', [128, 512], BF16, sw)
            zq = p.sb('zq', [128, 1024], BF16, sw)
            w_in = p.sb('w_in_sb', [128, 8, P_IN], BF16, sw)

            with ExitStack() as s1:
                stage = [p.sb('wstage%d' % i, [128, P_IN], F32, s1) for i in range(2)]
                load_weight(w_in, w_in_d, 8, P_IN, gcols['mix'], stage)
                load_weight(w_out, w_out_d, 8, D, None, stage)
                load_weight(wq, wq_d, 8, 512, gcols['mem'], stage)
                load_weight(wo, wo_d, 4, D, None, stage)
                wsb = p.sb('wsb', [128, 128], BF16, s1)
                for g in range(4):
                    stg = stage[g % 2]
                    p.dma('sp', lambda e, g=g, stg=stg: e.dma_start(out=stg[:, 0:128], in_=gm_ws_d[g, :, :]), writes=[stg])
                    p.op('dve', lambda e, stg=stg: e.tensor_copy(out=wsb[:], in_=stg[:, 0:128]), reads=[stg], writes=[wsb])
                    bk = nb()
                    bv = bk[:].bitcast(BF16)
                    tr(bv[:, 0:128], wsb[:, :], 128, [wsb], [bk])
                    p.op('dve', lambda e, g=g, bv=bv: e.tensor_tensor(out=WgT[:, g, :], in0=bv[:, 0:128], in1=trilT[:, :], op=ALU.mult),
                         reads=[bk, trilT], writes=[WgT])
                p.dma('sp', lambda e: e.dma_start(out=bsT[:], in_=gm_bs_d.rearrange("g t -> t g"), allow_slow_non_contiguous=True), writes=[bsT])
                w4f = p.sb('w4f', [64, 4, 64], F32, s1)
                blk64 = p.sb('blk64', [64, 64], F32, s1)
                p.dma('sp', lambda e: e.dma_start(out=blk64[:], in_=c_blk64[:, :]), writes=[blk64])
                p.op('pool', lambda e: e.memset(w4f[:], 0.0), writes=[w4f])
                for g in range(4):
                    for b in range(DEC_B):
                        p.dma('sp', lambda e, g=g, b=b: e.dma_start(
                            out=w4f[4 * b:4 * b + 4, g, 4 * b:4 * b + 4], in_=gm_ws_d[g, 0:4, 0:4].rearrange("t s -> s t"), allow_slow_non_contiguous=True),
                            writes=[w4f])
                    p.op('dve', lambda e, g=g: e.tensor_tensor(out=W4T[:, g, :], in0=w4f[:, g, :], in1=blk64[:, :], op=ALU.mult), reads=[w4f, blk64], writes=[W4T])
                for b in range(DEC_B):
                    p.dma('sp', lambda e, b=b: e.dma_start(out=bsT4[4 * b:4 * b + 4, :], in_=gm_bs_d[:, 0:4].rearrange("g t -> t g"), allow_slow_non_contiguous=True), writes=[bsT4])

                if 'P1' in do:
                    wkv = p.sb('wkv_sb', [128, 8, D], BF16, s1)
                    load_weight(wkv, wkv_d, 8, D, gcols['memn'], stage)
                    mkvf = p.sb('mkvf', [128, D], F32, s1)
                    mkb = p.sb('mkb', [128, 512], BF16, s1)
                    p.op('pool', lambda e: e.memset(mv_aug[:], 1.0), writes=[mv_aug])
                    for mt in range(2):
                        p.dma('sp', lambda e, mt=mt: e.dma_start(out=x0[:], in_=memp_d[mt * 128:(mt + 1) * 128, :]), writes=[x0])
                        norm_T(x0, 128, 'm', xn, xnT, ssd, sq)
                        b0, b1 = nb(), nb()
                        for half, bk in enumerate((b0, b1)):
                            for c in range(8):
                                mm(bk[:, :], xnT[:, c, :], wkv[:, c, half * 512:(half + 1) * 512], c == 0, c == 7, [xnT, wkv], [bk])
                        p.op('act', lambda e, b0=b0: e.copy(out=mkvf[:, 0:512], in_=b0[:, :]), reads=[b0], writes=[mkvf])
                        p.op('dve', lambda e, b1=b1: e.tensor_copy(out=mkvf[:, 512:1024], in_=b1[:, :]), reads=[b1], writes=[mkvf])
                        p.dma('sp', lambda e, mt=mt: e.dma_start(out=memk_p[mt * 128:(mt + 1) * 128, :], in_=mkvf[:, 0:512]), reads=[mkvf], writes=['memk_p'])
                        p.dma('sp', lambda e, mt=mt: e.dma_start(out=memv_p[mt * 128:(mt + 1) * 128, :], in_=mkvf[:, 512:1024]), reads=[mkvf], writes=['memv_p'])
                        p.op('dve', lambda e: e.tensor_copy(out=mkb[:], in_=mkvf[:, 0:512]), reads=[mkvf], writes=[mkb])
                        p.op('pool', lambda e, mt=mt: e.tensor_copy(out=mv_aug[:, mt, :, 0:128], in_=mkvf[:, 512:1024].rearrange("q (h d) -> q h d", h=4)),
                             reads=[mkvf], writes=[mv_aug])
                        bk = nb()
                        bv = bk[:].bitcast(BF16)
                        for h in range(4):
                            tr(bv[:, h * 128:(h + 1) * 128], mkb[:, h * 128:(h + 1) * 128], 128, [mkb], [bk])
                        p.op('act', lambda e, mt=mt, bv=bv: e.copy(out=mkT[:, :, mt * 128:(mt + 1) * 128], in_=bv[:, 0:512].rearrange("q (h m) -> q h m", h=4)),
                             reads=[bk], writes=[mkT])
            p.barrier()

            with ExitStack() as s3:
                kT = [p.sb('kT%d' % g, [64, SEQ], BF16, s3) for g in range(2)]
                kiT = p.sb('kiT', [64, SEQ], BF16, s3)
                v_aug = p.sb('v_aug', [128, NT, 2, 65], BF16, s3)
                sc = p.sb('sc', [128, SEQ], F32, s3)
                bis = p.sb('bis', [128, 8], F32, s3)
                Wtp = p.sb('Wtp', [128, 48], F32, s3)
                midp = p.sb('midp', [128, 1], F32, s3)
                pow2p = p.sb('pow2p', [128, 48], F32, s3)
                p.dma('sp', lambda e: e.dma_start(out=pow2p[:, :], in_=c_pow2[:, :]), writes=[pow2p])
                rbuf = [p.sb('rbuf%d' % i, [128, 512], F32, s3) for i in range(2)]
                m8 = p.sb('m8', [128, 8], F32, s3)
                maskb = p.sb('maskb', [128, SEQ], BF16, s3)
                maskT = p.sb('maskT', [128, NT, 128], BF16, s3)
                Eb = [p.sb('Eb%d' % i, [128, 512], BF16, s3) for i in range(3)]
                PTb = [p.sb('PTb%d' % i, [128, 4, 128], BF16, s3) for i in range(3)]
                p.op('pool', lambda e: e.memset(v_aug[:], 1.0), writes=[v_aug])
                if 'P4' in do and cfg.get('interleave_conv', True):
                    cub = [p.sb('cub%d' % i, [128, D], BF16, s3) for i in range(2)]
                    cut = [p.sb('cut%d' % i, [128, D], BF16, s3) for i in range(2)]
                    cvb = [p.sb('cvb%d' % i, [128, D], BF16, s3) for i in range(2)]

                def proj_tile(src_ap, T, ti, is_sample, x0b, ycatb):
                    p.dma('sp', lambda e: e.dma_start(out=x0b[0:T, :], in_=src_ap), writes=[x0b])
                    norm_T(x0b, T, 'a', xn, xnT, ssd, sq)
                    banks = [nb() for _ in range(5)]
                    for n5, bk in enumerate(banks):
                        c0 = n5 * 512
                        w = min(512, P_IN - c0)
                        for c in range(8):
                            mm(bk[0:T, 0:w], xnT[:, c, 0:T], w_in[:, c, c0:c0 + w], c == 0, c == 7, [xnT, w_in], [bk])
                    p.op('act', lambda e: e.activation(out=ub[0:T, :], in_=banks[0][0:T, :], func=AF.Gelu_apprx_tanh), reads=[banks[0]], writes=[ub])
                    p.op('act', lambda e: e.activation(out=gv[0:T, :], in_=banks[1][0:T, :], func=AF.Gelu_apprx_tanh), reads=[banks[1]], writes=[gv])
                    p.op('dve', lambda e: e.tensor_copy(out=zq[0:T, 0:512], in_=banks[2][0:T, :]), reads=[banks[2]], writes=[zq])
                    p.op('dve', lambda e: e.tensor_copy(out=kvf[0:T, 0:256], in_=banks[3][0:T, 0:256]), reads=[banks[3]], writes=[kvf])
                    p.op('dve', lambda e: e.tensor_copy(out=zq[0:T, 512:768], in_=banks[3][0:T, 256:512]), reads=[banks[3]], writes=[zq])
                    p.op('act', lambda e: e.copy(out=zq[0:T, 768:1024], in_=banks[4][0:T, 0:256]), reads=[banks[4]], writes=[zq])
                    p.op('act', lambda e: e.copy(out=kvf[0:T, 256:328], in_=banks[4][0:T, 256:328]), reads=[banks[4]], writes=[kvf])
                    r0 = ti * 128
                    ko, vo, kio = (k_s, v_s, ki_s) if is_sample else (k_p, v_p, ki_p)
                    p.dma('sp', lambda e: e.dma_start(out=ko[r0:r0 + T, :], in_=kvf[0:T, 0:128]), reads=[kvf], writes=['ko'])
                    p.dma('sp', lambda e: e.dma_start(out=vo[r0:r0 + T, :], in_=kvf[0:T, 128:256]), reads=[kvf], writes=['vo'])
                    p.dma('sp', lambda e: e.dma_start(out=kio[r0:r0 + T, :], in_=kvf[0:T, 256:320]), reads=[kvf], writes=['kio'])
                    p.op('dve', lambda e: e.bn_stats(out=bnst[0:T, 0:6], in_=gv[0:T, :]), reads=[gv], writes=[bnst])
                    p.op('dve', lambda e: e.bn_aggr(out=bnst[0:T, 6:8], in_=bnst[0:T, 0:6]), reads=[bnst], writes=[bnst])
                    p.op('dve', lambda e: e.tensor_scalar(out=bnst[0:T, 0:1], in0=bnst[0:T, 7:8], scalar1=EPS, scalar2=None, op0=ALU.add), reads=[bnst], writes=[bnst])
                    p.op('act', lambda e: e.activation(out=bnst[0:T, 1:2], in_=bnst[0:T, 0:1], func=AF.Sqrt), reads=[bnst], writes=[bnst])
                    p.op('dve', lambda e: e.reciprocal(out=bnst[0:T, 2:3], in_=bnst[0:T, 1:2]), reads=[bnst], writes=[bnst])
                    p.op('dve', lambda e: e.tensor_scalar(out=vn[0:T, :], in0=gv[0:T, :], scalar1=bnst[0:T, 6:7], scalar2=bnst[0:T, 2:3],
                                                          op0=ALU.subtract, op1=ALU.mult), reads=[gv, bnst], writes=[vn])
                    p.op('dve', lambda e: e.tensor_tensor(out=vn[0:T, :], in0=vn[0:T, :], in1=lng[0:T, :], op=ALU.mult), reads=[vn, lng], writes=[vn])
                    p.op('dve', lambda e: e.tensor_tensor(out=vn[0:T, :], in0=vn[0:T, :], in1=lnb[0:T, :], op=ALU.add), reads=[vn, lnb], writes=[vn])
                    if is_sample:
                        p.dma('sp', lambda e: e.dma_start(out=gmv_s[0:T, :], in_=vn[0:T, :]), reads=[vn], writes=['gmv_s'])
                    elif ti == NT - 1:
                        p.dma('sp', lambda e: e.dma_start(out=gmv_p[:, :], in_=vn[0:T, :]), reads=[vn], writes=['gmv_p'])
                    p.op('pool', lambda e: e.tensor_copy(out=vnb[0:T, :], in_=vn[0:T, :]), reads=[vn], writes=[vnb])
                    bk = nb()
                    Wm, bsm = (W4T, bsT4) if is_sample else (WgT, bsT)
                    for g in range(4):
                        mm(bk[0:T, g * 128:(g + 1) * 128], Wm[0:T, g, 0:T], vnb[0:T, g * 128:(g + 1) * 128], True, True, [Wm, vnb], [bk])
                    for g in range(4):
                        p.op('dve', lambda e, g=g, bk=bk: e.scalar_tensor_tensor(out=ycatb[0:T, g * 128:(g + 1) * 128], in0=bk[0:T, g * 128:(g + 1) * 128],
                                                                                  scalar=bsm[0:T, g:g + 1], in1=ub[0:T, g * 128:(g + 1) * 128],
                                                                                  op0=ALU.add, op1=ALU.mult), reads=[bk, bsm, ub], writes=[ycatb])

                def feature_major(T, ti, qTb, qiTb):
                    p.op('pool', lambda e: e.tensor_copy(out=kb[0:T, 0:128], in_=kvf[0:T, 0:128]), reads=[kvf], writes=[kb])
                    p.op('pool', lambda e: e.tensor_copy(out=kb[0:T, 128:192], in_=kvf[0:T, 256:320]), reads=[kvf], writes=[kb])
                    p.op('pool', lambda e: e.tensor_scalar(out=wsc[0:T, :], in0=kvf[0:T, 320:328], scalar1=8.0 ** -0.5, scalar2=None, op0=ALU.mult),
                         reads=[kvf], writes=[wsc])
                    for (src, off, dst) in ((zq, 0, qTb), (zq, 512, qiTb)):
                        bk = nb()
                        bv = bk[:].bitcast(BF16)
                        for h in range(8):
                            tr(bv[0:64, h * T:(h + 1) * T], src[0:T, off + h * 64: off + (h + 1) * 64], T, [src], [bk])
                        p.op('act', lambda e, bv=bv, dst=dst: e.copy(out=dst[:, :, 0:T], in_=bv[0:64, 0:8 * T].rearrange("q (h t) -> q h t", h=8)),
                             reads=[bk], writes=[dst])

                def prompt_dsa(ti):
                    T = 128
                    L = 128 * (ti + 1)
                    c_lo = ti * 128
                    bk = nb()
                    bv = bk[:].bitcast(BF16)
                    for g in range(3):
                        tr(bv[0:64, g * 128:(g + 1) * 128], kb[:, g * 64:(g + 1) * 64], 128, [kb], [bk])
                    p.op('act', lambda e, bv=bv: e.copy(out=kT[0][:, c_lo:c_lo + 128], in_=bv[0:64, 0:128]), reads=[bk], writes=[kT[0]])
                    p.op('act', lambda e, bv=bv: e.copy(out=kT[1][:, c_lo:c_lo + 128], in_=bv[0:64, 128:256]), reads=[bk], writes=[kT[1]])
                    p.op('act', lambda e, bv=bv: e.copy(out=kiT[:, c_lo:c_lo + 128], in_=bv[0:64, 256:384]), reads=[bk], writes=[kiT])
                    p.op('pool', lambda e: e.tensor_copy(out=v_aug[:, ti, :, 0:64], in_=kvf[:, 128:256].rearrange("q (g d) -> q g d", g=2)),
                         reads=[kvf], writes=[v_aug])
                    ri = 0
                    for c0 in range(0, L, 512):
                        w = min(512, L - c0)
                        for h in range(8):
                            bk = nb()
                            mm(bk[:, 0:w], qiT[:, h, :], kiT[:, c0:c0 + w], True, True, [qiT, kiT], [bk])
                            rb = rbuf[ri % 2]
                            ri += 1
                            p.op('act', lambda e, bk=bk, rb=rb, w=w: e.activation(out=rb[:, 0:w], in_=bk[:, 0:w], func=AF.Relu), reads=[bk], writes=[rb])
                            if h == 0:
                                p.op('dve', lambda e, rb=rb, w=w, c0=c0: e.tensor_scalar(out=sc[:, c0:c0 + w], in0=rb[:, 0:w], scalar1=wsc[:, 0:1], scalar2=None, op0=ALU.mult),
                                     reads=[rb, wsc], writes=[sc])
                            else:
                                p.op('dve', lambda e, rb=rb, w=w, c0=c0, h=h: e.scalar_tensor_tensor(out=sc[:, c0:c0 + w], in0=rb[:, 0:w], scalar=wsc[:, h:h + 1],
                                                                                                     in1=sc[:, c0:c0 + w], op0=ALU.mult, op1=ALU.add),
                                     reads=[rb, wsc, sc], writes=[sc])
                    if ti >= 2:
                        p.op('act', lambda e: e.activation(out=maskb[:, 0:L], in_=sc[:, 0:L], func=AF.Square, accum_out=bis[:, 0:1]), reads=[sc], writes=[maskb, bis])
                    p.op('dve', lambda e: e.tensor_tensor(out=sc[:, c_lo:c_lo + 128], in0=sc[:, c_lo:c_lo + 128], in1=negmask[:, :], op=ALU.add),
                         reads=[sc, negmask], writes=[sc])
                    if ti >= 2:
                        NITP = cfg.get('nit_p', 20)
                        p.op('act', lambda e: e.activation(out=bis[:, 1:2], in_=bis[:, 0:1], func=AF.Sqrt), reads=[bis], writes=[bis])
                        p.op('dve', lambda e: e.tensor_scalar(out=bis[:, 2:3], in0=bis[:, 1:2], scalar1=2.2, scalar2=2.0, op0=ALU.mult, op1=ALU.add), reads=[bis], writes=[bis])
                        p.op('dve', lambda e: e.tensor_scalar(out=Wtp[:, :], in0=pow2p[:, :], scalar1=bis[:, 2:3], scalar2=None, op0=ALU.mult), reads=[pow2p, bis], writes=[Wtp])
                        p.op('dve', lambda e: e.memset(midp[:, :], 0.0), writes=[midp])
                        for k in range(NITP):
                            p.op('dve', lambda e: e.tensor_scalar(out=maskb[:, 0:L], in0=sc[:, 0:L], scalar1=midp[:, 0:1], scalar2=None, op0=ALU.is_ge, op1=ALU.add,
                                                                  accum_out=bis[:, 3:4]), reads=[sc, midp], writes=[maskb, bis])
                            p.op('dve', lambda e: e.tensor_scalar(out=bis[:, 4:5], in0=bis[:, 3:4], scalar1=255.5, scalar2=0.5, op0=ALU.is_ge, op1=ALU.subtract), reads=[bis], writes=[bis])
                            p.op('dve', lambda e, k=k: e.scalar_tensor_tensor(out=midp[:, :], in0=bis[:, 4:5], scalar=Wtp[:, k:k + 1], in1=midp[:, :], op0=ALU.mult, op1=ALU.add),
                                 reads=[bis, Wtp, midp], writes=[midp])
                        p.op('dve', lambda e: e.tensor_tensor(out=bis[:, 5:6], in0=midp[:, :], in1=Wtp[:, NITP:NITP + 1], op=ALU.subtract), reads=[midp, Wtp], writes=[bis])
                        tau = bis[:, 5:6]
                        tau_t = bis
                    else:
                        tau = tau_c[:, 0:1]
                        tau_t = tau_c
                    p.op('dve', lambda e: e.tensor_scalar(out=maskb[:, 0:L], in0=sc[:, 0:L], scalar1=tau, scalar2=None, op0=ALU.is_ge),
                         reads=[sc, tau_t], writes=[maskb])
                    for j0 in range(0, ti + 1, 8):
                        nj = min(8, ti + 1 - j0)
                        bk = nb()
                        bv = bk[:].bitcast(BF16)
                        for jj in range(nj):
                            tr(bv[:, jj * 128:(jj + 1) * 128], maskb[:, (j0 + jj) * 128:(j0 + jj + 1) * 128], 128, [maskb], [bk])
                        p.op('act', lambda e, bv=bv, j0=j0, nj=nj: e.copy(out=maskT[:, j0:j0 + nj, :], in_=bv[:, 0:nj * 128].rearrange("q (j t) -> q j t", j=nj)),
                             reads=[bk], writes=[maskT])
                    seq = [(g, j) for g in range(2) for j in range(ti + 1)]
                    PTl = {}

                    def att_scores(idx):
                        g, j = seq[idx]
                        bk = nb()
                        mm(bk[:, :], kT[g][:, j * 128:(j + 1) * 128], qT[:, 4 * g:4 * g + 4, :].rearrange("q h t -> q (h t)"), True, True, [kT[g], qT], [bk])
                        E = Eb[idx % 3]
                        PT = PTb[idx % 3]
                        p.op('act', lambda e: e.activation(out=E[:, :], in_=bk[:, :], func=AF.Exp, scale=0.125), reads=[bk], writes=[E])
                        p.op('pool', lambda e: e.tensor_tensor(out=PT[:, :, :], in0=E[:, :].rearrange("q (h t) -> q h t", h=4),
                                                               in1=maskT[:, j:j + 1, :].to_broadcast([128, 4, 128]), op=ALU.mult),
                             reads=[E, maskT], writes=[PT])
                        PTl[idx] = PT

                    def att_pv(idx):
                        g, j = seq[idx]
                        ob = pb[6 + g]
                        PT = PTl[idx]
                        for hh in range(4):
                            mm(ob[:, hh * 65:(hh + 1) * 65], PT[:, hh, :], v_aug[:, j, g, :], (j == 0 and hh == 0), (j == ti), [PT, v_aug], [ob])
                        if j == ti:
                            p.op('dve', lambda e: e.reciprocal(out=rden[:, 4 * g:4 * g + 4], in_=ob[:, 0:260].rearrange("q (h d) -> q h d", h=4)[:, :, 64]),
                                 reads=[ob], writes=[rden])
                            p.op('dve', lambda e: e.tensor_tensor(out=ycat[:, 512 + 256 * g:512 + 256 * (g + 1)].rearrange("q (h d) -> q h d", h=4),
                                                                  in0=ob[:, 0:260].rearrange("q (h d) -> q h d", h=4)[:, :, 0:64],
                                                                  in1=rden[:, 4 * g:4 * g + 4].unsqueeze(2).to_broadcast([128, 4, 64]), op=ALU.mult),
                                 reads=[ob, rden], writes=[ycat])
                    for idx in range(len(seq) + 1):
                        if idx < len(seq):
                            att_scores(idx)
                        if idx >= 1:
                            att_pv(idx - 1)

                def out_proj_and_mem(T, row0, is_sample, x0b, ycatb):
                    bk = nb()
                    bv = bk[:].bitcast(BF16)
                    for c in range(8):
                        tr(bv[:, c * T:(c + 1) * T], ycatb[0:T, c * 128:(c + 1) * 128], T, [ycatb], [bk])
                    p.op('act', lambda e, bv=bv: e.copy(out=yT[:, :, 0:T], in_=bv[:, 0:8 * T].rearrange("q (c t) -> q c t", c=8)), reads=[bk], writes=[yT])
                    for half in range(2):
                        bk = nb()
                        for c in range(8):
                            mm(bk[0:T, :], yT[:, c, 0:T], w_out[:, c, half * 512:(half + 1) * 512], c == 0, c == 7, [yT, w_out], [bk])
                        p.op('dve', lambda e, bk=bk, half=half: e.tensor_tensor(out=x1[0:T, half * 512:(half + 1) * 512], in0=bk[0:T, :],
                                                                                in1=x0b[0:T, half * 512:(half + 1) * 512], op=ALU.add),
                             reads=[bk, x0b], writes=[x1])
                    norm_T(x1, T, 'b', xn, xnT, ssd, sq)
                    bk = nb()
                    for h in range(4):
                        for c in range(8):
                            mm(bk[:, h * T:(h + 1) * T], wq[:, c, h * 128:(h + 1) * 128], xnT[:, c, 0:T], c == 0, c == 7, [wq, xnT], [bk])
                    p.op('act', lambda e, bk=bk: e.copy(out=qmT[:, :, 0:T], in_=bk[:, 0:4 * T].rearrange("q (h t) -> q h t", h=4)), reads=[bk], writes=[qmT])
                    if not is_sample:
                        for mt in range(2):
                            bk = nb()
                            for h in range(4):
                                mm(bk[:, h * 128:(h + 1) * 128], mkT[:, h, mt * 128:(mt + 1) * 128], qmT[:, h, :], True, True, [mkT, qmT], [bk])
                            p.op('act', lambda e, bk=bk, mt=mt: e.activation(out=PmT[:, mt, :, :], in_=bk[:, :].rearrange("q (h t) -> q h t", h=4),
                                                                             func=AF.Exp, scale=128.0 ** -0.5), reads=[bk], writes=[PmT])
                        for hp in range(2):
                            ob = pb[6 + hp]
                            for mt in range(2):
                                for hh in range(2):
                                    h = 2 * hp + hh
                                    mm(ob[:, hh * 129:(hh + 1) * 129], PmT[:, mt, h, :], mv_aug[:, mt, h, :], (mt == 0 and hh == 0), (mt == 1), [PmT, mv_aug], [ob])
                            p.op('dve', lambda e, ob=ob, hp=hp: e.reciprocal(out=rden[:, 2 * hp:2 * hp + 2], in_=ob[:, 0:258].rearrange("q (h d) -> q h d", h=2)[:, :, 128]),
                                 reads=[ob], writes=[rden])
                            p.op('dve', lambda e, ob=ob, hp=hp: e.tensor_tensor(out=om[:, 256 * hp:256 * (hp + 1)].rearrange("q (h d) -> q h d", h=2),
                                                                                in0=ob[:, 0:258].rearrange("q (h d) -> q h d", h=2)[:, :, 0:128],
                                                                                in1=rden[:, 2 * hp:2 * hp + 2].unsqueeze(2).to_broadcast([128, 2, 128]), op=ALU.mult),
                                 reads=[ob, rden], writes=[om])
                        bk = nb()
                        bv = bk[:].bitcast(BF16)
                        for h in range(4):
                            tr(bv[:, h * T:(h + 1) * T], om[0:T, h * 128:(h + 1) * 128], T, [om], [bk])
                        p.op('act', lambda e, bv=bv: e.copy(out=omT[:, :, 0:T], in_=bv[:, 0:4 * T].rearrange("q (h t) -> q h t", h=4)), reads=[bk], writes=[omT])
                    else:
                        sample_mem_attn()
                    for half in range(2):
                        bk = nb()
                        for h in range(4):
                            mm(bk[0:T, :], omT[:, h, 0:T], wo[:, h, half * 512:(half + 1) * 512], h == 0, h == 3, [omT, wo], [bk])
                        p.op('dve', lambda e, bk=bk, half=half: e.tensor_tensor(out=x0b[0:T, half * 512:(half + 1) * 512], in0=bk[0:T, :],
                                                                                in1=x1[0:T, half * 512:(half + 1) * 512], op=ALU.add),
                             reads=[bk, x1], writes=[x0b])
                    p.dma('sp', lambda e: e.dma_start(out=x2s[row0:row0 + T, :], in_=x0b[0:T, :]), reads=[x0b], writes=['x2s'])
                    if dbg:
                        p.dma('sp', lambda e: e.dma_start(out=x2_dbg[row0:row0 + T, :], in_=x0b[0:T, :]), reads=[x0b], writes=['x2_dbg'])

                def sample_mem_attn():
                    for b in range(DEC_B):
                        mf, mb, mT = sm['mf'], sm['mb'], sm['mT']
                        for which, src_d in ((0, cmk_d), (1, cmv_d)):
                            p.dma('sp', lambda e, b=b, src_d=src_d, which=which: e.dma_start(out=mf[which][:, :, :], in_=src_d[b, :, :].rearrange("(mt m) f -> m mt f", mt=2)),
                                  writes=[mf[which]])
                            p.op('pool' if which else 'dve', lambda e, which=which: e.tensor_copy(out=mb[which][:, :, :], in_=mf[which][:, :, :]), reads=[mf[which]], writes=[mb[which]])
                        bk = nb()
                        bv = bk[:].bitcast(BF16)
                        for mt in range(2):
                            for h in range(4):
                                tr(bv[:, (mt * 4 + h) * 128:(mt * 4 + h + 1) * 128], mb[0][:, mt, h * 128:(h + 1) * 128], 128, [mb[0]], [bk])
                        p.op('act', lambda e, bv=bv: e.copy(out=mT[:, :, :], in_=bv[:, :].rearrange("q (k m) -> q k m", k=8)), reads=[bk], writes=[mT])
                        bk = nb()
                        for mt in range(2):
                            for h in range(4):
                                mm(bk[:, (mt * 4 + h) * 4:(mt * 4 + h + 1) * 4], mT[:, mt * 4 + h, :], qmT[:, h, 4 * b:4 * b + 4], True, True, [mT, qmT], [bk])
                        Pm = sm['Pm']
                        p.op('act', lambda e, bk=bk: e.activation(out=Pm[:, :], in_=bk[:, 0:32], func=AF.Exp, scale=128.0 ** -0.5), reads=[bk], writes=[Pm])
                        o6, o7 = pb[6], pb[7]
                        for h in range(4):
                            for mt in range(2):
                                mm(o6[:, h * 4:(h + 1) * 4], mb[1][:, mt, h * 128:(h + 1) * 128], Pm[:, (mt * 4 + h) * 4:(mt * 4 + h + 1) * 4], mt == 0, mt == 1, [mb[1], Pm], [o6])
                        for h in range(4):
                            for mt in range(2):
                                mm(o7[:, h * 4:(h + 1) * 4], ones_bf[:, :], Pm[:, (mt * 4 + h) * 4:(mt * 4 + h + 1) * 4], mt == 0, mt == 1, [ones_bf, Pm], [o7])
                        rc = sm['rc']
                        p.op('dve', lambda e: e.reciprocal(out=rc[:, 0:16], in_=o7[:, 0:16]), reads=[o7], writes=[rc])
                        p.op('dve', lambda e, b=b: e.tensor_tensor(out=omT[:, :, 4 * b:4 * b + 4], in0=o6[:, 0:16].rearrange("q (h t) -> q h t", h=4),
                                                                 in1=rc[:, 0:16].rearrange("q (h t) -> q h t", h=4), op=ALU.mult), reads=[o6, rc], writes=[omT])

                sm = {}

                def sample_dsa(stk):
                    T = NS
                    NIT = 36
                    gbufs = [p.sb('gbuf%d' % i, [64, 8192], F32, stk) for i in range(cfg.get('n_gbuf', 2))]
                    cbs = [p.sb('cbs%d' % i, [64, 8192], BF16, stk) for i in range(cfg.get('n_cbuf', 1))]
                    KTb = p.sb('KTb', [128, 64, 64], BF16, stk)
                    kiTc_l = [p.sb('kiTc%d' % i, [64, 16, 64], BF16, stk) for i in range(3)]
                    pt_sb = p.sb('pt_sb', [64, 16], I32, stk)
                    idx2 = p.sb('idx2', [64, 16, 2], I32, stk)
                    rS_l = [p.sb('rS%d' % i, [64, 16, 32], F32, stk) for i in range(3)]
                    NCH = cfg.get('nch', 4)
                    scTbs = [p.sb('scTb%d' % i, [64, 128, 4], F32, stk) for i in range(NCH)]
                    scns = [p.sb('scn%d' % i, [4, 4], F32, stk) for i in range(NCH)]
                    rSn_l = [p.sb('rSn%d' % i, [4, 32], F32, stk) for i in range(2)]
                    chunk_ctr = [0]
                    Wbc = p.sb('Wbc', [64, 16, 32], F32, stk)
                    Dg = p.sb('Dg', [64, 16, 8, 4], F32, stk)
                    q2T = p.sb('q2T', [128, 4, 64], BF16, stk)
                    kT2n = p.sb('kT2n', [128, 64], BF16, stk)
                    kiTs = p.sb('kiTs', [64, 64], BF16, stk)
                    vnf = p.sb('vnf', [4, 16, 128], F32, stk)
                    vnew = p.sb('vnew', [4, 16, 128], BF16, stk)
                    negm4 = p.sb('negm4', [4, 4], F32, stk)
                    pow2 = p.sb('pow2', [64, 48], F32, stk)
                    hs_l = [p.sb('hs%d' % i, [64, 4], F32, stk) for i in range(NCH)]
                    hsb = p.sb('hsb', [64, 2], BF16, stk)
                    Wsc_l = [p.sb('Wsc%d' % i, [64, 2], F32, stk) for i in range(NCH)]
                    Wtab_l = [p.sb('Wtab%d' % i, [64, 48], F32, stk) for i in range(NCH)]
                    lo_l = [p.sb('lo%d' % i, [64, 4], F32, stk) for i in range(NCH)]
                    mid_l = [p.sb('mid%d' % i, [64, 4], F32, stk) for i in range(NCH)]
                    ge_l = [p.sb('ge%d' % i, [64, 4], F32, stk) for i in range(NCH)]
                    cmpb_l = [p.sb('cmpb%d' % i, [64, 128, 4], BF16, stk) for i in range(NCH)]
                    cntp_l = [p.sb('cntp%d' % i, [64, 4], F32, stk) for i in range(NCH)]
                    cmpn_l = [p.sb('cmpn%d' % i, [4, 4], F32, stk) for i in range(NCH)]
                    mask_s_l = cmpb_l
                    maskn_l = [p.sb('maskn%d' % i, [4, 4], BF16, stk) for i in range(NCH)]
                    Ess = [p.sb('Es%d' % i, [64, 16, 32], BF16, stk) for i in range(2)]
                    PTss = [p.sb('PTs%d' % i, [64, 16, 32], BF16, stk) for i in range(2)]
                    En = p.sb('En', [4, 32], BF16, stk)
                    PTn = p.sb('PTn', [4, 32], BF16, stk)
                    rcs = p.sb('rcs', [128, 32], F32, stk)
                    dacc = p.sb('dacc', [64, 8, 32], F32, stk)
                    dtot = p.sb('dtot', [64, 32], F32, stk)
                    q2bd = p.sb('q2bd', [128, 16, 32], BF16, stk)
                    ybTs = p.sb('ybTs', [128, 4, 64], BF16, stk)

                    p.dma('sp', lambda e: e.dma_start(out=pt_sb[:, :], in_=pt_d.rearrange("b j -> j b"), allow_slow_non_contiguous=True), writes=[pt_sb])
                    p.dma('sp', lambda e: e.dma_start(out=negm4[:, :], in_=c_negm4[:, :]), writes=[negm4])
                    p.dma('sp', lambda e: e.dma_start(out=pow2[:, :], in_=c_pow2[0:64, :]), writes=[pow2])
                    for half in range(2):
                        p.op('dve', lambda e, half=half: e.tensor_scalar(out=idx2[:, :, half], in0=pt_sb[:, :], scalar1=2, scalar2=half, op0=ALU.mult, op1=ALU.add),
                             reads=[pt_sb], writes=[idx2])
                    p.op('dve', lambda e: e.tensor_tensor(out=Dg[:, :, :, :], in0=wsc_s[:, :].unsqueeze(1).unsqueeze(3).to_broadcast([64, 16, 8, 4]),
                                                          in1=identf[0:64, 0:64].rearrange("q (b t) -> q b t", b=16).unsqueeze(2).to_broadcast([64, 16, 8, 4]), op=ALU.mult),
                         reads=[wsc_s, identf], writes=[Dg])
                    bk = nb()
                    mm(bk[0:64, :], onesf[0:64, 0:64], Dg[:, :, :, :].rearrange("q b h t -> q (b h t)"), True, True, [onesf, Dg], [bk])
                    p.op('act', lambda e, bk=bk: e.copy(out=Wbc[:, :, :], in_=bk[0:64, :].rearrange("q (b x) -> q b x", b=16)), reads=[bk], writes=[Wbc])
                    p.op('pool', lambda e: e.tensor_copy(out=q2T[0:64, :, :], in_=qTs[:, 0:4, :]), reads=[qTs], writes=[q2T])
                    p.dma('sp', lambda e: e.dma_start(out=q2T[64:128, :, :], in_=qTs[:, 4:8, :]), reads=[qTs], writes=[q2T])
                    p.op('dve', lambda e: e.memset(q2bd[:, :, :], 0.0), writes=[q2bd])
                    for g in range(2):
                        p.op('dve', lambda e, g=g: e.tensor_copy(out=q2bd[g * 64:(g + 1) * 64, :, g * 16:(g + 1) * 16].rearrange("q b (r t) -> q b r t", r=4),
                                                                 in_=q2T[g * 64:(g + 1) * 64, :, :].rearrange("q r (b t) -> q b r t", t=4)), reads=[q2T], writes=[q2bd])
                    bk = nb()
                    bv = bk[:].bitcast(BF16)
                    tr(bv[:, 0:64], kb_s[:, 0:128], 64, [kb_s], [bk])
                    tr(bv[0:64, 64:128], kb_s[:, 128:192], 64, [kb_s], [bk])
                    p.op('act', lambda e, bv=bv: e.copy(out=kT2n[:, :], in_=bv[:, 0:64]), reads=[bk], writes=[kT2n])
                    p.op('act', lambda e, bv=bv: e.copy(out=kiTs[:, :], in_=bv[0:64, 64:128]), reads=[bk], writes=[kiTs])
                    p.dma('sp', lambda e: e.dma_start(out=vnf[:, :, :], in_=v_s.rearrange("(b t) d -> t b d", t=4)), reads=['vo'], writes=[vnf])
                    p.op('dve', lambda e: e.tensor_copy(out=vnew[:, :, :], in_=vnf[:, :, :]), reads=[vnf], writes=[vnew])

                    nb_ = cfg.get('n_sb', DEC_B)
                    NITB = cfg.get('nit', 22)
                    gseq = []
                    for b0_ in range(0, nb_, NCH):
                        grp_ = list(range(b0_, min(b0_ + NCH, nb_)))
                        gseq += [('ki', b_, 0) for b_ in grp_]
                        for b_ in grp_:
                            gseq += [('K', b_, 0), ('V', b_, 0), ('K', b_, 1), ('V', b_, 1)]
                    gpos = {it: i for i, it in enumerate(gseq)}
                    g_next = [0]

                    NGB = len(gbufs)

                    def emit_gather(i):
                        kind, b_, half = gseq[i]
                        gb = gbufs[i % NGB]
                        if kind == 'ki':
                            src, idx_ap, idx_t = ckidx_d, pt_sb[:, b_:b_ + 1], pt_sb
                        else:
                            src, idx_ap, idx_t = (ck_d if kind == 'K' else cv_d), idx2[:, b_, half:half + 1], idx2
                        p.dma('pool', lambda e: e.indirect_dma_start(out=gb[:, :], out_offset=None, in_=src[:, :],
                                                                     in_offset=bass.IndirectOffsetOnAxis(ap=idx_ap, axis=0)), reads=[idx_t], writes=[gb])

                    g_cast = [0]

                    def use(item):
                        i = gpos[item]
                        while g_next[0] < min(NGB, len(gseq)):
                            emit_gather(g_next[0])
                            g_next[0] += 1
                        while g_cast[0] <= i:
                            c = g_cast[0]
                            dst = cbs[c % len(cbs)]
                            gb = gbufs[c % NGB]
                            p.op('act', lambda e, dst=dst, gb=gb: e.copy(out=dst[:, 0:4096], in_=gb[:, 0:4096]), reads=[gb], writes=[dst])
                            p.op('dve', lambda e, dst=dst, gb=gb: e.tensor_copy(out=dst[:, 4096:8192], in_=gb[:, 4096:8192]), reads=[gb], writes=[dst])
                            g_cast[0] += 1
                            if g_next[0] < len(gseq):
                                emit_gather(g_next[0])
                                g_next[0] += 1
                        return cbs[i % len(cbs)]

                    def ki_steps(b):
                        qi_b = qiTs[:, :, 4 * b:4 * b + 4]
                        scT = scTbs[b % NCH]
                        scn_ = scns[b % NCH]
                        steps = []

                        def chunk(lc):
                            kiTc = kiTc_l[chunk_ctr[0] % 3]
                            rS = rS_l[chunk_ctr[0] % 3]
                            chunk_ctr[0] += 1
                            cb = use(('ki', b, 0))
                            bk = nb()
                            bv = bk[:].bitcast(BF16)
                            for i in range(16):
                                l = lc * 16 + i
                                tr(bv[0:64, i * 64:(i + 1) * 64], cb[:, l * 64:(l + 1) * 64], 64, [cb], [bk])
                            p.op('act', lambda e: e.copy(out=kiTc[:, :, :], in_=bv[0:64, :].rearrange("q (i j) -> q i j", i=16)), reads=[bk], writes=[kiTc])
                            bk2 = nb()
                            for i in range(16):
                                mm(bk2[0:64, i * 32:(i + 1) * 32], kiTc[:, i, :], qi_b, True, True, [kiTc, qiTs], [bk2])
                            p.op('act', lambda e: e.activation(out=rS[:, :, :], in_=bk2[0:64, :].rearrange("q (i x) -> q i x", i=16), func=AF.Relu), reads=[bk2], writes=[rS])
                            p.op('dve', lambda e: e.tensor_tensor(out=rS[:, :, :], in0=rS[:, :, :], in1=Wbc[:, b:b + 1, :].to_broadcast([64, 16, 32]), op=ALU.mult),
                                 reads=[rS, Wbc], writes=[rS])
                            p.op('dve', lambda e: e.tensor_reduce(out=scT[:, lc * 16:(lc + 1) * 16, :], in_=rS[:, :, :].rearrange("q i (h t) -> q i t h", h=8),
                                                                  op=ALU.add, axis=AX.X), reads=[rS], writes=[scT])

                        def newkeys():
                            rSn = rSn_l[b % 2]
                            bk = nb()
                            mm(bk[0:4, 0:32], kiTs[:, 4 * b:4 * b + 4], qi_b, True, True, [kiTs, qiTs], [bk])
                            p.op('act', lambda e: e.activation(out=rSn[:, :], in_=bk[0:4, 0:32], func=AF.Relu), reads=[bk], writes=[rSn])
                            p.op('dve', lambda e: e.tensor_tensor(out=rSn[:, :], in0=rSn[:, :], in1=Wbc[0:4, b, :], op=ALU.mult), reads=[rSn, Wbc], writes=[rSn])
                            p.op('dve', lambda e: e.tensor_reduce(out=scn_[:, :], in_=rSn[:, :].rearrange("q (h t) -> q t h", h=8), op=ALU.add, axis=AX.X), reads=[rSn], writes=[scn_])
                        for lc in range(8):
                            steps.append(lambda lc=lc: chunk(lc))
                        steps.append(newkeys)
                        return steps

                    def bisect_setup(b):
                        c = b % NCH
                        scT, scn_, hs, Wsc, Wtab, lo, cmpb = scTbs[c], scns[c], hs_l[c], Wsc_l[c], Wtab_l[c], lo_l[c], cmpb_l[c]
                        p.op('act', lambda e: e.activation(out=cmpb[:, :, :].rearrange("q l t -> q (l t)"), in_=scT[:, :, :].rearrange("q l t -> q (l t)"), func=AF.Square, accum_out=hs[:, 0:1]),
                             reads=[scT], writes=[cmpb, hs])
                        p.op('act', lambda e: e.activation(out=cmpb[0:4, 0, :], in_=scn_[:, :], func=AF.Square, accum_out=hs[0:4, 1:2]), reads=[scn_], writes=[cmpb, hs])
                        p.op('dve', lambda e: e.tensor_tensor(out=scn_[:, :], in0=scn_[:, :], in1=negm4[:, :], op=ALU.add), reads=[scn_, negm4], writes=[scn_])
                        bk = nb()
                        mm(bk[0:64, 0:1], onesf[0:64, 0:64], hs[:, 0:1], True, False, [onesf, hs], [bk])
                        mm(bk[0:64, 0:1], onesf[0:4, 0:64], hs[0:4, 1:2], False, True, [onesf, hs], [bk])
                        p.op('act', lambda e, bk=bk: e.activation(out=Wsc[:, 0:1], in_=bk[0:64, 0:1], func=AF.Sqrt), reads=[bk], writes=[Wsc])
                        p.op('dve', lambda e: e.tensor_scalar(out=Wsc[:, 0:1], in0=Wsc[:, 0:1], scalar1=2.2, scalar2=2.0, op0=ALU.mult, op1=ALU.add), reads=[Wsc], writes=[Wsc])
                        p.op('dve', lambda e: e.tensor_scalar(out=Wsc[:, 1:2], in0=Wsc[:, 0:1], scalar1=-0.5, scalar2=None, op0=ALU.mult), reads=[Wsc], writes=[Wsc])
                        p.op('dve', lambda e: e.tensor_scalar(out=Wtab[:, :], in0=pow2[:, :], scalar1=Wsc[:, 0:1], scalar2=None, op0=ALU.mult), reads=[pow2, Wsc], writes=[Wtab])
                        p.op('dve', lambda e: e.tensor_scalar(out=lo[:, :], in0=pow2[:, 0:4], scalar1=0.0, scalar2=Wsc[:, 1:2], op0=ALU.mult, op1=ALU.add), reads=[pow2, Wsc], writes=[lo])

                    def bisect_iter(b, k):
                        c = b % NCH
                        scT, scn_, Wtab, lo, mid, ge, cmpb, cntp, cmpn = scTbs[c], scns[c], Wtab_l[c], lo_l[c], mid_l[c], ge_l[c], cmpb_l[c], cntp_l[c], cmpn_l[c]
                        p.op('dve', lambda e: e.tensor_scalar(out=mid[:, :], in0=lo[:, :], scalar1=Wtab[:, k:k + 1], scalar2=None, op0=ALU.add), reads=[lo, Wtab], writes=[mid])
                        p.op('dve', lambda e: e.tensor_tensor(out=cmpb[:, :, :], in0=scT[:, :, :], in1=mid[:, :].unsqueeze(1).to_broadcast([64, 128, 4]), op=ALU.is_ge),
                             reads=[scT, mid], writes=[cmpb])
                        p.op('dve', lambda e: e.tensor_reduce(out=cntp[:, :], in_=cmpb[:, :, :].rearrange("q l t -> q t l"), op=ALU.add, axis=AX.X), reads=[cmpb], writes=[cntp])
                        p.op('dve', lambda e: e.tensor_tensor(out=cmpn[:, :], in0=scn_[:, :], in1=mid[0:4, :], op=ALU.is_ge), reads=[scn_, mid], writes=[cmpn])
                        bk = nb()
                        mm(bk[0:64, 0:4], onesf[0:64, 0:64], cntp[:, :], True, False, [onesf, cntp], [bk])
                        mm(bk[0:64, 0:4], onesf[0:4, 0:64], cmpn[:, :], False, True, [onesf, cmpn], [bk])
                        p.op('dve', lambda e: e.tensor_scalar(out=ge[:, :], in0=bk[0:64, 0:4], scalar1=255.5, scalar2=None, op0=ALU.is_ge), reads=[bk], writes=[ge])
                        p.op('dve', lambda e: e.scalar_tensor_tensor(out=lo[:, :], in0=ge[:, :], scalar=Wtab[:, k:k + 1], in1=lo[:, :], op0=ALU.mult, op1=ALU.add),
                             reads=[ge, Wtab, lo], writes=[lo])

                    def bisect_finish(b):
                        c = b % NCH
                        scT, scn_, lo, mask_s, maskn = scTbs[c], scns[c], lo_l[c], mask_s_l[c], maskn_l[c]
                        p.op('dve', lambda e: e.tensor_tensor(out=mask_s[:, :, :], in0=scT[:, :, :], in1=lo[:, :].unsqueeze(1).to_broadcast([64, 128, 4]), op=ALU.is_ge),
                             reads=[scT, lo], writes=[mask_s])
                        p.op('dve', lambda e: e.tensor_tensor(out=maskn[:, :], in0=scn_[:, :], in1=lo[0:4, :], op=ALU.is_ge), reads=[scn_, lo], writes=[maskn])

                    def attend(b):
                        mask_s, maskn = mask_s_l[b % NCH], maskn_l[b % NCH]
                        o6, o7 = pb[6], pb[7]
                        first = True
                        for half in range(2):
                            cbK = use(('K', b, half))
                            for lc in range(4):
                                bk = nb()
                                bv = bk[:].bitcast(BF16)
                                for i in range(16):
                                    l = lc * 16 + i
                                    tr(bv[:, i * 64:(i + 1) * 64], cbK[:, l * 128:(l + 1) * 128], 64, [cbK], [bk])
                                p.op('act', lambda e, bv=bv, lc=lc: e.copy(out=KTb[:, lc * 16:(lc + 1) * 16, :], in_=bv[:, :].rearrange("q (i j) -> q i j", i=16)), reads=[bk], writes=[KTb])
                            cbV = use(('V', b, half))
                            prev = None
                            for lc in range(5):
                                if lc < 4:
                                    bk = nb()
                                    for i in range(16):
                                        l = lc * 16 + i
                                        mm(bk[0:64, i * 32:(i + 1) * 32], KTb[:, l, :], q2bd[:, b, :], True, True, [KTb, q2bd], [bk])
                                    E_, P_ = Ess[lc % 2], PTss[lc % 2]
                                    p.op('act', lambda e, bk=bk, E_=E_: e.activation(out=E_[:, :, :], in_=bk[0:64, :].rearrange("q (i x) -> q i x", i=16), func=AF.Exp, scale=0.125),
                                         reads=[bk], writes=[E_])
                                    l0 = half * 64 + lc * 16
                                    p.op('dve', lambda e, l0=l0, E_=E_, P_=P_: e.tensor_tensor(out=P_[:, :, :].rearrange("q i (x t) -> q i x t", t=4), in0=E_[:, :, :].rearrange("q i (x t) -> q i x t", t=4),
                                                                                              in1=mask_s[:, l0:l0 + 16, :].unsqueeze(2).to_broadcast([64, 16, 8, 4]), op=ALU.mult),
                                         reads=[E_, mask_s], writes=[P_])
                                    ci = half * 4 + lc
                                    p.op('dve', lambda e, P_=P_, ci=ci: e.tensor_reduce(out=dacc[:, ci, :], in_=P_[:, :, :].rearrange("q i x -> q x i"), op=ALU.add, axis=AX.X),
                                         reads=[P_], writes=[dacc])
                                if prev is not None:
                                    plc, P_prev = prev
                                    for i in range(16):
                                        l = plc * 16 + i
                                        mm(o6[:, 0:32], cbV[:, l * 128:(l + 1) * 128], P_prev[:, i, :], first, False, [cbV, P_prev], [o6])
                                        first = False
                                prev = (lc, PTss[lc % 2]) if lc < 4 else None
                        bk = nb()
                        mm(bk[0:4, 0:32], kT2n[:, 4 * b:4 * b + 4], q2bd[:, b, :], True, True, [kT2n, q2bd], [bk])
                        p.op('act', lambda e, bk=bk: e.activation(out=En[:, :], in_=bk[0:4, 0:32], func=AF.Exp, scale=0.125), reads=[bk], writes=[En])
                        p.op('dve', lambda e: e.tensor_tensor(out=PTn[:, :].rearrange("q (x t) -> q x t", t=4), in0=En[:, :].rearrange("q (x t) -> q x t", t=4),
                                                               in1=maskn[:, :].unsqueeze(1).to_broadcast([4, 8, 4]), op=ALU.mult), reads=[En, maskn], writes=[PTn])
                        mm(o6[:, 0:32], vnew[:, b, :], PTn[:, :], False, True, [vnew, PTn], [o6])
                        p.op('dve', lambda e: e.tensor_reduce(out=dtot[:, :], in_=dacc[:, :, :].rearrange("q c x -> q x c"), op=ALU.add, axis=AX.X), reads=[dacc], writes=[dtot])
                        p.op('dve', lambda e: e.tensor_tensor(out=dtot[0:4, :], in0=dtot[0:4, :], in1=PTn[:, :], op=ALU.add), reads=[dtot, PTn], writes=[dtot])
                        mm(o7[:, 0:32], onesf[0:64, :], dtot[:, :], True, True, [onesf, dtot], [o7])
                        p.op('dve', lambda e: e.reciprocal(out=rcs[:, :], in_=o7[:, 0:32]), reads=[o7], writes=[rcs])
                        for g in range(2):
                            p.op('dve', lambda e, g=g: e.tensor_tensor(out=ybTs[g * 64:(g + 1) * 64, :, 4 * b:4 * b + 4],
                                                                     in0=o6[g * 64:(g + 1) * 64, g * 16:(g + 1) * 16].rearrange("q (r t) -> q r t", r=4),
                                                                     in1=rcs[g * 64:(g + 1) * 64, g * 16:(g + 1) * 16].rearrange("q (r t) -> q r t", r=4), op=ALU.mult),
                                 reads=[o6, rcs], writes=[ybTs])

                    for b0_ in range(0, nb_, NCH):
                        grp_ = list(range(b0_, min(b0_ + NCH, nb_)))
                        for b in grp_:
                            for st_ in ki_steps(b):
                                st_()
                        if cfg.get('sb_stage', 9) < 2:
                            continue
                        for b in grp_:
                            bisect_setup(b)
                        for k in range(NITB):
                            for b in grp_:
                                bisect_iter(b, k)
                        for b in grp_:
                            bisect_finish(b)
                        if cfg.get('sb_stage', 9) < 3:
                            continue
                        for b in grp_:
                            attend(b)
                    for r in range(4):
                        bk = nb()
                        bv = bk[:].bitcast(BF16)
                        tr(bv[0:64, 0:128], ybTs[:, r, :], 128, [ybTs], [bk])
                        p.op('act', lambda e, bv=bv, r=r: e.copy(out=ycats[:, 512:1024].rearrange("q (g r d) -> q g r d", g=2, r=4)[:, :, r, :],
                                                                 in_=bv[0:64, 0:128].rearrange("q (g d) -> q g d", g=2)), reads=[bk], writes=[ycats])


                if 'P3' in do:
                    proj_tile(xs_d[:, :], NS, 0, True, x0s, ycats)
                    feature_major(NS, 0, qTs, qiTs)
                    p.op('pool', lambda e: e.tensor_copy(out=kvf_s[:, :], in_=kvf[0:NS, :]), reads=[kvf], writes=[kvf_s])
                    p.op('pool', lambda e: e.tensor_copy(out=kb_s[:, :], in_=kb[0:NS, :]), reads=[kb], writes=[kb_s])
                    p.op('pool', lambda e: e.tensor_copy(out=wsc_s[:, :], in_=wsc[0:NS, :]), reads=[wsc], writes=[wsc_s])
                if 'P2' in do:
                    for ti in range(cfg.get('n_tiles', NT)):
                        proj_tile(xp_d[ti * 128:(ti + 1) * 128, :], 128, ti, False, x0, ycat)
                        feature_major(128, ti, qT, qiT)
                        if 'P4' in do and cfg.get('interleave_conv', True):
                            for ec in range(ti * 8, ti * 8 + 8):
                                convert_chunk(ec, cub[ec % 2], cut[ec % 2], cvb[ec % 2])
                            conv_done[0] = ti * 8 + 8
                        prompt_dsa(ti)
                        out_proj_and_mem(128, ti * 128, False, x0, ycat)
            sw.close()
            p.barrier()
            if 'P3' in do:
                with ExitStack() as s3s:
                    sample_dsa(s3s)
                p.barrier()
                with ExitStack() as s3m:
                    sm['mf'] = [p.sb('mf%d' % i, [128, 2, 512], F32, s3m) for i in range(2)]
                    sm['mb'] = [p.sb('mb%d' % i, [128, 2, 512], BF16, s3m) for i in range(2)]
                    sm['mT'] = p.sb('mT', [128, 8, 128], BF16, s3m)
                    sm['Pm'] = p.sb('Pm', [128, 32], BF16, s3m)
                    sm['rc'] = p.sb('rc', [128, 16], F32, s3m)
                    out_proj_and_mem(NS, SEQ, True, x0s, ycats)
            p.barrier()

        if 'P4' in do:
            nbmod[0] = 4
            with ExitStack() as s4:
                iota_f = p.sb('iota_f', [128, 128], F32, s4)
                gfin = p.sb('gfin', [128, D], F32, s4)
                p.dma('sp', lambda e: e.dma_start(out=iota_f[:], in_=c_iota[:, :]), writes=[iota_f])
                p.dma('sp', lambda e: e.dma_start(out=gfin[:], in_=g_fin_d.partition_broadcast(128)), writes=[gfin])
                pwq = p.sb('pwq_sb', [128, 8, D], BF16, s4)
                keysT = p.sb('keysT', [64, 16, 128], BF16, s4)
                with ExitStack() as s5:
                    stage = [p.sb('pstage%d' % i, [128, D], F32, s5) for i in range(2)]
                    load_weight(pwq, pwq_d, 8, D, None, stage)
                    kbf = p.sb('kbf', [128, 64], BF16, s5)
                    for hc in range(16):
                        stg = stage[hc % 2]
                        p.dma('sp', lambda e, hc=hc, stg=stg: e.dma_start(out=stg[:, 0:64], in_=pkeys_d[hc, :, :]), writes=[stg])
                        p.op('dve', lambda e, stg=stg: e.tensor_copy(out=kbf[:], in_=stg[:, 0:64]), reads=[stg], writes=[kbf])
                        bk = nb()
                        bv = bk[:].bitcast(BF16)
                        tr(bv[0:64, 0:128], kbf[:, :], 128, [kbf], [bk])
                        p.op('act', lambda e, hc=hc, bv=bv: e.copy(out=keysT[:, hc, :], in_=bv[0:64, 0:128]), reads=[bk], writes=[keysT])
                    ubf = [p.sb('ubf%d' % i, [128, D], BF16, s5) for i in range(2)]
                    utb = [p.sb('utb%d' % i, [128, D], BF16, s5) for i in range(2)]
                    vbf = [p.sb('vbf%d' % i, [128, D], BF16, s5) for i in range(2)]
                    for ec in range(conv_done[0], cfg.get('n_ec', 128)):
                        convert_chunk(ec, ubf[ec % 2], utb[ec % 2], vbf[ec % 2])

                p.barrier()
                xg_l = [p.sb('xg%d' % i, [128, 2, D], F32, s4) for i in range(2)]
                XT_l = [p.sb('XT%d' % i, [128, 8, 256], BF16, s4) for i in range(2)]
                hn = p.sb('hn', [128, D], BF16, s4)
                hnT = p.sb('hnT', [128, 8, 128], BF16, s4)
                sq4 = p.sb('sq4', [128, D], F32, s4)
                qpT = p.sb('qpT', [64, 16, 128], BF16, s4)
                ssb = p.sb('ssb', [128, 16, 128], F32, s4)
                wk = p.sb('wk', [128, 128], F32, s4)
                a16 = p.sb('a16', [128, 8, 16], F32, s4)
                b16 = p.sb('b16', [128, 8, 16], F32, s4)
                iau = p.sb('iau', [128, 8, 16], U32, s4)
                ibu = p.sb('ibu', [128, 8, 16], U32, s4)
                iaf = p.sb('iaf', [128, 8, 16], F32, s4)
                ibf = p.sb('ibf', [128, 8, 16], F32, s4)
                cand = p.sb('cand', [128, 8, 256], F32, s4)
                wkc = p.sb('wkc', [128, 256], F32, s4)
                c16 = p.sb('c16', [128, 8, 16], F32, s4)
                icu = p.sb('icu', [128, 8, 16], U32, s4)
                iju = p.sb('iju', [128, 2, 128], U32, s4)
                ijf = p.sb('ijf', [128, 2, 128], F32, s4)
                eq = p.sb('eq', [128, 128, 16], F32, s4)
                SEL = p.sb('SEL', [128, 3, 128], F32, s4)
                e16 = p.sb('e16', [128, 8, 16], F32, s4)
                z8 = p.sb('z8', [128, 16], F32, s4)
                selT_l = [p.sb('selT%d' % i, [128, 3, 256], F32, s4) for i in range(2)]
                Gt = p.sb('Gt', [128, 256, 128], BF16, s4)
                ohb1 = [p.sb('ohb1_%d' % i, [128, 8, 128], BF16, s4) for i in range(2)]
                ohb0 = [p.sb('ohb0_%d' % i, [128, 8, 128], BF16, s4) for i in range(2)]
                ohbg = [p.sb('ohbg_%d' % i, [128, 8, 128], BF16, s4) for i in range(2)]
                selTb_l = [p.sb('selTb%d' % i, [128, 3, 256], BF16, s4) for i in range(2)]
                iota_b = p.sb('iota_b', [128, 128], BF16, s4)
                p.op('pool', lambda e: e.tensor_copy(out=iota_b[:, :], in_=iota_f[:, :]), reads=[iota_f], writes=[iota_b])
                NBUF = 4
                UTc = [p.sb('UTc%d' % i, [128, 8, 128], BF16, s4) for i in range(NBUF)]
                Vc = [p.sb('Vc%d' % i, [128, D], BF16, s4) for i in range(NBUF)]
                gab = [p.sb('gab%d' % i, [128, 256], BF16, s4) for i in range(3)]
                GAb = [p.sb('GAb%d' % i, [128, 256], BF16, s4) for i in range(3)]
                pre = p.sb('pre', [128, D], F32, s4)
                yo = pre


                def peer_select(T, s, xg, XT, selT, selTb):
                    norm_T(xg[:, s, :], T, 'f', hn, hnT, ssd, sq4, gcol=gcols['ffn'])
                    p.op('pool', lambda e: e.tensor_copy(out=XT[:, :, s * 128:s * 128 + T], in_=hnT[:, :, 0:T]), reads=[hnT], writes=[XT])
                    for q4 in range(4):
                        bk = nb()
                        for k4 in range(4):
                            hc = q4 * 4 + k4
                            for c in range(8):
                                mm(bk[0:64, k4 * T:(k4 + 1) * T], pwq[:, c, hc * 64:(hc + 1) * 64], hnT[:, c, 0:T], c == 0, c == 7, [pwq, hnT], [bk])
                        p.op('act', lambda e, bk=bk, q4=q4: e.copy(out=qpT[:, q4 * 4:(q4 + 1) * 4, 0:T], in_=bk[0:64, 0:4 * T].rearrange("q (k t) -> q k t", k=4)),
                             reads=[bk], writes=[qpT])
                    for q4 in range(4):
                        bk = nb()
                        for k4 in range(4):
                            hc = q4 * 4 + k4
                            mm(bk[0:T, k4 * 128:(k4 + 1) * 128], qpT[:, hc, 0:T], keysT[:, hc, :], True, True, [qpT, keysT], [bk])
                        p.op('act', lambda e, bk=bk, q4=q4: e.copy(out=ssb[0:T, q4 * 4:(q4 + 1) * 4, :], in_=bk[0:T, :].rearrange("q (k n) -> q k n", k=4)),
                             reads=[bk], writes=[ssb])
                    for h in range(8):
                        for cc, (vals, idxs) in enumerate(((a16, iau), (b16, ibu))):
                            src = ssb[0:T, 2 * h + cc, :]
                            p.op('dve', lambda e, src=src, vals=vals, h=h: e.max(out=vals[0:T, h, 0:8], in_=src), reads=[ssb], writes=[vals])
                            p.op('dve', lambda e, src=src, vals=vals, idxs=idxs, h=h: e.max_index(out=idxs[0:T, h, 0:8], in_max=vals[0:T, h, 0:8], in_values=src),
                                 reads=[ssb, vals], writes=[idxs])
                            p.op('dve', lambda e, src=src, vals=vals, h=h: e.match_replace(out=wk[0:T, :], in_to_replace=vals[0:T, h, 0:8], in_values=src, imm_value=NEG),
                                 reads=[ssb, vals], writes=[wk])
                            p.op('dve', lambda e, vals=vals, h=h: e.max(out=vals[0:T, h, 8:16], in_=wk[0:T, :]), reads=[wk], writes=[vals])
                            p.op('dve', lambda e, vals=vals, idxs=idxs, h=h: e.max_index(out=idxs[0:T, h, 8:16], in_max=vals[0:T, h, 8:16], in_values=wk[0:T, :]),
                                 reads=[wk, vals], writes=[idxs])
                    for h in range(8):
                        p.op('dve', lambda e, h=h: e.tensor_tensor(out=cand[0:T, h, :].rearrange("q (i j) -> q i j", i=16),
                                                                    in0=a16[0:T, h, :].unsqueeze(2).to_broadcast([T, 16, 16]),
                                                                    in1=b16[0:T, h, :].unsqueeze(1).to_broadcast([T, 16, 16]), op=ALU.add),
                             reads=[a16, b16], writes=[cand])
                    for h in range(8):
                        src = cand[0:T, h, :]
                        p.op('dve', lambda e, src=src, h=h: e.max(out=c16[0:T, h, 0:8], in_=src), reads=[cand], writes=[c16])
                        p.op('dve', lambda e, src=src, h=h: e.max_index(out=icu[0:T, h, 0:8], in_max=c16[0:T, h, 0:8], in_values=src), reads=[cand, c16], writes=[icu])
                        p.op('dve', lambda e, src=src, h=h: e.match_replace(out=wkc[0:T, :], in_to_replace=c16[0:T, h, 0:8], in_values=src, imm_value=NEG),
                             reads=[cand, c16], writes=[wkc])
                        p.op('dve', lambda e, h=h: e.max(out=c16[0:T, h, 8:16], in_=wkc[0:T, :]), reads=[wkc], writes=[c16])
                        p.op('dve', lambda e, h=h: e.max_index(out=icu[0:T, h, 8:16], in_max=c16[0:T, h, 8:16], in_values=wkc[0:T, :]), reads=[wkc, c16], writes=[icu])
                    icf = icu[0:T, :, :].rearrange("q h k -> q (h k)")
                    p.op('dve', lambda e: e.tensor_scalar(out=iju[0:T, 0, :], in0=icf, scalar1=4, scalar2=None, op0=ALU.logical_shift_right), reads=[icu], writes=[iju])
                    p.op('dve', lambda e: e.tensor_scalar(out=iju[0:T, 1, :], in0=icf, scalar1=15, scalar2=None, op0=ALU.bitwise_and), reads=[icu], writes=[iju])
                    p.op('dve', lambda e: e.tensor_copy(out=ijf[0:T, :, :], in_=iju[0:T, :, :]), reads=[iju], writes=[ijf])
                    p.op('dve', lambda e: e.tensor_copy(out=iaf[0:T, :, :], in_=iau[0:T, :, :]), reads=[iau], writes=[iaf])
                    p.op('dve', lambda e: e.tensor_copy(out=ibf[0:T, :, :], in_=ibu[0:T, :, :]), reads=[ibu], writes=[ibf])
                    for w_, srcf in ((0, iaf), (1, ibf)):
                        p.op('dve', lambda e, w_=w_: e.tensor_tensor(out=eq[0:T, :, :], in0=ijf[0:T, w_, :].unsqueeze(2).to_broadcast([T, 128, 16]),
                                                                     in1=iota_f[0:T, 0:16].unsqueeze(1).to_broadcast([T, 128, 16]), op=ALU.is_equal),
                             reads=[ijf, iota_f], writes=[eq])
                        p.op('dve', lambda e, srcf=srcf: e.tensor_tensor(out=eq[0:T, :, :].rearrange("q (h k) i -> q h k i", h=8),
                                                                         in0=eq[0:T, :, :].rearrange("q (h k) i -> q h k i", h=8),
                                                                         in1=srcf[0:T, :, :].unsqueeze(2).to_broadcast([T, 8, 16, 16]), op=ALU.mult),
                             reads=[eq, srcf], writes=[eq])
                        p.op('dve', lambda e, w_=w_: e.tensor_reduce(out=SEL[0:T, w_, :], in_=eq[0:T, :, :], op=ALU.add, axis=AX.X), reads=[eq], writes=[SEL])
                    p.op('dve', lambda e: e.tensor_tensor(out=e16[0:T, :, :], in0=c16[0:T, :, :], in1=c16[0:T, :, 0:1].to_broadcast([T, 8, 16]), op=ALU.subtract),
                         reads=[c16], writes=[e16])
                    p.op('act', lambda e: e.activation(out=e16[0:T, :, :], in_=e16[0:T, :, :], func=AF.Exp), reads=[e16], writes=[e16])
                    p.op('dve', lambda e: e.tensor_reduce(out=z8[0:T, 0:8], in_=e16[0:T, :, :], op=ALU.add, axis=AX.X), reads=[e16], writes=[z8])
                    p.op('dve', lambda e: e.reciprocal(out=z8[0:T, 8:16], in_=z8[0:T, 0:8]), reads=[z8], writes=[z8])
                    p.op('dve', lambda e: e.tensor_tensor(out=SEL[0:T, 2, :].rearrange("q (h k) -> q h k", h=8), in0=e16[0:T, :, :],
                                                          in1=z8[0:T, 8:16].unsqueeze(2).to_broadcast([T, 8, 16]), op=ALU.mult), reads=[e16, z8], writes=[SEL])
                    bk = nb()
                    for w_ in range(3):
                        p.op('pe', lambda e, w_=w_, bk=bk: e.transpose(out=bk[:, w_ * T:(w_ + 1) * T], in_=SEL[0:T, w_, :], identity=identf[0:T, 0:T]),
                             reads=[SEL, identf], writes=[bk])
                    p.op('act', lambda e, bk=bk: e.copy(out=selT[:, :, s * 128:s * 128 + T], in_=bk[:, 0:3 * T].rearrange("q (w t) -> q w t", w=3)),
                         reads=[bk], writes=[selT])
                    p.op('pool', lambda e: e.tensor_copy(out=selTb[:, :, s * 128:s * 128 + T], in_=selT[:, :, s * 128:s * 128 + T]), reads=[selT], writes=[selTb])

                def peer_sel_part(row0, Tg, bufset):
                    xg, XT, selT, selTb = bufset
                    nsub = (Tg + 127) // 128
                    Ts = min(Tg, 128)
                    for s in range(nsub):
                        p.dma('sp', lambda e, s=s: e.dma_start(out=xg[0:Ts, s, :], in_=x2s[row0 + s * 128:row0 + s * 128 + Ts, :]), reads=['x2s'], writes=[xg])
                        peer_select(Ts, s, xg, XT, selT, selTb)

                def peer_group(Tg, y_out, yrow0, bufset, side_calls):
                    xg, XT, selT, selTb = bufset
                    nsub = (Tg + 127) // 128
                    Ts = min(Tg, 128)
                    side_i = [0]
                    for t0 in range(0, Tg, 8):
                        o1, o0, og = ohb1[(t0 // 8) % 2], ohb0[(t0 // 8) % 2], ohbg[(t0 // 8) % 2]
                        p.op('dve', lambda e, t0=t0, o1=o1: e.tensor_tensor(out=o1[:, :, :], in0=iota_b[:, :].unsqueeze(1).to_broadcast([128, 8, 128]),
                                                                          in1=selTb[:, 1, t0:t0 + 8].unsqueeze(2).to_broadcast([128, 8, 128]), op=ALU.is_equal),
                             reads=[iota_b, selTb], writes=[o1])
                        p.op('dve', lambda e, t0=t0, o0=o0: e.tensor_tensor(out=o0[:, :, :], in0=iota_b[:, :].unsqueeze(1).to_broadcast([128, 8, 128]),
                                                                          in1=selTb[:, 0, t0:t0 + 8].unsqueeze(2).to_broadcast([128, 8, 128]), op=ALU.is_equal),
                             reads=[iota_b, selTb], writes=[o0])
                        p.op('pool', lambda e, t0=t0, o0=o0, og=og: e.tensor_tensor(out=og[:, :, :], in0=o0[:, :, :],
                                                                                  in1=selTb[:, 2, t0:t0 + 8].unsqueeze(2).to_broadcast([128, 8, 128]), op=ALU.mult),
                             reads=[o0, selTb], writes=[og])
                        for q4 in range(2):
                            bk = nb()
                            for tt in range(4):
                                mm(bk[:, tt * 128:(tt + 1) * 128], o1[:, q4 * 4 + tt, :], og[:, q4 * 4 + tt, :], True, True, [o1, og], [bk])
                            p.op('act', lambda e, bk=bk, ta=t0 + q4 * 4: e.copy(out=Gt[:, ta:ta + 4, :], in_=bk[:, :].rearrange("q (t i) -> q t i", t=4)), reads=[bk], writes=[Gt])
                    acc = pb[4:8]
                    n_ec = cfg.get('n_ec', 128)

                    def fetch(ec):
                        p.dma('sp', lambda e, ec=ec: e.dma_start(out=UTc[ec % NBUF][:, :, :].rearrange("q c e -> q (c e)"), in_=UTs[ec, :, :]),
                              reads=[('UTs', ec)], writes=[UTc[ec % NBUF]])
                        p.dma('sp', lambda e, ec=ec: e.dma_start(out=Vc[ec % NBUF][:, :], in_=Vs[ec, :, :]), reads=[('Vs', ec)], writes=[Vc[ec % NBUF]])
                    for ec in range(min(NBUF, n_ec)):
                        fetch(ec)
                    for ec in range(n_ec + 1):
                        if ec < n_ec:
                            U_ = UTc[ec % NBUF]
                            bk = pb[ec % 2]
                            for c in range(8):
                                mm(bk[:, 0:Tg], U_[:, c, :], XT[:, c, 0:Tg], c == 0, c == 7, [U_, XT], [bk])
                            ga, GA = gab[ec % 3], GAb[ec % 3]
                            p.op('act', lambda e, bk=bk, ga=ga: e.activation(out=ga[:, 0:Tg], in_=bk[:, 0:Tg], func=AF.Gelu_apprx_tanh), reads=[bk], writes=[ga])
                            p.op('dve', lambda e, ga=ga, GA=GA, ec=ec: e.tensor_tensor(out=GA[:, 0:Tg], in0=ga[:, 0:Tg], in1=Gt[:, 0:Tg, ec], op=ALU.mult),
                                 reads=[ga, Gt], writes=[GA])
                        if ec >= 1:
                            pe_ = ec - 1
                            V_, GA = Vc[pe_ % NBUF], GAb[pe_ % 3]
                            for s in range(nsub):
                                for half in range(2):
                                    ab = acc[2 * s + half]
                                    mm(ab[0:Ts, :], GA[:, s * 128:s * 128 + Ts], V_[:, half * 512:(half + 1) * 512], pe_ == 0, pe_ == n_ec - 1, [GA, V_], [ab])
                            if pe_ + NBUF < n_ec:
                                fetch(pe_ + NBUF)
                        per = (len(side_calls) + n_ec - 1) // max(n_ec, 1) if side_calls else 0
                        for _ in range(per):
                            if side_i[0] < len(side_calls):
                                kind_, a_, k_ = side_calls[side_i[0]]
                                (orig_op if kind_ == 'op' else orig_dma)(*a_, **k_)
                                side_i[0] += 1
                    while side_i[0] < len(side_calls):
                        kind_, a_, k_ = side_calls[side_i[0]]
                        (orig_op if kind_ == 'op' else orig_dma)(*a_, **k_)
                        side_i[0] += 1
                    for s in range(nsub):
                        for half in range(2):
                            ab = acc[2 * s + half]
                            p.op('dve', lambda e, ab=ab, s=s, half=half: e.tensor_tensor(out=pre[0:Ts, half * 512:(half + 1) * 512], in0=ab[0:Ts, :],
                                                                                         in1=xg[0:Ts, s, half * 512:(half + 1) * 512], op=ALU.add),
                                 reads=[ab, xg], writes=[pre])
                        rs = rmsnorm_rstd(pre, Ts, ssd, sq4)
                        p.op('dve', lambda e, rs=rs: e.scalar_tensor_tensor(out=yo[0:Ts, :], in0=pre[0:Ts, :], scalar=rs, in1=gfin[0:Ts, :], op0=ALU.mult, op1=ALU.mult),
                             reads=[pre, ssd['ss'], gfin], writes=[yo])
                        p.dma('sp', lambda e, s=s: e.dma_start(out=y_out[yrow0 + s * 128:yrow0 + s * 128 + Ts, :], in_=yo[0:Ts, :]), reads=[yo], writes=['y_out'])

                groups = [(gi * 256, 256, y_p, gi * 256) for gi in range(cfg.get('n_groups', 8))]
                if cfg.get('peer_sample', True):
                    groups.append((SEQ, NS, y_s, 0))
                bufsets = [(xg_l[i], XT_l[i], selT_l[i], selTb_l[i]) for i in range(2)]
                orig_op, orig_dma = p.op, p.dma
                rec = {'on': False, 'calls': []}

                def op_wrap(*a, **k):
                    if rec['on']:
                        rec['calls'].append(('op', a, k))
                    else:
                        return orig_op(*a, **k)

                def dma_wrap(*a, **k):
                    if rec['on']:
                        rec['calls'].append(('dma', a, k))
                    else:
                        return orig_dma(*a, **k)
                p.op, p.dma = op_wrap, dma_wrap
                nbbase[0], nbmod[0] = 2, 2
                if groups:
                    peer_sel_part(groups[0][0], groups[0][1], bufsets[0])
                for n, (row0, Tg, y_out, yrow0) in enumerate(groups):
                    side = []
                    if n + 1 < len(groups) and cfg.get('overlap_sel', True):
                        rec['on'], rec['calls'] = True, []
                        peer_sel_part(groups[n + 1][0], groups[n + 1][1], bufsets[(n + 1) % 2])
                        rec['on'] = False
                        side = rec['calls']
                    elif n + 1 < len(groups):
                        pass
                    peer_group(Tg, y_out, yrow0, bufsets[n % 2], side)
                    if n + 1 < len(groups) and not cfg.get('overlap_sel', True):
                        peer_sel_part(groups[n + 1][0], groups[n + 1][1], bufsets[(n + 1) % 2])
                p.op, p.dma = orig_op, orig_dma

        p.finish()
        p.emit()
    return nc


_NC_CACHE = {}


def make_in_maps(inp, cfg, ncores=NCORES):
    c = host_consts()
    f = lambda a: np.ascontiguousarray(a, dtype=np.float32)
    maps = []
    for i in range(ncores):
        m = {
            'xp': f(inp['x_prompt'][i]),
            'xs': f(inp['x_sample'][DEC_B * i:DEC_B * (i + 1)].reshape(NS, D)),
            'memp': f(inp['mem_prompt'][i]),
            'w_in': f(inp['w_in'][0]), 'w_out': f(inp['w_out'][0]),
            'g_mix': f(inp['norm_mix_g'][0]), 'g_mem': f(inp['norm_mem_g'][0]), 'g_memn': f(inp['mem_norm_g'][0]),
            'g_ffn': f(inp['norm_ffn_g'][0]), 'g_fin': f(inp['final_norm_g']),
            'ln_g': f(inp['gm_ln_g'][0]), 'ln_b': f(inp['gm_ln_b'][0]),
            'gm_ws': f(inp['gm_ws'][0]), 'gm_bs': f(inp['gm_bs'][0]),
            'mem_wq': f(inp['mem_wq'][0]), 'mem_wkv': f(inp['mem_wkv'][0]), 'mem_wo': f(inp['mem_wo'][0]),
            'c_ident': c['ident'], 'c_trilT': c['trilT'], 'c_negmask': c['negmask'], 'c_blk64': c['blk64'], 'c_ones': c['ones'], 'c_iota': c['iota'],
            'peer_wq': f(inp['peer_wq'][0]), 'peer_keys': f(inp['peer_keys'][0].reshape(16, 128, 64)),
            'peer_u': f(inp['peer_u'][0]), 'peer_v': f(inp['peer_v'][0]),
            'cache_kidx': f(inp['cache_kidx'][0]).reshape(-1, 8192), 'cache_k': f(inp['cache_k'][0]).reshape(-1, 8192),
            'cache_v': f(inp['cache_v'][0]).reshape(-1, 8192),
            'page_table': np.ascontiguousarray(inp['page_table'][DEC_B * i:DEC_B * (i + 1)], dtype=np.int32),
            'cache_mem_k': f(inp['cache_mem_k'][0][DEC_B * i:DEC_B * (i + 1)]).reshape(DEC_B, 256, 512),
            'cache_mem_v': f(inp['cache_mem_v'][0][DEC_B * i:DEC_B * (i + 1)]).reshape(DEC_B, 256, 512),
            'c_pow2': c['pow2'], 'c_negm4': c['negm4'],
        }
        maps.append(m)
    return maps


def assemble(results, ncores=NCORES):
    cat = lambda k: np.stack([np.asarray(r[k], dtype=np.float32) for r in results])
    y_prompt = cat('y_p')
    y_sample = cat('y_s').reshape(ncores * DEC_B, DEC_T, D)
    k_prompt = cat('k_p').reshape(1, ncores, SEQ, 2, 64)
    v_prompt = cat('v_p').reshape(1, ncores, SEQ, 2, 64)
    kidx_prompt = cat('ki_p').reshape(1, ncores, SEQ, 64)
    gmv_prompt = cat('gmv_p').reshape(1, ncores, 128, 512)
    memk = cat('memk_p').reshape(1, ncores, 256, 4, 128)
    memv = cat('memv_p').reshape(1, ncores, 256, 4, 128)
    k_sample = cat('k_s').reshape(1, ncores * DEC_B, DEC_T, 2, 64)
    v_sample = cat('v_s').reshape(1, ncores * DEC_B, DEC_T, 2, 64)
    kidx_sample = cat('ki_s').reshape(1, ncores * DEC_B, DEC_T, 64)
    gmv_sample = cat('gmv_s').reshape(1, ncores * DEC_B, DEC_T, 512)
    return (y_prompt, y_sample, k_prompt, v_prompt, kidx_prompt, gmv_prompt, memk, memv,
            k_sample, v_sample, kidx_sample, gmv_sample)


def kernel(**inputs):
    cfg = {}
    nc = build(cfg)
    maps = make_in_maps(inputs, cfg)
    res = run_bass_kernel_spmd(nc, maps, core_ids=list(range(NCORES)))
    return assemble(res.results)
```

```python
import numpy as np
from contextlib import ExitStack
import concourse.bass as bass
import concourse.mybir as mybir
from concourse.bass_utils import run_bass_kernel_spmd

F32 = mybir.dt.float32
BF16 = mybir.dt.bfloat16
I32 = mybir.dt.int32
U32 = mybir.dt.uint32
AF = mybir.ActivationFunctionType
ALU = mybir.AluOpType
AX = mybir.AxisListType

NCORES = 8
D = 1024
SEQ = 2048
NT = SEQ // 128
P_IN = 2376
EPS = 1e-6
NEG = -1.0e30
DEC_B = 16
DEC_T = 4
NS = DEC_B * DEC_T
NPAGES = 64
NEXP = 16384


class Prog:
    ENG = ('sp', 'act', 'dve', 'pool', 'pe')

    def __init__(self, nc, stack, n_dma_sems=12):
        self.nc = nc
        self.stack = stack
        self.ops = {k: [] for k in self.ENG}
        self.cnt = {k: 0 for k in self.ENG}
        self.waited = {k: {} for k in self.ENG}
        self.res = {}
        self.sems = {}
        for k in ('act', 'dve', 'pool', 'pe'):
            self.sems[k] = stack.enter_context(nc.semaphore('prog_' + k))
        self.dma_sems = {}
        self.dma_rr = {}
        self.dma_uses = {}
        for q in ('sp', 'pool', 'act'):
            lst = []
            for i in range(n_dma_sems):
                key = 'dma_%s_%d' % (q, i)
                self.sems[key] = stack.enter_context(nc.semaphore(key))
                self.dma_uses[key] = 0
                lst.append(key)
            self.dma_sems[q] = lst
            self.dma_rr[q] = 0
        self.psum_names = set()

    def sb(self, name, shape, dtype, stack=None):
        return (stack or self.stack).enter_context(self.nc.sbuf_tensor(name, list(shape), dtype))

    def ps(self, name, shape, dtype):
        self.psum_names.add(name)
        return self.stack.enter_context(self.nc.psum_tensor(name, list(shape), dtype))

    @staticmethod
    def _key(x):
        if isinstance(x, (str, tuple)):
            return x
        t = getattr(x, 'tensor', x)
        n = getattr(t, 'name', None)
        if n is None:
            raise ValueError('cannot derive resource key from %r' % (x,))
        return n

    def _deps(self, reads, writes):
        deps = []
        for r in reads:
            st = self.res.get(self._key(r))
            if st and st['w']:
                deps.append(st['w'])
        for w in writes:
            st = self.res.get(self._key(w))
            if st:
                if st['w']:
                    deps.append(st['w'])
                deps.extend(st['r'])
        return deps

    def _commit(self, reads, writes, tok):
        for r in reads:
            st = self.res.setdefault(self._key(r), {'w': None, 'r': []})
            st['r'].append(tok)
        for w in writes:
            self.res[self._key(w)] = {'w': tok, 'r': []}

    def _filter_waits(self, eng, deps, skip_self=False):
        out = {}
        for (sk, val) in deps:
            if skip_self and sk == eng:
                continue
            if self.waited[eng].get(sk, 0) >= val:
                continue
            if out.get(sk, 0) < val:
                out[sk] = val
        for sk, val in out.items():
            self.waited[eng][sk] = val
        return list(out.items())

    def op(self, eng, fn, reads=(), writes=()):
        pr = [r for r in reads if self._key(r) in self.psum_names]
        if pr:
            reads = [r for r in reads if self._key(r) not in self.psum_names]
            writes = list(writes) + pr
        deps = self._deps(reads, writes)
        waits = self._filter_waits(eng, deps, skip_self=(eng == 'pe'))
        self.cnt[eng] += 1
        tok = (eng, self.cnt[eng])
        self._commit(reads, writes, tok)
        self.ops[eng].append((waits, fn, (eng, 1)))
        return tok

    def dma(self, q, fn, reads=(), writes=()):
        deps = self._deps(reads, writes)
        lst = self.dma_sems[q]
        sk = lst[self.dma_rr[q] % len(lst)]
        self.dma_rr[q] += 1
        if self.dma_uses[sk] > 0:
            deps.append((sk, 16 * self.dma_uses[sk]))
        waits = self._filter_waits(q, deps)
        self.dma_uses[sk] += 1
        tok = (sk, 16 * self.dma_uses[sk])
        self._commit(reads, writes, tok)
        self.ops[q].append((waits, fn, (sk, 16)))
        return tok

    def barrier(self):
        deps = [(k, self.cnt[k]) for k in ('act', 'dve', 'pool', 'pe') if self.cnt[k] > 0]
        deps += [(sk, 16 * n) for sk, n in self.dma_uses.items() if n > 0]
        for eng in self.ENG:
            waits = self._filter_waits(eng, [d for d in deps if d[0] != eng])
            if waits:
                self.ops[eng].append((waits, None, None))
        self.res = {}

    def finish(self):
        deps = [(sk, 16 * n) for sk, n in self.dma_uses.items() if n > 0]
        waits = self._filter_waits('sp', deps)
        self.ops['sp'].append((waits, None, None))

    def emit(self):
        nc = self.nc
        allsems = list(self.sems.values())
        with nc.Block() as b0:
            def clr(e):
                for s in allsems:
                    e.sem_clear(s)
            b0.sync(clr)
        with nc.Block() as block:
            for name, meth in (('sp', block.sync), ('act', block.scalar), ('dve', block.vector),
                               ('pool', block.gpsimd), ('pe', block.tensor)):
                ops = self.ops[name]
                if not ops:
                    continue

                def body(e, ops=ops):
                    for waits, fn, inc in ops:
                        for (sk, val) in waits:
                            e.wait_ge(self.sems[sk], val)
                        if fn is None:
                            continue
                        ins = fn(e)
                        if inc is not None:
                            ins.then_inc(self.sems[inc[0]], inc[1])
                meth(body)


def host_consts():
    c = {}
    c['ident'] = np.eye(128, dtype=np.float32)
    s = np.arange(128)
    c['trilT'] = (s[:, None] <= s[None, :]).astype(np.float32)
    c['negmask'] = np.where(s[None, :] <= s[:, None], 0.0, NEG).astype(np.float32)
    bt = np.arange(64)
    c['blk64'] = ((bt[:, None] // 4 == bt[None, :] // 4) & (bt[:, None] % 4 <= bt[None, :] % 4)).astype(np.float32)
    c['ones'] = np.ones((128, 128), dtype=np.float32)
    c['iota'] = np.tile(np.arange(128, dtype=np.float32)[None, :], (128, 1))
    c['pow2'] = np.tile((2.0 ** -(np.arange(48, dtype=np.float64) + 1)).astype(np.float32)[None, :], (128, 1))
    t4 = np.arange(4)
    c['negm4'] = np.where(t4[:, None] <= t4[None, :], 0.0, NEG).astype(np.float32)
    return c


def build(cfg):
    n_pool = cfg.get('n_pool', 10240)
    do = cfg.get('phases', ('P1', 'P2', 'P3', 'P4'))
    dbg = cfg.get('dbg', False)
    nc = bass.Bass("TRN2", target_bir_lowering=False)

    def din(name, shape, dt=F32):
        return nc.dram_tensor(name, list(shape), dt, kind="ExternalInput").ap()

    def dout(name, shape, dt=F32):
        return nc.dram_tensor(name, list(shape), dt, kind="ExternalOutput").ap()

    xp_d = din('xp', [SEQ, D])
    xs_d = din('xs', [NS, D])
    memp_d = din('memp', [256, D])
    w_in_d = din('w_in', [D, P_IN])
    w_out_d = din('w_out', [D, D])
    g_mix_d = din('g_mix', [D]); g_mem_d = din('g_mem', [D]); g_memn_d = din('g_memn', [D])
    g_ffn_d = din('g_ffn', [D]); g_fin_d = din('g_fin', [D])
    ln_g_d = din('ln_g', [512]); ln_b_d = din('ln_b', [512])
    gm_ws_d = din('gm_ws', [4, 128, 128]); gm_bs_d = din('gm_bs', [4, 128])
    wq_d = din('mem_wq', [D, 512]); wkv_d = din('mem_wkv', [D, D]); wo_d = din('mem_wo', [512, D])
    c_ident = din('c_ident', [128, 128]); c_trilT = din('c_trilT', [128, 128]); c_negmask = din('c_negmask', [128, 128])
    c_blk64 = din('c_blk64', [64, 64]); c_ones = din('c_ones', [128, 128]); c_iota = din('c_iota', [128, 128])
    pwq_d = din('peer_wq', [D, D]); pkeys_d = din('peer_keys', [16, 128, 64])
    pu_d = din('peer_u', [NEXP, D]); pv_d = din('peer_v', [NEXP, D])
    ckidx_d = din('cache_kidx', [n_pool, 8192]); ck_d = din('cache_k', [2 * n_pool, 8192]); cv_d = din('cache_v', [2 * n_pool, 8192])
    pt_d = din('page_table', [DEC_B, NPAGES], I32)
    cmk_d = din('cache_mem_k', [DEC_B, 256, 512]); cmv_d = din('cache_mem_v', [DEC_B, 256, 512])
    c_pow2 = din('c_pow2', [128, 48]); c_negm4 = din('c_negm4', [4, 4])

    y_p = dout('y_p', [SEQ, D]); y_s = dout('y_s', [NS, D])
    k_p = dout('k_p', [SEQ, 128]); v_p = dout('v_p', [SEQ, 128]); ki_p = dout('ki_p', [SEQ, 64])
    gmv_p = dout('gmv_p', [128, 512])
    memk_p = dout('memk_p', [256, 512]); memv_p = dout('memv_p', [256, 512])
    k_s = dout('k_s', [NS, 128]); v_s = dout('v_s', [NS, 128]); ki_s = dout('ki_s', [NS, 64]); gmv_s = dout('gmv_s', [NS, 512])
    if dbg:
        x2_dbg = dout('x2_dbg', [SEQ + NS, D])
    x2s = nc.dram_tensor('x2s', [SEQ + NS, D], F32, kind="Internal").ap()
    UTs = nc.dram_tensor('UTs', [128, 128, D], BF16, kind="Internal").ap()
    Vs = nc.dram_tensor('Vs', [128, 128, D], BF16, kind="Internal").ap()

    with ExitStack() as st:
        p = Prog(nc, st)
        pb = [p.ps('pb%d' % i, [128, 512], F32) for i in range(8)]
        rr = [0]
        nbmod = [6]
        nbbase = [0]

        def nb():
            b = pb[nbbase[0] + rr[0] % nbmod[0]]
            rr[0] += 1
            return b

        def mm(out, lhsT, rhs, start, stop, R, W):
            p.op('pe', lambda e: e.matmul(out, lhsT=lhsT, rhs=rhs, start=start, stop=stop), reads=R, writes=W)

        identf = p.sb('identf', [128, 128], F32)
        ident = p.sb('ident', [128, 128], BF16)
        trilT = p.sb('trilT', [128, 128], F32)
        negmask = p.sb('negmask', [128, 128], F32)
        ones_bf = p.sb('ones_bf', [128, 128], BF16)
        onesf = p.sb('onesf', [128, 128], F32)
        p.dma('sp', lambda e: e.dma_start(out=identf[:], in_=c_ident[:, :]), writes=[identf])
        p.dma('sp', lambda e: e.dma_start(out=trilT[:], in_=c_trilT[:, :]), writes=[trilT])
        p.dma('sp', lambda e: e.dma_start(out=negmask[:], in_=c_negmask[:, :]), writes=[negmask])
        p.dma('sp', lambda e: e.dma_start(out=onesf[:], in_=c_ones[:, :]), writes=[onesf])
        p.op('dve', lambda e: e.tensor_copy(out=ident[:], in_=identf[:]), reads=[identf], writes=[ident])
        p.op('dve', lambda e: e.tensor_copy(out=ones_bf[:], in_=onesf[:]), reads=[onesf], writes=[ones_bf])

        def tr(out, in_, K, R, W):
            p.op('pe', lambda e: e.transpose(out=out, in_=in_, identity=ident[0:K, 0:K]), reads=list(R) + [ident], writes=W)

        gcols = {}

        def load_gcol(name, g_d):
            t = p.sb('gc_' + name, [128, 8], F32)
            p.dma('sp', lambda e: e.dma_start(out=t[:], in_=g_d.rearrange("(c q) -> q c", q=128), allow_slow_non_contiguous=True), writes=[t])
            gcols[name] = t
            return t

        def load_weight(dst, w_d, nk, ncol, gcol, stage, eng_rot=[0]):
            for c in range(nk):
                stg = stage[c % len(stage)]
                p.dma('sp', lambda e, c=c, stg=stg: e.dma_start(out=stg[:, 0:ncol], in_=w_d[c * 128:(c + 1) * 128, :]), writes=[stg])
                eng = ('dve', 'pool')[eng_rot[0] % 2]
                eng_rot[0] += 1
                if gcol is not None:
                    p.op(eng, lambda e, c=c, stg=stg: e.tensor_scalar(out=dst[:, c, :], in0=stg[:, 0:ncol], scalar1=gcol[:, c:c + 1],
                                                                     scalar2=None, op0=ALU.mult), reads=[stg, gcol], writes=[dst])
                else:
                    p.op(eng, lambda e, c=c, stg=stg: e.tensor_copy(out=dst[:, c, :], in_=stg[:, 0:ncol]), reads=[stg], writes=[dst])

        def rmsnorm_rstd(x_t, T, rstd, scratch):
            ss = rstd['ss']
            p.op('act', lambda e: e.activation(out=scratch[0:T, :], in_=x_t[0:T, :], func=AF.Square, accum_out=ss[0:T, 0:1]),
                 reads=[x_t], writes=[scratch, ss])
            p.op('dve', lambda e: e.tensor_scalar(out=ss[0:T, 1:2], in0=ss[0:T, 0:1], scalar1=1.0 / D, scalar2=EPS, op0=ALU.mult, op1=ALU.add),
                 reads=[ss], writes=[ss])
            p.op('act', lambda e: e.activation(out=ss[0:T, 2:3], in_=ss[0:T, 1:2], func=AF.Sqrt), reads=[ss], writes=[ss])
            p.op('dve', lambda e: e.reciprocal(out=ss[0:T, 3:4], in_=ss[0:T, 2:3]), reads=[ss], writes=[ss])
            return ss[0:T, 3:4]

        def norm_T(x_t, T, tag, xn, xnT, ssd, scratch, gcol=None):
            rs = rmsnorm_rstd(x_t, T, ssd, scratch)
            p.op('act', lambda e: e.activation(out=xn[0:T, :], in_=x_t[0:T, :], func=AF.Copy, scale=rs), reads=[x_t, ssd['ss']], writes=[xn])
            bk = nb()
            bv = bk[:].bitcast(BF16)
            for c in range(8):
                tr(bv[:, c * T:(c + 1) * T], xn[0:T, c * 128:(c + 1) * 128], T, [xn], [bk])
            if gcol is None:
                p.op('dve', lambda e: e.tensor_copy(out=xnT[:, :, 0:T], in_=bv[:, 0:8 * T].rearrange("q (c t) -> q c t", c=8)),
                     reads=[bk], writes=[xnT])
            else:
                for c in range(8):
                    p.op('dve', lambda e, c=c: e.tensor_scalar(out=xnT[:, c, 0:T], in0=bv[:, c * T:(c + 1) * T], scalar1=gcol[:, c:c + 1], scalar2=None, op0=ALU.mult),
                         reads=[bk, gcol], writes=[xnT])

        ssd = {'ss': p.sb('ss', [128, 4], F32)}

        for nm, gd in (('mix', g_mix_d), ('mem', g_mem_d), ('memn', g_memn_d), ('ffn', g_ffn_d)):
            load_gcol(nm, gd)
        conv_done = [0]

        def convert_chunk(ec, u_, t_, v_):
            p.dma('pool', lambda e: e.dma_start(out=u_[:, :], in_=pu_d[ec * 128:(ec + 1) * 128, :]), writes=[u_])
            p.dma('pool', lambda e: e.dma_start(out=v_[:, :], in_=pv_d[ec * 128:(ec + 1) * 128, :]), writes=[v_])
            bk = nb()
            bv = bk[:].bitcast(BF16)
            for c in range(8):
                tr(bv[:, c * 128:(c + 1) * 128], u_[:, c * 128:(c + 1) * 128], 128, [u_], [bk])
            if ec % 2 == 0:
                p.op('act', lambda e: e.copy(out=t_[:, :], in_=bv[:, :]), reads=[bk], writes=[t_])
            else:
                p.op('pool', lambda e: e.tensor_copy(out=t_[:, :], in_=bv[:, :]), reads=[bk], writes=[t_]) if False else \
                    p.op('act', lambda e: e.copy(out=t_[:, :], in_=bv[:, :]), reads=[bk], writes=[t_])
            p.dma('sp', lambda e: e.dma_start(out=UTs[ec, :, :], in_=t_[:, :]), reads=[t_], writes=[('UTs', ec)])
            p.dma('sp', lambda e: e.dma_start(out=Vs[ec, :, :], in_=v_[:, :]), reads=[v_], writes=[('Vs', ec)])

        with ExitStack() as s2:
            w_out = p.sb('w_out_sb', [128, 8, D], BF16, s2)
            wq = p.sb('wq_sb', [128, 8, 512], BF16, s2)
            wo = p.sb('wo_sb', [128, 4, D], BF16, s2)
            sq = p.sb('sq_scratch', [128, D], BF16, s2)
            xn = p.sb('xn', [128, D], BF16, s2)
            xnT = p.sb('xnT', [128, 8, 128], BF16, s2)
            W4T = p.sb('W4T', [64, 4, 64], BF16, s2)
            bsT4 = p.sb('bsT4', [64, 4], F32, s2)
            tau_c = p.sb('tau_c', [128, 1], F32, s2)
            p.op('pool', lambda e: e.memset(tau_c[:], -1.0e29), writes=[tau_c])

            bnst = p.sb('bnst', [128, 8], F32, s2)
            wsc = p.sb('wsc', [128, 8], F32, s2)
            ycat = p.sb('ycat', [128, D], BF16, s2)
            yT = p.sb('yT', [128, 8, 128], BF16, s2)
            x1 = p.sb('x1', [128, D], F32, s2)
            rden = p.sb('rden', [128, 8], F32, s2)
            qmT = p.sb('qmT', [128, 4, 128], BF16, s2)
            PmT = p.sb('PmT', [128, 2, 4, 128], BF16, s2)
            om = p.sb('om', [128, 512], BF16, s2)
            omT = p.sb('omT', [128, 4, 128], BF16, s2)
            x0s = p.sb('x0s', [64, D], F32, s2)
            ycats = p.sb('ycats', [64, D], BF16, s2)
            kvf_s = p.sb('kvf_s', [64, 328], F32, s2)
            kb_s = p.sb('kb_s', [64, 192], BF16, s2)
            wsc_s = p.sb('wsc_s', [64, 8], F32, s2)
            qTs = p.sb('qTs', [64, 8, 64], BF16, s2)
            qiTs = p.sb('qiTs', [64, 8, 64], BF16, s2)
            sw = ExitStack()
            x0 = p.sb('x0', [128, D], F32, sw)
            mkT = p.sb('mkT', [128, 4, 256], BF16, sw)
            mv_aug = p.sb('mv_aug', [128, 2, 4, 129], BF16, sw)
            WgT = p.sb('WgT', [128, 4, 128], BF16, sw)
            bsT = p.sb('bsT', [128, 4], F32, sw)
            lng = p.sb('lng', [128, 512], F32, sw)
            lnb = p.sb('lnb', [128, 512], F32, sw)
            kvf = p.sb('kvf', [128, 328], F32, sw)
            kb = p.sb('kb', [128, 192], BF16, sw)
            qT = p.sb('qT', [64, 8, 128], BF16, sw)
            qiT = p.sb('qiT', [64, 8, 128], BF16, sw)
            p.dma('sp', lambda e: e.dma_start(out=lng[:], in_=ln_g_d.partition_broadcast(128)), writes=[lng])
            p.dma('sp', lambda e: e.dma_start(out=lnb[:], in_=ln_b_d.partition_broadcast(128)), writes=[lnb])
            ub = p.sb('ub', [128, 512], F32, sw)
            gv = p.sb('gv', [128, 512], F32, sw)
            vn = p.sb('vn', [128, 512], F32, sw)
            vnb = p.sb('vnb', [128, 512], BF16, sw)
            zq = p.sb('zq', [128, 1024], BF16, sw)
            w_in = p.sb('w_in_sb', [128, 8, P_IN], BF16, sw)

            with ExitStack() as s1:
                stage = [p.sb('wstage%d' % i, [128, P_IN], F32, s1) for i in range(2)]
                load_weight(w_in, w_in_d, 8, P_IN, gcols['mix'], stage)
                load_weight(w_out, w_out_d, 8, D, None, stage)
                load_weight(wq, wq_d, 8, 512, gcols['mem'], stage)
                load_weight(wo, wo_d, 4, D, None, stage)
                wsb = p.sb('wsb', [128, 128], BF16, s1)
                for g in range(4):
                    stg = stage[g % 2]
                    p.dma('sp', lambda e, g=g, stg=stg: e.dma_start(out=stg[:, 0:128], in_=gm_ws_d[g, :, :]), writes=[stg])
                    p.op('dve', lambda e, stg=stg: e.tensor_copy(out=wsb[:], in_=stg[:, 0:128]), reads=[stg], writes=[wsb])
                    bk = nb()
                    bv = bk[:].bitcast(BF16)
                    tr(bv[:, 0:128], wsb[:, :], 128, [wsb], [bk])
                    p.op('dve', lambda e, g=g, bv=bv: e.tensor_tensor(out=WgT[:, g, :], in0=bv[:, 0:128], in1=trilT[:, :], op=ALU.mult),
                         reads=[bk, trilT], writes=[WgT])
                p.dma('sp', lambda e: e.dma_start(out=bsT[:], in_=gm_bs_d.rearrange("g t -> t g"), allow_slow_non_contiguous=True), writes=[bsT])
                w4f = p.sb('w4f', [64, 4, 64], F32, s1)
                blk64 = p.sb('blk64', [64, 64], F32, s1)
                p.dma('sp', lambda e: e.dma_start(out=blk64[:], in_=c_blk64[:, :]), writes=[blk64])
                p.op('pool', lambda e: e.memset(w4f[:], 0.0), writes=[w4f])
                for g in range(4):
                    for b in range(DEC_B):
                        p.dma('sp', lambda e, g=g, b=b: e.dma_start(
                            out=w4f[4 * b:4 * b + 4, g, 4 * b:4 * b + 4], in_=gm_ws_d[g, 0:4, 0:4].rearrange("t s -> s t"), allow_slow_non_contiguous=True),
                            writes=[w4f])
                    p.op('dve', lambda e, g=g: e.tensor_tensor(out=W4T[:, g, :], in0=w4f[:, g, :], in1=blk64[:, :], op=ALU.mult), reads=[w4f, blk64], writes=[W4T])
                for b in range(DEC_B):
                    p.dma('sp', lambda e, b=b: e.dma_start(out=bsT4[4 * b:4 * b + 4, :], in_=gm_bs_d[:, 0:4].rearrange("g t -> t g"), allow_slow_non_contiguous=True), writes=[bsT4])

                if 'P1' in do:
                    wkv = p.sb('wkv_sb', [128, 8, D], BF16, s1)
                    load_weight(wkv, wkv_d, 8, D, gcols['memn'], stage)
                    mkvf = p.sb('mkvf', [128, D], F32, s1)
                    mkb = p.sb('mkb', [128, 512], BF16, s1)
                    p.op('pool', lambda e: e.memset(mv_aug[:], 1.0), writes=[mv_aug])
                    for mt in range(2):
                        p.dma('sp', lambda e, mt=mt: e.dma_start(out=x0[:], in_=memp_d[mt * 128:(mt + 1) * 128, :]), writes=[x0])
                        norm_T(x0, 128, 'm', xn, xnT, ssd, sq)
                        b0, b1 = nb(), nb()
                        for half, bk in enumerate((b0, b1)):
                            for c in range(8):
                                mm(bk[:, :], xnT[:, c, :], wkv[:, c, half * 512:(half + 1) * 512], c == 0, c == 7, [xnT, wkv], [bk])
                        p.op('act', lambda e, b0=b0: e.copy(out=mkvf[:, 0:512], in_=b0[:, :]), reads=[b0], writes=[mkvf])
                        p.op('dve', lambda e, b1=b1: e.tensor_copy(out=mkvf[:, 512:1024], in_=b1[:, :]), reads=[b1], writes=[mkvf])
                        p.dma('sp', lambda e, mt=mt: e.dma_start(out=memk_p[mt * 128:(mt + 1) * 128, :], in_=mkvf[:, 0:512]), reads=[mkvf], writes=['memk_p'])
                        p.dma('sp', lambda e, mt=mt: e.dma_start(out=memv_p[mt * 128:(mt + 1) * 128, :], in_=mkvf[:, 512:1024]), reads=[mkvf], writes=['memv_p'])
                        p.op('dve', lambda e: e.tensor_copy(out=mkb[:], in_=mkvf[:, 0:512]), reads=[mkvf], writes=[mkb])
                        p.op('pool', lambda e, mt=mt: e.tensor_copy(out=mv_aug[:, mt, :, 0:128], in_=mkvf[:, 512:1024].rearrange("q (h d) -> q h d", h=4)),
                             reads=[mkvf], writes=[mv_aug])
                        bk = nb()
                        bv = bk[:].bitcast(BF16)
                        for h in range(4):
                            tr(bv[:, h * 128:(h + 1) * 128], mkb[:, h * 128:(h + 1) * 128], 128, [mkb], [bk])
                        p.op('act', lambda e, mt=mt, bv=bv: e.copy(out=mkT[:, :, mt * 128:(mt + 1) * 128], in_=bv[:, 0:512].rearrange("q (h m) -> q h m", h=4)),
                             reads=[bk], writes=[mkT])
            p.barrier()

            with ExitStack() as s3:
                kT = [p.sb('kT%d' % g, [64, SEQ], BF16, s3) for g in range(2)]
                kiT = p.sb('kiT', [64, SEQ], BF16, s3)
                v_aug = p.sb('v_aug', [128, NT, 2, 65], BF16, s3)
                sc = p.sb('sc', [128, SEQ], F32, s3)
                bis = p.sb('bis', [128, 8], F32, s3)
                Wtp = p.sb('Wtp', [128, 48], F32, s3)
                midp = p.sb('midp', [128, 1], F32, s3)
                pow2p = p.sb('pow2p', [128, 48], F32, s3)
                p.dma('sp', lambda e: e.dma_start(out=pow2p[:, :], in_=c_pow2[:, :]), writes=[pow2p])
                rbuf = [p.sb('rbuf%d' % i, [128, 512], F32, s3) for i in range(2)]
                m8 = p.sb('m8', [128, 8], F32, s3)
                maskb = p.sb('maskb', [128, SEQ], BF16, s3)
                maskT = p.sb('maskT', [128, NT, 128], BF16, s3)
                Eb = [p.sb('Eb%d' % i, [128, 512], BF16, s3) for i in range(3)]
                PTb = [p.sb('PTb%d' % i, [128, 4, 128], BF16, s3) for i in range(3)]
                p.op('pool', lambda e: e.memset(v_aug[:], 1.0), writes=[v_aug])
                if 'P4' in do and cfg.get('interleave_conv', True):
                    cub = [p.sb('cub%d' % i, [128, D], BF16, s3) for i in range(2)]
                    cut = [p.sb('cut%d' % i, [128, D], BF16, s3) for i in range(2)]
                    cvb = [p.sb('cvb%d' % i, [128, D], BF16, s3) for i in range(2)]

                def proj_tile(src_ap, T, ti, is_sample, x0b, ycatb):
                    p.dma('sp', lambda e: e.dma_start(out=x0b[0:T, :], in_=src_ap), writes=[x0b])
                    norm_T(x0b, T, 'a', xn, xnT, ssd, sq)
                    banks = [nb() for _ in range(5)]
                    for n5, bk in enumerate(banks):
                        c0 = n5 * 512
                        w = min(512, P_IN - c0)
                        for c in range(8):
                            mm(bk[0:T, 0:w], xnT[:, c, 0:T], w_in[:, c, c0:c0 + w], c == 0, c == 7, [xnT, w_in], [bk])
                    p.op('act', lambda e: e.activation(out=ub[0:T, :], in_=banks[0][0:T, :], func=AF.Gelu_apprx_tanh), reads=[banks[0]], writes=[ub])
                    p.op('act', lambda e: e.activation(out=gv[0:T, :], in_=banks[1][0:T, :], func=AF.Gelu_apprx_tanh), reads=[banks[1]], writes=[gv])
                    p.op('dve', lambda e: e.tensor_copy(out=zq[0:T, 0:512], in_=banks[2][0:T, :]), reads=[banks[2]], writes=[zq])
                    p.op('dve', lambda e: e.tensor_copy(out=kvf[0:T, 0:256], in_=banks[3][0:T, 0:256]), reads=[banks[3]], writes=[kvf])
                    p.op('dve', lambda e: e.tensor_copy(out=zq[0:T, 512:768], in_=banks[3][0:T, 256:512]), reads=[banks[3]], writes=[zq])
                    p.op('act', lambda e: e.copy(out=zq[0:T, 768:1024], in_=banks[4][0:T, 0:256]), reads=[banks[4]], writes=[zq])
                    p.op('act', lambda e: e.copy(out=kvf[0:T, 256:328], in_=banks[4][0:T, 256:328]), reads=[banks[4]], writes=[kvf])
                    r0 = ti * 128
                    ko, vo, kio = (k_s, v_s, ki_s) if is_sample else (k_p, v_p, ki_p)
                    p.dma('sp', lambda e: e.dma_start(out=ko[r0:r0 + T, :], in_=kvf[0:T, 0:128]), reads=[kvf], writes=['ko'])
                    p.dma('sp', lambda e: e.dma_start(out=vo[r0:r0 + T, :], in_=kvf[0:T, 128:256]), reads=[kvf], writes=['vo'])
                    p.dma('sp', lambda e: e.dma_start(out=kio[r0:r0 + T, :], in_=kvf[0:T, 256:320]), reads=[kvf], writes=['kio'])
                    p.op('dve', lambda e: e.bn_stats(out=bnst[0:T, 0:6], in_=gv[0:T, :]), reads=[gv], writes=[bnst])
                    p.op('dve', lambda e: e.bn_aggr(out=bnst[0:T, 6:8], in_=bnst[0:T, 0:6]), reads=[bnst], writes=[bnst])
                    p.op('dve', lambda e: e.tensor_scalar(out=bnst[0:T, 0:1], in0=bnst[0:T, 7:8], scalar1=EPS, scalar2=None, op0=ALU.add), reads=[bnst], writes=[bnst])
                    p.op('act', lambda e: e.activation(out=bnst[0:T, 1:2], in_=bnst[0:T, 0:1], func=AF.Sqrt), reads=[bnst], writes=[bnst])
                    p.op('dve', lambda e: e.reciprocal(out=bnst[0:T, 2:3], in_=bnst[0:T, 1:2]), reads=[bnst], writes=[bnst])
                    p.op('dve', lambda e: e.tensor_scalar(out=vn[0:T, :], in0=gv[0:T, :], scalar1=bnst[0:T, 6:7], scalar2=bnst[0:T, 2:3],
                                                          op0=ALU.subtract, op1=ALU.mult), reads=[gv, bnst], writes=[vn])
                    p.op('dve', lambda e: e.tensor_tensor(out=vn[0:T, :], in0=vn[0:T, :], in1=lng[0:T, :], op=ALU.mult), reads=[vn, lng], writes=[vn])
                    p.op('dve', lambda e: e.tensor_tensor(out=vn[0:T, :], in0=vn[0:T, :], in1=lnb[0:T, :], op=ALU.add), reads=[vn, lnb], writes=[vn])
                    if is_sample:
                        p.dma('sp', lambda e: e.dma_start(out=gmv_s[0:T, :], in_=vn[0:T, :]), reads=[vn], writes=['gmv_s'])
                    elif ti == NT - 1:
                        p.dma('sp', lambda e: e.dma_start(out=gmv_p[:, :], in_=vn[0:T, :]), reads=[vn], writes=['gmv_p'])
                    p.op('pool', lambda e: e.tensor_copy(out=vnb[0:T, :], in_=vn[0:T, :]), reads=[vn], writes=[vnb])
                    bk = nb()
                    Wm, bsm = (W4T, bsT4) if is_sample else (WgT, bsT)
                    for g in range(4):
                        mm(bk[0:T, g * 128:(g + 1) * 128], Wm[0:T, g, 0:T], vnb[0:T, g * 128:(g + 1) * 128], True, True, [Wm, vnb], [bk])
                    for g in range(4):
                        p.op('dve', lambda e, g=g, bk=bk: e.scalar_tensor_tensor(out=ycatb[0:T, g * 128:(g + 1) * 128], in0=bk[0:T, g * 128:(g + 1) * 128],
                                                                                  scalar=bsm[0:T, g:g + 1], in1=ub[0:T, g * 128:(g + 1) * 128],
                                                                                  op0=ALU.add, op1=ALU.mult), reads=[bk, bsm, ub], writes=[ycatb])

                def feature_major(T, ti, qTb, qiTb):
                    p.op('pool', lambda e: e.tensor_copy(out=kb[0:T, 0:128], in_=kvf[0:T, 0:128]), reads=[kvf], writes=[kb])
                    p.op('pool', lambda e: e.tensor_copy(out=kb[0:T, 128:192], in_=kvf[0:T, 256:320]), reads=[kvf], writes=[kb])
                    p.op('pool', lambda e: e.tensor_scalar(out=wsc[0:T, :], in0=kvf[0:T, 320:328], scalar1=8.0 ** -0.5, scalar2=None, op0=ALU.mult),
                         reads=[kvf], writes=[wsc])
                    for (src, off, dst) in ((zq, 0, qTb), (zq, 512, qiTb)):
                        bk = nb()
                        bv = bk[:].bitcast(BF16)
                        for h in range(8):
                            tr(bv[0:64, h * T:(h + 1) * T], src[0:T, off + h * 64: off + (h + 1) * 64], T, [src], [bk])
                        p.op('act', lambda e, bv=bv, dst=dst: e.copy(out=dst[:, :, 0:T], in_=bv[0:64, 0:8 * T].rearrange("q (h t) -> q h t", h=8)),
                             reads=[bk], writes=[dst])

                def prompt_dsa(ti):
                    T = 128
                    L = 128 * (ti + 1)
                    c_lo = ti * 128
                    bk = nb()
                    bv = bk[:].bitcast(BF16)
                    for g in range(3):
                        tr(bv[0:64, g * 128:(g + 1) * 128], kb[:, g * 64:(g + 1) * 64], 128, [kb], [bk])
                    p.op('act', lambda e, bv=bv: e.copy(out=kT[0][:, c_lo:c_lo + 128], in_=bv[0:64, 0:128]), reads=[bk], writes=[kT[0]])
                    p.op('act', lambda e, bv=bv: e.copy(out=kT[1][:, c_lo:c_lo + 128], in_=bv[0:64, 128:256]), reads=[bk], writes=[kT[1]])
                    p.op('act', lambda e, bv=bv: e.copy(out=kiT[:, c_lo:c_lo + 128], in_=bv[0:64, 256:384]), reads=[bk], writes=[kiT])
                    p.op('pool', lambda e: e.tensor_copy(out=v_aug[:, ti, :, 0:64], in_=kvf[:, 128:256].rearrange("q (g d) -> q g d", g=2)),
                         reads=[kvf], writes=[v_aug])
                    ri = 0
                    for c0 in range(0, L, 512):
                        w = min(512, L - c0)
                        for h in range(8):
                            bk = nb()
                            mm(bk[:, 0:w], qiT[:, h, :], kiT[:, c0:c0 + w], True, True, [qiT, kiT], [bk])
                            rb = rbuf[ri % 2]
                            ri += 1
                            p.op('act', lambda e, bk=bk, rb=rb, w=w: e.activation(out=rb[:, 0:w], in_=bk[:, 0:w], func=AF.Relu), reads=[bk], writes=[rb])
                            if h == 0:
                                p.op('dve', lambda e, rb=rb, w=w, c0=c0: e.tensor_scalar(out=sc[:, c0:c0 + w], in0=rb[:, 0:w], scalar1=wsc[:, 0:1], scalar2=None, op0=ALU.mult),
                                     reads=[rb, wsc], writes=[sc])
                            else:
                                p.op('dve', lambda e, rb=rb, w=w, c0=c0, h=h: e.scalar_tensor_tensor(out=sc[:, c0:c0 + w], in0=rb[:, 0:w], scalar=wsc[:, h:h + 1],
                                                                                                     in1=sc[:, c0:c0 + w], op0=ALU.mult, op1=ALU.add),
                                     reads=[rb, wsc, sc], writes=[sc])
                    if ti >= 2:
                        p.op('act', lambda e: e.activation(out=maskb[:, 0:L], in_=sc[:, 0:L], func=AF.Square, accum_out=bis[:, 0:1]), reads=[sc], writes=[maskb, bis])
                    p.op('dve', lambda e: e.tensor_tensor(out=sc[:, c_lo:c_lo + 128], in0=sc[:, c_lo:c_lo + 128], in1=negmask[:, :], op=ALU.add),
                         reads=[sc, negmask], writes=[sc])
                    if ti >= 2:
                        NITP = cfg.get('nit_p', 20)
                        p.op('act', lambda e: e.activation(out=bis[:, 1:2], in_=bis[:, 0:1], func=AF.Sqrt), reads=[bis], writes=[bis])
                        p.op('dve', lambda e: e.tensor_scalar(out=bis[:, 2:3], in0=bis[:, 1:2], scalar1=2.2, scalar2=2.0, op0=ALU.mult, op1=ALU.add), reads=[bis], writes=[bis])
                        p.op('dve', lambda e: e.tensor_scalar(out=Wtp[:, :], in0=pow2p[:, :], scalar1=bis[:, 2:3], scalar2=None, op0=ALU.mult), reads=[pow2p, bis], writes=[Wtp])
                        p.op('dve', lambda e: e.memset(midp[:, :], 0.0), writes=[midp])
                        for k in range(NITP):
                            p.op('dve', lambda e: e.tensor_scalar(out=maskb[:, 0:L], in0=sc[:, 0:L], scalar1=midp[:, 0:1], scalar2=None, op0=ALU.is_ge, op1=ALU.add,
                                                                  accum_out=bis[:, 3:4]), reads=[sc, midp], writes=[maskb, bis])
                            p.op('dve', lambda e: e.tensor_scalar(out=bis[:, 4:5], in0=bis[:, 3:4], scalar1=255.5, scalar2=0.5, op0=ALU.is_ge, op1=ALU.subtract), reads=[bis], writes=[bis])
                            p.op('dve', lambda e, k=k: e.scalar_tensor_tensor(out=midp[:, :], in0=bis[:, 4:5], scalar=Wtp[:, k:k + 1], in1=midp[:, :], op0=ALU.mult, op1=ALU.add),
                                 reads=[bis, Wtp, midp], writes=[midp])
                        p.op('dve', lambda e: e.tensor_tensor(out=bis[:, 5:6], in0=midp[:, :], in1=Wtp[:, NITP:NITP + 1], op=ALU.subtract), reads=[midp, Wtp], writes=[bis])
                        tau = bis[:, 5:6]
                        tau_t = bis
                    else:
                        tau = tau_c[:, 0:1]
                        tau_t = tau_c
                    p.op('dve', lambda e: e.tensor_scalar(out=maskb[:, 0:L], in0=sc[:, 0:L], scalar1=tau, scalar2=None, op0=ALU.is_ge),
                         reads=[sc, tau_t], writes=[maskb])
                    for j0 in range(0, ti + 1, 8):
                        nj = min(8, ti + 1 - j0)
                        bk = nb()
                        bv = bk[:].bitcast(BF16)
                        for jj in range(nj):
                            tr(bv[:, jj * 128:(jj + 1) * 128], maskb[:, (j0 + jj) * 128:(j0 + jj + 1) * 128], 128, [maskb], [bk])
                        p.op('act', lambda e, bv=bv, j0=j0, nj=nj: e.copy(out=maskT[:, j0:j0 + nj, :], in_=bv[:, 0:nj * 128].rearrange("q (j t) -> q j t", j=nj)),
                             reads=[bk], writes=[maskT])
                    seq = [(g, j) for g in range(2) for j in range(ti + 1)]
                    PTl = {}

                    def att_scores(idx):
                        g, j = seq[idx]
                        bk = nb()
                        mm(bk[:, :], kT[g][:, j * 128:(j + 1) * 128], qT[:, 4 * g:4 * g + 4, :].rearrange("q h t -> q (h t)"), True, True, [kT[g], qT], [bk])
                        E = Eb[idx % 3]
                        PT = PTb[idx % 3]
                        p.op('act', lambda e: e.activation(out=E[:, :], in_=bk[:, :], func=AF.Exp, scale=0.125), reads=[bk], writes=[E])
                        p.op('dve', lambda e: e.tensor_tensor(out=PT[:, :, :], in0=E[:, :].rearrange("q (h t) -> q h t", h=4),
                                                               in1=maskT[:, j:j + 1, :].to_broadcast([128, 4, 128]), op=ALU.mult),
                             reads=[E, maskT], writes=[PT])
                        PTl[idx] = PT

                    def att_pv(idx):
                        g, j = seq[idx]
                        ob = pb[6 + g]
                        PT = PTl[idx]
                        for hh in range(4):
                            mm(ob[:, hh * 65:(hh + 1) * 65], PT[:, hh, :], v_aug[:, j, g, :], (j == 0 and hh == 0), (j == ti), [PT, v_aug], [ob])
                        if j == ti:
                            p.op('dve', lambda e: e.reciprocal(out=rden[:, 4 * g:4 * g + 4], in_=ob[:, 0:260].rearrange("q (h d) -> q h d", h=4)[:, :, 64]),
                                 reads=[ob], writes=[rden])
                            p.op('dve', lambda e: e.tensor_tensor(out=ycat[:, 512 + 256 * g:512 + 256 * (g + 1)].rearrange("q (h d) -> q h d", h=4),
                                                                  in0=ob[:, 0:260].rearrange("q (h d) -> q h d", h=4)[:, :, 0:64],
                                                                  in1=rden[:, 4 * g:4 * g + 4].unsqueeze(2).to_broadcast([128, 4, 64]), op=ALU.mult),
                                 reads=[ob, rden], writes=[ycat])
                    for idx in range(len(seq) + 1):
                        if idx < len(seq):
                            att_scores(idx)
                        if idx >= 1:
                            att_pv(idx - 1)

                def out_proj_and_mem(T, row0, is_sample, x0b, ycatb):
                    bk = nb()
                    bv = bk[:].bitcast(BF16)
                    for c in range(8):
                        tr(bv[:, c * T:(c + 1) * T], ycatb[0:T, c * 128:(c + 1) * 128], T, [ycatb], [bk])
                    p.op('act', lambda e, bv=bv: e.copy(out=yT[:, :, 0:T], in_=bv[:, 0:8 * T].rearrange("q (c t) -> q c t", c=8)), reads=[bk], writes=[yT])
                    for half in range(2):
                        bk = nb()
                        for c in range(8):
                            mm(bk[0:T, :], yT[:, c, 0:T], w_out[:, c, half * 512:(half + 1) * 512], c == 0, c == 7, [yT, w_out], [bk])
                        p.op('dve', lambda e, bk=bk, half=half: e.tensor_tensor(out=x1[0:T, half * 512:(half + 1) * 512], in0=bk[0:T, :],
                                                                                in1=x0b[0:T, half * 512:(half + 1) * 512], op=ALU.add),
                             reads=[bk, x0b], writes=[x1])
                    norm_T(x1, T, 'b', xn, xnT, ssd, sq)
                    bk = nb()
                    for h in range(4):
                        for c in range(8):
                            mm(bk[:, h * T:(h + 1) * T], wq[:, c, h * 128:(h + 1) * 128], xnT[:, c, 0:T], c == 0, c == 7, [wq, xnT], [bk])
                    p.op('act', lambda e, bk=bk: e.copy(out=qmT[:, :, 0:T], in_=bk[:, 0:4 * T].rearrange("q (h t) -> q h t", h=4)), reads=[bk], writes=[qmT])
                    if not is_sample:
                        for mt in range(2):
                            bk = nb()
                            for h in range(4):
                                mm(bk[:, h * 128:(h + 1) * 128], mkT[:, h, mt * 128:(mt + 1) * 128], qmT[:, h, :], True, True, [mkT, qmT], [bk])
                            p.op('act', lambda e, bk=bk, mt=mt: e.activation(out=PmT[:, mt, :, :], in_=bk[:, :].rearrange("q (h t) -> q h t", h=4),
                                                                             func=AF.Exp, scale=128.0 ** -0.5), reads=[bk], writes=[PmT])
                        for hp in range(2):
                            ob = pb[6 + hp]
                            for mt in range(2):
                                for hh in range(2):
                                    h = 2 * hp + hh
                                    mm(ob[:, hh * 129:(hh + 1) * 129], PmT[:, mt, h, :], mv_aug[:, mt, h, :], (mt == 0 and hh == 0), (mt == 1), [PmT, mv_aug], [ob])
                            p.op('dve', lambda e, ob=ob, hp=hp: e.reciprocal(out=rden[:, 2 * hp:2 * hp + 2], in_=ob[:, 0:258].rearrange("q (h d) -> q h d", h=2)[:, :, 128]),
                                 reads=[ob], writes=[rden])
                            p.op('dve', lambda e, ob=ob, hp=hp: e.tensor_tensor(out=om[:, 256 * hp:256 * (hp + 1)].rearrange("q (h d) -> q h d", h=2),
                                                                                in0=ob[:, 0:258].rearrange("q (h d) -> q h d", h=2)[:, :, 0:128],
                                                                                in1=rden[:, 2 * hp:2 * hp + 2].unsqueeze(2).to_broadcast([128, 2, 128]), op=ALU.mult),
                                 reads=[ob, rden], writes=[om])
                        bk = nb()
                        bv = bk[:].bitcast(BF16)
                        for h in range(4):
                            tr(bv[:, h * T:(h + 1) * T], om[0:T, h * 128:(h + 1) * 128], T, [om], [bk])
                        p.op('act', lambda e, bv=bv: e.copy(out=omT[:, :, 0:T], in_=bv[:, 0:4 * T].rearrange("q (h t) -> q h t", h=4)), reads=[bk], writes=[omT])
                    else:
                        sample_mem_attn()
                    for half in range(2):
                        bk = nb()
                        for h in range(4):
                            mm(bk[0:T, :], omT[:, h, 0:T], wo[:, h, half * 512:(half + 1) * 512], h == 0, h == 3, [omT, wo], [bk])
                        p.op('dve', lambda e, bk=bk, half=half: e.tensor_tensor(out=x0b[0:T, half * 512:(half + 1) * 512], in0=bk[0:T, :],
                                                                                in1=x1[0:T, half * 512:(half + 1) * 512], op=ALU.add),
                             reads=[bk, x1], writes=[x0b])
                    p.dma('sp', lambda e: e.dma_start(out=x2s[row0:row0 + T, :], in_=x0b[0:T, :]), reads=[x0b], writes=['x2s'])
                    if dbg:
                        p.dma('sp', lambda e: e.dma_start(out=x2_dbg[row0:row0 + T, :], in_=x0b[0:T, :]), reads=[x0b], writes=['x2_dbg'])

                def sample_mem_attn():
                    for b in range(DEC_B):
                        mf, mb, mT = sm['mf'], sm['mb'], sm['mT']
                        for which, src_d in ((0, cmk_d), (1, cmv_d)):
                            p.dma('sp', lambda e, b=b, src_d=src_d, which=which: e.dma_start(out=mf[which][:, :, :], in_=src_d[b, :, :].rearrange("(mt m) f -> m mt f", mt=2)),
                                  writes=[mf[which]])
                            p.op('pool' if which else 'dve', lambda e, which=which: e.tensor_copy(out=mb[which][:, :, :], in_=mf[which][:, :, :]), reads=[mf[which]], writes=[mb[which]])
                        bk = nb()
                        bv = bk[:].bitcast(BF16)
                        for mt in range(2):
                            for h in range(4):
                                tr(bv[:, (mt * 4 + h) * 128:(mt * 4 + h + 1) * 128], mb[0][:, mt, h * 128:(h + 1) * 128], 128, [mb[0]], [bk])
                        p.op('act', lambda e, bv=bv: e.copy(out=mT[:, :, :], in_=bv[:, :].rearrange("q (k m) -> q k m", k=8)), reads=[bk], writes=[mT])
                        bk = nb()
                        for mt in range(2):
                            for h in range(4):
                                mm(bk[:, (mt * 4 + h) * 4:(mt * 4 + h + 1) * 4], mT[:, mt * 4 + h, :], qmT[:, h, 4 * b:4 * b + 4], True, True, [mT, qmT], [bk])
                        Pm = sm['Pm']
                        p.op('act', lambda e, bk=bk: e.activation(out=Pm[:, :], in_=bk[:, 0:32], func=AF.Exp, scale=128.0 ** -0.5), reads=[bk], writes=[Pm])
                        o6, o7 = pb[6], pb[7]
                        for h in range(4):
                            for mt in range(2):
                                mm(o6[:, h * 4:(h + 1) * 4], mb[1][:, mt, h * 128:(h + 1) * 128], Pm[:, (mt * 4 + h) * 4:(mt * 4 + h + 1) * 4], mt == 0, mt == 1, [mb[1], Pm], [o6])
                        for h in range(4):
                            for mt in range(2):
                                mm(o7[:, h * 4:(h + 1) * 4], ones_bf[:, :], Pm[:, (mt * 4 + h) * 4:(mt * 4 + h + 1) * 4], mt == 0, mt == 1, [ones_bf, Pm], [o7])
                        rc = sm['rc']
                        p.op('dve', lambda e: e.reciprocal(out=rc[:, 0:16], in_=o7[:, 0:16]), reads=[o7], writes=[rc])
                        p.op('dve', lambda e, b=b: e.tensor_tensor(out=omT[:, :, 4 * b:4 * b + 4], in0=o6[:, 0:16].rearrange("q (h t) -> q h t", h=4),
                                                                 in1=rc[:, 0:16].rearrange("q (h t) -> q h t", h=4), op=ALU.mult), reads=[o6, rc], writes=[omT])

                sm = {}

                def sample_dsa(stk):
                    T = NS
                    NIT = 36
                    gbufs = [p.sb('gbuf%d' % i, [64, 8192], F32, stk) for i in range(cfg.get('n_gbuf', 2))]
                    cbs = [p.sb('cbs%d' % i, [64, 8192], BF16, stk) for i in range(cfg.get('n_cbuf', 1))]
                    KTb = p.sb('KTb', [128, 64, 64], BF16, stk)
                    kiTc_l = [p.sb('kiTc%d' % i, [64, 16, 64], BF16, stk) for i in range(3)]
                    pt_sb = p.sb('pt_sb', [64, 16], I32, stk)
                    idx2 = p.sb('idx2', [64, 16, 2], I32, stk)
                    rS_l = [p.sb('rS%d' % i, [64, 16, 32], F32, stk) for i in range(3)]
                    NCH = cfg.get('nch', 4)
                    scTbs = [p.sb('scTb%d' % i, [64, 128, 4], F32, stk) for i in range(NCH)]
                    scns = [p.sb('scn%d' % i, [4, 4], F32, stk) for i in range(NCH)]
                    rSn_l = [p.sb('rSn%d' % i, [4, 32], F32, stk) for i in range(2)]
                    chunk_ctr = [0]
                    Wbc = p.sb('Wbc', [64, 16, 32], F32, stk)
                    Dg = p.sb('Dg', [64, 16, 8, 4], F32, stk)
                    q2T = p.sb('q2T', [128, 4, 64], BF16, stk)
                    kT2n = p.sb('kT2n', [128, 64], BF16, stk)
                    kiTs = p.sb('kiTs', [64, 64], BF16, stk)
                    vnf = p.sb('vnf', [4, 16, 128], F32, stk)
                    vnew = p.sb('vnew', [4, 16, 128], BF16, stk)
                    negm4 = p.sb('negm4', [4, 4], F32, stk)
                    pow2 = p.sb('pow2', [64, 48], F32, stk)
                    hs_l = [p.sb('hs%d' % i, [64, 4], F32, stk) for i in range(NCH)]
                    hsb = p.sb('hsb', [64, 2], BF16, stk)
                    Wsc_l = [p.sb('Wsc%d' % i, [64, 2], F32, stk) for i in range(NCH)]
                    Wtab_l = [p.sb('Wtab%d' % i, [64, 48], F32, stk) for i in range(NCH)]
                    lo_l = [p.sb('lo%d' % i, [64, 4], F32, stk) for i in range(NCH)]
                    mid_l = [p.sb('mid%d' % i, [64, 4], F32, stk) for i in range(NCH)]
                    ge_l = [p.sb('ge%d' % i, [64, 4], F32, stk) for i in range(NCH)]
                    cmpb_l = [p.sb('cmpb%d' % i, [64, 128, 4], BF16, stk) for i in range(NCH)]
                    cntp_l = [p.sb('cntp%d' % i, [64, 4], F32, stk) for i in range(NCH)]
                    cmpn_l = [p.sb('cmpn%d' % i, [4, 4], F32, stk) for i in range(NCH)]
                    mask_s_l = cmpb_l
                    maskn_l = [p.sb('maskn%d' % i, [4, 4], BF16, stk) for i in range(NCH)]
                    Ess = [p.sb('Es%d' % i, [64, 16, 32], BF16, stk) for i in range(2)]
                    PTss = [p.sb('PTs%d' % i, [64, 16, 32], BF16, stk) for i in range(2)]
                    En = p.sb('En', [4, 32], BF16, stk)
                    PTn = p.sb('PTn', [4, 32], BF16, stk)
                    rcs = p.sb('rcs', [128, 32], F32, stk)
                    dacc = p.sb('dacc', [64, 8, 32], F32, stk)
                    dtot = p.sb('dtot', [64, 32], F32, stk)
                    q2bd = p.sb('q2bd', [128, 16, 32], BF16, stk)
                    ybTs = p.sb('ybTs', [128, 4, 64], BF16, stk)

                    p.dma('sp', lambda e: e.dma_start(out=pt_sb[:, :], in_=pt_d.rearrange("b j -> j b"), allow_slow_non_contiguous=True), writes=[pt_sb])
                    p.dma('sp', lambda e: e.dma_start(out=negm4[:, :], in_=c_negm4[:, :]), writes=[negm4])
                    p.dma('sp', lambda e: e.dma_start(out=pow2[:, :], in_=c_pow2[0:64, :]), writes=[pow2])
                    for half in range(2):
                        p.op('dve', lambda e, half=half: e.tensor_scalar(out=idx2[:, :, half], in0=pt_sb[:, :], scalar1=2, scalar2=half, op0=ALU.mult, op1=ALU.add),
                             reads=[pt_sb], writes=[idx2])
                    p.op('dve', lambda e: e.tensor_tensor(out=Dg[:, :, :, :], in0=wsc_s[:, :].unsqueeze(1).unsqueeze(3).to_broadcast([64, 16, 8, 4]),
                                                          in1=identf[0:64, 0:64].rearrange("q (b t) -> q b t", b=16).unsqueeze(2).to_broadcast([64, 16, 8, 4]), op=ALU.mult),
                         reads=[wsc_s, identf], writes=[Dg])
                    bk = nb()
                    mm(bk[0:64, :], onesf[0:64, 0:64], Dg[:, :, :, :].rearrange("q b h t -> q (b h t)"), True, True, [onesf, Dg], [bk])
                    p.op('act', lambda e, bk=bk: e.copy(out=Wbc[:, :, :], in_=bk[0:64, :].rearrange("q (b x) -> q b x", b=16)), reads=[bk], writes=[Wbc])
                    p.op('pool', lambda e: e.tensor_copy(out=q2T[0:64, :, :], in_=qTs[:, 0:4, :]), reads=[qTs], writes=[q2T])
                    p.dma('sp', lambda e: e.dma_start(out=q2T[64:128, :, :], in_=qTs[:, 4:8, :]), reads=[qTs], writes=[q2T])
                    p.op('dve', lambda e: e.memset(q2bd[:, :, :], 0.0), writes=[q2bd])
                    for g in range(2):
                        p.op('dve', lambda e, g=g: e.tensor_copy(out=q2bd[g * 64:(g + 1) * 64, :, g * 16:(g + 1) * 16].rearrange("q b (r t) -> q b r t", r=4),
                                                                 in_=q2T[g * 64:(g + 1) * 64, :, :].rearrange("q r (b t) -> q b r t", t=4)), reads=[q2T], writes=[q2bd])
                    bk = nb()
                    bv = bk[:].bitcast(BF16)
                    tr(bv[:, 0:64], kb_s[:, 0:128], 64, [kb_s], [bk])
                    tr(bv[0:64, 64:128], kb_s[:, 128:192], 64, [kb_s], [bk])
                    p.op('act', lambda e, bv=bv: e.copy(out=kT2n[:, :], in_=bv[:, 0:64]), reads=[bk], writes=[kT2n])
                    p.op('act', lambda e, bv=bv: e.copy(out=kiTs[:, :], in_=bv[0:64, 64:128]), reads=[bk], writes=[kiTs])
                    p.dma('sp', lambda e: e.dma_start(out=vnf[:, :, :], in_=v_s.rearrange("(b t) d -> t b d", t=4)), reads=['vo'], writes=[vnf])
                    p.op('dve', lambda e: e.tensor_copy(out=vnew[:, :, :], in_=vnf[:, :, :]), reads=[vnf], writes=[vnew])

                    nb_ = cfg.get('n_sb', DEC_B)
                    NITB = cfg.get('nit', 22)
                    gseq = []
                    for b0_ in range(0, nb_, NCH):
                        grp_ = list(range(b0_, min(b0_ + NCH, nb_)))
                        gseq += [('ki', b_, 0) for b_ in grp_]
                        for b_ in grp_:
                            gseq += [('K', b_, 0), ('V', b_, 0), ('K', b_, 1), ('V', b_, 1)]
                    gpos = {it: i for i, it in enumerate(gseq)}
                    g_next = [0]

                    NGB = len(gbufs)

                    def emit_gather(i):
                        kind, b_, half = gseq[i]
                        gb = gbufs[i % NGB]
                        if kind == 'ki':
                            src, idx_ap, idx_t = ckidx_d, pt_sb[:, b_:b_ + 1], pt_sb
                        else:
                            src, idx_ap, idx_t = (ck_d if kind == 'K' else cv_d), idx2[:, b_, half:half + 1], idx2
                        p.dma('pool', lambda e: e.indirect_dma_start(out=gb[:, :], out_offset=None, in_=src[:, :],
                                                                     in_offset=bass.IndirectOffsetOnAxis(ap=idx_ap, axis=0)), reads=[idx_t], writes=[gb])

                    g_cast = [0]

                    def use(item):
                        i = gpos[item]
                        while g_next[0] < min(NGB, len(gseq)):
                            emit_gather(g_next[0])
                            g_next[0] += 1
                        while g_cast[0] <= i:
                            c = g_cast[0]
                            dst = cbs[c % len(cbs)]
                            gb = gbufs[c % NGB]
                            p.op('act', lambda e, dst=dst, gb=gb: e.copy(out=dst[:, 0:4096], in_=gb[:, 0:4096]), reads=[gb], writes=[dst])
                            p.op('dve', lambda e, dst=dst, gb=gb: e.tensor_copy(out=dst[:, 4096:8192], in_=gb[:, 4096:8192]), reads=[gb], writes=[dst])
                            g_cast[0] += 1
                            if g_next[0] < len(gseq):
                                emit_gather(g_next[0])
                                g_next[0] += 1
                        return cbs[i % len(cbs)]

                    def ki_steps(b):
                        qi_b = qiTs[:, :, 4 * b:4 * b + 4]
                        scT = scTbs[b % NCH]
                        scn_ = scns[b % NCH]
                        steps = []

                        def chunk(lc):
                            kiTc = kiTc_l[chunk_ctr[0] % 3]
                            rS = rS_l[chunk_ctr[0] % 3]
                            chunk_ctr[0] += 1
                            cb = use(('ki', b, 0))
                            bk = nb()
                            bv = bk[:].bitcast(BF16)
                            for i in range(16):
                                l = lc * 16 + i
                                tr(bv[0:64, i * 64:(i + 1) * 64], cb[:, l * 64:(l + 1) * 64], 64, [cb], [bk])
                            p.op('act', lambda e: e.copy(out=kiTc[:, :, :], in_=bv[0:64, :].rearrange("q (i j) -> q i j", i=16)), reads=[bk], writes=[kiTc])
                            bk2 = nb()
                            for i in range(16):
                                mm(bk2[0:64, i * 32:(i + 1) * 32], kiTc[:, i, :], qi_b, True, True, [kiTc, qiTs], [bk2])
                            p.op('act', lambda e: e.activation(out=rS[:, :, :], in_=bk2[0:64, :].rearrange("q (i x) -> q i x", i=16), func=AF.Relu), reads=[bk2], writes=[rS])
                            p.op('dve', lambda e: e.tensor_tensor(out=rS[:, :, :], in0=rS[:, :, :], in1=Wbc[:, b:b + 1, :].to_broadcast([64, 16, 32]), op=ALU.mult),
                                 reads=[rS, Wbc], writes=[rS])
                            p.op('dve', lambda e: e.tensor_reduce(out=scT[:, lc * 16:(lc + 1) * 16, :], in_=rS[:, :, :].rearrange("q i (h t) -> q i t h", h=8),
                                                                  op=ALU.add, axis=AX.X), reads=[rS], writes=[scT])

                        def newkeys():
                            rSn = rSn_l[b % 2]
                            bk = nb()
                            mm(bk[0:4, 0:32], kiTs[:, 4 * b:4 * b + 4], qi_b, True, True, [kiTs, qiTs], [bk])
                            p.op('act', lambda e: e.activation(out=rSn[:, :], in_=bk[0:4, 0:32], func=AF.Relu), reads=[bk], writes=[rSn])
                            p.op('dve', lambda e: e.tensor_tensor(out=rSn[:, :], in0=rSn[:, :], in1=Wbc[0:4, b, :], op=ALU.mult), reads=[rSn, Wbc], writes=[rSn])
                            p.op('dve', lambda e: e.tensor_reduce(out=scn_[:, :], in_=rSn[:, :].rearrange("q (h t) -> q t h", h=8), op=ALU.add, axis=AX.X), reads=[rSn], writes=[scn_])
                        for lc in range(8):
                            steps.append(lambda lc=lc: chunk(lc))
                        steps.append(newkeys)
                        return steps

                    def bisect_setup(b):
                        c = b % NCH
                        scT, scn_, hs, Wsc, Wtab, lo, cmpb = scTbs[c], scns[c], hs_l[c], Wsc_l[c], Wtab_l[c], lo_l[c], cmpb_l[c]
                        p.op('act', lambda e: e.activation(out=cmpb[:, :, :].rearrange("q l t -> q (l t)"), in_=scT[:, :, :].rearrange("q l t -> q (l t)"), func=AF.Square, accum_out=hs[:, 0:1]),
                             reads=[scT], writes=[cmpb, hs])
                        p.op('act', lambda e: e.activation(out=cmpb[0:4, 0, :], in_=scn_[:, :], func=AF.Square, accum_out=hs[0:4, 1:2]), reads=[scn_], writes=[cmpb, hs])
                        p.op('dve', lambda e: e.tensor_tensor(out=scn_[:, :], in0=scn_[:, :], in1=negm4[:, :], op=ALU.add), reads=[scn_, negm4], writes=[scn_])
                        bk = nb()
                        mm(bk[0:64, 0:1], onesf[0:64, 0:64], hs[:, 0:1], True, False, [onesf, hs], [bk])
                        mm(bk[0:64, 0:1], onesf[0:4, 0:64], hs[0:4, 1:2], False, True, [onesf, hs], [bk])
                        p.op('act', lambda e, bk=bk: e.activation(out=Wsc[:, 0:1], in_=bk[0:64, 0:1], func=AF.Sqrt), reads=[bk], writes=[Wsc])
                        p.op('dve', lambda e: e.tensor_scalar(out=Wsc[:, 0:1], in0=Wsc[:, 0:1], scalar1=2.2, scalar2=2.0, op0=ALU.mult, op1=ALU.add), reads=[Wsc], writes=[Wsc])
                        p.op('dve', lambda e: e.tensor_scalar(out=Wsc[:, 1:2], in0=Wsc[:, 0:1], scalar1=-0.5, scalar2=None, op0=ALU.mult), reads=[Wsc], writes=[Wsc])
                        p.op('dve', lambda e: e.tensor_scalar(out=Wtab[:, :], in0=pow2[:, :], scalar1=Wsc[:, 0:1], scalar2=None, op0=ALU.mult), reads=[pow2, Wsc], writes=[Wtab])
                        p.op('dve', lambda e: e.tensor_scalar(out=lo[:, :], in0=pow2[:, 0:4], scalar1=0.0, scalar2=Wsc[:, 1:2], op0=ALU.mult, op1=ALU.add), reads=[pow2, Wsc], writes=[lo])

                    def bisect_iter(b, k):
                        c = b % NCH
                        scT, scn_, Wtab, lo, mid, ge, cmpb, cntp, cmpn = scTbs[c], scns[c], Wtab_l[c], lo_l[c], mid_l[c], ge_l[c], cmpb_l[c], cntp_l[c], cmpn_l[c]
                        p.op('dve', lambda e: e.tensor_scalar(out=mid[:, :], in0=lo[:, :], scalar1=Wtab[:, k:k + 1], scalar2=None, op0=ALU.add), reads=[lo, Wtab], writes=[mid])
                        p.op('dve', lambda e: e.tensor_tensor(out=cmpb[:, :, :], in0=scT[:, :, :], in1=mid[:, :].unsqueeze(1).to_broadcast([64, 128, 4]), op=ALU.is_ge),
                             reads=[scT, mid], writes=[cmpb])
                        p.op('dve', lambda e: e.tensor_reduce(out=cntp[:, :], in_=cmpb[:, :, :].rearrange("q l t -> q t l"), op=ALU.add, axis=AX.X), reads=[cmpb], writes=[cntp])
                        p.op('dve', lambda e: e.tensor_tensor(out=cmpn[:, :], in0=scn_[:, :], in1=mid[0:4, :], op=ALU.is_ge), reads=[scn_, mid], writes=[cmpn])
                        bk = nb()
                        mm(bk[0:64, 0:4], onesf[0:64, 0:64], cntp[:, :], True, False, [onesf, cntp], [bk])
                        mm(bk[0:64, 0:4], onesf[0:4, 0:64], cmpn[:, :], False, True, [onesf, cmpn], [bk])
                        p.op('dve', lambda e: e.tensor_scalar(out=ge[:, :], in0=bk[0:64, 0:4], scalar1=255.5, scalar2=None, op0=ALU.is_ge), reads=[bk], writes=[ge])
                        p.op('dve', lambda e: e.scalar_tensor_tensor(out=lo[:, :], in0=ge[:, :], scalar=Wtab[:, k:k + 1], in1=lo[:, :], op0=ALU.mult, op1=ALU.add),
                             reads=[ge, Wtab, lo], writes=[lo])

                    def bisect_finish(b):
                        c = b % NCH
                        scT, scn_, lo, mask_s, maskn = scTbs[c], scns[c], lo_l[c], mask_s_l[c], maskn_l[c]
                        p.op('dve', lambda e: e.tensor_tensor(out=mask_s[:, :, :], in0=scT[:, :, :], in1=lo[:, :].unsqueeze(1).to_broadcast([64, 128, 4]), op=ALU.is_ge),
                             reads=[scT, lo], writes=[mask_s])
                        p.op('dve', lambda e: e.tensor_tensor(out=maskn[:, :], in0=scn_[:, :], in1=lo[0:4, :], op=ALU.is_ge), reads=[scn_, lo], writes=[maskn])

                    def attend(b):
                        mask_s, maskn = mask_s_l[b % NCH], maskn_l[b % NCH]
                        o6, o7 = pb[6], pb[7]
                        first = True
                        for half in range(2):
                            cbK = use(('K', b, half))
                            for lc in range(4):
                                bk = nb()
                                bv = bk[:].bitcast(BF16)
                                for i in range(16):
                                    l = lc * 16 + i
                                    tr(bv[:, i * 64:(i + 1) * 64], cbK[:, l * 128:(l + 1) * 128], 64, [cbK], [bk])
                                p.op('act', lambda e, bv=bv, lc=lc: e.copy(out=KTb[:, lc * 16:(lc + 1) * 16, :], in_=bv[:, :].rearrange("q (i j) -> q i j", i=16)), reads=[bk], writes=[KTb])
                            cbV = use(('V', b, half))
                            prev = None
                            for lc in range(5):
                                if lc < 4:
                                    bk = nb()
                                    for i in range(16):
                                        l = lc * 16 + i
                                        mm(bk[0:64, i * 32:(i + 1) * 32], KTb[:, l, :], q2bd[:, b, :], True, True, [KTb, q2bd], [bk])
                                    E_, P_ = Ess[lc % 2], PTss[lc % 2]
                                    p.op('act', lambda e, bk=bk, E_=E_: e.activation(out=E_[:, :, :], in_=bk[0:64, :].rearrange("q (i x) -> q i x", i=16), func=AF.Exp, scale=0.125),
                                         reads=[bk], writes=[E_])
                                    l0 = half * 64 + lc * 16
                                    p.op('dve', lambda e, l0=l0, E_=E_, P_=P_: e.tensor_tensor(out=P_[:, :, :].rearrange("q i (x t) -> q i x t", t=4), in0=E_[:, :, :].rearrange("q i (x t) -> q i x t", t=4),
                                                                                              in1=mask_s[:, l0:l0 + 16, :].unsqueeze(2).to_broadcast([64, 16, 8, 4]), op=ALU.mult),
                                         reads=[E_, mask_s], writes=[P_])
                                    ci = half * 4 + lc
                                    p.op('dve', lambda e, P_=P_, ci=ci: e.tensor_reduce(out=dacc[:, ci, :], in_=P_[:, :, :].rearrange("q i x -> q x i"), op=ALU.add, axis=AX.X),
                                         reads=[P_], writes=[dacc])
                                if prev is not None:
                                    plc, P_prev = prev
                                    for i in range(16):
                                        l = plc * 16 + i
                                        mm(o6[:, 0:32], cbV[:, l * 128:(l + 1) * 128], P_prev[:, i, :], first, False, [cbV, P_prev], [o6])
                                        first = False
                                prev = (lc, PTss[lc % 2]) if lc < 4 else None
                        bk = nb()
                        mm(bk[0:4, 0:32], kT2n[:, 4 * b:4 * b + 4], q2bd[:, b, :], True, True, [kT2n, q2bd], [bk])
                        p.op('act', lambda e, bk=bk: e.activation(out=En[:, :], in_=bk[0:4, 0:32], func=AF.Exp, scale=0.125), reads=[bk], writes=[En])
                        p.op('dve', lambda e: e.tensor_tensor(out=PTn[:, :].rearrange("q (x t) -> q x t", t=4), in0=En[:, :].rearrange("q (x t) -> q x t", t=4),
                                                               in1=maskn[:, :].unsqueeze(1).to_broadcast([4, 8, 4]), op=ALU.mult), reads=[En, maskn], writes=[PTn])
                        mm(o6[:, 0:32], vnew[:, b, :], PTn[:, :], False, True, [vnew, PTn], [o6])
                        p.op('dve', lambda e: e.tensor_reduce(out=dtot[:, :], in_=dacc[:, :, :].rearrange("q c x -> q x c"), op=ALU.add, axis=AX.X), reads=[dacc], writes=[dtot])
                        p.op('dve', lambda e: e.tensor_tensor(out=dtot[0:4, :], in0=dtot[0:4, :], in1=PTn[:, :], op=ALU.add), reads=[dtot, PTn], writes=[dtot])
                        mm(o7[:, 0:32], onesf[0:64, :], dtot[:, :], True, True, [onesf, dtot], [o7])
                        p.op('dve', lambda e: e.reciprocal(out=rcs[:, :], in_=o7[:, 0:32]), reads=[o7], writes=[rcs])
                        for g in range(2):
                            p.op('dve', lambda e, g=g: e.tensor_tensor(out=ybTs[g * 64:(g + 1) * 64, :, 4 * b:4 * b + 4],
                                                                     in0=o6[g * 64:(g + 1) * 64, g * 16:(g + 1) * 16].rearrange("q (r t) -> q r t", r=4),
                                                                     in1=rcs[g * 64:(g + 1) * 64, g * 16:(g + 1) * 16].rearrange("q (r t) -> q r t", r=4), op=ALU.mult),
                                 reads=[o6, rcs], writes=[ybTs])

                    for b0_ in range(0, nb_, NCH):
                        grp_ = list(range(b0_, min(b0_ + NCH, nb_)))
                        for b in grp_:
                            for st_ in ki_steps(b):
                                st_()
                        if cfg.get('sb_stage', 9) < 2:
                            continue
                        for b in grp_:
                            bisect_setup(b)
                        for k in range(NITB):
                            for b in grp_:
                                bisect_iter(b, k)
                        for b in grp_:
                            bisect_finish(b)
                        if cfg.get('sb_stage', 9) < 3:
                            continue
                        for b in grp_:
                            attend(b)
                    for r in range(4):
                        bk = nb()
                        bv = bk[:].bitcast(BF16)
                        tr(bv[0:64, 0:128], ybTs[:, r, :], 128, [ybTs], [bk])
                        p.op('act', lambda e, bv=bv, r=r: e.copy(out=ycats[:, 512:1024].rearrange("q (g r d) -> q g r d", g=2, r=4)[:, :, r, :],
                                                                 in_=bv[0:64, 0:128].rearrange("q (g d) -> q g d", g=2)), reads=[bk], writes=[ycats])


                if 'P3' in do:
                    proj_tile(xs_d[:, :], NS, 0, True, x0s, ycats)
                    feature_major(NS, 0, qTs, qiTs)
                    p.op('pool', lambda e: e.tensor_copy(out=kvf_s[:, :], in_=kvf[0:NS, :]), reads=[kvf], writes=[kvf_s])
                    p.op('pool', lambda e: e.tensor_copy(out=kb_s[:, :], in_=kb[0:NS, :]), reads=[kb], writes=[kb_s])
                    p.op('pool', lambda e: e.tensor_copy(out=wsc_s[:, :], in_=wsc[0:NS, :]), reads=[wsc], writes=[wsc_s])
                if 'P2' in do:
                    for ti in range(cfg.get('n_tiles', NT)):
                        proj_tile(xp_d[ti * 128:(ti + 1) * 128, :], 128, ti, False, x0, ycat)
                        feature_major(128, ti, qT, qiT)
                        if 'P4' in do and cfg.get('interleave_conv', True):
                            for ec in range(ti * 8, ti * 8 + 8):
                                convert_chunk(ec, cub[ec % 2], cut[ec % 2], cvb[ec % 2])
                            conv_done[0] = ti * 8 + 8
                        prompt_dsa(ti)
                        out_proj_and_mem(128, ti * 128, False, x0, ycat)
            sw.close()
            p.barrier()
            if 'P3' in do:
                with ExitStack() as s3s:
                    sample_dsa(s3s)
                p.barrier()
                with ExitStack() as s3m:
                    sm['mf'] = [p.sb('mf%d' % i, [128, 2, 512], F32, s3m) for i in range(2)]
                    sm['mb'] = [p.sb('mb%d' % i, [128, 2, 512], BF16, s3m) for i in range(2)]
                    sm['mT'] = p.sb('mT', [128, 8, 128], BF16, s3m)
                    sm['Pm'] = p.sb('Pm', [128, 32], BF16, s3m)
                    sm['rc'] = p.sb('rc', [128, 16], F32, s3m)
                    out_proj_and_mem(NS, SEQ, True, x0s, ycats)
            p.barrier()

        if 'P4' in do:
            nbmod[0] = 4
            with ExitStack() as s4:
                iota_f = p.sb('iota_f', [128, 128], F32, s4)
                gfin = p.sb('gfin', [128, D], F32, s4)
                p.dma('sp', lambda e: e.dma_start(out=iota_f[:], in_=c_iota[:, :]), writes=[iota_f])
                p.dma('sp', lambda e: e.dma_start(out=gfin[:], in_=g_fin_d.partition_broadcast(128)), writes=[gfin])
                pwq = p.sb('pwq_sb', [128, 8, D], BF16, s4)
                keysT = p.sb('keysT', [64, 16, 128], BF16, s4)
                with ExitStack() as s5:
                    stage = [p.sb('pstage%d' % i, [128, D], F32, s5) for i in range(2)]
                    load_weight(pwq, pwq_d, 8, D, None, stage)
                    kbf = p.sb('kbf', [128, 64], BF16, s5)
                    for hc in range(16):
                        stg = stage[hc % 2]
                        p.dma('sp', lambda e, hc=hc, stg=stg: e.dma_start(out=stg[:, 0:64], in_=pkeys_d[hc, :, :]), writes=[stg])
                        p.op('dve', lambda e, stg=stg: e.tensor_copy(out=kbf[:], in_=stg[:, 0:64]), reads=[stg], writes=[kbf])
                        bk = nb()
                        bv = bk[:].bitcast(BF16)
                        tr(bv[0:64, 0:128], kbf[:, :], 128, [kbf], [bk])
                        p.op('act', lambda e, hc=hc, bv=bv: e.copy(out=keysT[:, hc, :], in_=bv[0:64, 0:128]), reads=[bk], writes=[keysT])
                    ubf = [p.sb('ubf%d' % i, [128, D], BF16, s5) for i in range(2)]
                    utb = [p.sb('utb%d' % i, [128, D], BF16, s5) for i in range(2)]
                    vbf = [p.sb('vbf%d' % i, [128, D], BF16, s5) for i in range(2)]
                    for ec in range(conv_done[0], cfg.get('n_ec', 128)):
                        convert_chunk(ec, ubf[ec % 2], utb[ec % 2], vbf[ec % 2])

                p.barrier()
                xg_l = [p.sb('xg%d' % i, [128, 2, D], F32, s4) for i in range(2)]
                XT_l = [p.sb('XT%d' % i, [128, 8, 256], BF16, s4) for i in range(2)]
                hn = p.sb('hn', [128, D], BF16, s4)
                hnT = p.sb('hnT', [128, 8, 128], BF16, s4)
                sq4 = p.sb('sq4', [128, D], F32, s4)
                qpT = p.sb('qpT', [64, 16, 128], BF16, s4)
                ssb = p.sb('ssb', [128, 16, 128], F32, s4)
                wk = p.sb('wk', [128, 128], F32, s4)
                a16 = p.sb('a16', [128, 8, 16], F32, s4)
                b16 = p.sb('b16', [128, 8, 16], F32, s4)
                iau = p.sb('iau', [128, 8, 16], U32, s4)
                ibu = p.sb('ibu', [128, 8, 16], U32, s4)
                iaf = p.sb('iaf', [128, 8, 16], F32, s4)
                ibf = p.sb('ibf', [128, 8, 16], F32, s4)
                cand = p.sb('cand', [128, 8, 256], F32, s4)
                wkc = p.sb('wkc', [128, 256], F32, s4)
                c16 = p.sb('c16', [128, 8, 16], F32, s4)
                icu = p.sb('icu', [128, 8, 16], U32, s4)
                iju = p.sb('iju', [128, 2, 128], U32, s4)
                ijf = p.sb('ijf', [128, 2, 128], F32, s4)
                eq = p.sb('eq', [128, 128, 16], F32, s4)
                SEL = p.sb('SEL', [128, 3, 128], F32, s4)
                e16 = p.sb('e16', [128, 8, 16], F32, s4)
                z8 = p.sb('z8', [128, 16], F32, s4)
                selT_l = [p.sb('selT%d' % i, [128, 3, 256], F32, s4) for i in range(2)]
                Gt = p.sb('Gt', [128, 256, 128], BF16, s4)
                ohb1 = [p.sb('ohb1_%d' % i, [128, 8, 128], BF16, s4) for i in range(2)]
                ohb0 = [p.sb('ohb0_%d' % i, [128, 8, 128], BF16, s4) for i in range(2)]
                ohbg = [p.sb('ohbg_%d' % i, [128, 8, 128], BF16, s4) for i in range(2)]
                selTb_l = [p.sb('selTb%d' % i, [128, 3, 256], BF16, s4) for i in range(2)]
                iota_b = p.sb('iota_b', [128, 128], BF16, s4)
                p.op('pool', lambda e: e.tensor_copy(out=iota_b[:, :], in_=iota_f[:, :]), reads=[iota_f], writes=[iota_b])
                NBUF = 4
                UTc = [p.sb('UTc%d' % i, [128, 8, 128], BF16, s4) for i in range(NBUF)]
                Vc = [p.sb('Vc%d' % i, [128, D], BF16, s4) for i in range(NBUF)]
                gab = [p.sb('gab%d' % i, [128, 256], BF16, s4) for i in range(3)]
                GAb = [p.sb('GAb%d' % i, [128, 256], BF16, s4) for i in range(3)]
                pre = p.sb('pre', [128, D], F32, s4)
                yo = pre


                def peer_select(T, s, xg, XT, selT, selTb):
                    norm_T(xg[:, s, :], T, 'f', hn, hnT, ssd, sq4, gcol=gcols['ffn'])
                    p.op('pool', lambda e: e.tensor_copy(out=XT[:, :, s * 128:s * 128 + T], in_=hnT[:, :, 0:T]), reads=[hnT], writes=[XT])
                    for q4 in range(4):
                        bk = nb()
                        for k4 in range(4):
                            hc = q4 * 4 + k4
                            for c in range(8):
                                mm(bk[0:64, k4 * T:(k4 + 1) * T], pwq[:, c, hc * 64:(hc + 1) * 64], hnT[:, c, 0:T], c == 0, c == 7, [pwq, hnT], [bk])
                        p.op('act', lambda e, bk=bk, q4=q4: e.copy(out=qpT[:, q4 * 4:(q4 + 1) * 4, 0:T], in_=bk[0:64, 0:4 * T].rearrange("q (k t) -> q k t", k=4)),
                             reads=[bk], writes=[qpT])
                    for q4 in range(4):
                        bk = nb()
                        for k4 in range(4):
                            hc = q4 * 4 + k4
                            mm(bk[0:T, k4 * 128:(k4 + 1) * 128], qpT[:, hc, 0:T], keysT[:, hc, :], True, True, [qpT, keysT], [bk])
                        p.op('act', lambda e, bk=bk, q4=q4: e.copy(out=ssb[0:T, q4 * 4:(q4 + 1) * 4, :], in_=bk[0:T, :].rearrange("q (k n) -> q k n", k=4)),
                             reads=[bk], writes=[ssb])
                    for h in range(8):
                        for cc, (vals, idxs) in enumerate(((a16, iau), (b16, ibu))):
                            src = ssb[0:T, 2 * h + cc, :]
                            p.op('dve', lambda e, src=src, vals=vals, h=h: e.max(out=vals[0:T, h, 0:8], in_=src), reads=[ssb], writes=[vals])
                            p.op('dve', lambda e, src=src, vals=vals, idxs=idxs, h=h: e.max_index(out=idxs[0:T, h, 0:8], in_max=vals[0:T, h, 0:8], in_values=src),
                                 reads=[ssb, vals], writes=[idxs])
                            p.op('dve', lambda e, src=src, vals=vals, h=h: e.match_replace(out=wk[0:T, :], in_to_replace=vals[0:T, h, 0:8], in_values=src, imm_value=NEG),
                                 reads=[ssb, vals], writes=[wk])
                            p.op('dve', lambda e, vals=vals, h=h: e.max(out=vals[0:T, h, 8:16], in_=wk[0:T, :]), reads=[wk], writes=[vals])
                            p.op('dve', lambda e, vals=vals, idxs=idxs, h=h: e.max_index(out=idxs[0:T, h, 8:16], in_max=vals[0:T, h, 8:16], in_values=wk[0:T, :]),
                                 reads=[wk, vals], writes=[idxs])
                    for h in range(8):
                        p.op('dve', lambda e, h=h: e.tensor_tensor(out=cand[0:T, h, :].rearrange("q (i j) -> q i j", i=16),
                                                                    in0=a16[0:T, h, :].unsqueeze(2).to_broadcast([T, 16, 16]),
                                                                    in1=b16[0:T, h, :].unsqueeze(1).to_broadcast([T, 16, 16]), op=ALU.add),
                             reads=[a16, b16], writes=[cand])
                    for h in range(8):
                        src = cand[0:T, h, :]
                        p.op('dve', lambda e, src=src, h=h: e.max(out=c16[0:T, h, 0:8], in_=src), reads=[cand], writes=[c16])
                        p.op('dve', lambda e, src=src, h=h: e.max_index(out=icu[0:T, h, 0:8], in_max=c16[0:T, h, 0:8], in_values=src), reads=[cand, c16], writes=[icu])
                        p.op('dve', lambda e, src=src, h=h: e.match_replace(out=wkc[0:T, :], in_to_replace=c16[0:T, h, 0:8], in_values=src, imm_value=NEG),
                             reads=[cand, c16], writes=[wkc])
                        p.op('dve', lambda e, h=h: e.max(out=c16[0:T, h, 8:16], in_=wkc[0:T, :]), reads=[wkc], writes=[c16])
                        p.op('dve', lambda e, h=h: e.max_index(out=icu[0:T, h, 8:16], in_max=c16[0:T, h, 8:16], in_values=wkc[0:T, :]), reads=[wkc, c16], writes=[icu])
                    icf = icu[0:T, :, :].rearrange("q h k -> q (h k)")
                    p.op('dve', lambda e: e.tensor_scalar(out=iju[0:T, 0, :], in0=icf, scalar1=4, scalar2=None, op0=ALU.logical_shift_right), reads=[icu], writes=[iju])
                    p.op('dve', lambda e: e.tensor_scalar(out=iju[0:T, 1, :], in0=icf, scalar1=15, scalar2=None, op0=ALU.bitwise_and), reads=[icu], writes=[iju])
                    p.op('dve', lambda e: e.tensor_copy(out=ijf[0:T, :, :], in_=iju[0:T, :, :]), reads=[iju], writes=[ijf])
                    p.op('dve', lambda e: e.tensor_copy(out=iaf[0:T, :, :], in_=iau[0:T, :, :]), reads=[iau], writes=[iaf])
                    p.op('dve', lambda e: e.tensor_copy(out=ibf[0:T, :, :], in_=ibu[0:T, :, :]), reads=[ibu], writes=[ibf])
                    for w_, srcf in ((0, iaf), (1, ibf)):
                        p.op('dve', lambda e, w_=w_: e.tensor_tensor(out=eq[0:T, :, :], in0=ijf[0:T, w_, :].unsqueeze(2).to_broadcast([T, 128, 16]),
                                                                     in1=iota_f[0:T, 0:16].unsqueeze(1).to_broadcast([T, 128, 16]), op=ALU.is_equal),
                             reads=[ijf, iota_f], writes=[eq])
                        p.op('dve', lambda e, srcf=srcf: e.tensor_tensor(out=eq[0:T, :, :].rearrange("q (h k) i -> q h k i", h=8),
                                                                         in0=eq[0:T, :, :].rearrange("q (h k) i -> q h k i", h=8),
                                                                         in1=srcf[0:T, :, :].unsqueeze(2).to_broadcast([T, 8, 16, 16]), op=ALU.mult),
                             reads=[eq, srcf], writes=[eq])
                        p.op('dve', lambda e, w_=w_: e.tensor_reduce(out=SEL[0:T, w_, :], in_=eq[0:T, :, :], op=ALU.add, axis=AX.X), reads=[eq], writes=[SEL])
                    p.op('dve', lambda e: e.tensor_tensor(out=e16[0:T, :, :], in0=c16[0:T, :, :], in1=c16[0:T, :, 0:1].to_broadcast([T, 8, 16]), op=ALU.subtract),
                         reads=[c16], writes=[e16])
                    p.op('act', lambda e: e.activation(out=e16[0:T, :, :], in_=e16[0:T, :, :], func=AF.Exp), reads=[e16], writes=[e16])
                    p.op('dve', lambda e: e.tensor_reduce(out=z8[0:T, 0:8], in_=e16[0:T, :, :], op=ALU.add, axis=AX.X), reads=[e16], writes=[z8])
                    p.op('dve', lambda e: e.reciprocal(out=z8[0:T, 8:16], in_=z8[0:T, 0:8]), reads=[z8], writes=[z8])
                    p.op('dve', lambda e: e.tensor_tensor(out=SEL[0:T, 2, :].rearrange("q (h k) -> q h k", h=8), in0=e16[0:T, :, :],
                                                          in1=z8[0:T, 8:16].unsqueeze(2).to_broadcast([T, 8, 16]), op=ALU.mult), reads=[e16, z8], writes=[SEL])
                    bk = nb()
                    for w_ in range(3):
                        p.op('pe', lambda e, w_=w_, bk=bk: e.transpose(out=bk[:, w_ * T:(w_ + 1) * T], in_=SEL[0:T, w_, :], identity=identf[0:T, 0:T]),
                             reads=[SEL, identf], writes=[bk])
                    p.op('act', lambda e, bk=bk: e.copy(out=selT[:, :, s * 128:s * 128 + T], in_=bk[:, 0:3 * T].rearrange("q (w t) -> q w t", w=3)),
                         reads=[bk], writes=[selT])
                    p.op('pool', lambda e: e.tensor_copy(out=selTb[:, :, s * 128:s * 128 + T], in_=selT[:, :, s * 128:s * 128 + T]), reads=[selT], writes=[selTb])

                def peer_sel_part(row0, Tg, bufset):
                    xg, XT, selT, selTb = bufset
                    nsub = (Tg + 127) // 128
                    Ts = min(Tg, 128)
                    for s in range(nsub):
                        p.dma('sp', lambda e, s=s: e.dma_start(out=xg[0:Ts, s, :], in_=x2s[row0 + s * 128:row0 + s * 128 + Ts, :]), reads=['x2s'], writes=[xg])
                        peer_select(Ts, s, xg, XT, selT, selTb)

                def peer_group(Tg, y_out, yrow0, bufset, side_calls):
                    xg, XT, selT, selTb = bufset
                    nsub = (Tg + 127) // 128
                    Ts = min(Tg, 128)
                    side_i = [0]
                    for t0 in range(0, Tg, 8):
                        o1, o0, og = ohb1[(t0 // 8) % 2], ohb0[(t0 // 8) % 2], ohbg[(t0 // 8) % 2]
                        p.op('dve', lambda e, t0=t0, o1=o1: e.tensor_tensor(out=o1[:, :, :], in0=iota_b[:, :].unsqueeze(1).to_broadcast([128, 8, 128]),
                                                                          in1=selTb[:, 1, t0:t0 + 8].unsqueeze(2).to_broadcast([128, 8, 128]), op=ALU.is_equal),
                             reads=[iota_b, selTb], writes=[o1])
                        p.op('dve', lambda e, t0=t0, o0=o0: e.tensor_tensor(out=o0[:, :, :], in0=iota_b[:, :].unsqueeze(1).to_broadcast([128, 8, 128]),
                                                                          in1=selTb[:, 0, t0:t0 + 8].unsqueeze(2).to_broadcast([128, 8, 128]), op=ALU.is_equal),
                             reads=[iota_b, selTb], writes=[o0])
                        p.op('pool', lambda e, t0=t0, o0=o0, og=og: e.tensor_tensor(out=og[:, :, :], in0=o0[:, :, :],
                                                                                  in1=selTb[:, 2, t0:t0 + 8].unsqueeze(2).to_broadcast([128, 8, 128]), op=ALU.mult),
                             reads=[o0, selTb], writes=[og])
                        for q4 in range(2):
                            bk = nb()
                            for tt in range(4):
                                mm(bk[:, tt * 128:(tt + 1) * 128], o1[:, q4 * 4 + tt, :], og[:, q4 * 4 + tt, :], True, True, [o1, og], [bk])
                            p.op('act', lambda e, bk=bk, ta=t0 + q4 * 4: e.copy(out=Gt[:, ta:ta + 4, :], in_=bk[:, :].rearrange("q (t i) -> q t i", t=4)), reads=[bk], writes=[Gt])
                    acc = pb[4:8]
                    n_ec = cfg.get('n_ec', 128)

                    def fetch(ec):
                        p.dma('sp', lambda e, ec=ec: e.dma_start(out=UTc[ec % NBUF][:, :, :].rearrange("q c e -> q (c e)"), in_=UTs[ec, :, :]),
                              reads=[('UTs', ec)], writes=[UTc[ec % NBUF]])
                        p.dma(cfg.get('vq', 'pool'), lambda e, ec=ec: e.dma_start(out=Vc[ec % NBUF][:, :], in_=Vs[ec, :, :]), reads=[('Vs', ec)], writes=[Vc[ec % NBUF]])
                    for ec in range(min(NBUF, n_ec)):
                        fetch(ec)
                    for ec in range(n_ec + 1):
                        if ec < n_ec:
                            U_ = UTc[ec % NBUF]
                            bk = pb[ec % 2]
                            for c in range(8):
                                mm(bk[:, 0:Tg], U_[:, c, :], XT[:, c, 0:Tg], c == 0, c == 7, [U_, XT], [bk])
                            ga, GA = gab[ec % 3], GAb[ec % 3]
                            p.op('act', lambda e, bk=bk, ga=ga: e.activation(out=ga[:, 0:Tg], in_=bk[:, 0:Tg], func=AF.Gelu_apprx_tanh), reads=[bk], writes=[ga])
                            p.op('dve', lambda e, ga=ga, GA=GA, ec=ec: e.tensor_tensor(out=GA[:, 0:Tg], in0=ga[:, 0:Tg], in1=Gt[:, 0:Tg, ec], op=ALU.mult),
                                 reads=[ga, Gt], writes=[GA])
                        if ec >= 1:
                            pe_ = ec - 1
                            V_, GA = Vc[pe_ % NBUF], GAb[pe_ % 3]
                            for s in range(nsub):
                                for half in range(2):
                                    ab = acc[2 * s + half]
                                    mm(ab[0:Ts, :], GA[:, s * 128:s * 128 + Ts], V_[:, half * 512:(half + 1) * 512], pe_ == 0, pe_ == n_ec - 1, [GA, V_], [ab])
                            if pe_ + NBUF < n_ec:
                                fetch(pe_ + NBUF)
                        per = (len(side_calls) + n_ec - 1) // max(n_ec, 1) if side_calls else 0
                        for _ in range(per):
                            if side_i[0] < len(side_calls):
                                kind_, a_, k_ = side_calls[side_i[0]]
                                (orig_op if kind_ == 'op' else orig_dma)(*a_, **k_)
                                side_i[0] += 1
                    while side_i[0] < len(side_calls):
                        kind_, a_, k_ = side_calls[side_i[0]]
                        (orig_op if kind_ == 'op' else orig_dma)(*a_, **k_)
                        side_i[0] += 1
                    for s in range(nsub):
                        for half in range(2):
                            ab = acc[2 * s + half]
                            p.op('dve', lambda e, ab=ab, s=s, half=half: e.tensor_tensor(out=pre[0:Ts, half * 512:(half + 1) * 512], in0=ab[0:Ts, :],
                                                                                         in1=xg[0:Ts, s, half * 512:(half + 1) * 512], op=ALU.add),
                                 reads=[ab, xg], writes=[pre])
                        rs = rmsnorm_rstd(pre, Ts, ssd, sq4)
                        p.op('dve', lambda e, rs=rs: e.scalar_tensor_tensor(out=yo[0:Ts, :], in0=pre[0:Ts, :], scalar=rs, in1=gfin[0:Ts, :], op0=ALU.mult, op1=ALU.mult),
                             reads=[pre, ssd['ss'], gfin], writes=[yo])
                        p.dma('sp', lambda e, s=s: e.dma_start(out=y_out[yrow0 + s * 128:yrow0 + s * 128 + Ts, :], in_=yo[0:Ts, :]), reads=[yo], writes=['y_out'])

                groups = [(gi * 256, 256, y_p, gi * 256) for gi in range(cfg.get('n_groups', 8))]
                if cfg.get('peer_sample', True):
                    groups.append((SEQ, NS, y_s, 0))
                bufsets = [(xg_l[i], XT_l[i], selT_l[i], selTb_l[i]) for i in range(2)]
                orig_op, orig_dma = p.op, p.dma
                rec = {'on': False, 'calls': []}

                def op_wrap(*a, **k):
                    if rec['on']:
                        rec['calls'].append(('op', a, k))
                    else:
                        return orig_op(*a, **k)

                def dma_wrap(*a, **k):
                    if rec['on']:
                        rec['calls'].append(('dma', a, k))
                    else:
                        return orig_dma(*a, **k)
                p.op, p.dma = op_wrap, dma_wrap
                nbbase[0], nbmod[0] = 2, 2
                if groups:
                    peer_sel_part(groups[0][0], groups[0][1], bufsets[0])
                for n, (row0, Tg, y_out, yrow0) in enumerate(groups):
                    side = []
                    if n + 1 < len(groups) and cfg.get('overlap_sel', True):
                        rec['on'], rec['calls'] = True, []
                        peer_sel_part(groups[n + 1][0], groups[n + 1][1], bufsets[(n + 1) % 2])
                        rec['on'] = False
                        side = rec['calls']
                    elif n + 1 < len(groups):
                        pass
                    peer_group(Tg, y_out, yrow0, bufsets[n % 2], side)
                    if n + 1 < len(groups) and not cfg.get('overlap_sel', True):
                        peer_sel_part(groups[n + 1][0], groups[n + 1][1], bufsets[(n + 1) % 2])
                p.op, p.dma = orig_op, orig_dma

        p.finish()
        p.emit()
    return nc


_NC_CACHE = {}


def make_in_maps(inp, cfg, ncores=NCORES):
    c = host_consts()
    f = lambda a: np.ascontiguousarray(a, dtype=np.float32)
    maps = []
    for i in range(ncores):
        m = {
            'xp': f(inp['x_prompt'][i]),
            'xs': f(inp['x_sample'][DEC_B * i:DEC_B * (i + 1)].reshape(NS, D)),
            'memp': f(inp['mem_prompt'][i]),
            'w_in': f(inp['w_in'][0]), 'w_out': f(inp['w_out'][0]),
            'g_mix': f(inp['norm_mix_g'][0]), 'g_mem': f(inp['norm_mem_g'][0]), 'g_memn': f(inp['mem_norm_g'][0]),
            'g_ffn': f(inp['norm_ffn_g'][0]), 'g_fin': f(inp['final_norm_g']),
            'ln_g': f(inp['gm_ln_g'][0]), 'ln_b': f(inp['gm_ln_b'][0]),
            'gm_ws': f(inp['gm_ws'][0]), 'gm_bs': f(inp['gm_bs'][0]),
            'mem_wq': f(inp['mem_wq'][0]), 'mem_wkv': f(inp['mem_wkv'][0]), 'mem_wo': f(inp['mem_wo'][0]),
            'c_ident': c['ident'], 'c_trilT': c['trilT'], 'c_negmask': c['negmask'], 'c_blk64': c['blk64'], 'c_ones': c['ones'], 'c_iota': c['iota'],
            'peer_wq': f(inp['peer_wq'][0]), 'peer_keys': f(inp['peer_keys'][0].reshape(16, 128, 64)),
            'peer_u': f(inp['peer_u'][0]), 'peer_v': f(inp['peer_v'][0]),
            'cache_kidx': f(inp['cache_kidx'][0]).reshape(-1, 8192), 'cache_k': f(inp['cache_k'][0]).reshape(-1, 8192),
            'cache_v': f(inp['cache_v'][0]).reshape(-1, 8192),
            'page_table': np.ascontiguousarray(inp['page_table'][DEC_B * i:DEC_B * (i + 1)], dtype=np.int32),
            'cache_mem_k': f(inp['cache_mem_k'][0][DEC_B * i:DEC_B * (i + 1)]).reshape(DEC_B, 256, 512),
            'cache_mem_v': f(inp['cache_mem_v'][0][DEC_B * i:DEC_B * (i + 1)]).reshape(DEC_B, 256, 512),
            'c_pow2': c['pow2'], 'c_negm4': c['negm4'],
        }
        maps.append(m)
    return maps


def assemble(results, ncores=NCORES):
    cat = lambda k: np.stack([np.asarray(r[k], dtype=np.float32) for r in results])
    y_prompt = cat('y_p')
    y_sample = cat('y_s').reshape(ncores * DEC_B, DEC_T, D)
    k_prompt = cat('k_p').reshape(1, ncores, SEQ, 2, 64)
    v_prompt = cat('v_p').reshape(1, ncores, SEQ, 2, 64)
    kidx_prompt = cat('ki_p').reshape(1, ncores, SEQ, 64)
    gmv_prompt = cat('gmv_p').reshape(1, ncores, 128, 512)
    memk = cat('memk_p').reshape(1, ncores, 256, 4, 128)
    memv = cat('memv_p').reshape(1, ncores, 256, 4, 128)
    k_sample = cat('k_s').reshape(1, ncores * DEC_B, DEC_T, 2, 64)
    v_sample = cat('v_s').reshape(1, ncores * DEC_B, DEC_T, 2, 64)
    kidx_sample = cat('ki_s').reshape(1, ncores * DEC_B, DEC_T, 64)
    gmv_sample = cat('gmv_s').reshape(1, ncores * DEC_B, DEC_T, 512)
    return (y_prompt, y_sample, k_prompt, v_prompt, kidx_prompt, gmv_prompt, memk, memv,
            k_sample, v_sample, kidx_sample, gmv_sample)


def kernel(**inputs):
    cfg = {}
    nc = build(cfg)
    maps = make_in_maps(inputs, cfg)
    res = run_bass_kernel_spmd(nc, maps, core_ids=list(range(NCORES)))
    return assemble(res.results)
```

```python
import numpy as np
from contextlib import ExitStack
import concourse.bass as bass
import concourse.mybir as mybir
from concourse.bass_utils import run_bass_kernel_spmd

F32 = mybir.dt.float32
BF16 = mybir.dt.bfloat16
I32 = mybir.dt.int32
U32 = mybir.dt.uint32
AF = mybir.ActivationFunctionType
ALU = mybir.AluOpType
AX = mybir.AxisListType

NCORES = 8
D = 1024
SEQ = 2048
NT = SEQ // 128
P_IN = 2376
EPS = 1e-6
NEG = -1.0e30
DEC_B = 16
DEC_T = 4
NS = DEC_B * DEC_T
NPAGES = 64
NEXP = 16384


class Prog:
    ENG = ('sp', 'act', 'dve', 'pool', 'pe')

    def __init__(self, nc, stack, n_dma_sems=12):
        self.nc = nc
        self.stack = stack
        self.ops = {k: [] for k in self.ENG}
        self.cnt = {k: 0 for k in self.ENG}
        self.waited = {k: {} for k in self.ENG}
        self.res = {}
        self.sems = {}
        for k in ('act', 'dve', 'pool', 'pe'):
            self.sems[k] = stack.enter_context(nc.semaphore('prog_' + k))
        self.dma_sems = {}
        self.dma_rr = {}
        self.dma_uses = {}
        for q in ('sp', 'pool', 'act'):
            lst = []
            for i in range(n_dma_sems):
                key = 'dma_%s_%d' % (q, i)
                self.sems[key] = stack.enter_context(nc.semaphore(key))
                self.dma_uses[key] = 0
                lst.append(key)
            self.dma_sems[q] = lst
            self.dma_rr[q] = 0
        self.psum_names = set()

    def sb(self, name, shape, dtype, stack=None):
        return (stack or self.stack).enter_context(self.nc.sbuf_tensor(name, list(shape), dtype))

    def ps(self, name, shape, dtype):
        self.psum_names.add(name)
        return self.stack.enter_context(self.nc.psum_tensor(name, list(shape), dtype))

    @staticmethod
    def _key(x):
        if isinstance(x, (str, tuple)):
            return x
        t = getattr(x, 'tensor', x)
        n = getattr(t, 'name', None)
        if n is None:
            raise ValueError('cannot derive resource key from %r' % (x,))
        return n

    def _deps(self, reads, writes):
        deps = []
        for r in reads:
            st = self.res.get(self._key(r))
            if st and st['w']:
                deps.append(st['w'])
        for w in writes:
            st = self.res.get(self._key(w))
            if st:
                if st['w']:
                    deps.append(st['w'])
                deps.extend(st['r'])
        return deps

    def _commit(self, reads, writes, tok):
        for r in reads:
            st = self.res.setdefault(self._key(r), {'w': None, 'r': []})
            st['r'].append(tok)
        for w in writes:
            self.res[self._key(w)] = {'w': tok, 'r': []}

    def _filter_waits(self, eng, deps, skip_self=False):
        out = {}
        for (sk, val) in deps:
            if skip_self and sk == eng:
                continue
            if self.waited[eng].get(sk, 0) >= val:
                continue
            if out.get(sk, 0) < val:
                out[sk] = val
        for sk, val in out.items():
            self.waited[eng][sk] = val
        return list(out.items())

    def op(self, eng, fn, reads=(), writes=()):
        pr = [r for r in reads if self._key(r) in self.psum_names]
        if pr:
            reads = [r for r in reads if self._key(r) not in self.psum_names]
            writes = list(writes) + pr
        deps = self._deps(reads, writes)
        waits = self._filter_waits(eng, deps, skip_self=(eng == 'pe'))
        self.cnt[eng] += 1
        tok = (eng, self.cnt[eng])
        self._commit(reads, writes, tok)
        self.ops[eng].append((waits, fn, (eng, 1)))
        return tok

    def dma(self, q, fn, reads=(), writes=()):
        deps = self._deps(reads, writes)
        lst = self.dma_sems[q]
        sk = lst[self.dma_rr[q] % len(lst)]
        self.dma_rr[q] += 1
        if self.dma_uses[sk] > 0:
            deps.append((sk, 16 * self.dma_uses[sk]))
        waits = self._filter_waits(q, deps)
        self.dma_uses[sk] += 1
        tok = (sk, 16 * self.dma_uses[sk])
        self._commit(reads, writes, tok)
        self.ops[q].append((waits, fn, (sk, 16)))
        return tok

    def barrier(self):
        deps = [(k, self.cnt[k]) for k in ('act', 'dve', 'pool', 'pe') if self.cnt[k] > 0]
        deps += [(sk, 16 * n) for sk, n in self.dma_uses.items() if n > 0]
        for eng in self.ENG:
            waits = self._filter_waits(eng, [d for d in deps if d[0] != eng])
            if waits:
                self.ops[eng].append((waits, None, None))
        self.res = {}

    def finish(self):
        deps = [(sk, 16 * n) for sk, n in self.dma_uses.items() if n > 0]
        waits = self._filter_waits('sp', deps)
        self.ops['sp'].append((waits, None, None))

    def emit(self):
        nc = self.nc
        allsems = list(self.sems.values())
        with nc.Block() as b0:
            def clr(e):
                for s in allsems:
                    e.sem_clear(s)
            b0.sync(clr)
        with nc.Block() as block:
            for name, meth in (('sp', block.sync), ('act', block.scalar), ('dve', block.vector),
                               ('pool', block.gpsimd), ('pe', block.tensor)):
                ops = self.ops[name]
                if not ops:
                    continue

                def body(e, ops=ops):
                    for waits, fn, inc in ops:
                        for (sk, val) in waits:
                            e.wait_ge(self.sems[sk], val)
                        if fn is None:
                            continue
                        ins = fn(e)
                        if inc is not None:
                            ins.then_inc(self.sems[inc[0]], inc[1])
                meth(body)


def host_consts():
    c = {}
    c['ident'] = np.eye(128, dtype=np.float32)
    s = np.arange(128)
    c['trilT'] = (s[:, None] <= s[None, :]).astype(np.float32)
    c['negmask'] = np.where(s[None, :] <= s[:, None], 0.0, NEG).astype(np.float32)
    bt = np.arange(64)
    c['blk64'] = ((bt[:, None] // 4 == bt[None, :] // 4) & (bt[:, None] % 4 <= bt[None, :] % 4)).astype(np.float32)
    c['ones'] = np.ones((128, 128), dtype=np.float32)
    c['iota'] = np.tile(np.arange(128, dtype=np.float32)[None, :], (128, 1))
    c['pow2'] = np.tile((2.0 ** -(np.arange(48, dtype=np.float64) + 1)).astype(np.float32)[None, :], (128, 1))
    t4 = np.arange(4)
    c['negm4'] = np.where(t4[:, None] <= t4[None, :], 0.0, NEG).astype(np.float32)
    return c


def build(cfg):
    n_pool = cfg.get('n_pool', 10240)
    do = cfg.get('phases', ('P1', 'P2', 'P3', 'P4'))
    dbg = cfg.get('dbg', False)
    nc = bass.Bass("TRN2", target_bir_lowering=False)

    def din(name, shape, dt=F32):
        return nc.dram_tensor(name, list(shape), dt, kind="ExternalInput").ap()

    def dout(name, shape, dt=F32):
        return nc.dram_tensor(name, list(shape), dt, kind="ExternalOutput").ap()

    xp_d = din('xp', [SEQ, D])
    xs_d = din('xs', [NS, D])
    memp_d = din('memp', [256, D])
    w_in_d = din('w_in', [D, P_IN])
    w_out_d = din('w_out', [D, D])
    g_mix_d = din('g_mix', [D]); g_mem_d = din('g_mem', [D]); g_memn_d = din('g_memn', [D])
    g_ffn_d = din('g_ffn', [D]); g_fin_d = din('g_fin', [D])
    ln_g_d = din('ln_g', [512]); ln_b_d = din('ln_b', [512])
    gm_ws_d = din('gm_ws', [4, 128, 128]); gm_bs_d = din('gm_bs', [4, 128])
    wq_d = din('mem_wq', [D, 512]); wkv_d = din('mem_wkv', [D, D]); wo_d = din('mem_wo', [512, D])
    c_ident = din('c_ident', [128, 128]); c_trilT = din('c_trilT', [128, 128]); c_negmask = din('c_negmask', [128, 128])
    c_blk64 = din('c_blk64', [64, 64]); c_ones = din('c_ones', [128, 128]); c_iota = din('c_iota', [128, 128])
    pwq_d = din('peer_wq', [D, D]); pkeys_d = din('peer_keys', [16, 128, 64])
    pu_d = din('peer_u', [NEXP, D]); pv_d = din('peer_v', [NEXP, D])
    ckidx_d = din('cache_kidx', [n_pool, 8192]); ck_d = din('cache_k', [2 * n_pool, 8192]); cv_d = din('cache_v', [2 * n_pool, 8192])
    pt_d = din('page_table', [DEC_B, NPAGES], I32)
    cmk_d = din('cache_mem_k', [DEC_B, 256, 512]); cmv_d = din('cache_mem_v', [DEC_B, 256, 512])
    c_pow2 = din('c_pow2', [128, 48]); c_negm4 = din('c_negm4', [4, 4])

    y_p = dout('y_p', [SEQ, D]); y_s = dout('y_s', [NS, D])
    k_p = dout('k_p', [SEQ, 128]); v_p = dout('v_p', [SEQ, 128]); ki_p = dout('ki_p', [SEQ, 64])
    gmv_p = dout('gmv_p', [128, 512])
    memk_p = dout('memk_p', [256, 512]); memv_p = dout('memv_p', [256, 512])
    k_s = dout('k_s', [NS, 128]); v_s = dout('v_s', [NS, 128]); ki_s = dout('ki_s', [NS, 64]); gmv_s = dout('gmv_s', [NS, 512])
    if dbg:
        x2_dbg = dout('x2_dbg', [SEQ + NS, D])
    x2s = nc.dram_tensor('x2s', [SEQ + NS, D], F32, kind="Internal").ap()
    UTs = nc.dram_tensor('UTs', [128, 128, D], BF16, kind="Internal").ap()
    Vs = nc.dram_tensor('Vs', [128, 128, D], BF16, kind="Internal").ap()

    with ExitStack() as st:
        p = Prog(nc, st)
        pb = [p.ps('pb%d' % i, [128, 512], F32) for i in range(8)]
        rr = [0]
        nbmod = [6]
        nbbase = [0]

        def nb():
            b = pb[nbbase[0] + rr[0] % nbmod[0]]
            rr[0] += 1
            return b

        def mm(out, lhsT, rhs, start, stop, R, W):
            p.op('pe', lambda e: e.matmul(out, lhsT=lhsT, rhs=rhs, start=start, stop=stop), reads=R, writes=W)

        identf = p.sb('identf', [128, 128], F32)
        ident = p.sb('ident', [128, 128], BF16)
        trilT = p.sb('trilT', [128, 128], F32)
        negmask = p.sb('negmask', [128, 128], F32)
        ones_bf = p.sb('ones_bf', [128, 128], BF16)
        onesf = p.sb('onesf', [128, 128], F32)
        p.dma('sp', lambda e: e.dma_start(out=identf[:], in_=c_ident[:, :]), writes=[identf])
        p.dma('sp', lambda e: e.dma_start(out=trilT[:], in_=c_trilT[:, :]), writes=[trilT])
        p.dma('sp', lambda e: e.dma_start(out=negmask[:], in_=c_negmask[:, :]), writes=[negmask])
        p.dma('sp', lambda e: e.dma_start(out=onesf[:], in_=c_ones[:, :]), writes=[onesf])
        p.op('dve', lambda e: e.tensor_copy(out=ident[:], in_=identf[:]), reads=[identf], writes=[ident])
        p.op('dve', lambda e: e.tensor_copy(out=ones_bf[:], in_=onesf[:]), reads=[onesf], writes=[ones_bf])

        def tr(out, in_, K, R, W):
            p.op('pe', lambda e: e.transpose(out=out, in_=in_, identity=ident[0:K, 0:K]), reads=list(R) + [ident], writes=W)

        gcols = {}

        def load_gcol(name, g_d):
            t = p.sb('gc_' + name, [128, 8], F32)
            p.dma('sp', lambda e: e.dma_start(out=t[:], in_=g_d.rearrange("(c q) -> q c", q=128), allow_slow_non_contiguous=True), writes=[t])
            gcols[name] = t
            return t

        def load_weight(dst, w_d, nk, ncol, gcol, stage, eng_rot=[0]):
            for c in range(nk):
                stg = stage[c % len(stage)]
                p.dma('sp', lambda e, c=c, stg=stg: e.dma_start(out=stg[:, 0:ncol], in_=w_d[c * 128:(c + 1) * 128, :]), writes=[stg])
                eng = ('dve', 'pool')[eng_rot[0] % 2]
                eng_rot[0] += 1
                if gcol is not None:
                    p.op(eng, lambda e, c=c, stg=stg: e.tensor_scalar(out=dst[:, c, :], in0=stg[:, 0:ncol], scalar1=gcol[:, c:c + 1],
                                                                     scalar2=None, op0=ALU.mult), reads=[stg, gcol], writes=[dst])
                else:
                    p.op(eng, lambda e, c=c, stg=stg: e.tensor_copy(out=dst[:, c, :], in_=stg[:, 0:ncol]), reads=[stg], writes=[dst])

        def rmsnorm_rstd(x_t, T, rstd, scratch):
            ss = rstd['ss']
            p.op('act', lambda e: e.activation(out=scratch[0:T, :], in_=x_t[0:T, :], func=AF.Square, accum_out=ss[0:T, 0:1]),
                 reads=[x_t], writes=[scratch, ss])
            p.op('dve', lambda e: e.tensor_scalar(out=ss[0:T, 1:2], in0=ss[0:T, 0:1], scalar1=1.0 / D, scalar2=EPS, op0=ALU.mult, op1=ALU.add),
                 reads=[ss], writes=[ss])
            p.op('act', lambda e: e.activation(out=ss[0:T, 2:3], in_=ss[0:T, 1:2], func=AF.Sqrt), reads=[ss], writes=[ss])
            p.op('dve', lambda e: e.reciprocal(out=ss[0:T, 3:4], in_=ss[0:T, 2:3]), reads=[ss], writes=[ss])
            return ss[0:T, 3:4]

        def norm_T(x_t, T, tag, xn, xnT, ssd, scratch, gcol=None):
            rs = rmsnorm_rstd(x_t, T, ssd, scratch)
            p.op('act', lambda e: e.activation(out=xn[0:T, :], in_=x_t[0:T, :], func=AF.Copy, scale=rs), reads=[x_t, ssd['ss']], writes=[xn])
            bk = nb()
            bv = bk[:].bitcast(BF16)
            for c in range(8):
                tr(bv[:, c * T:(c + 1) * T], xn[0:T, c * 128:(c + 1) * 128], T, [xn], [bk])
            if gcol is None:
                p.op('dve', lambda e: e.tensor_copy(out=xnT[:, :, 0:T], in_=bv[:, 0:8 * T].rearrange("q (c t) -> q c t", c=8)),
                     reads=[bk], writes=[xnT])
            else:
                for c in range(8):
                    p.op('dve', lambda e, c=c: e.tensor_scalar(out=xnT[:, c, 0:T], in0=bv[:, c * T:(c + 1) * T], scalar1=gcol[:, c:c + 1], scalar2=None, op0=ALU.mult),
                         reads=[bk, gcol], writes=[xnT])

        ssd = {'ss': p.sb('ss', [128, 4], F32)}

        for nm, gd in (('mix', g_mix_d), ('mem', g_mem_d), ('memn', g_memn_d), ('ffn', g_ffn_d)):
            load_gcol(nm, gd)
        conv_done = [0]

        def convert_chunk(ec, u_, t_, v_):
            p.dma('pool', lambda e: e.dma_start(out=u_[:, :], in_=pu_d[ec * 128:(ec + 1) * 128, :]), writes=[u_])
            p.dma('pool', lambda e: e.dma_start(out=v_[:, :], in_=pv_d[ec * 128:(ec + 1) * 128, :]), writes=[v_])
            bk = nb()
            bv = bk[:].bitcast(BF16)
            for c in range(8):
                tr(bv[:, c * 128:(c + 1) * 128], u_[:, c * 128:(c + 1) * 128], 128, [u_], [bk])
            if ec % 2 == 0:
                p.op('act', lambda e: e.copy(out=t_[:, :], in_=bv[:, :]), reads=[bk], writes=[t_])
            else:
                p.op('pool', lambda e: e.tensor_copy(out=t_[:, :], in_=bv[:, :]), reads=[bk], writes=[t_]) if False else \
                    p.op('act', lambda e: e.copy(out=t_[:, :], in_=bv[:, :]), reads=[bk], writes=[t_])
            p.dma('sp', lambda e: e.dma_start(out=UTs[ec, :, :], in_=t_[:, :]), reads=[t_], writes=[('UTs', ec)])
            p.dma('sp', lambda e: e.dma_start(out=Vs[ec, :, :], in_=v_[:, :]), reads=[v_], writes=[('Vs', ec)])

        with ExitStack() as s2:
            w_out = p.sb('w_out_sb', [128, 8, D], BF16, s2)
            wq = p.sb('wq_sb', [128, 8, 512], BF16, s2)
            wo = p.sb('wo_sb', [128, 4, D], BF16, s2)
            sq = p.sb('sq_scratch', [128, D], BF16, s2)
            xn = p.sb('xn', [128, D], BF16, s2)
            xnT = p.sb('xnT', [128, 8, 128], BF16, s2)
            W4T = p.sb('W4T', [64, 4, 64], BF16, s2)
            bsT4 = p.sb('bsT4', [64, 4], F32, s2)
            tau_c = p.sb('tau_c', [128, 1], F32, s2)
            p.op('pool', lambda e: e.memset(tau_c[:], -1.0e29), writes=[tau_c])

            bnst = p.sb('bnst', [128, 8], F32, s2)
            wsc = p.sb('wsc', [128, 8], F32, s2)
            ycat = p.sb('ycat', [128, D], BF16, s2)
            yT = p.sb('yT', [128, 8, 128], BF16, s2)
            x1 = p.sb('x1', [128, D], F32, s2)
            rden = p.sb('rden', [128, 8], F32, s2)
            qmT = p.sb('qmT', [128, 4, 128], BF16, s2)
            PmT = p.sb('PmT', [128, 2, 4, 128], BF16, s2)
            om = p.sb('om', [128, 512], BF16, s2)
            omT = p.sb('omT', [128, 4, 128], BF16, s2)
            x0s = p.sb('x0s', [64, D], F32, s2)
            ycats = p.sb('ycats', [64, D], BF16, s2)
            kvf_s = p.sb('kvf_s', [64, 328], F32, s2)
            kb_s = p.sb('kb_s', [64, 192], BF16, s2)
            wsc_s = p.sb('wsc_s', [64, 8], F32, s2)
            qTs = p.sb('qTs', [64, 8, 64], BF16, s2)
            qiTs = p.sb('qiTs', [64, 8, 64], BF16, s2)
            sw = ExitStack()
            x0 = p.sb('x0', [128, D], F32, sw)
            mkT = p.sb('mkT', [128, 4, 256], BF16, sw)
            mv_aug = p.sb('mv_aug', [128, 2, 4, 129], BF16, sw)
            WgT = p.sb('WgT', [128, 4, 128], BF16, sw)
            bsT = p.sb('bsT', [128, 4], F32, sw)
            lng = p.sb('lng', [128, 512], F32, sw)
            lnb = p.sb('lnb', [128, 512], F32, sw)
            kvf = p.sb('kvf', [128, 328], F32, sw)
            kb = p.sb('kb', [128, 192], BF16, sw)
            qT = p.sb('qT', [64, 8, 128], BF16, sw)
            qiT = p.sb('qiT', [64, 8, 128], BF16, sw)
            p.dma('sp', lambda e: e.dma_start(out=lng[:], in_=ln_g_d.partition_broadcast(128)), writes=[lng])
            p.dma('sp', lambda e: e.dma_start(out=lnb[:], in_=ln_b_d.partition_broadcast(128)), writes=[lnb])
            ub = p.sb('ub', [128, 512], F32, sw)
            gv = p.sb('gv', [128, 512], F32, sw)
            vn = p.sb('vn', [128, 512], F32, sw)
            vnb = p.sb('vnb', [128, 512], BF16, sw)
            zq = p.sb('zq', [128, 1024], BF16, sw)
            w_in = p.sb('w_in_sb', [128, 8, P_IN], BF16, sw)

            with ExitStack() as s1:
                stage = [p.sb('wstage%d' % i, [128, P_IN], F32, s1) for i in range(2)]
                load_weight(w_in, w_in_d, 8, P_IN, gcols['mix'], stage)
                load_weight(w_out, w_out_d, 8, D, None, stage)
                load_weight(wq, wq_d, 8, 512, gcols['mem'], stage)
                load_weight(wo, wo_d, 4, D, None, stage)
                wsb = p.sb('wsb', [128, 128], BF16, s1)
                for g in range(4):
                    stg = stage[g % 2]
                    p.dma('sp', lambda e, g=g, stg=stg: e.dma_start(out=stg[:, 0:128], in_=gm_ws_d[g, :, :]), writes=[stg])
                    p.op('dve', lambda e, stg=stg: e.tensor_copy(out=wsb[:], in_=stg[:, 0:128]), reads=[stg], writes=[wsb])
                    bk = nb()
                    bv = bk[:].bitcast(BF16)
                    tr(bv[:, 0:128], wsb[:, :], 128, [wsb], [bk])
                    p.op('dve', lambda e, g=g, bv=bv: e.tensor_tensor(out=WgT[:, g, :], in0=bv[:, 0:128], in1=trilT[:, :], op=ALU.mult),
                         reads=[bk, trilT], writes=[WgT])
                p.dma('sp', lambda e: e.dma_start(out=bsT[:], in_=gm_bs_d.rearrange("g t -> t g"), allow_slow_non_contiguous=True), writes=[bsT])
                w4f = p.sb('w4f', [64, 4, 64], F32, s1)
                blk64 = p.sb('blk64', [64, 64], F32, s1)
                p.dma('sp', lambda e: e.dma_start(out=blk64[:], in_=c_blk64[:, :]), writes=[blk64])
                p.op('pool', lambda e: e.memset(w4f[:], 0.0), writes=[w4f])
                for g in range(4):
                    for b in range(DEC_B):
                        p.dma('sp', lambda e, g=g, b=b: e.dma_start(
                            out=w4f[4 * b:4 * b + 4, g, 4 * b:4 * b + 4], in_=gm_ws_d[g, 0:4, 0:4].rearrange("t s -> s t"), allow_slow_non_contiguous=True),
                            writes=[w4f])
                    p.op('dve', lambda e, g=g: e.tensor_tensor(out=W4T[:, g, :], in0=w4f[:, g, :], in1=blk64[:, :], op=ALU.mult), reads=[w4f, blk64], writes=[W4T])
                for b in range(DEC_B):
                    p.dma('sp', lambda e, b=b: e.dma_start(out=bsT4[4 * b:4 * b + 4, :], in_=gm_bs_d[:, 0:4].rearrange("g t -> t g"), allow_slow_non_contiguous=True), writes=[bsT4])

                if 'P1' in do:
                    wkv = p.sb('wkv_sb', [128, 8, D], BF16, s1)
                    load_weight(wkv, wkv_d, 8, D, gcols['memn'], stage)
                    mkvf = p.sb('mkvf', [128, D], F32, s1)
                    mkb = p.sb('mkb', [128, 512], BF16, s1)
                    p.op('pool', lambda e: e.memset(mv_aug[:], 1.0), writes=[mv_aug])
                    for mt in range(2):
                        p.dma('sp', lambda e, mt=mt: e.dma_start(out=x0[:], in_=memp_d[mt * 128:(mt + 1) * 128, :]), writes=[x0])
                        norm_T(x0, 128, 'm', xn, xnT, ssd, sq)
                        b0, b1 = nb(), nb()
                        for half, bk in enumerate((b0, b1)):
                            for c in range(8):
                                mm(bk[:, :], xnT[:, c, :], wkv[:, c, half * 512:(half + 1) * 512], c == 0, c == 7, [xnT, wkv], [bk])
                        p.op('act', lambda e, b0=b0: e.copy(out=mkvf[:, 0:512], in_=b0[:, :]), reads=[b0], writes=[mkvf])
                        p.op('dve', lambda e, b1=b1: e.tensor_copy(out=mkvf[:, 512:1024], in_=b1[:, :]), reads=[b1], writes=[mkvf])
                        p.dma('sp', lambda e, mt=mt: e.dma_start(out=memk_p[mt * 128:(mt + 1) * 128, :], in_=mkvf[:, 0:512]), reads=[mkvf], writes=['memk_p'])
                        p.dma('sp', lambda e, mt=mt: e.dma_start(out=memv_p[mt * 128:(mt + 1) * 128, :], in_=mkvf[:, 512:1024]), reads=[mkvf], writes=['memv_p'])
                        p.op('dve', lambda e: e.tensor_copy(out=mkb[:], in_=mkvf[:, 0:512]), reads=[mkvf], writes=[mkb])
                        p.op('pool', lambda e, mt=mt: e.tensor_copy(out=mv_aug[:, mt, :, 0:128], in_=mkvf[:, 512:1024].rearrange("q (h d) -> q h d", h=4)),
                             reads=[mkvf], writes=[mv_aug])
                        bk = nb()
                        bv = bk[:].bitcast(BF16)
                        for h in range(4):
                            tr(bv[:, h * 128:(h + 1) * 128], mkb[:, h * 128:(h + 1) * 128], 128, [mkb], [bk])
                        p.op('act', lambda e, mt=mt, bv=bv: e.copy(out=mkT[:, :, mt * 128:(mt + 1) * 128], in_=bv[:, 0:512].rearrange("q (h m) -> q h m", h=4)),
                             reads=[bk], writes=[mkT])
            p.barrier()

            with ExitStack() as s3:
                kT = [p.sb('kT%d' % g, [64, SEQ], BF16, s3) for g in range(2)]
                kiT = p.sb('kiT', [64, SEQ], BF16, s3)
                v_aug = p.sb('v_aug', [128, NT, 2, 65], BF16, s3)
                sc = p.sb('sc', [128, SEQ], F32, s3)
                bis = p.sb('bis', [128, 8], F32, s3)
                Wtp = p.sb('Wtp', [128, 48], F32, s3)
                midp = p.sb('midp', [128, 1], F32, s3)
                pow2p = p.sb('pow2p', [128, 48], F32, s3)
                p.dma('sp', lambda e: e.dma_start(out=pow2p[:, :], in_=c_pow2[:, :]), writes=[pow2p])
                rbuf = [p.sb('rbuf%d' % i, [128, 512], F32, s3) for i in range(2)]
                m8 = p.sb('m8', [128, 8], F32, s3)
                maskb = p.sb('maskb', [128, SEQ], BF16, s3)
                maskT = p.sb('maskT', [128, NT, 128], BF16, s3)
                Eb = [p.sb('Eb%d' % i, [128, 512], BF16, s3) for i in range(3)]
                PTb = [p.sb('PTb%d' % i, [128, 4, 128], BF16, s3) for i in range(3)]
                p.op('pool', lambda e: e.memset(v_aug[:], 1.0), writes=[v_aug])
                if 'P4' in do and cfg.get('interleave_conv', True):
                    cub = [p.sb('cub%d' % i, [128, D], BF16, s3) for i in range(2)]
                    cut = [p.sb('cut%d' % i, [128, D], BF16, s3) for i in range(2)]
                    cvb = [p.sb('cvb%d' % i, [128, D], BF16, s3) for i in range(2)]

                def proj_tile(src_ap, T, ti, is_sample, x0b, ycatb):
                    p.dma('sp', lambda e: e.dma_start(out=x0b[0:T, :], in_=src_ap), writes=[x0b])
                    norm_T(x0b, T, 'a', xn, xnT, ssd, sq)
                    banks = [nb() for _ in range(5)]
                    for n5, bk in enumerate(banks):
                        c0 = n5 * 512
                        w = min(512, P_IN - c0)
                        for c in range(8):
                            mm(bk[0:T, 0:w], xnT[:, c, 0:T], w_in[:, c, c0:c0 + w], c == 0, c == 7, [xnT, w_in], [bk])
                    p.op('act', lambda e: e.activation(out=ub[0:T, :], in_=banks[0][0:T, :], func=AF.Gelu_apprx_tanh), reads=[banks[0]], writes=[ub])
                    p.op('act', lambda e: e.activation(out=gv[0:T, :], in_=banks[1][0:T, :], func=AF.Gelu_apprx_tanh), reads=[banks[1]], writes=[gv])
                    p.op('dve', lambda e: e.tensor_copy(out=zq[0:T, 0:512], in_=banks[2][0:T, :]), reads=[banks[2]], writes=[zq])
                    p.op('dve', lambda e: e.tensor_copy(out=kvf[0:T, 0:256], in_=banks[3][0:T, 0:256]), reads=[banks[3]], writes=[kvf])
                    p.op('dve', lambda e: e.tensor_copy(out=zq[0:T, 512:768], in_=banks[3][0:T, 256:512]), reads=[banks[3]], writes=[zq])
                    p.op('act', lambda e: e.copy(out=zq[0:T, 768:1024], in_=banks[4][0:T, 0:256]), reads=[banks[4]], writes=[zq])
                    p.op('act', lambda e: e.copy(out=kvf[0:T, 256:328], in_=banks[4][0:T, 256:328]), reads=[banks[4]], writes=[kvf])
                    r0 = ti * 128
                    ko, vo, kio = (k_s, v_s, ki_s) if is_sample else (k_p, v_p, ki_p)
                    p.dma('sp', lambda e: e.dma_start(out=ko[r0:r0 + T, :], in_=kvf[0:T, 0:128]), reads=[kvf], writes=['ko'])
                    p.dma('sp', lambda e: e.dma_start(out=vo[r0:r0 + T, :], in_=kvf[0:T, 128:256]), reads=[kvf], writes=['vo'])
                    p.dma('sp', lambda e: e.dma_start(out=kio[r0:r0 + T, :], in_=kvf[0:T, 256:320]), reads=[kvf], writes=['kio'])
                    p.op('dve', lambda e: e.bn_stats(out=bnst[0:T, 0:6], in_=gv[0:T, :]), reads=[gv], writes=[bnst])
                    p.op('dve', lambda e: e.bn_aggr(out=bnst[0:T, 6:8], in_=bnst[0:T, 0:6]), reads=[bnst], writes=[bnst])
                    p.op('dve', lambda e: e.tensor_scalar(out=bnst[0:T, 0:1], in0=bnst[0:T, 7:8], scalar1=EPS, scalar2=None, op0=ALU.add), reads=[bnst], writes=[bnst])
                    p.op('act', lambda e: e.activation(out=bnst[0:T, 1:2], in_=bnst[0:T, 0:1], func=AF.Sqrt), reads=[bnst], writes=[bnst])
                    p.op('dve', lambda e: e.reciprocal(out=bnst[0:T, 2:3], in_=bnst[0:T, 1:2]), reads=[bnst], writes=[bnst])
                    p.op('dve', lambda e: e.tensor_scalar(out=vn[0:T, :], in0=gv[0:T, :], scalar1=bnst[0:T, 6:7], scalar2=bnst[0:T, 2:3],
                                                          op0=ALU.subtract, op1=ALU.mult), reads=[gv, bnst], writes=[vn])
                    p.op('dve', lambda e: e.tensor_tensor(out=vn[0:T, :], in0=vn[0:T, :], in1=lng[0:T, :], op=ALU.mult), reads=[vn, lng], writes=[vn])
                    p.op('dve', lambda e: e.tensor_tensor(out=vn[0:T, :], in0=vn[0:T, :], in1=lnb[0:T, :], op=ALU.add), reads=[vn, lnb], writes=[vn])
                    if is_sample:
                        p.dma('sp', lambda e: e.dma_start(out=gmv_s[0:T, :], in_=vn[0:T, :]), reads=[vn], writes=['gmv_s'])
                    elif ti == NT - 1:
                        p.dma('sp', lambda e: e.dma_start(out=gmv_p[:, :], in_=vn[0:T, :]), reads=[vn], writes=['gmv_p'])
                    p.op('pool', lambda e: e.tensor_copy(out=vnb[0:T, :], in_=vn[0:T, :]), reads=[vn], writes=[vnb])
                    bk = nb()
                    Wm, bsm = (W4T, bsT4) if is_sample else (WgT, bsT)
                    for g in range(4):
                        mm(bk[0:T, g * 128:(g + 1) * 128], Wm[0:T, g, 0:T], vnb[0:T, g * 128:(g + 1) * 128], True, True, [Wm, vnb], [bk])
                    for g in range(4):
                        p.op('dve', lambda e, g=g, bk=bk: e.scalar_tensor_tensor(out=ycatb[0:T, g * 128:(g + 1) * 128], in0=bk[0:T, g * 128:(g + 1) * 128],
                                                                                  scalar=bsm[0:T, g:g + 1], in1=ub[0:T, g * 128:(g + 1) * 128],
                                                                                  op0=ALU.add, op1=ALU.mult), reads=[bk, bsm, ub], writes=[ycatb])

                def feature_major(T, ti, qTb, qiTb):
                    p.op('pool', lambda e: e.tensor_copy(out=kb[0:T, 0:128], in_=kvf[0:T, 0:128]), reads=[kvf], writes=[kb])
                    p.op('pool', lambda e: e.tensor_copy(out=kb[0:T, 128:192], in_=kvf[0:T, 256:320]), reads=[kvf], writes=[kb])
                    p.op('pool', lambda e: e.tensor_scalar(out=wsc[0:T, :], in0=kvf[0:T, 320:328], scalar1=8.0 ** -0.5, scalar2=None, op0=ALU.mult),
                         reads=[kvf], writes=[wsc])
                    for (src, off, dst) in ((zq, 0, qTb), (zq, 512, qiTb)):
                        bk = nb()
                        bv = bk[:].bitcast(BF16)
                        for h in range(8):
                            tr(bv[0:64, h * T:(h + 1) * T], src[0:T, off + h * 64: off + (h + 1) * 64], T, [src], [bk])
                        p.op('act', lambda e, bv=bv, dst=dst: e.copy(out=dst[:, :, 0:T], in_=bv[0:64, 0:8 * T].rearrange("q (h t) -> q h t", h=8)),
                             reads=[bk], writes=[dst])

                def prompt_dsa(ti):
                    T = 128
                    L = 128 * (ti + 1)
                    c_lo = ti * 128
                    bk = nb()
                    bv = bk[:].bitcast(BF16)
                    for g in range(3):
                        tr(bv[0:64, g * 128:(g + 1) * 128], kb[:, g * 64:(g + 1) * 64], 128, [kb], [bk])
                    p.op('act', lambda e, bv=bv: e.copy(out=kT[0][:, c_lo:c_lo + 128], in_=bv[0:64, 0:128]), reads=[bk], writes=[kT[0]])
                    p.op('act', lambda e, bv=bv: e.copy(out=kT[1][:, c_lo:c_lo + 128], in_=bv[0:64, 128:256]), reads=[bk], writes=[kT[1]])
                    p.op('act', lambda e, bv=bv: e.copy(out=kiT[:, c_lo:c_lo + 128], in_=bv[0:64, 256:384]), reads=[bk], writes=[kiT])
                    p.op('pool', lambda e: e.tensor_copy(out=v_aug[:, ti, :, 0:64], in_=kvf[:, 128:256].rearrange("q (g d) -> q g d", g=2)),
                         reads=[kvf], writes=[v_aug])
                    ri = 0
                    for c0 in range(0, L, 512):
                        w = min(512, L - c0)
                        for h in range(8):
                            bk = nb()
                            mm(bk[:, 0:w], qiT[:, h, :], kiT[:, c0:c0 + w], True, True, [qiT, kiT], [bk])
                            rb = rbuf[ri % 2]
                            ri += 1
                            p.op('act', lambda e, bk=bk, rb=rb, w=w: e.activation(out=rb[:, 0:w], in_=bk[:, 0:w], func=AF.Relu), reads=[bk], writes=[rb])
                            if h == 0:
                                p.op('dve', lambda e, rb=rb, w=w, c0=c0: e.tensor_scalar(out=sc[:, c0:c0 + w], in0=rb[:, 0:w], scalar1=wsc[:, 0:1], scalar2=None, op0=ALU.mult),
                                     reads=[rb, wsc], writes=[sc])
                            else:
                                p.op('dve', lambda e, rb=rb, w=w, c0=c0, h=h: e.scalar_tensor_tensor(out=sc[:, c0:c0 + w], in0=rb[:, 0:w], scalar=wsc[:, h:h + 1],
                                                                                                     in1=sc[:, c0:c0 + w], op0=ALU.mult, op1=ALU.add),
                                     reads=[rb, wsc, sc], writes=[sc])
                    if ti >= 2:
                        p.op('act', lambda e: e.activation(out=maskb[:, 0:L], in_=sc[:, 0:L], func=AF.Square, accum_out=bis[:, 0:1]), reads=[sc], writes=[maskb, bis])
                    p.op('dve', lambda e: e.tensor_tensor(out=sc[:, c_lo:c_lo + 128], in0=sc[:, c_lo:c_lo + 128], in1=negmask[:, :], op=ALU.add),
                         reads=[sc, negmask], writes=[sc])
                    if ti >= 2:
                        NITP = cfg.get('nit_p', 20)
                        p.op('act', lambda e: e.activation(out=bis[:, 1:2], in_=bis[:, 0:1], func=AF.Sqrt), reads=[bis], writes=[bis])
                        p.op('dve', lambda e: e.tensor_scalar(out=bis[:, 2:3], in0=bis[:, 1:2], scalar1=2.2, scalar2=2.0, op0=ALU.mult, op1=ALU.add), reads=[bis], writes=[bis])
                        p.op('dve', lambda e: e.tensor_scalar(out=Wtp[:, :], in0=pow2p[:, :], scalar1=bis[:, 2:3], scalar2=None, op0=ALU.mult), reads=[pow2p, bis], writes=[Wtp])
                        p.op('dve', lambda e: e.memset(midp[:, :], 0.0), writes=[midp])
                        for k in range(NITP):
                            p.op('dve', lambda e: e.tensor_scalar(out=maskb[:, 0:L], in0=sc[:, 0:L], scalar1=midp[:, 0:1], scalar2=None, op0=ALU.is_ge, op1=ALU.add,
                                                                  accum_out=bis[:, 3:4]), reads=[sc, midp], writes=[maskb, bis])
                            p.op('dve', lambda e: e.tensor_scalar(out=bis[:, 4:5], in0=bis[:, 3:4], scalar1=255.5, scalar2=0.5, op0=ALU.is_ge, op1=ALU.subtract), reads=[bis], writes=[bis])
                            p.op('dve', lambda e, k=k: e.scalar_tensor_tensor(out=midp[:, :], in0=bis[:, 4:5], scalar=Wtp[:, k:k + 1], in1=midp[:, :], op0=ALU.mult, op1=ALU.add),
                                 reads=[bis, Wtp, midp], writes=[midp])
                        p.op('dve', lambda e: e.tensor_tensor(out=bis[:, 5:6], in0=midp[:, :], in1=Wtp[:, NITP:NITP + 1], op=ALU.subtract), reads=[midp, Wtp], writes=[bis])
                        tau = bis[:, 5:6]
                        tau_t = bis
                    else:
                        tau = tau_c[:, 0:1]
                        tau_t = tau_c
                    p.op('dve', lambda e: e.tensor_scalar(out=maskb[:, 0:L], in0=sc[:, 0:L], scalar1=tau, scalar2=None, op0=ALU.is_ge),
                         reads=[sc, tau_t], writes=[maskb])
                    for j0 in range(0, ti + 1, 8):
                        nj = min(8, ti + 1 - j0)
                        bk = nb()
                        bv = bk[:].bitcast(BF16)
                        for jj in range(nj):
                            tr(bv[:, jj * 128:(jj + 1) * 128], maskb[:, (j0 + jj) * 128:(j0 + jj + 1) * 128], 128, [maskb], [bk])
                        p.op('act', lambda e, bv=bv, j0=j0, nj=nj: e.copy(out=maskT[:, j0:j0 + nj, :], in_=bv[:, 0:nj * 128].rearrange("q (j t) -> q j t", j=nj)),
                             reads=[bk], writes=[maskT])
                    seq = [(g, j) for g in range(2) for j in range(ti + 1)]
                    PTl = {}

                    def att_scores(idx):
                        g, j = seq[idx]
                        bk = nb()
                        mm(bk[:, :], kT[g][:, j * 128:(j + 1) * 128], qT[:, 4 * g:4 * g + 4, :].rearrange("q h t -> q (h t)"), True, True, [kT[g], qT], [bk])
                        E = Eb[idx % 3]
                        PT = PTb[idx % 3]
                        p.op('act', lambda e: e.activation(out=E[:, :], in_=bk[:, :], func=AF.Exp, scale=0.125), reads=[bk], writes=[E])
                        p.op('dve', lambda e: e.tensor_tensor(out=PT[:, :, :], in0=E[:, :].rearrange("q (h t) -> q h t", h=4),
                                                               in1=maskT[:, j:j + 1, :].to_broadcast([128, 4, 128]), op=ALU.mult),
                             reads=[E, maskT], writes=[PT])
                        PTl[idx] = PT

                    def att_pv(idx):
                        g, j = seq[idx]
                        ob = pb[6 + g]
                        PT = PTl[idx]
                        for hh in range(4):
                            mm(ob[:, hh * 65:(hh + 1) * 65], PT[:, hh, :], v_aug[:, j, g, :], (j == 0 and hh == 0), (j == ti), [PT, v_aug], [ob])
                        if j == ti:
                            p.op('dve', lambda e: e.reciprocal(out=rden[:, 4 * g:4 * g + 4], in_=ob[:, 0:260].rearrange("q (h d) -> q h d", h=4)[:, :, 64]),
                                 reads=[ob], writes=[rden])
                            p.op('dve', lambda e: e.tensor_tensor(out=ycat[:, 512 + 256 * g:512 + 256 * (g + 1)].rearrange("q (h d) -> q h d", h=4),
                                                                  in0=ob[:, 0:260].rearrange("q (h d) -> q h d", h=4)[:, :, 0:64],
                                                                  in1=rden[:, 4 * g:4 * g + 4].unsqueeze(2).to_broadcast([128, 4, 64]), op=ALU.mult),
                                 reads=[ob, rden], writes=[ycat])
                    for idx in range(len(seq) + 1):
                        if idx < len(seq):
                            att_scores(idx)
                        if idx >= 1:
                            att_pv(idx - 1)

                def out_proj_and_mem(T, row0, is_sample, x0b, ycatb):
                    bk = nb()
                    bv = bk[:].bitcast(BF16)
                    for c in range(8):
                        tr(bv[:, c * T:(c + 1) * T], ycatb[0:T, c * 128:(c + 1) * 128], T, [ycatb], [bk])
                    p.op('act', lambda e, bv=bv: e.copy(out=yT[:, :, 0:T], in_=bv[:, 0:8 * T].rearrange("q (c t) -> q c t", c=8)), reads=[bk], writes=[yT])
                    for half in range(2):
                        bk = nb()
                        for c in range(8):
                            mm(bk[0:T, :], yT[:, c, 0:T], w_out[:, c, half * 512:(half + 1) * 512], c == 0, c == 7, [yT, w_out], [bk])
                        p.op('dve', lambda e, bk=bk, half=half: e.tensor_tensor(out=x1[0:T, half * 512:(half + 1) * 512], in0=bk[0:T, :],
                                                                                in1=x0b[0:T, half * 512:(half + 1) * 512], op=ALU.add),
                             reads=[bk, x0b], writes=[x1])
                    norm_T(x1, T, 'b', xn, xnT, ssd, sq)
                    bk = nb()
                    for h in range(4):
                        for c in range(8):
                            mm(bk[:, h * T:(h + 1) * T], wq[:, c, h * 128:(h + 1) * 128], xnT[:, c, 0:T], c == 0, c == 7, [wq, xnT], [bk])
                    p.op('act', lambda e, bk=bk: e.copy(out=qmT[:, :, 0:T], in_=bk[:, 0:4 * T].rearrange("q (h t) -> q h t", h=4)), reads=[bk], writes=[qmT])
                    if not is_sample:
                        for mt in range(2):
                            bk = nb()
                            for h in range(4):
                                mm(bk[:, h * 128:(h + 1) * 128], mkT[:, h, mt * 128:(mt + 1) * 128], qmT[:, h, :], True, True, [mkT, qmT], [bk])
                            p.op('act', lambda e, bk=bk, mt=mt: e.activation(out=PmT[:, mt, :, :], in_=bk[:, :].rearrange("q (h t) -> q h t", h=4),
                                                                             func=AF.Exp, scale=128.0 ** -0.5), reads=[bk], writes=[PmT])
                        for hp in range(2):
                            ob = pb[6 + hp]
                            for mt in range(2):
                                for hh in range(2):
                                    h = 2 * hp + hh
                                    mm(ob[:, hh * 129:(hh + 1) * 129], PmT[:, mt, h, :], mv_aug[:, mt, h, :], (mt == 0 and hh == 0), (mt == 1), [PmT, mv_aug], [ob])
                            p.op('dve', lambda e, ob=ob, hp=hp: e.reciprocal(out=rden[:, 2 * hp:2 * hp + 2], in_=ob[:, 0:258].rearrange("q (h d) -> q h d", h=2)[:, :, 128]),
                                 reads=[ob], writes=[rden])
                            p.op('dve', lambda e, ob=ob, hp=hp: e.tensor_tensor(out=om[:, 256 * hp:256 * (hp + 1)].rearrange("q (h d) -> q h d", h=2),
                                                                                in0=ob[:, 0:258].rearrange("q (h d) -> q h d", h=2)[:, :, 0:128],
                                                                                in1=rden[:, 2 * hp:2 * hp + 2].unsqueeze(2).to_broadcast([128, 2, 128]), op=ALU.mult),
                                 reads=[ob, rden], writes=[om])
                        bk = nb()
                        bv = bk[:].bitcast(BF16)
                        for h in range(4):
                            tr(bv[:, h * T:(h + 1) * T], om[0:T, h * 128:(h + 1) * 128], T, [om], [bk])
                        p.op('act', lambda e, bv=bv: e.copy(out=omT[:, :, 0:T], in_=bv[:, 0:4 * T].rearrange("q (h t) -> q h t", h=4)), reads=[bk], writes=[omT])
                    else:
                        sample_mem_attn()
                    for half in range(2):
                        bk = nb()
                        for h in range(4):
                            mm(bk[0:T, :], omT[:, h, 0:T], wo[:, h, half * 512:(half + 1) * 512], h == 0, h == 3, [omT, wo], [bk])
                        p.op('dve', lambda e, bk=bk, half=half: e.tensor_tensor(out=x0b[0:T, half * 512:(half + 1) * 512], in0=bk[0:T, :],
                                                                                in1=x1[0:T, half * 512:(half + 1) * 512], op=ALU.add),
                             reads=[bk, x1], writes=[x0b])
                    p.dma('sp', lambda e: e.dma_start(out=x2s[row0:row0 + T, :], in_=x0b[0:T, :]), reads=[x0b], writes=['x2s'])
                    if dbg:
                        p.dma('sp', lambda e: e.dma_start(out=x2_dbg[row0:row0 + T, :], in_=x0b[0:T, :]), reads=[x0b], writes=['x2_dbg'])

                def sample_mem_attn():
                    for b in range(DEC_B):
                        mf, mb, mT = sm['mf'], sm['mb'], sm['mT']
                        for which, src_d in ((0, cmk_d), (1, cmv_d)):
                            p.dma('sp', lambda e, b=b, src_d=src_d, which=which: e.dma_start(out=mf[which][:, :, :], in_=src_d[b, :, :].rearrange("(mt m) f -> m mt f", mt=2)),
                                  writes=[mf[which]])
                            p.op('pool' if which else 'dve', lambda e, which=which: e.tensor_copy(out=mb[which][:, :, :], in_=mf[which][:, :, :]), reads=[mf[which]], writes=[mb[which]])
                        bk = nb()
                        bv = bk[:].bitcast(BF16)
                        for mt in range(2):
                            for h in range(4):
                                tr(bv[:, (mt * 4 + h) * 128:(mt * 4 + h + 1) * 128], mb[0][:, mt, h * 128:(h + 1) * 128], 128, [mb[0]], [bk])
                        p.op('act', lambda e, bv=bv: e.copy(out=mT[:, :, :], in_=bv[:, :].rearrange("q (k m) -> q k m", k=8)), reads=[bk], writes=[mT])
                        bk = nb()
                        for mt in range(2):
                            for h in range(4):
                                mm(bk[:, (mt * 4 + h) * 4:(mt * 4 + h + 1) * 4], mT[:, mt * 4 + h, :], qmT[:, h, 4 * b:4 * b + 4], True, True, [mT, qmT], [bk])
                        Pm = sm['Pm']
                        p.op('act', lambda e, bk=bk: e.activation(out=Pm[:, :], in_=bk[:, 0:32], func=AF.Exp, scale=128.0 ** -0.5), reads=[bk], writes=[Pm])
                        o6, o7 = pb[6], pb[7]
                        for h in range(4):
                            for mt in range(2):
                                mm(o6[:, h * 4:(h + 1) * 4], mb[1][:, mt, h * 128:(h + 1) * 128], Pm[:, (mt * 4 + h) * 4:(mt * 4 + h + 1) * 4], mt == 0, mt == 1, [mb[1], Pm], [o6])
                        for h in range(4):
                            for mt in range(2):
                                mm(o7[:, h * 4:(h + 1) * 4], ones_bf[:, :], Pm[:, (mt * 4 + h) * 4:(mt * 4 + h + 1) * 4], mt == 0, mt == 1, [ones_bf, Pm], [o7])
                        rc = sm['rc']
                        p.op('dve', lambda e: e.reciprocal(out=rc[:, 0:16], in_=o7[:, 0:16]), reads=[o7], writes=[rc])
                        p.op('dve', lambda e, b=b: e.tensor_tensor(out=omT[:, :, 4 * b:4 * b + 4], in0=o6[:, 0:16].rearrange("q (h t) -> q h t", h=4),
                                                                 in1=rc[:, 0:16].rearrange("q (h t) -> q h t", h=4), op=ALU.mult), reads=[o6, rc], writes=[omT])

                sm = {}

                def sample_dsa(stk):
                    T = NS
                    NIT = 36
                    gbufs = [p.sb('gbuf%d' % i, [64, 8192], F32, stk) for i in range(cfg.get('n_gbuf', 2))]
                    cbs = [p.sb('cbs%d' % i, [64, 8192], BF16, stk) for i in range(cfg.get('n_cbuf', 1))]
                    KTb = p.sb('KTb', [128, 64, 64], BF16, stk)
                    kiTc_l = [p.sb('kiTc%d' % i, [64, 16, 64], BF16, stk) for i in range(3)]
                    pt_sb = p.sb('pt_sb', [64, 16], I32, stk)
                    idx2 = p.sb('idx2', [64, 16, 2], I32, stk)
                    rS_l = [p.sb('rS%d' % i, [64, 16, 32], F32, stk) for i in range(3)]
                    NCH = cfg.get('nch', 4)
                    scTbs = [p.sb('scTb%d' % i, [64, 128, 4], F32, stk) for i in range(NCH)]
                    scns = [p.sb('scn%d' % i, [4, 4], F32, stk) for i in range(NCH)]
                    rSn_l = [p.sb('rSn%d' % i, [4, 32], F32, stk) for i in range(2)]
                    chunk_ctr = [0]
                    Wbc = p.sb('Wbc', [64, 16, 32], F32, stk)
                    Dg = p.sb('Dg', [64, 16, 8, 4], F32, stk)
                    q2T = p.sb('q2T', [128, 4, 64], BF16, stk)
                    kT2n = p.sb('kT2n', [128, 64], BF16, stk)
                    kiTs = p.sb('kiTs', [64, 64], BF16, stk)
                    vnf = p.sb('vnf', [4, 16, 128], F32, stk)
                    vnew = p.sb('vnew', [4, 16, 128], BF16, stk)
                    negm4 = p.sb('negm4', [4, 4], F32, stk)
                    pow2 = p.sb('pow2', [64, 48], F32, stk)
                    hs_l = [p.sb('hs%d' % i, [64, 4], F32, stk) for i in range(NCH)]
                    hsb = p.sb('hsb', [64, 2], BF16, stk)
                    Wsc_l = [p.sb('Wsc%d' % i, [64, 2], F32, stk) for i in range(NCH)]
                    Wtab_l = [p.sb('Wtab%d' % i, [64, 48], F32, stk) for i in range(NCH)]
                    lo_l = [p.sb('lo%d' % i, [64, 4], F32, stk) for i in range(NCH)]
                    mid_l = [p.sb('mid%d' % i, [64, 4], F32, stk) for i in range(NCH)]
                    ge_l = [p.sb('ge%d' % i, [64, 4], F32, stk) for i in range(NCH)]
                    cmpb_l = [p.sb('cmpb%d' % i, [64, 128, 4], BF16, stk) for i in range(NCH)]
                    cntp_l = [p.sb('cntp%d' % i, [64, 4], F32, stk) for i in range(NCH)]
                    cmpn_l = [p.sb('cmpn%d' % i, [4, 4], F32, stk) for i in range(NCH)]
                    mask_s_l = cmpb_l
                    maskn_l = [p.sb('maskn%d' % i, [4, 4], BF16, stk) for i in range(NCH)]
                    Ess = [p.sb('Es%d' % i, [64, 16, 32], BF16, stk) for i in range(2)]
                    PTss = [p.sb('PTs%d' % i, [64, 16, 32], BF16, stk) for i in range(2)]
                    En = p.sb('En', [4, 32], BF16, stk)
                    PTn = p.sb('PTn', [4, 32], BF16, stk)
                    rcs = p.sb('rcs', [128, 32], F32, stk)
                    dacc = p.sb('dacc', [64, 8, 32], F32, stk)
                    dtot = p.sb('dtot', [64, 32], F32, stk)
                    q2bd = p.sb('q2bd', [128, 16, 32], BF16, stk)
                    ybTs = p.sb('ybTs', [128, 4, 64], BF16, stk)

                    p.dma('sp', lambda e: e.dma_start(out=pt_sb[:, :], in_=pt_d.rearrange("b j -> j b"), allow_slow_non_contiguous=True), writes=[pt_sb])
                    p.dma('sp', lambda e: e.dma_start(out=negm4[:, :], in_=c_negm4[:, :]), writes=[negm4])
                    p.dma('sp', lambda e: e.dma_start(out=pow2[:, :], in_=c_pow2[0:64, :]), writes=[pow2])
                    for half in range(2):
                        p.op('dve', lambda e, half=half: e.tensor_scalar(out=idx2[:, :, half], in0=pt_sb[:, :], scalar1=2, scalar2=half, op0=ALU.mult, op1=ALU.add),
                             reads=[pt_sb], writes=[idx2])
                    p.op('dve', lambda e: e.tensor_tensor(out=Dg[:, :, :, :], in0=wsc_s[:, :].unsqueeze(1).unsqueeze(3).to_broadcast([64, 16, 8, 4]),
                                                          in1=identf[0:64, 0:64].rearrange("q (b t) -> q b t", b=16).unsqueeze(2).to_broadcast([64, 16, 8, 4]), op=ALU.mult),
                         reads=[wsc_s, identf], writes=[Dg])
                    bk = nb()
                    mm(bk[0:64, :], onesf[0:64, 0:64], Dg[:, :, :, :].rearrange("q b h t -> q (b h t)"), True, True, [onesf, Dg], [bk])
                    p.op('act', lambda e, bk=bk: e.copy(out=Wbc[:, :, :], in_=bk[0:64, :].rearrange("q (b x) -> q b x", b=16)), reads=[bk], writes=[Wbc])
                    p.op('pool', lambda e: e.tensor_copy(out=q2T[0:64, :, :], in_=qTs[:, 0:4, :]), reads=[qTs], writes=[q2T])
                    p.dma('sp', lambda e: e.dma_start(out=q2T[64:128, :, :], in_=qTs[:, 4:8, :]), reads=[qTs], writes=[q2T])
                    p.op('dve', lambda e: e.memset(q2bd[:, :, :], 0.0), writes=[q2bd])
                    for g in range(2):
                        p.op('dve', lambda e, g=g: e.tensor_copy(out=q2bd[g * 64:(g + 1) * 64, :, g * 16:(g + 1) * 16].rearrange("q b (r t) -> q b r t", r=4),
                                                                 in_=q2T[g * 64:(g + 1) * 64, :, :].rearrange("q r (b t) -> q b r t", t=4)), reads=[q2T], writes=[q2bd])
                    bk = nb()
                    bv = bk[:].bitcast(BF16)
                    tr(bv[:, 0:64], kb_s[:, 0:128], 64, [kb_s], [bk])
                    tr(bv[0:64, 64:128], kb_s[:, 128:192], 64, [kb_s], [bk])
                    p.op('act', lambda e, bv=bv: e.copy(out=kT2n[:, :], in_=bv[:, 0:64]), reads=[bk], writes=[kT2n])
                    p.op('act', lambda e, bv=bv: e.copy(out=kiTs[:, :], in_=bv[0:64, 64:128]), reads=[bk], writes=[kiTs])
                    p.dma('sp', lambda e: e.dma_start(out=vnf[:, :, :], in_=v_s.rearrange("(b t) d -> t b d", t=4)), reads=['vo'], writes=[vnf])
                    p.op('dve', lambda e: e.tensor_copy(out=vnew[:, :, :], in_=vnf[:, :, :]), reads=[vnf], writes=[vnew])

                    nb_ = cfg.get('n_sb', DEC_B)
                    NITB = cfg.get('nit', 22)
                    gseq = []
                    for b0_ in range(0, nb_, NCH):
                        grp_ = list(range(b0_, min(b0_ + NCH, nb_)))
                        gseq += [('ki', b_, 0) for b_ in grp_]
                        for b_ in grp_:
                            gseq += [('K', b_, 0), ('V', b_, 0), ('K', b_, 1), ('V', b_, 1)]
                    gpos = {it: i for i, it in enumerate(gseq)}
                    g_next = [0]

                    NGB = len(gbufs)

                    def emit_gather(i):
                        kind, b_, half = gseq[i]
                        gb = gbufs[i % NGB]
                        if kind == 'ki':
                            src, idx_ap, idx_t = ckidx_d, pt_sb[:, b_:b_ + 1], pt_sb
                        else:
                            src, idx_ap, idx_t = (ck_d if kind == 'K' else cv_d), idx2[:, b_, half:half + 1], idx2
                        p.dma('pool', lambda e: e.indirect_dma_start(out=gb[:, :], out_offset=None, in_=src[:, :],
                                                                     in_offset=bass.IndirectOffsetOnAxis(ap=idx_ap, axis=0)), reads=[idx_t], writes=[gb])

                    g_cast = [0]

                    def use(item):
                        i = gpos[item]
                        while g_next[0] < min(NGB, len(gseq)):
                            emit_gather(g_next[0])
                            g_next[0] += 1
                        while g_cast[0] <= i:
                            c = g_cast[0]
                            dst = cbs[c % len(cbs)]
                            gb = gbufs[c % NGB]
                            p.op('act', lambda e, dst=dst, gb=gb: e.copy(out=dst[:, 0:4096], in_=gb[:, 0:4096]), reads=[gb], writes=[dst])
                            p.op('dve', lambda e, dst=dst, gb=gb: e.tensor_copy(out=dst[:, 4096:8192], in_=gb[:, 4096:8192]), reads=[gb], writes=[dst])
                            g_cast[0] += 1
                            if g_next[0] < len(gseq):
                                emit_gather(g_next[0])
                                g_next[0] += 1
                        return cbs[i % len(cbs)]

                    def ki_steps(b):
                        qi_b = qiTs[:, :, 4 * b:4 * b + 4]
                        scT = scTbs[b % NCH]
                        scn_ = scns[b % NCH]
                        steps = []

                        def chunk(lc):
                            kiTc = kiTc_l[chunk_ctr[0] % 3]
                            rS = rS_l[chunk_ctr[0] % 3]
                            chunk_ctr[0] += 1
                            cb = use(('ki', b, 0))
                            bk = nb()
                            bv = bk[:].bitcast(BF16)
                            for i in range(16):
                                l = lc * 16 + i
                                tr(bv[0:64, i * 64:(i + 1) * 64], cb[:, l * 64:(l + 1) * 64], 64, [cb], [bk])
                            p.op('act', lambda e: e.copy(out=kiTc[:, :, :], in_=bv[0:64, :].rearrange("q (i j) -> q i j", i=16)), reads=[bk], writes=[kiTc])
                            bk2 = nb()
                            for i in range(16):
                                mm(bk2[0:64, i * 32:(i + 1) * 32], kiTc[:, i, :], qi_b, True, True, [kiTc, qiTs], [bk2])
                            p.op('act', lambda e: e.activation(out=rS[:, :, :], in_=bk2[0:64, :].rearrange("q (i x) -> q i x", i=16), func=AF.Relu), reads=[bk2], writes=[rS])
                            p.op('dve', lambda e: e.tensor_tensor(out=rS[:, :, :], in0=rS[:, :, :], in1=Wbc[:, b:b + 1, :].to_broadcast([64, 16, 32]), op=ALU.mult),
                                 reads=[rS, Wbc], writes=[rS])
                            p.op('dve', lambda e: e.tensor_reduce(out=scT[:, lc * 16:(lc + 1) * 16, :], in_=rS[:, :, :].rearrange("q i (h t) -> q i t h", h=8),
                                                                  op=ALU.add, axis=AX.X), reads=[rS], writes=[scT])

                        def newkeys():
                            rSn = rSn_l[b % 2]
                            bk = nb()
                            mm(bk[0:4, 0:32], kiTs[:, 4 * b:4 * b + 4], qi_b, True, True, [kiTs, qiTs], [bk])
                            p.op('act', lambda e: e.activation(out=rSn[:, :], in_=bk[0:4, 0:32], func=AF.Relu), reads=[bk], writes=[rSn])
                            p.op('dve', lambda e: e.tensor_tensor(out=rSn[:, :], in0=rSn[:, :], in1=Wbc[0:4, b, :], op=ALU.mult), reads=[rSn, Wbc], writes=[rSn])
                            p.op('dve', lambda e: e.tensor_reduce(out=scn_[:, :], in_=rSn[:, :].rearrange("q (h t) -> q t h", h=8), op=ALU.add, axis=AX.X), reads=[rSn], writes=[scn_])
                        for lc in range(8):
                            steps.append(lambda lc=lc: chunk(lc))
                        steps.append(newkeys)
                        return steps

                    def bisect_setup(b):
                        c = b % NCH
                        scT, scn_, hs, Wsc, Wtab, lo, cmpb = scTbs[c], scns[c], hs_l[c], Wsc_l[c], Wtab_l[c], lo_l[c], cmpb_l[c]
                        p.op('act', lambda e: e.activation(out=cmpb[:, :, :].rearrange("q l t -> q (l t)"), in_=scT[:, :, :].rearrange("q l t -> q (l t)"), func=AF.Square, accum_out=hs[:, 0:1]),
                             reads=[scT], writes=[cmpb, hs])
                        p.op('act', lambda e: e.activation(out=cmpb[0:4, 0, :], in_=scn_[:, :], func=AF.Square, accum_out=hs[0:4, 1:2]), reads=[scn_], writes=[cmpb, hs])
                        p.op('dve', lambda e: e.tensor_tensor(out=scn_[:, :], in0=scn_[:, :], in1=negm4[:, :], op=ALU.add), reads=[scn_, negm4], writes=[scn_])
                        bk = nb()
                        mm(bk[0:64, 0:1], onesf[0:64, 0:64], hs[:, 0:1], True, False, [onesf, hs], [bk])
                        mm(bk[0:64, 0:1], onesf[0:4, 0:64], hs[0:4, 1:2], False, True, [onesf, hs], [bk])
                        p.op('act', lambda e, bk=bk: e.activation(out=Wsc[:, 0:1], in_=bk[0:64, 0:1], func=AF.Sqrt), reads=[bk], writes=[Wsc])
                        p.op('dve', lambda e: e.tensor_scalar(out=Wsc[:, 0:1], in0=Wsc[:, 0:1], scalar1=2.2, scalar2=2.0, op0=ALU.mult, op1=ALU.add), reads=[Wsc], writes=[Wsc])
                        p.op('dve', lambda e: e.tensor_scalar(out=Wsc[:, 1:2], in0=Wsc[:, 0:1], scalar1=-0.5, scalar2=None, op0=ALU.mult), reads=[Wsc], writes=[Wsc])
                        p.op('dve', lambda e: e.tensor_scalar(out=Wtab[:, :], in0=pow2[:, :], scalar1=Wsc[:, 0:1], scalar2=None, op0=ALU.mult), reads=[pow2, Wsc], writes=[Wtab])
                        p.op('dve', lambda e: e.tensor_scalar(out=lo[:, :], in0=pow2[:, 0:4], scalar1=0.0, scalar2=Wsc[:, 1:2], op0=ALU.mult, op1=ALU.add), reads=[pow2, Wsc], writes=[lo])

                    def bisect_iter(b, k):
                        c = b % NCH
                        scT, scn_, Wtab, lo, mid, ge, cmpb, cntp, cmpn = scTbs[c], scns[c], Wtab_l[c], lo_l[c], mid_l[c], ge_l[c], cmpb_l[c], cntp_l[c], cmpn_l[c]
                        p.op('dve', lambda e: e.tensor_scalar(out=mid[:, :], in0=lo[:, :], scalar1=Wtab[:, k:k + 1], scalar2=None, op0=ALU.add), reads=[lo, Wtab], writes=[mid])
                        p.op('dve', lambda e: e.tensor_tensor(out=cmpb[:, :, :], in0=scT[:, :, :], in1=mid[:, :].unsqueeze(1).to_broadcast([64, 128, 4]), op=ALU.is_ge),
                             reads=[scT, mid], writes=[cmpb])
                        p.op('dve', lambda e: e.tensor_reduce(out=cntp[:, :], in_=cmpb[:, :, :].rearrange("q l t -> q t l"), op=ALU.add, axis=AX.X), reads=[cmpb], writes=[cntp])
                        p.op('dve', lambda e: e.tensor_tensor(out=cmpn[:, :], in0=scn_[:, :], in1=mid[0:4, :], op=ALU.is_ge), reads=[scn_, mid], writes=[cmpn])
                        bk = nb()
                        mm(bk[0:64, 0:4], onesf[0:64, 0:64], cntp[:, :], True, False, [onesf, cntp], [bk])
                        mm(bk[0:64, 0:4], onesf[0:4, 0:64], cmpn[:, :], False, True, [onesf, cmpn], [bk])
                        p.op('dve', lambda e: e.tensor_scalar(out=ge[:, :], in0=bk[0:64, 0:4], scalar1=255.5, scalar2=None, op0=ALU.is_ge), reads=[bk], writes=[ge])
                        p.op('dve', lambda e: e.scalar_tensor_tensor(out=lo[:, :], in0=ge[:, :], scalar=Wtab[:, k:k + 1], in1=lo[:, :], op0=ALU.mult, op1=ALU.add),
                             reads=[ge, Wtab, lo], writes=[lo])

                    def bisect_finish(b):
                        c = b % NCH
                        scT, scn_, lo, mask_s, maskn = scTbs[c], scns[c], lo_l[c], mask_s_l[c], maskn_l[c]
                        p.op('dve', lambda e: e.tensor_tensor(out=mask_s[:, :, :], in0=scT[:, :, :], in1=lo[:, :].unsqueeze(1).to_broadcast([64, 128, 4]), op=ALU.is_ge),
                             reads=[scT, lo], writes=[mask_s])
                        p.op('dve', lambda e: e.tensor_tensor(out=maskn[:, :], in0=scn_[:, :], in1=lo[0:4, :], op=ALU.is_ge), reads=[scn_, lo], writes=[maskn])

                    def attend(b):
                        mask_s, maskn = mask_s_l[b % NCH], maskn_l[b % NCH]
                        o6, o7 = pb[6], pb[7]
                        first = True
                        for half in range(2):
                            cbK = use(('K', b, half))
                            for lc in range(4):
                                bk = nb()
                                bv = bk[:].bitcast(BF16)
                                for i in range(16):
                                    l = lc * 16 + i
                                    tr(bv[:, i * 64:(i + 1) * 64], cbK[:, l * 128:(l + 1) * 128], 64, [cbK], [bk])
                                p.op('act', lambda e, bv=bv, lc=lc: e.copy(out=KTb[:, lc * 16:(lc + 1) * 16, :], in_=bv[:, :].rearrange("q (i j) -> q i j", i=16)), reads=[bk], writes=[KTb])
                            cbV = use(('V', b, half))
                            prev = None
                            for lc in range(5):
                                if lc < 4:
                                    bk = nb()
                                    for i in range(16):
                                        l = lc * 16 + i
                                        mm(bk[0:64, i * 32:(i + 1) * 32], KTb[:, l, :], q2bd[:, b, :], True, True, [KTb, q2bd], [bk])
                                    E_, P_ = Ess[lc % 2], PTss[lc % 2]
                                    p.op('act', lambda e, bk=bk, E_=E_: e.activation(out=E_[:, :, :], in_=bk[0:64, :].rearrange("q (i x) -> q i x", i=16), func=AF.Exp, scale=0.125),
                                         reads=[bk], writes=[E_])
                                    l0 = half * 64 + lc * 16
                                    p.op('dve', lambda e, l0=l0, E_=E_, P_=P_: e.tensor_tensor(out=P_[:, :, :].rearrange("q i (x t) -> q i x t", t=4), in0=E_[:, :, :].rearrange("q i (x t) -> q i x t", t=4),
                                                                                              in1=mask_s[:, l0:l0 + 16, :].unsqueeze(2).to_broadcast([64, 16, 8, 4]), op=ALU.mult),
                                         reads=[E_, mask_s], writes=[P_])
                                    ci = half * 4 + lc
                                    p.op('dve', lambda e, P_=P_, ci=ci: e.tensor_reduce(out=dacc[:, ci, :], in_=P_[:, :, :].rearrange("q i x -> q x i"), op=ALU.add, axis=AX.X),
                                         reads=[P_], writes=[dacc])
                                if prev is not None:
                                    plc, P_prev = prev
                                    for i in range(16):
                                        l = plc * 16 + i
                                        mm(o6[:, 0:32], cbV[:, l * 128:(l + 1) * 128], P_prev[:, i, :], first, False, [cbV, P_prev], [o6])
                                        first = False
                                prev = (lc, PTss[lc % 2]) if lc < 4 else None
                        bk = nb()
                        mm(bk[0:4, 0:32], kT2n[:, 4 * b:4 * b + 4], q2bd[:, b, :], True, True, [kT2n, q2bd], [bk])
                        p.op('act', lambda e, bk=bk: e.activation(out=En[:, :], in_=bk[0:4, 0:32], func=AF.Exp, scale=0.125), reads=[bk], writes=[En])
                        p.op('dve', lambda e: e.tensor_tensor(out=PTn[:, :].rearrange("q (x t) -> q x t", t=4), in0=En[:, :].rearrange("q (x t) -> q x t", t=4),
                                                               in1=maskn[:, :].unsqueeze(1).to_broadcast([4, 8, 4]), op=ALU.mult), reads=[En, maskn], writes=[PTn])
                        mm(o6[:, 0:32], vnew[:, b, :], PTn[:, :], False, True, [vnew, PTn], [o6])
                        p.op('dve', lambda e: e.tensor_reduce(out=dtot[:, :], in_=dacc[:, :, :].rearrange("q c x -> q x c"), op=ALU.add, axis=AX.X), reads=[dacc], writes=[dtot])
                        p.op('dve', lambda e: e.tensor_tensor(out=dtot[0:4, :], in0=dtot[0:4, :], in1=PTn[:, :], op=ALU.add), reads=[dtot, PTn], writes=[dtot])
                        mm(o7[:, 0:32], onesf[0:64, :], dtot[:, :], True, True, [onesf, dtot], [o7])
                        p.op('dve', lambda e: e.reciprocal(out=rcs[:, :], in_=o7[:, 0:32]), reads=[o7], writes=[rcs])
                        for g in range(2):
                            p.op('dve', lambda e, g=g: e.tensor_tensor(out=ybTs[g * 64:(g + 1) * 64, :, 4 * b:4 * b + 4],
                                                                     in0=o6[g * 64:(g + 1) * 64, g * 16:(g + 1) * 16].rearrange("q (r t) -> q r t", r=4),
                                                                     in1=rcs[g * 64:(g + 1) * 64, g * 16:(g + 1) * 16].rearrange("q (r t) -> q r t", r=4), op=ALU.mult),
                                 reads=[o6, rcs], writes=[ybTs])

                    for b0_ in range(0, nb_, NCH):
                        grp_ = list(range(b0_, min(b0_ + NCH, nb_)))
                        for b in grp_:
                            for st_ in ki_steps(b):
                                st_()
                        if cfg.get('sb_stage', 9) < 2:
                            continue
                        for b in grp_:
                            bisect_setup(b)
                        for k in range(NITB):
                            for b in grp_:
                                bisect_iter(b, k)
                        for b in grp_:
                            bisect_finish(b)
                        if cfg.get('sb_stage', 9) < 3:
                            continue
                        for b in grp_:
                            attend(b)
                    for r in range(4):
                        bk = nb()
                        bv = bk[:].bitcast(BF16)
                        tr(bv[0:64, 0:128], ybTs[:, r, :], 128, [ybTs], [bk])
                        p.op('act', lambda e, bv=bv, r=r: e.copy(out=ycats[:, 512:1024].rearrange("q (g r d) -> q g r d", g=2, r=4)[:, :, r, :],
                                                                 in_=bv[0:64, 0:128].rearrange("q (g d) -> q g d", g=2)), reads=[bk], writes=[ycats])


                if 'P3' in do:
                    proj_tile(xs_d[:, :], NS, 0, True, x0s, ycats)
                    feature_major(NS, 0, qTs, qiTs)
                    p.op('pool', lambda e: e.tensor_copy(out=kvf_s[:, :], in_=kvf[0:NS, :]), reads=[kvf], writes=[kvf_s])
                    p.op('pool', lambda e: e.tensor_copy(out=kb_s[:, :], in_=kb[0:NS, :]), reads=[kb], writes=[kb_s])
                    p.op('pool', lambda e: e.tensor_copy(out=wsc_s[:, :], in_=wsc[0:NS, :]), reads=[wsc], writes=[wsc_s])
                if 'P2' in do:
                    for ti in range(cfg.get('n_tiles', NT)):
                        proj_tile(xp_d[ti * 128:(ti + 1) * 128, :], 128, ti, False, x0, ycat)
                        feature_major(128, ti, qT, qiT)
                        if 'P4' in do and cfg.get('interleave_conv', True):
                            for ec in range(ti * 8, ti * 8 + 8):
                                convert_chunk(ec, cub[ec % 2], cut[ec % 2], cvb[ec % 2])
                            conv_done[0] = ti * 8 + 8
                        prompt_dsa(ti)
                        out_proj_and_mem(128, ti * 128, False, x0, ycat)
            sw.close()
            p.barrier()
            if 'P3' in do:
                with ExitStack() as s3s:
                    sample_dsa(s3s)
                p.barrier()
                with ExitStack() as s3m:
                    sm['mf'] = [p.sb('mf%d' % i, [128, 2, 512], F32, s3m) for i in range(2)]
                    sm['mb'] = [p.sb('mb%d' % i, [128, 2, 512], BF16, s3m) for i in range(2)]
                    sm['mT'] = p.sb('mT', [128, 8, 128], BF16, s3m)
                    sm['Pm'] = p.sb('Pm', [128, 32], BF16, s3m)
                    sm['rc'] = p.sb('rc', [128, 16], F32, s3m)
                    out_proj_and_mem(NS, SEQ, True, x0s, ycats)
            p.barrier()

        if 'P4' in do:
            nbmod[0] = 4
            with ExitStack() as s4:
                iota_f = p.sb('iota_f', [128, 128], F32, s4)
                gfin = p.sb('gfin', [128, D], F32, s4)
                p.dma('sp', lambda e: e.dma_start(out=iota_f[:], in_=c_iota[:, :]), writes=[iota_f])
                p.dma('sp', lambda e: e.dma_start(out=gfin[:], in_=g_fin_d.partition_broadcast(128)), writes=[gfin])
                pwq = p.sb('pwq_sb', [128, 8, D], BF16, s4)
                keysT = p.sb('keysT', [64, 16, 128], BF16, s4)
                with ExitStack() as s5:
                    stage = [p.sb('pstage%d' % i, [128, D], F32, s5) for i in range(2)]
                    load_weight(pwq, pwq_d, 8, D, None, stage)
                    kbf = p.sb('kbf', [128, 64], BF16, s5)
                    for hc in range(16):
                        stg = stage[hc % 2]
                        p.dma('sp', lambda e, hc=hc, stg=stg: e.dma_start(out=stg[:, 0:64], in_=pkeys_d[hc, :, :]), writes=[stg])
                        p.op('dve', lambda e, stg=stg: e.tensor_copy(out=kbf[:], in_=stg[:, 0:64]), reads=[stg], writes=[kbf])
                        bk = nb()
                        bv = bk[:].bitcast(BF16)
                        tr(bv[0:64, 0:128], kbf[:, :], 128, [kbf], [bk])
                        p.op('act', lambda e, hc=hc, bv=bv: e.copy(out=keysT[:, hc, :], in_=bv[0:64, 0:128]), reads=[bk], writes=[keysT])
                    ubf = [p.sb('ubf%d' % i, [128, D], BF16, s5) for i in range(2)]
                    utb = [p.sb('utb%d' % i, [128, D], BF16, s5) for i in range(2)]
                    vbf = [p.sb('vbf%d' % i, [128, D], BF16, s5) for i in range(2)]
                    for ec in range(conv_done[0], cfg.get('n_ec', 128)):
                        convert_chunk(ec, ubf[ec % 2], utb[ec % 2], vbf[ec % 2])

                p.barrier()
                xg_l = [p.sb('xg%d' % i, [128, 2, D], F32, s4) for i in range(2)]
                XT_l = [p.sb('XT%d' % i, [128, 8, 256], BF16, s4) for i in range(2)]
                hn = p.sb('hn', [128, D], BF16, s4)
                hnT = p.sb('hnT', [128, 8, 128], BF16, s4)
                sq4 = p.sb('sq4', [128, D], F32, s4)
                qpT = p.sb('qpT', [64, 16, 128], BF16, s4)
                ssb = p.sb('ssb', [128, 16, 128], F32, s4)
                wk = p.sb('wk', [128, 128], F32, s4)
                a16 = p.sb('a16', [128, 8, 16], F32, s4)
                b16 = p.sb('b16', [128, 8, 16], F32, s4)
                iau = p.sb('iau', [128, 8, 16], U32, s4)
                ibu = p.sb('ibu', [128, 8, 16], U32, s4)
                iaf = p.sb('iaf', [128, 8, 16], F32, s4)
                ibf = p.sb('ibf', [128, 8, 16], F32, s4)
                cand = p.sb('cand', [128, 8, 256], F32, s4)
                wkc = p.sb('wkc', [128, 256], F32, s4)
                c16 = p.sb('c16', [128, 8, 16], F32, s4)
                icu = p.sb('icu', [128, 8, 16], U32, s4)
                iju = p.sb('iju', [128, 2, 128], U32, s4)
                ijf = p.sb('ijf', [128, 2, 128], F32, s4)
                eq = p.sb('eq', [128, 128, 16], F32, s4)
                SEL = p.sb('SEL', [128, 3, 128], F32, s4)
                e16 = p.sb('e16', [128, 8, 16], F32, s4)
                z8 = p.sb('z8', [128, 16], F32, s4)
                selT_l = [p.sb('selT%d' % i, [128, 3, 256], F32, s4) for i in range(2)]
                Gt = p.sb('Gt', [128, 256, 128], BF16, s4)
                ohb1 = [p.sb('ohb1_%d' % i, [128, 8, 128], BF16, s4) for i in range(2)]
                ohb0 = [p.sb('ohb0_%d' % i, [128, 8, 128], BF16, s4) for i in range(2)]
                ohbg = [p.sb('ohbg_%d' % i, [128, 8, 128], BF16, s4) for i in range(2)]
                selTb_l = [p.sb('selTb%d' % i, [128, 3, 256], BF16, s4) for i in range(2)]
                iota_b = p.sb('iota_b', [128, 128], BF16, s4)
                p.op('pool', lambda e: e.tensor_copy(out=iota_b[:, :], in_=iota_f[:, :]), reads=[iota_f], writes=[iota_b])
                NBUF = 4
                UTc = [p.sb('UTc%d' % i, [128, 8, 128], BF16, s4) for i in range(NBUF)]
                Vc = [p.sb('Vc%d' % i, [128, D], BF16, s4) for i in range(NBUF)]
                gab = [p.sb('gab%d' % i, [128, 256], BF16, s4) for i in range(3)]
                GAb = [p.sb('GAb%d' % i, [128, 256], BF16, s4) for i in range(3)]
                pre = p.sb('pre', [128, D], F32, s4)
                yo = pre


                def peer_select(T, s, xg, XT, selT, selTb):
                    norm_T(xg[:, s, :], T, 'f', hn, hnT, ssd, sq4, gcol=gcols['ffn'])
                    p.op('pool', lambda e: e.tensor_copy(out=XT[:, :, s * 128:s * 128 + T], in_=hnT[:, :, 0:T]), reads=[hnT], writes=[XT])
                    for q4 in range(4):
                        bk = nb()
                        for k4 in range(4):
                            hc = q4 * 4 + k4
                            for c in range(8):
                                mm(bk[0:64, k4 * T:(k4 + 1) * T], pwq[:, c, hc * 64:(hc + 1) * 64], hnT[:, c, 0:T], c == 0, c == 7, [pwq, hnT], [bk])
                        p.op('act', lambda e, bk=bk, q4=q4: e.copy(out=qpT[:, q4 * 4:(q4 + 1) * 4, 0:T], in_=bk[0:64, 0:4 * T].rearrange("q (k t) -> q k t", k=4)),
                             reads=[bk], writes=[qpT])
                    for q4 in range(4):
                        bk = nb()
                        for k4 in range(4):
                            hc = q4 * 4 + k4
                            mm(bk[0:T, k4 * 128:(k4 + 1) * 128], qpT[:, hc, 0:T], keysT[:, hc, :], True, True, [qpT, keysT], [bk])
                        p.op('act', lambda e, bk=bk, q4=q4: e.copy(out=ssb[0:T, q4 * 4:(q4 + 1) * 4, :], in_=bk[0:T, :].rearrange("q (k n) -> q k n", k=4)),
                             reads=[bk], writes=[ssb])
                    for h in range(8):
                        for cc, (vals, idxs) in enumerate(((a16, iau), (b16, ibu))):
                            src = ssb[0:T, 2 * h + cc, :]
                            p.op('dve', lambda e, src=src, vals=vals, h=h: e.max(out=vals[0:T, h, 0:8], in_=src), reads=[ssb], writes=[vals])
                            p.op('dve', lambda e, src=src, vals=vals, idxs=idxs, h=h: e.max_index(out=idxs[0:T, h, 0:8], in_max=vals[0:T, h, 0:8], in_values=src),
                                 reads=[ssb, vals], writes=[idxs])
                            p.op('dve', lambda e, src=src, vals=vals, h=h: e.match_replace(out=wk[0:T, :], in_to_replace=vals[0:T, h, 0:8], in_values=src, imm_value=NEG),
                                 reads=[ssb, vals], writes=[wk])
                            p.op('dve', lambda e, vals=vals, h=h: e.max(out=vals[0:T, h, 8:16], in_=wk[0:T, :]), reads=[wk], writes=[vals])
                            p.op('dve', lambda e, vals=vals, idxs=idxs, h=h: e.max_index(out=idxs[0:T, h, 8:16], in_max=vals[0:T, h, 8:16], in_values=wk[0:T, :]),
                                 reads=[wk, vals], writes=[idxs])
                    for h in range(8):
                        p.op('dve', lambda e, h=h: e.tensor_tensor(out=cand[0:T, h, :].rearrange("q (i j) -> q i j", i=16),
                                                                    in0=a16[0:T, h, :].unsqueeze(2).to_broadcast([T, 16, 16]),
                                                                    in1=b16[0:T, h, :].unsqueeze(1).to_broadcast([T, 16, 16]), op=ALU.add),
                             reads=[a16, b16], writes=[cand])
                    for h in range(8):
                        src = cand[0:T, h, :]
                        p.op('dve', lambda e, src=src, h=h: e.max(out=c16[0:T, h, 0:8], in_=src), reads=[cand], writes=[c16])
                        p.op('dve', lambda e, src=src, h=h: e.max_index(out=icu[0:T, h, 0:8], in_max=c16[0:T, h, 0:8], in_values=src), reads=[cand, c16], writes=[icu])
                        p.op('dve', lambda e, src=src, h=h: e.match_replace(out=wkc[0:T, :], in_to_replace=c16[0:T, h, 0:8], in_values=src, imm_value=NEG),
                             reads=[cand, c16], writes=[wkc])
                        p.op('dve', lambda e, h=h: e.max(out=c16[0:T, h, 8:16], in_=wkc[0:T, :]), reads=[wkc], writes=[c16])
                        p.op('dve', lambda e, h=h: e.max_index(out=icu[0:T, h, 8:16], in_max=c16[0:T, h, 8:16], in_values=wkc[0:T, :]), reads=[wkc, c16], writes=[icu])
                    icf = icu[0:T, :, :].rearrange("q h k -> q (h k)")
                    p.op('dve', lambda e: e.tensor_scalar(out=iju[0:T, 0, :], in0=icf, scalar1=4, scalar2=None, op0=ALU.logical_shift_right), reads=[icu], writes=[iju])
                    p.op('dve', lambda e: e.tensor_scalar(out=iju[0:T, 1, :], in0=icf, scalar1=15, scalar2=None, op0=ALU.bitwise_and), reads=[icu], writes=[iju])
                    p.op('dve', lambda e: e.tensor_copy(out=ijf[0:T, :, :], in_=iju[0:T, :, :]), reads=[iju], writes=[ijf])
                    p.op('dve', lambda e: e.tensor_copy(out=iaf[0:T, :, :], in_=iau[0:T, :, :]), reads=[iau], writes=[iaf])
                    p.op('dve', lambda e: e.tensor_copy(out=ibf[0:T, :, :], in_=ibu[0:T, :, :]), reads=[ibu], writes=[ibf])
                    for w_, srcf in ((0, iaf), (1, ibf)):
                        p.op('dve', lambda e, w_=w_: e.tensor_tensor(out=eq[0:T, :, :], in0=ijf[0:T, w_, :].unsqueeze(2).to_broadcast([T, 128, 16]),
                                                                     in1=iota_f[0:T, 0:16].unsqueeze(1).to_broadcast([T, 128, 16]), op=ALU.is_equal),
                             reads=[ijf, iota_f], writes=[eq])
                        p.op('dve', lambda e, srcf=srcf: e.tensor_tensor(out=eq[0:T, :, :].rearrange("q (h k) i -> q h k i", h=8),
                                                                         in0=eq[0:T, :, :].rearrange("q (h k) i -> q h k i", h=8),
                                                                         in1=srcf[0:T, :, :].unsqueeze(2).to_broadcast([T, 8, 16, 16]), op=ALU.mult),
                             reads=[eq, srcf], writes=[eq])
                        p.op('dve', lambda e, w_=w_: e.tensor_reduce(out=SEL[0:T, w_, :], in_=eq[0:T, :, :], op=ALU.add, axis=AX.X), reads=[eq], writes=[SEL])
                    p.op('dve', lambda e: e.tensor_tensor(out=e16[0:T, :, :], in0=c16[0:T, :, :], in1=c16[0:T, :, 0:1].to_broadcast([T, 8, 16]), op=ALU.subtract),
                         reads=[c16], writes=[e16])
                    p.op('act', lambda e: e.activation(out=e16[0:T, :, :], in_=e16[0:T, :, :], func=AF.Exp), reads=[e16], writes=[e16])
                    p.op('dve', lambda e: e.tensor_reduce(out=z8[0:T, 0:8], in_=e16[0:T, :, :], op=ALU.add, axis=AX.X), reads=[e16], writes=[z8])
                    p.op('dve', lambda e: e.reciprocal(out=z8[0:T, 8:16], in_=z8[0:T, 0:8]), reads=[z8], writes=[z8])
                    p.op('dve', lambda e: e.tensor_tensor(out=SEL[0:T, 2, :].rearrange("q (h k) -> q h k", h=8), in0=e16[0:T, :, :],
                                                          in1=z8[0:T, 8:16].unsqueeze(2).to_broadcast([T, 8, 16]), op=ALU.mult), reads=[e16, z8], writes=[SEL])
                    bk = nb()
                    for w_ in range(3):
                        p.op('pe', lambda e, w_=w_, bk=bk: e.transpose(out=bk[:, w_ * T:(w_ + 1) * T], in_=SEL[0:T, w_, :], identity=identf[0:T, 0:T]),
                             reads=[SEL, identf], writes=[bk])
                    p.op('act', lambda e, bk=bk: e.copy(out=selT[:, :, s * 128:s * 128 + T], in_=bk[:, 0:3 * T].rearrange("q (w t) -> q w t", w=3)),
                         reads=[bk], writes=[selT])
                    p.op('pool', lambda e: e.tensor_copy(out=selTb[:, :, s * 128:s * 128 + T], in_=selT[:, :, s * 128:s * 128 + T]), reads=[selT], writes=[selTb])

                def peer_sel_part(row0, Tg, bufset):
                    xg, XT, selT, selTb = bufset
                    nsub = (Tg + 127) // 128
                    Ts = min(Tg, 128)
                    for s in range(nsub):
                        p.dma('sp', lambda e, s=s: e.dma_start(out=xg[0:Ts, s, :], in_=x2s[row0 + s * 128:row0 + s * 128 + Ts, :]), reads=['x2s'], writes=[xg])
                        peer_select(Ts, s, xg, XT, selT, selTb)

                def peer_group(Tg, y_out, yrow0, bufset, side_calls):
                    xg, XT, selT, selTb = bufset
                    nsub = (Tg + 127) // 128
                    Ts = min(Tg, 128)
                    side_i = [0]
                    for t0 in range(0, Tg, 8):
                        o1, o0, og = ohb1[(t0 // 8) % 2], ohb0[(t0 // 8) % 2], ohbg[(t0 // 8) % 2]
                        p.op('dve', lambda e, t0=t0, o1=o1: e.tensor_tensor(out=o1[:, :, :], in0=iota_b[:, :].unsqueeze(1).to_broadcast([128, 8, 128]),
                                                                          in1=selTb[:, 1, t0:t0 + 8].unsqueeze(2).to_broadcast([128, 8, 128]), op=ALU.is_equal),
                             reads=[iota_b, selTb], writes=[o1])
                        p.op('dve', lambda e, t0=t0, o0=o0: e.tensor_tensor(out=o0[:, :, :], in0=iota_b[:, :].unsqueeze(1).to_broadcast([128, 8, 128]),
                                                                          in1=selTb[:, 0, t0:t0 + 8].unsqueeze(2).to_broadcast([128, 8, 128]), op=ALU.is_equal),
                             reads=[iota_b, selTb], writes=[o0])
                        p.op('dve', lambda e, t0=t0, o0=o0, og=og: e.tensor_tensor(out=og[:, :, :], in0=o0[:, :, :],
                                                                                  in1=selTb[:, 2, t0:t0 + 8].unsqueeze(2).to_broadcast([128, 8, 128]), op=ALU.mult),
                             reads=[o0, selTb], writes=[og])
                        for q4 in range(2):
                            bk = nb()
                            for tt in range(4):
                                mm(bk[:, tt * 128:(tt + 1) * 128], o1[:, q4 * 4 + tt, :], og[:, q4 * 4 + tt, :], True, True, [o1, og], [bk])
                            p.op('act', lambda e, bk=bk, ta=t0 + q4 * 4: e.copy(out=Gt[:, ta:ta + 4, :], in_=bk[:, :].rearrange("q (t i) -> q t i", t=4)), reads=[bk], writes=[Gt])
                    acc = pb[4:8]
                    n_ec = cfg.get('n_ec', 128)

                    def fetch(ec):
                        p.dma('sp', lambda e, ec=ec: e.dma_start(out=UTc[ec % NBUF][:, :, :].rearrange("q c e -> q (c e)"), in_=UTs[ec, :, :]),
                              reads=[('UTs', ec)], writes=[UTc[ec % NBUF]])
                        p.dma(cfg.get('vq', 'pool'), lambda e, ec=ec: e.dma_start(out=Vc[ec % NBUF][:, :], in_=Vs[ec, :, :]), reads=[('Vs', ec)], writes=[Vc[ec % NBUF]])
                    for ec in range(min(NBUF, n_ec)):
                        fetch(ec)
                    for ec in range(n_ec + 1):
                        if ec < n_ec:
                            U_ = UTc[ec % NBUF]
                            bk = pb[ec % 2]
                            for c in range(8):
                                mm(bk[:, 0:Tg], U_[:, c, :], XT[:, c, 0:Tg], c == 0, c == 7, [U_, XT], [bk])
                            ga, GA = gab[ec % 3], GAb[ec % 3]
                            p.op('act', lambda e, bk=bk, ga=ga: e.activation(out=ga[:, 0:Tg], in_=bk[:, 0:Tg], func=AF.Gelu_apprx_tanh), reads=[bk], writes=[ga])
                            p.op('dve', lambda e, ga=ga, GA=GA, ec=ec: e.tensor_tensor(out=GA[:, 0:Tg], in0=ga[:, 0:Tg], in1=Gt[:, 0:Tg, ec], op=ALU.mult),
                                 reads=[ga, Gt], writes=[GA])
                        if ec >= 1:
                            pe_ = ec - 1
                            V_, GA = Vc[pe_ % NBUF], GAb[pe_ % 3]
                            for s in range(nsub):
                                for half in range(2):
                                    ab = acc[2 * s + half]
                                    mm(ab[0:Ts, :], GA[:, s * 128:s * 128 + Ts], V_[:, half * 512:(half + 1) * 512], pe_ == 0, pe_ == n_ec - 1, [GA, V_], [ab])
                            if pe_ + NBUF < n_ec:
                                fetch(pe_ + NBUF)
                        per = (len(side_calls) + n_ec - 1) // max(n_ec, 1) if side_calls else 0
                        for _ in range(per):
                            if side_i[0] < len(side_calls):
                                kind_, a_, k_ = side_calls[side_i[0]]
                                (orig_op if kind_ == 'op' else orig_dma)(*a_, **k_)
                                side_i[0] += 1
                    while side_i[0] < len(side_calls):
                        kind_, a_, k_ = side_calls[side_i[0]]
                        (orig_op if kind_ == 'op' else orig_dma)(*a_, **k_)
                        side_i[0] += 1
                    for s in range(nsub):
                        for half in range(2):
                            ab = acc[2 * s + half]
                            p.op('dve', lambda e, ab=ab, s=s, half=half: e.tensor_tensor(out=pre[0:Ts, half * 512:(half + 1) * 512], in0=ab[0:Ts, :],
                                                                                         in1=xg[0:Ts, s, half * 512:(half + 1) * 512], op=ALU.add),
                                 reads=[ab, xg], writes=[pre])
                        rs = rmsnorm_rstd(pre, Ts, ssd, sq4)
                        p.op('dve', lambda e, rs=rs: e.scalar_tensor_tensor(out=yo[0:Ts, :], in0=pre[0:Ts, :], scalar=rs, in1=gfin[0:Ts, :], op0=ALU.mult, op1=ALU.mult),
                             reads=[pre, ssd['ss'], gfin], writes=[yo])
                        p.dma('sp', lambda e, s=s: e.dma_start(out=y_out[yrow0 + s * 128:yrow0 + s * 128 + Ts, :], in_=yo[0:Ts, :]), reads=[yo], writes=['y_out'])

                groups = [(gi * 256, 256, y_p, gi * 256) for gi in range(cfg.get('n_groups', 8))]
                if cfg.get('peer_sample', True):
                    groups.append((SEQ, NS, y_s, 0))
                bufsets = [(xg_l[i], XT_l[i], selT_l[i], selTb_l[i]) for i in range(2)]
                orig_op, orig_dma = p.op, p.dma
                rec = {'on': False, 'calls': []}

                def op_wrap(*a, **k):
                    if rec['on']:
                        rec['calls'].append(('op', a, k))
                    else:
                        return orig_op(*a, **k)

                def dma_wrap(*a, **k):
                    if rec['on']:
                        rec['calls'].append(('dma', a, k))
                    else:
                        return orig_dma(*a, **k)
                p.op, p.dma = op_wrap, dma_wrap
                nbbase[0], nbmod[0] = 2, 2
                if groups:
                    peer_sel_part(groups[0][0], groups[0][1], bufsets[0])
                for n, (row0, Tg, y_out, yrow0) in enumerate(groups):
                    side = []
                    if n + 1 < len(groups) and cfg.get('overlap_sel', True):
                        rec['on'], rec['calls'] = True, []
                        peer_sel_part(groups[n + 1][0], groups[n + 1][1], bufsets[(n + 1) % 2])
                        rec['on'] = False
                        side = rec['calls']
                    elif n + 1 < len(groups):
                        pass
                    peer_group(Tg, y_out, yrow0, bufsets[n % 2], side)
                    if n + 1 < len(groups) and not cfg.get('overlap_sel', True):
                        peer_sel_part(groups[n + 1][0], groups[n + 1][1], bufsets[(n + 1) % 2])
                p.op, p.dma = orig_op, orig_dma

        p.finish()
        p.emit()
    return nc


_NC_CACHE = {}


def make_in_maps(inp, cfg, ncores=NCORES):
    c = host_consts()
    f = lambda a: np.ascontiguousarray(a, dtype=np.float32)
    maps = []
    for i in range(ncores):
        m = {
            'xp': f(inp['x_prompt'][i]),
            'xs': f(inp['x_sample'][DEC_B * i:DEC_B * (i + 1)].reshape(NS, D)),
            'memp': f(inp['mem_prompt'][i]),
            'w_in': f(inp['w_in'][0]), 'w_out': f(inp['w_out'][0]),
            'g_mix': f(inp['norm_mix_g'][0]), 'g_mem': f(inp['norm_mem_g'][0]), 'g_memn': f(inp['mem_norm_g'][0]),
            'g_ffn': f(inp['norm_ffn_g'][0]), 'g_fin': f(inp['final_norm_g']),
            'ln_g': f(inp['gm_ln_g'][0]), 'ln_b': f(inp['gm_ln_b'][0]),
            'gm_ws': f(inp['gm_ws'][0]), 'gm_bs': f(inp['gm_bs'][0]),
            'mem_wq': f(inp['mem_wq'][0]), 'mem_wkv': f(inp['mem_wkv'][0]), 'mem_wo': f(inp['mem_wo'][0]),
            'c_ident': c['ident'], 'c_trilT': c['trilT'], 'c_negmask': c['negmask'], 'c_blk64': c['blk64'], 'c_ones': c['ones'], 'c_iota': c['iota'],
            'peer_wq': f(inp['peer_wq'][0]), 'peer_keys': f(inp['peer_keys'][0].reshape(16, 128, 64)),
            'peer_u': f(inp['peer_u'][0]), 'peer_v': f(inp['peer_v'][0]),
            'cache_kidx': f(inp['cache_kidx'][0]).reshape(-1, 8192), 'cache_k': f(inp['cache_k'][0]).reshape(-1, 8192),
            'cache_v': f(inp['cache_v'][0]).reshape(-1, 8192),
            'page_table': np.ascontiguousarray(inp['page_table'][DEC_B * i:DEC_B * (i + 1)], dtype=np.int32),
            'cache_mem_k': f(inp['cache_mem_k'][0][DEC_B * i:DEC_B * (i + 1)]).reshape(DEC_B, 256, 512),
            'cache_mem_v': f(inp['cache_mem_v'][0][DEC_B * i:DEC_B * (i + 1)]).reshape(DEC_B, 256, 512),
            'c_pow2': c['pow2'], 'c_negm4': c['negm4'],
        }
        maps.append(m)
    return maps


def assemble(results, ncores=NCORES):
    cat = lambda k: np.stack([np.asarray(r[k], dtype=np.float32) for r in results])
    y_prompt = cat('y_p')
    y_sample = cat('y_s').reshape(ncores * DEC_B, DEC_T, D)
    k_prompt = cat('k_p').reshape(1, ncores, SEQ, 2, 64)
    v_prompt = cat('v_p').reshape(1, ncores, SEQ, 2, 64)
    kidx_prompt = cat('ki_p').reshape(1, ncores, SEQ, 64)
    gmv_prompt = cat('gmv_p').reshape(1, ncores, 128, 512)
    memk = cat('memk_p').reshape(1, ncores, 256, 4, 128)
    memv = cat('memv_p').reshape(1, ncores, 256, 4, 128)
    k_sample = cat('k_s').reshape(1, ncores * DEC_B, DEC_T, 2, 64)
    v_sample = cat('v_s').reshape(1, ncores * DEC_B, DEC_T, 2, 64)
    kidx_sample = cat('ki_s').reshape(1, ncores * DEC_B, DEC_T, 64)
    gmv_sample = cat('gmv_s').reshape(1, ncores * DEC_B, DEC_T, 512)
    return (y_prompt, y_sample, k_prompt, v_prompt, kidx_prompt, gmv_prompt, memk, memv,
            k_sample, v_sample, kidx_sample, gmv_sample)


def kernel(**inputs):
    cfg = {}
    nc = build(cfg)
    maps = make_in_maps(inputs, cfg)
    res = run_bass_kernel_spmd(nc, maps, core_ids=list(range(NCORES)))
    return assemble(res.results)
```
